# Optimizing a Trainium2 kernel written in Bass

```python
import jax, jax.numpy as jnp
from jax import lax
import numpy as np

D_MODEL = 2048
BATCH = 8
SEQ = 2048
DEPTH = 1

CHUNK = 64
D_MIX = D_MODEL
FOX_HEAD_DIM = 128
FOX_WIDTH = D_MIX // 2
FOX_HEADS = FOX_WIDTH // FOX_HEAD_DIM
Q_BLOCK = 128
MLSTM_HEAD_DIM = 256
MLSTM_WIDTH = D_MIX - FOX_WIDTH
MLSTM_HEADS = MLSTM_WIDTH // MLSTM_HEAD_DIM
CONV_K = 4
PEER_HEADS = 8
N_KEYS = 128
N_EXPERTS = N_KEYS * N_KEYS
PEER_TOPK = 16
PEER_QDIM = 256
PEER_TOKEN_BLOCK = 128
RMS_EPS = 1e-6
N_MOD = 6

kernel_name = "hybrid_fox_mlstm_peer_block"


def rms_norm(x, gain):
    xf = x.astype(jnp.float32)
    r = lax.rsqrt(jnp.mean(xf * xf, axis=-1, keepdims=True) + RMS_EPS)
    return (xf * r).astype(x.dtype) * gain


def causal_depthwise_conv(x, w, b):
    C = x.shape[-1]
    y = lax.conv_general_dilated(
        x, w[:, None, :].astype(x.dtype), window_strides=(1,),
        padding=[(CONV_K - 1, 0)], dimension_numbers=('NWC', 'WIO', 'NWC'),
        feature_group_count=C)
    return y + b


def fox_attention(q, k, v, f_pre, q_gain, k_gain):
    B, S, H, Dh = q.shape
    q = rms_norm(q, q_gain).transpose(0, 2, 1, 3)
    k = rms_norm(k, k_gain).transpose(0, 2, 1, 3)
    v = v.transpose(0, 2, 1, 3)
    F = jnp.cumsum(jax.nn.log_sigmoid(f_pre.astype(jnp.float32)), axis=1).transpose(0, 2, 1)
    scale = FOX_HEAD_DIM ** -0.5
    outs = []
    for blk in range(S // Q_BLOCK):
        lo, hi = blk * Q_BLOCK, (blk + 1) * Q_BLOCK
        s = jnp.einsum('bhqd,bhkd->bhqk', q[:, :, lo:hi], k[:, :, :hi]).astype(jnp.float32) * scale
        s = s + F[:, :, lo:hi, None] - F[:, :, None, :hi]
        causal = (lo + jnp.arange(Q_BLOCK))[:, None] >= jnp.arange(hi)[None, :]
        s = jnp.where(causal, s, -jnp.inf)
        p = jax.nn.softmax(s, axis=-1).astype(v.dtype)
        outs.append(jnp.einsum('bhqk,bhkd->bhqd', p, v[:, :, :hi]))
    o = jnp.concatenate(outs, axis=2)
    return o.transpose(0, 2, 1, 3).reshape(B, S, H * Dh)


def mlstm_chunkwise(q, k, v, i_pre, f_pre):
    B, S, H, Dh = q.shape
    NC = S // CHUNK

    def to_chunks(t):
        return t.reshape(B, NC, CHUNK, *t.shape[2:]).swapaxes(0, 1)

    k = k * (Dh ** -0.5)
    xs = (to_chunks(q.astype(jnp.float32)), to_chunks(k.astype(jnp.float32)),
          to_chunks(v.astype(jnp.float32)), to_chunks(i_pre.astype(jnp.float32)),
          to_chunks(jax.nn.log_sigmoid(f_pre.astype(jnp.float32))))
    causal = jnp.tril(jnp.ones((CHUNK, CHUNK), dtype=bool))

    def step(carry, inp):
        C, n, m = carry
        qc, kc, vc, ic, fc = inp
        b = jnp.cumsum(fc, axis=1).transpose(0, 2, 1)
        ic = ic.transpose(0, 2, 1)
        logD = b[:, :, :, None] - b[:, :, None, :] + ic[:, :, None, :]
        logD = jnp.where(causal, logD, -jnp.inf)
        m_inter = b + m[:, :, None]
        m_t = jnp.maximum(m_inter, jnp.max(logD, axis=-1))
        Dm = jnp.exp(logD - m_t[..., None])
        inter = jnp.exp(m_inter - m_t)
        W = jnp.einsum('blhd,bshd->bhls', qc, kc) * Dm
        num = (jnp.einsum('bhls,bshd->bhld', W, vc)
               + inter[..., None] * jnp.einsum('blhd,bhde->bhle', qc, C))
        den = jnp.sum(W, axis=-1) + inter * jnp.einsum('blhd,bhd->bhl', qc, n)
        h = num / jnp.maximum(jnp.abs(den), jnp.exp(-m_t))[..., None]
        bL = b[:, :, -1]
        w_s = bL[..., None] - b + ic
        m_new = jnp.maximum(bL + m, jnp.max(w_s, axis=-1))
        decay = jnp.exp(bL + m - m_new)
        ws = jnp.exp(w_s - m_new[..., None])
        C_new = decay[..., None, None] * C + jnp.einsum('bhs,bshd,bshe->bhde', ws, kc, vc)
        n_new = decay[..., None] * n + jnp.einsum('bhs,bshd->bhd', ws, kc)
        return (C_new, n_new, m_new), h.transpose(0, 2, 1, 3)

    init = (jnp.zeros((B, H, Dh, Dh), jnp.float32), jnp.zeros((B, H, Dh), jnp.float32),
            jnp.zeros((B, H), jnp.float32))
    _, hs = lax.scan(step, init, xs)
    return hs.swapaxes(0, 1).reshape(B, S, H, Dh).astype(q.dtype)


def mixer(h, w_in, fox_f_bias, fox_q_gain, fox_k_gain, mlstm_conv_w, mlstm_conv_b,
          mlstm_i_bias, mlstm_f_bias, mlstm_head_gain, w_out):
    B, S, _ = h.shape
    sizes = [FOX_WIDTH, FOX_WIDTH, FOX_WIDTH, FOX_HEADS,
             MLSTM_WIDTH, MLSTM_WIDTH, MLSTM_WIDTH, MLSTM_WIDTH, MLSTM_HEADS, MLSTM_HEADS]
    split_at = np.cumsum(sizes)[:-1].tolist()
    p = h @ w_in
    fq, fk, fv, ff, mq, mk, mv, mo, mi, mf = jnp.split(p, split_at, axis=-1)
    fox_out = fox_attention(fq.reshape(B, S, FOX_HEADS, FOX_HEAD_DIM),
                            fk.reshape(B, S, FOX_HEADS, FOX_HEAD_DIM),
                            fv.reshape(B, S, FOX_HEADS, FOX_HEAD_DIM),
                            ff + fox_f_bias, fox_q_gain, fox_k_gain)
    mqk = jax.nn.silu(causal_depthwise_conv(jnp.concatenate([mq, mk], axis=-1),
                                            mlstm_conv_w, mlstm_conv_b))
    mq, mk = jnp.split(mqk, 2, axis=-1)
    hm = mlstm_chunkwise(mq.reshape(B, S, MLSTM_HEADS, MLSTM_HEAD_DIM),
                         mk.reshape(B, S, MLSTM_HEADS, MLSTM_HEAD_DIM),
                         mv.reshape(B, S, MLSTM_HEADS, MLSTM_HEAD_DIM),
                         mi + mlstm_i_bias, mf + mlstm_f_bias)
    hm = rms_norm(hm, mlstm_head_gain.reshape(MLSTM_HEADS, MLSTM_HEAD_DIM))
    mlstm_out = hm.reshape(B, S, MLSTM_WIDTH) * jax.nn.sigmoid(mo)
    return jnp.concatenate([fox_out, mlstm_out], axis=-1) @ w_out


def peer(h, w_query, sub_keys_1, sub_keys_2, expert_down, expert_up):
    B, S, D = h.shape
    half = PEER_QDIM // 2
    q = (h @ w_query).reshape(B, S, PEER_HEADS, PEER_QDIM)
    s1 = jnp.einsum('bshd,nd->bshn', q[..., :half], sub_keys_1).astype(jnp.float32)
    s2 = jnp.einsum('bshd,nd->bshn', q[..., half:], sub_keys_2).astype(jnp.float32)
    v1, i1 = lax.top_k(s1, PEER_TOPK)
    v2, i2 = lax.top_k(s2, PEER_TOPK)
    cand = (v1[..., :, None] + v2[..., None, :]).reshape(B, S, PEER_HEADS, PEER_TOPK * PEER_TOPK)
    vs, pos = lax.top_k(cand, PEER_TOPK)
    e1 = jnp.take_along_axis(i1, pos // PEER_TOPK, axis=-1)
    e2 = jnp.take_along_axis(i2, pos % PEER_TOPK, axis=-1)
    idx = e1 * N_KEYS + e2
    g = jax.nn.softmax(vs, axis=-1)
    T = B * S
    nb = T // PEER_TOKEN_BLOCK

    def block(args):
        xb, ib, gb = args
        u = jnp.take(expert_down, ib, axis=0)
        a = jax.nn.gelu(jnp.einsum('td,thkd->thk', xb, u), approximate=False)
        w = (gb * a).astype(xb.dtype)
        vv = jnp.take(expert_up, ib, axis=0)
        return jnp.einsum('thk,thkd->td', w, vv)

    out = lax.map(block, (h.reshape(nb, PEER_TOKEN_BLOCK, D),
                          idx.reshape(nb, PEER_TOKEN_BLOCK, PEER_HEADS, PEER_TOPK),
                          g.reshape(nb, PEER_TOKEN_BLOCK, PEER_HEADS, PEER_TOPK)))
    return out.reshape(B, S, D)


def setup_inputs(seed: int = 0) -> dict:
    key = jax.random.key(seed)
    ks = jax.random.split(key, 24)
    n = jax.random.normal
    L = DEPTH
    in_cols = 3 * FOX_WIDTH + FOX_HEADS + 4 * MLSTM_WIDTH + 2 * MLSTM_HEADS
    return {
        "x": n(ks[0], (BATCH, SEQ, D_MODEL), jnp.float32),
        "c": n(ks[1], (BATCH, D_MODEL), jnp.float32),
        "w_ada": n(ks[2], (L, D_MODEL, N_MOD * D_MODEL), jnp.float32) * (0.5 * D_MODEL ** -0.5),
        "b_ada": n(ks[3], (L, N_MOD * D_MODEL), jnp.float32) * 0.02,
        "norm1_gain": 1.0 + 0.02 * n(ks[4], (L, D_MODEL), jnp.float32),
        "norm2_gain": 1.0 + 0.02 * n(ks[5], (L, D_MODEL), jnp.float32),
        "w_in": n(ks[6], (L, D_MODEL, in_cols), jnp.float32) * D_MODEL ** -0.5,
        "fox_f_bias": 3.0 + 0.1 * n(ks[7], (L, FOX_HEADS), jnp.float32),
        "fox_q_gain": 1.0 + 0.02 * n(ks[8], (L, FOX_HEAD_DIM), jnp.float32),
        "fox_k_gain": 1.0 + 0.02 * n(ks[9], (L, FOX_HEAD_DIM), jnp.float32),
        "mlstm_conv_w": n(ks[10], (L, CONV_K, 2 * MLSTM_WIDTH), jnp.float32) * CONV_K ** -0.5,
        "mlstm_conv_b": 0.02 * n(ks[11], (L, 2 * MLSTM_WIDTH), jnp.float32),
        "mlstm_i_bias": -1.0 + 0.1 * n(ks[12], (L, MLSTM_HEADS), jnp.float32),
        "mlstm_f_bias": 3.0 + 0.1 * n(ks[13], (L, MLSTM_HEADS), jnp.float32),
        "mlstm_head_gain": 1.0 + 0.02 * n(ks[14], (L, MLSTM_WIDTH), jnp.float32),
        "w_out": n(ks[15], (L, D_MIX, D_MODEL), jnp.float32) * D_MIX ** -0.5,
        "peer_w_query": n(ks[16], (L, D_MODEL, PEER_HEADS * PEER_QDIM), jnp.float32) * D_MODEL ** -0.5,
        "peer_sub_keys_1": n(ks[17], (L, N_KEYS, PEER_QDIM // 2), jnp.float32) * (PEER_QDIM // 2) ** -0.5,
        "peer_sub_keys_2": n(ks[18], (L, N_KEYS, PEER_QDIM // 2), jnp.float32) * (PEER_QDIM // 2) ** -0.5,
        "peer_expert_down": n(ks[19], (L, N_EXPERTS, D_MODEL), jnp.float32) * D_MODEL ** -0.5,
        "peer_expert_up": n(ks[20], (L, N_EXPERTS, D_MODEL), jnp.float32) * PEER_HEADS ** -0.5,
    }


def reference(x, c, w_ada, b_ada, norm1_gain, norm2_gain, w_in, fox_f_bias, fox_q_gain,
              fox_k_gain, mlstm_conv_w, mlstm_conv_b, mlstm_i_bias, mlstm_f_bias,
              mlstm_head_gain, w_out, peer_w_query, peer_sub_keys_1, peer_sub_keys_2,
              peer_expert_down, peer_expert_up):
    for l in range(DEPTH):
        mod = jax.nn.silu(c) @ w_ada[l] + b_ada[l]
        shift1, scale1, gate1, shift2, scale2, gate2 = jnp.split(mod[:, None, :], N_MOD, axis=-1)
        h = rms_norm(x, norm1_gain[l]) * (1.0 + scale1) + shift1
        y = mixer(h, w_in[l], fox_f_bias[l], fox_q_gain[l], fox_k_gain[l], mlstm_conv_w[l],
                  mlstm_conv_b[l], mlstm_i_bias[l], mlstm_f_bias[l], mlstm_head_gain[l], w_out[l])
        x = x + gate1 * y
        h = rms_norm(x, norm2_gain[l]) * (1.0 + scale2) + shift2
        y = peer(h, peer_w_query[l], peer_sub_keys_1[l], peer_sub_keys_2[l],
                 peer_expert_down[l], peer_expert_up[l])
        x = x + gate2 * y
    return x
```

```python
from contextlib import ExitStack
import numpy as np
import concourse.bass as bass
import concourse.mybir as mybir
from concourse.bass_utils import run_bass_kernel_spmd

F32 = mybir.dt.float32
BF16 = mybir.dt.bfloat16
ALU = mybir.AluOpType
AF = mybir.ActivationFunctionType
AX = mybir.AxisListType

import os
NBLK = int(os.environ.get("NBLK", "24"))
ADAQ = os.environ.get("ADAQ", "pool")
D = 2048
T = 2048
KC = 16
NT = 16
EPS = 1e-6


class Res:
    __slots__ = ("name", "w", "r")

    def __init__(self, name=""):
        self.name = name
        self.w = None
        self.r = []


class Sched:
    SEM_LIMIT = 30000

    def __init__(self, nc, es):
        self.nc = nc
        self.es = es
        self.engs = {"pe": nc.tensor, "act": nc.scalar, "dve": nc.vector,
                     "pool": nc.gpsimd, "sp": nc.sync}
        self.sem = {}
        self.cnt = {}
        self.nsem = 0
        self.pe_sems = []
        self.pend = {}
        for e in ("pe", "act", "dve", "pool"):
            self._new_sem(e)
        self.waited = {e: {} for e in self.engs}
        self.dma_slots = {}
        self.dma_next = {}
        for q, n in (("sp", 8), ("pool", 2), ("act", 4)):
            self.dma_slots[q] = [[self._mk(f"d{q}{i}"), 0] for i in range(n)]
            self.dma_next[q] = 0
        self.ninstr = 0

    def _mk(self, name):
        self.nsem += 1
        return self.es.enter_context(self.nc.semaphore(f"{name}_{self.nsem}"))

    def _new_sem(self, e):
        self.sem[e] = self._mk(f"s{e}")
        self.cnt[e] = 0
        if e == "pe":
            self.pe_sems.append(self.sem[e])

    def _wait(self, e, tok):
        sem, val = tok
        if e == "pe" and sem in self.pe_sems:
            return
        key = id(sem)
        if self.waited[e].get(key, 0) >= val:
            return
        self.engs[e].wait_ge(sem, val)
        self.waited[e][key] = val

    def _deps(self, e, reads, writes):
        toks = []
        for r in reads:
            if r.w is not None:
                toks.append(r.w)
        for w in writes:
            if w.w is not None:
                toks.append(w.w)
            toks.extend(w.r)
        for t in toks:
            self._wait(e, t)

    def _mark(self, tok, reads, writes):
        for w in writes:
            w.w = tok
            w.r = []
        for r in reads:
            if r in writes:
                continue
            r.r.append(tok)
            if len(r.r) > 24:
                r.r = r.r[-24:]

    def op(self, e, fn, reads=(), writes=(), inc=True):
        self._deps(e, reads, writes)
        if not self.pend.get(e, False) and self.cnt[e] >= self.SEM_LIMIT:
            self._new_sem(e)
        self.pend[e] = not inc
        ins = fn()
        self.ninstr += 1
        if inc:
            ins.then_inc(self.sem[e], 1)
            self.cnt[e] += 1
            tok = (self.sem[e], self.cnt[e])
            self._pending_ok = True
        else:
            tok = (self.sem[e], self.cnt[e] + 1)
        self._mark(tok, reads, writes)
        return tok

    def dma(self, q, out, in_, reads=(), writes=(), **kw):
        slots = self.dma_slots[q]
        i = self.dma_next[q]
        self.dma_next[q] = (i + 1) % len(slots)
        sem, val = slots[i]
        if val > 0:
            self._wait(q, (sem, val))
        self._deps(q, reads, writes)
        ins = self.engs[q].dma_start(out=out, in_=in_, **kw)
        ins.then_inc(sem, 16)
        slots[i][1] = val + 16
        tok = (sem, val + 16)
        self._mark(tok, reads, writes)
        self.ninstr += 1
        return tok

    def barrier(self):
        toks = []
        for e in ("pe", "act", "dve", "pool"):
            if self.cnt[e] > 0:
                assert not self.pend.get(e, False), f"open group on {e} at barrier"
                toks.append((self.sem[e], self.cnt[e]))
        for q in self.dma_slots:
            for sem, val in self.dma_slots[q]:
                if val > 0:
                    toks.append((sem, val))
        for e in self.engs:
            for t in toks:
                self._wait(e, t)

    def wait_all(self, e, ress):
        for r in ress:
            if r.w is not None:
                self._wait(e, r.w)


class Buf:
    def __init__(self, t, name):
        self.t = t
        self.r = Res(name)

    def __getitem__(self, idx):
        return self.t[idx]


class Ring:
    def __init__(self, bufs):
        self.bufs = bufs
        self.i = 0

    def next(self):
        b = self.bufs[self.i]
        self.i = (self.i + 1) % len(self.bufs)
        return b


class Ctx:
    def __init__(self, nc, es):
        self.nc = nc
        self.es = es
        self.S = Sched(nc, es)
        self.n = 0

    def sb(self, shape, dt, name, es=None):
        self.n += 1
        t = (es or self.es).enter_context(self.nc.sbuf_tensor(f"{name}_{self.n}", list(shape), dt))
        return Buf(t, name)

    def ps(self, shape, dt, name, es=None):
        self.n += 1
        t = (es or self.es).enter_context(self.nc.psum_tensor(f"{name}_{self.n}", list(shape), dt))
        return Buf(t, name)

    def sbring(self, n, shape, dt, name, es=None):
        return Ring([self.sb(shape, dt, f"{name}{i}", es) for i in range(n)])


NE1 = int(os.environ.get("NE1", "128"))
NQ = int(os.environ.get("NQ", "4"))
NFOX = int(os.environ.get("NFOX", "8"))
NML = int(os.environ.get("NML", "4"))
GK = 4
NEG = -1.0e30
CORES_PER_LAUNCH = 8


def build_program(stage=99, dbg=False):
    nc = bass.Bass("TRN2", target_bir_lowering=False)

    def din(name, shape, dt=F32):
        return nc.dram_tensor(name, list(shape), dt, kind="ExternalInput").ap()

    x_d = din("x", [T, D])
    c_d = din("c_r", [128, KC])
    wada_d = din("wada_r", [24, 128, KC, 512])
    bada_d = din("bada_r", [128, 96])
    g1_d = din("g1_r", [128, KC])
    g2_d = din("g2_r", [128, KC])
    wfm_d = din("wfm_r", [56, 128, KC, 128])
    wg_d = din("wg_r", [128, KC, 16])
    gb_d = din("gb_r", [16, 1])
    qkg_d = din("qkg_r", [128, 2])
    cw_d = din("convw_r", [128, 16, 4])
    cb_d = din("convb_r", [128, 16])
    hg_d = din("hg_r", [128, 8])
    wout_d = din("wout_r", [4, 128, KC, 512])
    wq_d = din("wq_r", [16, 128, KC, 128])
    k1t_d = din("k1t_r", [128, 128])
    k2t_d = din("k2t_r", [128, 128])
    edt_d = din("edt_r", [128, 128, KC, 128])
    eu_d = din("eu_r", [128, 128, D])
    out_d = nc.dram_tensor("out", [T, D], F32, kind="ExternalOutput").ap()
    mix_d = nc.dram_tensor("mix_scr", [KC, 128, T], BF16, kind="Internal").ap()
    if dbg:
        dbg_d = nc.dram_tensor("dbg", [128, 4096], F32, kind="ExternalOutput").ap()

    with ExitStack() as es:
        C = Ctx(nc, es)
        S = C.S
        V, A, P, G = nc.vector, nc.scalar, nc.tensor, nc.gpsimd

        psb = Ring([C.ps([128, 512], F32, f"ps{i}") for i in range(4)])
        acc_ps = [C.ps([128, 512], F32, f"pacc{i}") for i in range(4)]
        out_res = [Res(f"out{i}") for i in range(NT)]
        mix_res = [Res(f"mix{i}") for i in range(KC)]

        ident_f = C.sb([128, 128], F32, "ident_f")
        ident_b = C.sb([128, 128], BF16, "ident_b")
        ones_f = C.sb([128, 128], F32, "ones_f")
        ones_b = C.sb([128, 128], BF16, "ones_b")
        sel127 = C.sb([128, 128], F32, "sel127")
        tri_b = C.sb([128, 128], BF16, "tri_b")
        zcol = C.sb([128, 1], F32, "zcol")
        lncol = C.sb([128, 1], F32, "lncol")
        S.op("pool", lambda: G.memset(ones_f[:], 1.0), writes=[ones_f.r])
        S.op("pool", lambda: G.memset(ones_b[:], 1.0), writes=[ones_b.r])
        S.op("pool", lambda: G.memset(zcol[:], 0.0), writes=[zcol.r])
        S.op("pool", lambda: G.memset(lncol[:], float(np.log(1.0 / 16.0))), writes=[lncol.r])
        S.op("pool", lambda: G.affine_select(out=ident_f[:], in_=ones_f[:], pattern=[[-1, 128]],
                                             compare_op=ALU.is_equal, fill=0.0, base=0,
                                             channel_multiplier=1),
             reads=[ones_f.r], writes=[ident_f.r])
        S.op("pool", lambda: G.tensor_copy(out=ident_b[:], in_=ident_f[:]),
             reads=[ident_f.r], writes=[ident_b.r])
        S.op("pool", lambda: G.affine_select(out=sel127[:], in_=ones_f[:], pattern=[[0, 128]],
                                             compare_op=ALU.is_equal, fill=0.0, base=-127,
                                             channel_multiplier=1),
             reads=[ones_f.r], writes=[sel127.r])
        S.op("pool", lambda: G.affine_select(out=tri_b[:], in_=ones_b[:], pattern=[[1, 128]],
                                             compare_op=ALU.is_ge, fill=0.0, base=0,
                                             channel_multiplier=-1),
             reads=[ones_b.r], writes=[tri_b.r])

        def load_small(d_ap, shape, name, dt=F32):
            b = C.sb(shape, dt, name)
            S.dma("sp", b[:], d_ap, writes=[b.r])
            return b

        c_sb = load_small(c_d, [128, KC], "c_sb")
        bada_sb = load_small(bada_d, [128, 96], "bada")
        g1_sb = load_small(g1_d, [128, KC], "g1")
        g2_sb = load_small(g2_d, [128, KC], "g2")
        gb_sb = load_small(gb_d, [16, 1], "gb")
        qkg_sb = load_small(qkg_d, [128, 2], "qkg")
        cw_sb = load_small(cw_d, [128, 16, 4], "cw")
        cb_sb = load_small(cb_d, [128, 16], "cb")
        hg_sb = load_small(hg_d, [128, 8], "hg")
        k1t_f = load_small(k1t_d, [128, 128], "k1tf")
        k2t_f = load_small(k2t_d, [128, 128], "k2tf")
        wg_f = load_small(wg_d, [128, KC, 16], "wgf")
        k1t_b = C.sb([128, 128], BF16, "k1tb")
        k2t_b = C.sb([128, 128], BF16, "k2tb")
        wg_b = C.sb([128, KC, 16], BF16, "wgb")
        S.op("pool", lambda: G.tensor_copy(out=k1t_b[:], in_=k1t_f[:]), reads=[k1t_f.r], writes=[k1t_b.r])
        S.op("pool", lambda: G.tensor_copy(out=k2t_b[:], in_=k2t_f[:]), reads=[k2t_f.r], writes=[k2t_b.r])
        S.op("pool", lambda: G.tensor_copy(out=wg_b[:], in_=wg_f[:]), reads=[wg_f.r], writes=[wg_b.r])
        qsc = C.sb([128, 2], F32, "qsc")
        S.op("dve", lambda: V.tensor_scalar(out=qsc[:, 0:1], in0=qkg_sb[:, 0:1], scalar1=128.0 ** -0.5,
                                            scalar2=None, op0=ALU.mult), reads=[qkg_sb.r], writes=[qsc.r])
        S.op("dve", lambda: V.tensor_copy(out=qsc[:, 1:2], in_=qkg_sb[:, 1:2]), reads=[qkg_sb.r, qsc.r], writes=[qsc.r])

        sc_sb = C.sb([128, KC], F32, "sc_sb")
        mod = C.sb([128, 96], F32, "mod")
        S.op("act", lambda: A.activation(out=sc_sb[:], in_=c_sb[:], func=AF.Silu),
             reads=[c_sb.r], writes=[sc_sb.r])
        es_ada = ExitStack()
        wada_ring = C.sbring(2, [128, KC, 512], F32, "wada", es_ada)
        for jb in range(24):
            wb = wada_ring.next()
            S.dma("sp" if jb % 2 == 0 else "act", wb[:], wada_d[jb], writes=[wb.r])
            pm = psb.next()
            for jj in range(4):
                for kc in range(KC):
                    S.op("pe", lambda kc=kc, jj=jj, pm=pm, wb=wb: P.matmul(
                        pm[:, jj:jj + 1], lhsT=wb[:, kc, jj * 128:(jj + 1) * 128],
                        rhs=sc_sb[:, kc:kc + 1], start=(kc == 0), stop=(kc == KC - 1)),
                        reads=[wb.r, sc_sb.r], writes=[pm.r], inc=(kc == KC - 1 and jj == 3))
            S.op("dve", lambda jb=jb, pm=pm: V.tensor_tensor(
                out=mod[:, jb * 4:jb * 4 + 4], in0=pm[:, 0:4],
                in1=bada_sb[:, jb * 4:jb * 4 + 4], op=ALU.add),
                reads=[pm.r, bada_sb.r], writes=[mod.r])
        S.barrier()
        es_ada.close()
        A1 = C.sb([128, KC], F32, "A1")
        A2 = C.sb([128, KC], F32, "A2")
        S.op("dve", lambda: V.scalar_tensor_tensor(out=A1[:], in0=mod[:, 16:32], scalar=1.0, in1=g1_sb[:],
                                                   op0=ALU.add, op1=ALU.mult),
             reads=[mod.r, g1_sb.r], writes=[A1.r])
        S.op("dve", lambda: V.scalar_tensor_tensor(out=A2[:], in0=mod[:, 64:80], scalar=1.0, in1=g2_sb[:],
                                                   op0=ALU.add, op1=ALU.mult),
             reads=[mod.r, g2_sb.r], writes=[A2.r])

        dg = C.sb([128, 128], F32, "diag")

        def build_gate(gt, c0):
            for kc in range(KC):
                S.op("dve", lambda kc=kc: V.tensor_scalar(
                    out=dg[:], in0=ident_f[:], scalar1=mod[:, c0 + kc:c0 + kc + 1], scalar2=None,
                    op0=ALU.mult), reads=[ident_f.r, mod.r], writes=[dg.r])
                pm = psb.next()
                S.op("pe", lambda pm=pm: P.matmul(pm[:, 0:128], lhsT=ones_f[:], rhs=dg[:], start=True, stop=True),
                     reads=[ones_f.r, dg.r], writes=[pm.r])
                S.op("act", lambda pm=pm, kc=kc: A.copy(out=gt[:, kc * 128:(kc + 1) * 128], in_=pm[:, 0:128]),
                     reads=[pm.r], writes=[gt.r])

        def norm_to_T(es_l, src_rows, src_res, Asc, shift_c0, dstT, ntiles, nring):
            xr = C.sbring(nring, [128, D], F32, "xt", es_l)
            xn_r = C.sbring(nring, [128, D], BF16, "xn", es_l)
            ssr = C.sbring(2, [128, 2], F32, "ss", es_l)
            for i in range(ntiles):
                xt = xr.next()
                S.dma("sp", xt[:], src_rows(i), reads=[src_res(i)] if src_res else [], writes=[xt.r])
                ss = ssr.next()
                S.op("dve", lambda ss=ss: V.memset(ss[:], 0.0), writes=[ss.r])
                xn = xn_r.next()
                S.op("act", lambda xt=xt, ss=ss, xn=xn: A.activation(out=xn[:], in_=xt[:], func=AF.Square,
                                                                     accum_out=ss[:, 0:1]),
                     reads=[xt.r, ss.r], writes=[xn.r, ss.r])
                S.op("dve", lambda ss=ss: V.tensor_scalar(out=ss[:, 1:2], in0=ss[:, 0:1], scalar1=1.0 / D,
                                                          scalar2=EPS, op0=ALU.mult, op1=ALU.add),
                     reads=[ss.r], writes=[ss.r])
                S.op("act", lambda ss=ss: A.activation(out=ss[:, 1:2], in_=ss[:, 1:2], func=AF.Sqrt),
                     reads=[ss.r], writes=[ss.r])
                S.op("dve", lambda ss=ss: V.reciprocal(out=ss[:, 1:2], in_=ss[:, 1:2]),
                     reads=[ss.r], writes=[ss.r])
                S.op("act", lambda xt=xt, ss=ss, xn=xn: A.activation(out=xn[:], in_=xt[:], func=AF.Copy,
                                                                     scale=ss[:, 1:2]),
                     reads=[xt.r, ss.r], writes=[xn.r])
                for k4 in range(4):
                    pm = psb.next()
                    pmb = pm.t.bitcast(BF16)
                    for j in range(4):
                        kc = k4 * 4 + j
                        S.op("pe", lambda kc=kc, j=j, pmb=pmb, xn=xn: P.transpose(
                            out=pmb[:, j * 128:(j + 1) * 128], in_=xn[:, kc * 128:(kc + 1) * 128],
                            identity=ident_b[:]), reads=[xn.r, ident_b.r], writes=[pm.r], inc=(j == 3))
                    for j in range(4):
                        kc = k4 * 4 + j
                        S.op("dve", lambda kc=kc, j=j, pmb=pmb, i=i: V.tensor_scalar(
                            out=dstT[:, kc, i * 128:(i + 1) * 128], in0=pmb[:, j * 128:(j + 1) * 128],
                            scalar1=Asc[:, kc:kc + 1], scalar2=mod[:, shift_c0 + kc:shift_c0 + kc + 1],
                            op0=ALU.mult, op1=ALU.add),
                            reads=[pm.r, Asc.r, mod.r], writes=[dstT.r])

        es_m = ExitStack()
        wst = C.sbring(2, [128, KC, 128], F32, "wst", es_m)
        wbf = C.sbring(2, [128, KC, 128], BF16, "wbf", es_m)
        wq_flip = [0]

        def load_wchunk(d_ap):
            st = wst.next()
            q = "sp" if wq_flip[0] % 2 == 0 else "act"
            wq_flip[0] += 1
            S.dma(q, st[:], d_ap, writes=[st.r])
            wb = wbf.next()
            S.op("pool", lambda: G.tensor_copy(out=wb[:], in_=st[:]), reads=[st.r], writes=[wb.r])
            return wb

        h1T = C.sb([128, KC, T], BF16, "h1T", es_m)
        Ltok = C.sb([128, NT, 16], F32, "Ltok", es_m)
        Gtok = C.sb([128, NT, 16], F32, "Gtok", es_m)
        LrefB = C.sb([128, 64], F32, "LrefB", es_m)
        EQ = C.sb([16, T], BF16, "EQ", es_m)
        selm = C.sb([16, 4 * 128], BF16, "selm", es_m)
        qTr = [C.sb([128, T], BF16, f"qT{i}", es_m) for i in range(2)]
        kTr = [C.sb([128, T], BF16, f"kT{i}", es_m) for i in range(2)]
        qpr = [C.sbring(2, [128, 512], BF16, f"qp{i}", es_m) for i in range(2)]
        vtok = C.sb([128, NT, 256], BF16, "vtok", es_m)
        sigo = [C.sb([128, T], BF16, f"sigo{i}", es_m) for i in range(2)]
        rawc = C.sb([128, T + 4], F32, "rawc", es_m)
        cv = C.sb([128, T], F32, "cv", es_m)
        rawf_r = C.sbring(2, [128, 512], F32, "rawf", es_m)
        sqb_r = C.sbring(2, [128, 512], BF16, "sqb", es_m)
        rs_r = C.sbring(2, [128, 512], F32, "rs", es_m)
        pt_r = C.sbring(3, [128, 512], BF16, "pt", es_m)
        kb_r = C.sbring(2, [128, NT], F32, "kb", es_m)
        ktmp = C.sb([128, NT], F32, "ktmp", es_m)
        hT_r = [C.sbring(1, [128, 512], F32, f"hT{i}", es_m) for i in range(2)]
        mixo_r = C.sbring(2, [128, T], BF16, "mixo", es_m)
        es_n1 = ExitStack()
        norm_to_T(es_n1, lambda i: x_d[i * 128:(i + 1) * 128, :], None, A1, 0, h1T, NT, 2)
        S.barrier()
        es_n1.close()

        es_g = ExitStack()
        graw = C.sb([16, T], F32, "graw", es_g)
        lsp = C.sb([16, T], F32, "lsp", es_g)
        Lc = C.sb([16, T], F32, "Lc", es_g)
        for c in range(4):
            pm = psb.next()
            for kc in range(KC):
                S.op("pe", lambda kc=kc, pm=pm, c=c: P.matmul(
                    pm[0:16, :], lhsT=wg_b[:, kc, :], rhs=h1T[:, kc, c * 512:(c + 1) * 512],
                    start=(kc == 0), stop=(kc == KC - 1)),
                    reads=[wg_b.r, h1T.r], writes=[pm.r], inc=(kc == KC - 1))
            S.op("act", lambda pm=pm, c=c: A.activation(out=graw[:, c * 512:(c + 1) * 512], in_=pm[0:16, :],
                                                        func=AF.Identity, bias=gb_sb[:, 0:1]),
                 reads=[pm.r, gb_sb.r], writes=[graw.r])
        S.op("act", lambda: A.activation(out=lsp[:], in_=graw[:], func=AF.Exp, scale=-1.0),
             reads=[graw.r], writes=[lsp.r])
        S.op("act", lambda: A.activation(out=lsp[:], in_=lsp[:], func=AF.Ln, bias=ones_f[0:16, 0:1]),
             reads=[lsp.r, ones_f.r], writes=[lsp.r])
        S.op("dve", lambda: V.tensor_tensor_scan(out=Lc[:], data0=ones_f[0:16, 0:1].broadcast_to([16, T]), data1=lsp[:],
                                                 initial=zcol[0:16, 0:1], op0=ALU.mult, op1=ALU.add),
             reads=[ones_f.r, lsp.r, zcol.r], writes=[Lc.r])
        for (src, dst) in ((Lc, Ltok), (graw, Gtok)):
            for i4 in range(4):
                pm = psb.next()
                for j in range(4):
                    i = i4 * 4 + j
                    S.op("pe", lambda i=i, j=j, pm=pm, src=src: P.transpose(
                        out=pm[:, j * 16:(j + 1) * 16], in_=src[0:16, i * 128:(i + 1) * 128],
                        identity=ident_f[0:16, 0:16]), reads=[src.r, ident_f.r], writes=[pm.r], inc=(j == 3))
                S.op("dve", lambda i4=i4, pm=pm, dst=dst: V.tensor_copy(
                    out=dst[:, i4 * 4:(i4 + 1) * 4, :],
                    in_=pm[:, 0:64].rearrange("p (a b) -> p a b", a=4)),
                    reads=[pm.r], writes=[dst.r])
        pm = psb.next()
        for c in range(4):
            S.op("pe", lambda c=c, pm=pm: P.matmul(pm[:, c * 16:(c + 1) * 16], lhsT=sel127[:],
                                                   rhs=Ltok[:, 4 * c + 3, :], start=True, stop=True),
                 reads=[sel127.r, Ltok.r], writes=[pm.r], inc=(c == 3))
        S.op("dve", lambda pm=pm: V.tensor_copy(out=LrefB[:], in_=pm[:, 0:64]), reads=[pm.r], writes=[LrefB.r])
        for c in range(4):
            S.op("act", lambda c=c: A.activation(out=EQ[0:12, c * 512:(c + 1) * 512], in_=Lc[0:12, c * 512:(c + 1) * 512],
                                                 func=AF.Exp, scale=-1.0,
                                                 bias=Lc[0:12, c * 512 + 511:c * 512 + 512]),
                 reads=[Lc.r], writes=[EQ.r])
        for m in range(4):
            S.op("dve", lambda m=m: V.tensor_scalar(out=selm[:, m * 128:(m + 1) * 128], in0=ones_f[0:16, :],
                                                    scalar1=ident_f[0:16, 8 + m:9 + m], scalar2=None,
                                                    op0=ALU.mult),
                 reads=[ones_f.r, ident_f.r], writes=[selm.r])

        S.barrier()
        es_g.close()
        S.op("pool", lambda: G.memset(rawc[:, 0:4], 0.0), writes=[rawc.r])

        def proj_fm(wb, c, pm):
            for kc in range(KC):
                S.op("pe", lambda kc=kc: P.matmul(pm[:], lhsT=wb[:, kc, :], rhs=h1T[:, kc, c * 512:(c + 1) * 512],
                                                  start=(kc == 0), stop=(kc == KC - 1)),
                     reads=[wb.r, h1T.r], writes=[pm.r], inc=(kc == KC - 1))

        def fox_qk(wb, dst, gcol):
            for c in range(4):
                pm = psb.next()
                proj_fm(wb, c, pm)
                sqb = sqb_r.next()
                rawf = rawf_r.next()
                S.op("act", lambda: A.activation(out=sqb[:], in_=pm[:], func=AF.Square), reads=[pm.r], writes=[sqb.r])
                S.op("act", lambda: A.copy(out=rawf[:], in_=pm[:]), reads=[pm.r], writes=[rawf.r])
                pn = psb.next()
                S.op("pe", lambda: P.matmul(pn[:], lhsT=ones_b[:], rhs=sqb[:], start=True, stop=True),
                     reads=[ones_b.r, sqb.r], writes=[pn.r])
                rs = rs_r.next()
                S.op("dve", lambda: V.tensor_scalar(out=rs[:], in0=pn[:], scalar1=1.0 / 128, scalar2=EPS,
                                                    op0=ALU.mult, op1=ALU.add), reads=[pn.r], writes=[rs.r])
                S.op("act", lambda: A.activation(out=rs[:], in_=rs[:], func=AF.Sqrt), reads=[rs.r], writes=[rs.r])
                S.op("dve", lambda: V.reciprocal(out=rs[:], in_=rs[:]), reads=[rs.r], writes=[rs.r])
                S.op("dve", lambda: V.scalar_tensor_tensor(out=dst[:, c * 512:(c + 1) * 512], in0=rawf[:],
                                                           scalar=qsc[:, gcol:gcol + 1], in1=rs[:],
                                                           op0=ALU.mult, op1=ALU.mult),
                     reads=[rawf.r, qsc.r, rs.r], writes=[dst.r])

        def v_tok(wb, dvc):
            for i4 in range(4):
                pm = psb.next()
                for j in range(4):
                    i = i4 * 4 + j
                    for kc in range(KC):
                        S.op("pe", lambda kc=kc, i=i, j=j: P.matmul(
                            pm[:, j * 128:(j + 1) * 128], lhsT=h1T[:, kc, i * 128:(i + 1) * 128], rhs=wb[:, kc, :],
                            start=(kc == 0), stop=(kc == KC - 1)),
                            reads=[h1T.r, wb.r], writes=[pm.r], inc=(kc == KC - 1 and j == 3))
                S.op("act", lambda: A.copy(out=vtok[:, i4 * 4:(i4 + 1) * 4, dvc * 128:(dvc + 1) * 128],
                                           in_=pm[:].rearrange("p (a b) -> p a b", a=4)),
                     reads=[pm.r], writes=[vtok.r])

        def ml_qk(wb, dst, cch):
            for c in range(4):
                pm = psb.next()
                proj_fm(wb, c, pm)
                S.op("act", lambda: A.copy(out=rawc[:, 4 + c * 512:4 + (c + 1) * 512], in_=pm[:]),
                     reads=[pm.r], writes=[rawc.r])
            S.op("dve", lambda: V.tensor_scalar(out=cv[:], in0=rawc[:, 4:4 + T], scalar1=cw_sb[:, cch, 3:4],
                                                scalar2=cb_sb[:, cch:cch + 1], op0=ALU.mult, op1=ALU.add),
                 reads=[rawc.r, cw_sb.r, cb_sb.r], writes=[cv.r])
            for j in range(3):
                S.op("dve", lambda j=j: V.scalar_tensor_tensor(out=cv[:], in0=rawc[:, 1 + j:1 + j + T],
                                                               scalar=cw_sb[:, cch, j:j + 1], in1=cv[:],
                                                               op0=ALU.mult, op1=ALU.add),
                     reads=[rawc.r, cw_sb.r, cv.r], writes=[cv.r])
            S.op("act", lambda: A.activation(out=dst[:], in_=cv[:], func=AF.Silu), reads=[cv.r], writes=[dst.r])

        def ml_o(wb, dst):
            for c in range(4):
                pm = psb.next()
                proj_fm(wb, c, pm)
                S.op("act", lambda: A.activation(out=dst[:, c * 512:(c + 1) * 512], in_=pm[:], func=AF.Sigmoid),
                     reads=[pm.r], writes=[dst.r])

        def attend(nd, ndv, row, is_fox, mrow, out_chunks, hgc0):
            mixo = [mixo_r.next() for _ in range(ndv)]
            for c in range(4):
                kb = kb_r.next()
                if is_fox:
                    S.op("dve", lambda: V.tensor_scalar(out=kb[:], in0=Ltok[:, :, row],
                                                        scalar1=LrefB[:, c * 16 + row:c * 16 + row + 1],
                                                        scalar2=None, op0=ALU.subtract),
                         reads=[Ltok.r, LrefB.r], writes=[kb.r])
                    qs = [qTr[d][:, c * 512:(c + 1) * 512] for d in range(nd)]
                    qres = [qTr[d].r for d in range(nd)]
                else:
                    S.op("dve", lambda: V.scalar_tensor_tensor(out=ktmp[:, 0:4 * c + 4], in0=Ltok[:, 0:4 * c + 4, row],
                                                               scalar=LrefB[:, c * 16 + row:c * 16 + row + 1],
                                                               in1=Gtok[:, 0:4 * c + 4, 12 + mrow],
                                                               op0=ALU.subtract, op1=ALU.add),
                         reads=[Ltok.r, LrefB.r, Gtok.r], writes=[ktmp.r])
                    S.op("act", lambda: A.activation(out=kb[:, 0:4 * c + 4], in_=ktmp[:, 0:4 * c + 4], func=AF.Exp, bias=lncol[:, 0:1]),
                         reads=[ktmp.r, lncol.r], writes=[kb.r])
                    pe_ = psb.next()
                    S.op("pe", lambda: P.matmul(pe_[:], lhsT=selm[0:12, mrow * 128:(mrow + 1) * 128],
                                                rhs=EQ[0:12, c * 512:(c + 1) * 512], start=True, stop=True),
                         reads=[selm.r, EQ.r], writes=[pe_.r])
                    qs, qres = [], []
                    for d in range(nd):
                        qp = qpr[d].next()
                        S.op("dve", lambda d=d, qp=qp: V.tensor_tensor(out=qp[:], in0=qTr[d][:, c * 512:(c + 1) * 512],
                                                                       in1=pe_[:], op=ALU.mult),
                             reads=[qTr[d].r, pe_.r], writes=[qp.r])
                        qs.append(qp[:])
                        qres.append(qp.r)
                pO = [acc_ps[d] for d in range(ndv)]
                pD = acc_ps[2]
                nj = 4 * c + 4
                for j in range(nj):
                    lo = 128 * (j - 4 * c) if j >= 4 * c else 0
                    pS = psb.next()
                    for d in range(nd):
                        S.op("pe", lambda d=d: P.matmul(pS[:, lo:512], lhsT=kTr[d][:, j * 128:(j + 1) * 128],
                                                        rhs=qs[d][:, lo:512], start=(d == 0), stop=(d == nd - 1)),
                             reads=[kTr[d].r, qres[d]], writes=[pS.r], inc=(d == nd - 1))
                    pt = pt_r.next()
                    if is_fox:
                        S.op("act", lambda: A.activation(out=pt[:, lo:512], in_=pS[:, lo:512], func=AF.Exp,
                                                         bias=kb[:, j:j + 1]),
                             reads=[pS.r, kb.r], writes=[pt.r])
                    else:
                        S.op("dve", lambda: V.tensor_scalar(out=pt[:, lo:512], in0=pS[:, lo:512],
                                                            scalar1=kb[:, j:j + 1], scalar2=None, op0=ALU.mult),
                             reads=[pS.r, kb.r], writes=[pt.r])
                    if j >= 4 * c:
                        S.op("pool", lambda: G.tensor_tensor(out=pt[:, lo:lo + 128], in0=pt[:, lo:lo + 128],
                                                             in1=tri_b[:], op=ALU.mult),
                             reads=[pt.r, tri_b.r], writes=[pt.r])
                    for dv in range(ndv):
                        S.op("pe", lambda dv=dv: P.matmul(pO[dv][:, lo:512], lhsT=vtok[:, j, dv * 128:(dv + 1) * 128],
                                                          rhs=pt[:, lo:512], start=(j == 0), stop=(j == nj - 1)),
                             reads=[vtok.r, pt.r], writes=[pO[dv].r], inc=False)
                    S.op("pe", lambda: P.matmul(pD[:, lo:512], lhsT=ones_b[:], rhs=pt[:, lo:512],
                                                start=(j == 0), stop=(j == nj - 1)),
                         reads=[ones_b.r, pt.r], writes=[pD.r])
                rs = rs_r.next()
                if is_fox:
                    S.op("dve", lambda: V.reciprocal(out=rs[:], in_=pD[:]), reads=[pD.r], writes=[rs.r])
                    S.op("dve", lambda: V.tensor_tensor(out=mixo[0][:, c * 512:(c + 1) * 512], in0=pO[0][:],
                                                        in1=rs[:], op=ALU.mult),
                         reads=[pO[0].r, rs.r], writes=[mixo[0].r])
                else:
                    S.op("dve", lambda: V.tensor_scalar(out=rs[:], in0=pD[:], scalar1=-1.0, scalar2=1.0,
                                                        op0=ALU.mult, op1=ALU.max), reads=[pD.r], writes=[rs.r])
                    S.op("dve", lambda: V.scalar_tensor_tensor(out=rs[:], in0=pD[:], scalar=1.0, in1=rs[:],
                                                               op0=ALU.max, op1=ALU.max),
                         reads=[pD.r, rs.r], writes=[rs.r])
                    S.op("dve", lambda: V.reciprocal(out=rs[:], in_=rs[:]), reads=[rs.r], writes=[rs.r])
                    hTs = []
                    pn = psb.next()
                    for dv in range(ndv):
                        hT = hT_r[dv].next()
                        S.op("dve", lambda dv=dv, hT=hT: V.tensor_tensor(out=hT[:], in0=pO[dv][:], in1=rs[:], op=ALU.mult),
                             reads=[pO[dv].r, rs.r], writes=[hT.r])
                        sqb = sqb_r.next()
                        S.op("act", lambda hT=hT, sqb=sqb: A.activation(out=sqb[:], in_=hT[:], func=AF.Square),
                             reads=[hT.r], writes=[sqb.r])
                        S.op("pe", lambda dv=dv, sqb=sqb: P.matmul(pn[:], lhsT=ones_b[:], rhs=sqb[:],
                                                                    start=(dv == 0), stop=(dv == ndv - 1)),
                             reads=[ones_b.r, sqb.r], writes=[pn.r], inc=(dv == ndv - 1))
                        hTs.append(hT)
                    rs2 = rs_r.next()
                    S.op("dve", lambda: V.tensor_scalar(out=rs2[:], in0=pn[:], scalar1=1.0 / 256, scalar2=EPS,
                                                        op0=ALU.mult, op1=ALU.add), reads=[pn.r], writes=[rs2.r])
                    S.op("act", lambda: A.activation(out=rs2[:], in_=rs2[:], func=AF.Sqrt), reads=[rs2.r], writes=[rs2.r])
                    S.op("dve", lambda: V.reciprocal(out=rs2[:], in_=rs2[:]), reads=[rs2.r], writes=[rs2.r])
                    for dv in range(ndv):
                        S.op("dve", lambda dv=dv: V.scalar_tensor_tensor(
                            out=hTs[dv][:], in0=hTs[dv][:], scalar=hg_sb[:, hgc0 + dv:hgc0 + dv + 1], in1=rs2[:],
                            op0=ALU.mult, op1=ALU.mult), reads=[hTs[dv].r, hg_sb.r, rs2.r], writes=[hTs[dv].r])
                        S.op("dve", lambda dv=dv: V.tensor_tensor(
                            out=mixo[dv][:, c * 512:(c + 1) * 512], in0=hTs[dv][:],
                            in1=sigo[dv][:, c * 512:(c + 1) * 512], op=ALU.mult),
                            reads=[hTs[dv].r, sigo[dv].r], writes=[mixo[dv].r])
            for dv in range(ndv):
                S.dma("sp", mix_d[out_chunks[dv]], mixo[dv][:], reads=[mixo[dv].r],
                      writes=[mix_res[out_chunks[dv]]])

        for h in range(NFOX):
            fox_qk(load_wchunk(wfm_d[3 * h]), qTr[0], 0)
            fox_qk(load_wchunk(wfm_d[3 * h + 1]), kTr[0], 1)
            v_tok(load_wchunk(wfm_d[3 * h + 2]), 0)
            attend(1, 1, h, True, 0, [h], 0)
        for m in range(NML):
            b0 = 24 + 8 * m
            ml_qk(load_wchunk(wfm_d[b0 + 0]), qTr[0], 2 * m)
            ml_qk(load_wchunk(wfm_d[b0 + 1]), qTr[1], 2 * m + 1)
            ml_qk(load_wchunk(wfm_d[b0 + 2]), kTr[0], 8 + 2 * m)
            ml_qk(load_wchunk(wfm_d[b0 + 3]), kTr[1], 8 + 2 * m + 1)
            v_tok(load_wchunk(wfm_d[b0 + 4]), 0)
            v_tok(load_wchunk(wfm_d[b0 + 5]), 1)
            ml_o(load_wchunk(wfm_d[b0 + 6]), sigo[0])
            ml_o(load_wchunk(wfm_d[b0 + 7]), sigo[1])
            attend(2, 2, 8 + m, False, m, [8 + 2 * m, 8 + 2 * m + 1], 2 * m)
        S.barrier()
        es_m.close()

        es_o = ExitStack()
        mixT = C.sb([128, KC, T], BF16, "mixT", es_o)
        written = list(range(NFOX)) + [8 + j for j in range(2 * NML)]
        if len(written) < KC:
            S.op("pool", lambda: G.memset(mixT[:], 0.0), writes=[mixT.r])
        for kc in written:
            S.dma("sp" if kc % 2 == 0 else "act", mixT[:, kc, :], mix_d[kc], reads=[mix_res[kc]], writes=[mixT.r])
        gate1b = C.sb([128, D], F32, "gate1b", es_o)
        build_gate(gate1b, 32)
        wo_st = C.sb([128, KC, 512], F32, "wo_st", es_o)
        wo_bf = C.sbring(2, [128, KC, 512], BF16, "wo_bf", es_o)
        xs_r = C.sbring(3, [128, 512], F32, "xs", es_o)
        t1_r = C.sbring(3, [128, 512], F32, "t1", es_o)
        for cb in range(4):
            S.dma("sp", wo_st[:], wout_d[cb], writes=[wo_st.r])
            wob = wo_bf.next()
            S.op("pool", lambda: G.tensor_copy(out=wob[:], in_=wo_st[:]), reads=[wo_st.r], writes=[wob.r])
            for i in range(NT):
                pm = psb.next()
                for kc in range(KC):
                    S.op("pe", lambda kc=kc: P.matmul(pm[:], lhsT=mixT[:, kc, i * 128:(i + 1) * 128], rhs=wob[:, kc, :],
                                                      start=(kc == 0), stop=(kc == KC - 1)),
                         reads=[mixT.r, wob.r], writes=[pm.r], inc=(kc == KC - 1))
                xs = xs_r.next()
                S.dma("act", xs[:], x_d[i * 128:(i + 1) * 128, cb * 512:(cb + 1) * 512], writes=[xs.r])
                t1 = t1_r.next()
                S.op("dve", lambda: V.tensor_tensor(out=t1[:], in0=pm[:], in1=gate1b[:, cb * 512:(cb + 1) * 512],
                                                    op=ALU.mult), reads=[pm.r, gate1b.r], writes=[t1.r])
                S.op("pool", lambda: G.tensor_tensor(out=t1[:], in0=t1[:], in1=xs[:], op=ALU.add),
                     reads=[t1.r, xs.r], writes=[t1.r])
                S.dma("sp", out_d[i * 128:(i + 1) * 128, cb * 512:(cb + 1) * 512], t1[:], reads=[t1.r],
                      writes=[out_res[i]])
        S.barrier()
        es_o.close()

        es_p = ExitStack()
        psb = Ring(psb.bufs + acc_ps)
        wst = C.sbring(3, [128, KC, 128], F32, "wstp", es_p)
        wbf = C.sbring(2, [128, KC, 128], BF16, "wbfp", es_p)
        gate2b = C.sb([128, D], F32, "gate2b", es_p)
        build_gate(gate2b, 80)
        h2T = C.sb([128, KC, 512], BF16, "h2T", es_p)
        qT = C.sb([128, 16, 512], BF16, "qTp", es_p)
        acc = C.sb([128, 4, D], F32, "acc", es_p)
        Cb = C.sb([128, 8, 512], BF16, "Cb", es_p)
        statsT = C.sb([32, 512], BF16, "statsT", es_p)
        selT = C.sb([32, 8 * 128], BF16, "selT", es_p)
        selg = C.sb([32, 8 * 128], BF16, "selg", es_p)
        ucol = C.sb([32, 8], F32, "ucol", es_p)
        for h in range(8):
            S.op("dve", lambda h=h: V.tensor_tensor(out=ucol[:, h:h + 1], in0=ident_f[0:32, h:h + 1],
                                                    in1=ident_f[0:32, 8 + h:9 + h], op=ALU.add),
                 reads=[ident_f.r, ucol.r], writes=[ucol.r])
            S.op("dve", lambda h=h: V.scalar_tensor_tensor(out=ucol[:, h:h + 1], in0=ucol[:, h:h + 1], scalar=-1.0,
                                                           in1=ident_f[0:32, 16 + h:17 + h],
                                                           op0=ALU.mult, op1=ALU.subtract),
                 reads=[ident_f.r, ucol.r], writes=[ucol.r])
            S.op("dve", lambda h=h: V.tensor_scalar(out=selT[:, h * 128:(h + 1) * 128], in0=ones_f[0:32, :],
                                                    scalar1=ucol[:, h:h + 1], scalar2=None, op0=ALU.mult),
                 reads=[ones_f.r, ucol.r], writes=[selT.r])
            S.op("dve", lambda h=h: V.tensor_scalar(out=selg[:, h * 128:(h + 1) * 128], in0=ones_f[0:32, :],
                                                    scalar1=ident_f[0:32, 24 + h:25 + h], scalar2=None, op0=ALU.mult),
                 reads=[ones_f.r, ident_f.r], writes=[selg.r])
        k1bc_r = C.sbring(2, [128, 128], BF16, "k1bc", es_p)
        eu_st = C.sbring(2, [128, D], F32, "eu_st", es_p)
        eu_bf = C.sbring(GK + 2, [128, D], BF16, "eu_bf", es_p)
        wT_r = C.sbring(2 * GK, [128, 512], BF16, "wT", es_p)
        gA_r = C.sbring(2, [128, 512], BF16, "gA", es_p)
        E_r = C.sbring(2, [128, 512], BF16, "E", es_p)
        Mm2_r = C.sbring(2, [128, 2, 512], BF16, "Mm2", es_p)
        Tt2_r = C.sbring(2, [128, 2, 512], BF16, "Tt2", es_p)
        Ga2_r = C.sbring(2, [128, 2, 512], BF16, "Ga2", es_p)
        sc_r = C.sbring(2, [128, 256], F32, "sc", es_p)
        sc2_r = C.sbring(1, [128, 256], F32, "sc2", es_p)
        v12_r = C.sbring(2, [128, 32], F32, "v12", es_p)
        cand_r = C.sbring(2, [128, 256], F32, "cand", es_p)
        cand2_r = C.sbring(1, [128, 256], F32, "cand2", es_p)
        c16_r = C.sbring(2, [128, 16], F32, "c16", es_p)
        e16_r = C.sbring(2, [128, 16], F32, "e16", es_p)
        sm_r = C.sbring(2, [128, 4], F32, "sm", es_p)
        statf = C.sb([128, 48], F32, "statf", es_p)
        statb = C.sb([128, 32], BF16, "statb", es_p)

        for Q in range(NQ):
            es_n2 = ExitStack()
            norm_to_T(es_n2, lambda i: out_d[(Q * 4 + i) * 128:(Q * 4 + i + 1) * 128, :],
                      lambda i: out_res[Q * 4 + i], A2, 48, h2T, 4, 1)
            S.barrier()
            es_n2.close()
            for cc in range(16):
                st = wst.next()
                S.dma("sp" if cc % 2 == 0 else "act", st[:], wq_d[cc], writes=[st.r])
                wb = wbf.next()
                S.op("pool", lambda: G.tensor_copy(out=wb[:], in_=st[:]), reads=[st.r], writes=[wb.r])
                pm = psb.next()
                for kc in range(KC):
                    S.op("pe", lambda kc=kc: P.matmul(pm[:], lhsT=wb[:, kc, :], rhs=h2T[:, kc, :],
                                                      start=(kc == 0), stop=(kc == KC - 1)),
                         reads=[wb.r, h2T.r], writes=[pm.r], inc=(kc == KC - 1))
                S.op("act", lambda: A.copy(out=qT[:, cc, :], in_=pm[:]), reads=[pm.r], writes=[qT.r])
            for ti in range(4):
                for h in range(8):
                    pm = psb.next()
                    S.op("pe", lambda: P.matmul(pm[:, 0:128], lhsT=qT[:, 2 * h, ti * 128:(ti + 1) * 128],
                                                rhs=k1t_b[:], start=True, stop=True),
                         reads=[qT.r, k1t_b.r], writes=[pm.r], inc=False)
                    S.op("pe", lambda: P.matmul(pm[:, 128:256], lhsT=qT[:, 2 * h + 1, ti * 128:(ti + 1) * 128],
                                                rhs=k2t_b[:], start=True, stop=True),
                         reads=[qT.r, k2t_b.r], writes=[pm.r])
                    sc = sc_r.next()
                    sc2 = sc2_r.next()
                    v12 = v12_r.next()
                    S.op("act", lambda: A.copy(out=sc[:], in_=pm[:, 0:256]), reads=[pm.r], writes=[sc.r])
                    for half in range(2):
                        sl = slice(half * 128, (half + 1) * 128)
                        vo = half * 16
                        S.op("dve", lambda: V.max(out=v12[:, vo:vo + 8], in_=sc[:, sl]), reads=[sc.r, v12.r], writes=[v12.r])
                        S.op("dve", lambda: V.match_replace(out=sc2[:, sl], in_to_replace=v12[:, vo:vo + 8],
                                                            in_values=sc[:, sl], imm_value=NEG),
                             reads=[sc.r, v12.r, sc2.r], writes=[sc2.r])
                        S.op("dve", lambda: V.max(out=v12[:, vo + 8:vo + 16], in_=sc2[:, sl]),
                             reads=[sc2.r, v12.r], writes=[v12.r])
                    cand = cand_r.next()
                    cand2 = cand2_r.next()
                    c16 = c16_r.next()
                    e16 = e16_r.next()
                    sm = sm_r.next()
                    S.op("dve", lambda: V.tensor_tensor(
                        out=cand[:].rearrange("p (a b) -> p a b", a=16),
                        in0=v12[:, 0:16].unsqueeze(2).broadcast_to([128, 16, 16]),
                        in1=v12[:, 16:32].unsqueeze(1).broadcast_to([128, 16, 16]), op=ALU.add),
                        reads=[v12.r], writes=[cand.r])
                    S.op("dve", lambda: V.max(out=c16[:, 0:8], in_=cand[:]), reads=[cand.r, c16.r], writes=[c16.r])
                    S.op("dve", lambda: V.match_replace(out=cand2[:], in_to_replace=c16[:, 0:8], in_values=cand[:],
                                                        imm_value=NEG), reads=[cand.r, c16.r], writes=[cand2.r])
                    S.op("dve", lambda: V.max(out=c16[:, 8:16], in_=cand2[:]), reads=[cand2.r, c16.r], writes=[c16.r])
                    S.op("dve", lambda: V.tensor_scalar(out=sm[:, 0:1], in0=c16[:, 0:1], scalar1=-1.0, scalar2=None,
                                                        op0=ALU.mult), reads=[c16.r, sm.r], writes=[sm.r])
                    S.op("dve", lambda: V.memset(sm[:, 1:2], 0.0), reads=[sm.r], writes=[sm.r])
                    S.op("act", lambda: A.activation(out=e16[:], in_=c16[:], func=AF.Exp, bias=sm[:, 0:1],
                                                     accum_out=sm[:, 1:2]),
                         reads=[c16.r, sm.r], writes=[e16.r, sm.r])
                    S.op("dve", lambda: V.reciprocal(out=sm[:, 2:3], in_=sm[:, 1:2]), reads=[sm.r], writes=[sm.r])
                    S.op("dve", lambda: V.tensor_scalar(out=statf[:, h:h + 1], in0=c16[:, 15:16], scalar1=-3.0e-5,
                                                        scalar2=None, op0=ALU.add),
                         reads=[c16.r, statf.r], writes=[statf.r])
                    S.op("dve", lambda: V.tensor_tensor(out=statf[:, 32 + h:33 + h], in0=e16[:, 15:16], in1=sm[:, 2:3],
                                                        op=ALU.mult), reads=[e16.r, sm.r, statf.r], writes=[statf.r])
                S.op("dve", lambda: V.tensor_copy(out=statb[:, 0:8], in_=statf[:, 0:8]), reads=[statf.r, statb.r], writes=[statb.r])
                S.op("dve", lambda: V.tensor_tensor(out=statf[:, 8:16], in0=statf[:, 0:8], in1=statb[:, 0:8],
                                                    op=ALU.subtract), reads=[statf.r, statb.r], writes=[statf.r])
                S.op("dve", lambda: V.tensor_copy(out=statb[:, 8:16], in_=statf[:, 8:16]), reads=[statf.r, statb.r], writes=[statb.r])
                S.op("dve", lambda: V.tensor_tensor(out=statf[:, 16:24], in0=statf[:, 8:16], in1=statb[:, 8:16],
                                                    op=ALU.subtract), reads=[statf.r, statb.r], writes=[statf.r])
                S.op("dve", lambda: V.tensor_copy(out=statb[:, 16:24], in_=statf[:, 16:24]), reads=[statf.r, statb.r], writes=[statb.r])
                S.op("dve", lambda: V.tensor_copy(out=statb[:, 24:32], in_=statf[:, 32:40]), reads=[statf.r, statb.r], writes=[statb.r])
                pm = psb.next()
                pmb = pm.t.bitcast(BF16)
                S.op("pe", lambda: P.transpose(out=pmb[0:32, 0:128], in_=statb[:, 0:32], identity=ident_b[:]),
                     reads=[statb.r, ident_b.r], writes=[pm.r])
                S.op("act", lambda: A.copy(out=statsT[:, ti * 128:(ti + 1) * 128], in_=pmb[0:32, 0:128]),
                     reads=[pm.r], writes=[statsT.r])
            for h in range(8):
                pm = psb.next()
                S.op("pe", lambda: P.matmul(pm[:], lhsT=selg[:, h * 128:(h + 1) * 128], rhs=statsT[:],
                                            start=True, stop=True), reads=[selg.r, statsT.r], writes=[pm.r])
                S.op("act", lambda: A.copy(out=Cb[:, h, :], in_=pm[:]), reads=[pm.r], writes=[Cb.r])

            grp = []
            ngrp = 0
            for e1 in range(NE1):
                st = wst.next()
                S.dma("sp", st[:], edt_d[e1], writes=[st.r])
                edb = wbf.next()
                S.op("act", lambda: A.copy(out=edb[:], in_=st[:]), reads=[st.r], writes=[edb.r])
                es_ = eu_st.next()
                S.dma("act", es_[:], eu_d[e1], writes=[es_.r])
                eub = eu_bf.next()
                S.op("dve", lambda: V.tensor_copy(out=eub[:], in_=es_[:]), reads=[es_.r], writes=[eub.r])
                k1bc = k1bc_r.next()
                S.op("pool", lambda: G.tensor_copy(out=k1bc[:], in_=k1t_b[:, e1:e1 + 1].broadcast_to([128, 128])),
                     reads=[k1t_b.r], writes=[k1bc.r])
                pA = psb.next()
                for kc in range(KC):
                    S.op("pe", lambda kc=kc: P.matmul(pA[:], lhsT=edb[:, kc, :], rhs=h2T[:, kc, :],
                                                      start=(kc == 0), stop=(kc == KC - 1)),
                         reads=[edb.r, h2T.r], writes=[pA.r], inc=(kc == KC - 1))
                gA = gA_r.next()
                S.op("act", lambda: A.activation(out=gA[:], in_=pA[:], func=AF.Gelu), reads=[pA.r], writes=[gA.r])
                Ga2 = Ga2_r.next()
                for hp in range(4):
                    Mm2 = Mm2_r.next()
                    for hh in range(2):
                        h = 2 * hp + hh
                        pX = psb.next()
                        S.op("pe", lambda: P.matmul(pX[:], lhsT=k2t_b[:], rhs=qT[:, 2 * h + 1, :], start=True, stop=False),
                             reads=[k2t_b.r, qT.r], writes=[pX.r], inc=False)
                        S.op("pe", lambda: P.matmul(pX[:], lhsT=k1bc[:], rhs=qT[:, 2 * h, :], start=False, stop=False),
                             reads=[k1bc.r, qT.r], writes=[pX.r], inc=False)
                        S.op("pe", lambda: P.matmul(pX[:], lhsT=selT[:, h * 128:(h + 1) * 128], rhs=statsT[:],
                                                    start=False, stop=True),
                             reads=[selT.r, statsT.r], writes=[pX.r])
                        E = E_r.next()
                        S.op("act", lambda: A.activation(out=E[:], in_=pX[:], func=AF.Exp), reads=[pX.r], writes=[E.r])
                        S.op("dve", lambda: V.scalar_tensor_tensor(out=Mm2[:, hh, :], in0=pX[:], scalar=0.0, in1=E[:],
                                                                   op0=ALU.is_ge, op1=ALU.mult),
                             reads=[pX.r, E.r, Mm2.r], writes=[Mm2.r])
                    if hp == 0:
                        S.op("dve", lambda: V.tensor_tensor(out=Ga2[:], in0=Mm2[:], in1=Cb[:, 0:2, :], op=ALU.mult),
                             reads=[Mm2.r, Cb.r], writes=[Ga2.r])
                    else:
                        Tt2 = Tt2_r.next()
                        S.op("dve", lambda: V.tensor_tensor(out=Tt2[:], in0=Mm2[:], in1=Cb[:, 2 * hp:2 * hp + 2, :],
                                                            op=ALU.mult), reads=[Mm2.r, Cb.r], writes=[Tt2.r])
                        S.op("pool", lambda: G.tensor_tensor(out=Ga2[:], in0=Ga2[:], in1=Tt2[:], op=ALU.add),
                             reads=[Ga2.r, Tt2.r], writes=[Ga2.r])
                S.op("pool", lambda: G.tensor_tensor(out=Ga2[:, 0, :], in0=Ga2[:, 0, :], in1=Ga2[:, 1, :], op=ALU.add),
                     reads=[Ga2.r], writes=[Ga2.r])
                wT = wT_r.next()
                S.op("dve", lambda: V.tensor_tensor(out=wT[:], in0=Ga2[:, 0, :], in1=gA[:], op=ALU.mult),
                     reads=[Ga2.r, gA.r], writes=[wT.r])
                grp.append((wT, eub))
                if len(grp) == GK or e1 == NE1 - 1:
                    for ti in range(4):
                        for cbk in range(4):
                            pm = psb.next()
                            for gi, (wT_, eub_) in enumerate(grp):
                                S.op("pe", lambda gi=gi, wT_=wT_, eub_=eub_: P.matmul(
                                    pm[:], lhsT=wT_[:, ti * 128:(ti + 1) * 128], rhs=eub_[:, cbk * 512:(cbk + 1) * 512],
                                    start=(gi == 0), stop=(gi == len(grp) - 1)),
                                    reads=[wT_.r, eub_.r], writes=[pm.r], inc=(gi == len(grp) - 1))
                            if ngrp == 0:
                                S.op("act", lambda: A.copy(out=acc[:, ti, cbk * 512:(cbk + 1) * 512], in_=pm[:]),
                                     reads=[pm.r], writes=[acc.r])
                            else:
                                S.op("dve", lambda: V.tensor_tensor(out=acc[:, ti, cbk * 512:(cbk + 1) * 512],
                                                                    in0=pm[:], in1=acc[:, ti, cbk * 512:(cbk + 1) * 512],
                                                                    op=ALU.add), reads=[pm.r, acc.r], writes=[acc.r])
                    grp = []
                    ngrp += 1
            es_f = ExitStack()
            x1_r = C.sbring(1, [128, D], F32, "x1t", es_f)
            for ti in range(4):
                i = Q * 4 + ti
                x1 = x1_r.next()
                S.dma("sp", x1[:], out_d[i * 128:(i + 1) * 128, :], reads=[out_res[i]], writes=[x1.r])
                S.op("dve", lambda: V.tensor_tensor(out=acc[:, ti, :], in0=acc[:, ti, :], in1=gate2b[:], op=ALU.mult),
                     reads=[acc.r, gate2b.r], writes=[acc.r])
                S.op("pool", lambda: G.tensor_tensor(out=acc[:, ti, :], in0=acc[:, ti, :], in1=x1[:], op=ALU.add),
                     reads=[acc.r, x1.r], writes=[acc.r])
                S.dma("sp", out_d[i * 128:(i + 1) * 128, :], acc[:, ti, :], reads=[acc.r, out_res[i]], writes=[out_res[i]])
            S.barrier()
            es_f.close()
        S.barrier()
        es_p.close()

        for q in ("sp", "pool", "act"):
            for sem, val in S.dma_slots[q]:
                if val > 0:
                    S._wait("sp", (sem, val))
        print("instructions:", S.ninstr)
    return nc


def _host_layouts(inp):
    f = lambda a: np.ascontiguousarray(a, dtype=np.float32)
    L = {}
    w_ada = inp["w_ada"][0]
    L["wada_r"] = f(w_ada.reshape(KC, 128, 24, 512).transpose(2, 1, 0, 3))
    L["bada_r"] = f(inp["b_ada"][0].reshape(96, 128).T)
    L["g1_r"] = f(inp["norm1_gain"][0].reshape(KC, 128).T)
    L["g2_r"] = f(inp["norm2_gain"][0].reshape(KC, 128).T)
    w_in = inp["w_in"][0]
    o_fq, o_fk, o_fv, o_ff, o_mq, o_mk, o_mv, o_mo, o_mi, o_mf = 0, 1024, 2048, 3072, 3080, 4104, 5128, 6152, 7176, 7180
    cols = []
    for h in range(8):
        cols += [o_fq + 128 * h, o_fk + 128 * h, o_fv + 128 * h]
    for m in range(4):
        cols += [o_mq + 256 * m, o_mq + 256 * m + 128, o_mk + 256 * m, o_mk + 256 * m + 128,
                 o_mv + 256 * m, o_mv + 256 * m + 128, o_mo + 256 * m, o_mo + 256 * m + 128]
    wfm = np.empty((56, 128, KC, 128), np.float32)
    for i, c0 in enumerate(cols):
        wfm[i] = w_in[:, c0:c0 + 128].reshape(KC, 128, 128).transpose(1, 0, 2)
    L["wfm_r"] = wfm
    gcols = list(range(o_ff, o_ff + 8)) + list(range(o_mf, o_mf + 4)) + list(range(o_mi, o_mi + 4))
    L["wg_r"] = f(w_in[:, gcols].reshape(KC, 128, 16).transpose(1, 0, 2))
    L["gb_r"] = f(np.concatenate([inp["fox_f_bias"][0], inp["mlstm_f_bias"][0], inp["mlstm_i_bias"][0]]).reshape(16, 1))
    L["qkg_r"] = f(np.stack([inp["fox_q_gain"][0], inp["fox_k_gain"][0]], axis=1))
    L["convw_r"] = f(inp["mlstm_conv_w"][0].reshape(4, 16, 128).transpose(2, 1, 0))
    L["convb_r"] = f(inp["mlstm_conv_b"][0].reshape(16, 128).T)
    L["hg_r"] = f(inp["mlstm_head_gain"][0].reshape(8, 128).T)
    L["wout_r"] = f(inp["w_out"][0].reshape(KC, 128, 4, 512).transpose(2, 1, 0, 3))
    L["wq_r"] = f(inp["peer_w_query"][0].reshape(KC, 128, 16, 128).transpose(2, 1, 0, 3))
    L["k1t_r"] = f(inp["peer_sub_keys_1"][0].T)
    L["k2t_r"] = f(inp["peer_sub_keys_2"][0].T)
    ed = inp["peer_expert_down"][0]
    L["edt_r"] = f(ed.reshape(128, 128, KC, 128).transpose(0, 3, 2, 1))
    L["eu_r"] = f(inp["peer_expert_up"][0].reshape(128, 128, D))
    return L


def _core_inputs(inputs, b):
    return {
        "x": np.ascontiguousarray(inputs["x"][b], dtype=np.float32),
        "c_r": np.ascontiguousarray(inputs["c"][b].reshape(KC, 128).T, dtype=np.float32),
    }


def kernel(**inputs):
    inputs = {k: np.asarray(v) for k, v in inputs.items()}
    L = _host_layouts(inputs)
    nc = build_program()
    outs = []
    for g0 in range(0, 8, CORES_PER_LAUNCH):
        in_maps = []
        for b in range(g0, g0 + CORES_PER_LAUNCH):
            m = dict(L)
            m.update(_core_inputs(inputs, b))
            in_maps.append(m)
        res = run_bass_kernel_spmd(nc, in_maps, core_ids=list(range(CORES_PER_LAUNCH)))
        outs += [np.asarray(r["out"], dtype=np.float32) for r in res.results]
    return np.stack(outs, axis=0)
```

```python
from contextlib import ExitStack
import numpy as np
import concourse.bass as bass
import concourse.mybir as mybir
from concourse.bass_utils import run_bass_kernel_spmd

F32 = mybir.dt.float32
BF16 = mybir.dt.bfloat16
ALU = mybir.AluOpType
AF = mybir.ActivationFunctionType
AX = mybir.AxisListType

import os
NBLK = int(os.environ.get("NBLK", "24"))
ADAQ = os.environ.get("ADAQ", "pool")
D = 2048
T = 2048
KC = 16
NT = 16
EPS = 1e-6


class Res:
    __slots__ = ("name", "w", "r")

    def __init__(self, name=""):
        self.name = name
        self.w = None
        self.r = []


class Sched:
    SEM_LIMIT = 30000

    def __init__(self, nc, es):
        self.nc = nc
        self.es = es
        self.engs = {"pe": nc.tensor, "act": nc.scalar, "dve": nc.vector,
                     "pool": nc.gpsimd, "sp": nc.sync}
        self.sem = {}
        self.cnt = {}
        self.nsem = 0
        self.pe_sems = []
        self.pend = {}
        for e in ("pe", "act", "dve", "pool"):
            self._new_sem(e)
        self.waited = {e: {} for e in self.engs}
        self.dma_slots = {}
        self.dma_next = {}
        for q, n in (("sp", 8), ("pool", 2), ("act", 4)):
            self.dma_slots[q] = [[self._mk(f"d{q}{i}"), 0] for i in range(n)]
            self.dma_next[q] = 0
        self.ninstr = 0

    def _mk(self, name):
        self.nsem += 1
        return self.es.enter_context(self.nc.semaphore(f"{name}_{self.nsem}"))

    def _new_sem(self, e):
        self.sem[e] = self._mk(f"s{e}")
        self.cnt[e] = 0
        if e == "pe":
            self.pe_sems.append(self.sem[e])

    def _wait(self, e, tok):
        sem, val = tok
        if e == "pe" and sem in self.pe_sems:
            return
        key = id(sem)
        if self.waited[e].get(key, 0) >= val:
            return
        self.engs[e].wait_ge(sem, val)
        self.waited[e][key] = val

    def _deps(self, e, reads, writes):
        toks = []
        for r in reads:
            if r.w is not None:
                toks.append(r.w)
        for w in writes:
            if w.w is not None:
                toks.append(w.w)
            toks.extend(w.r)
        for t in toks:
            self._wait(e, t)

    def _mark(self, tok, reads, writes):
        for w in writes:
            w.w = tok
            w.r = []
        for r in reads:
            if r in writes:
                continue
            r.r.append(tok)
            if len(r.r) > 24:
                r.r = r.r[-24:]

    def op(self, e, fn, reads=(), writes=(), inc=True):
        self._deps(e, reads, writes)
        if not self.pend.get(e, False) and self.cnt[e] >= self.SEM_LIMIT:
            self._new_sem(e)
        self.pend[e] = not inc
        ins = fn()
        self.ninstr += 1
        if inc:
            ins.then_inc(self.sem[e], 1)
            self.cnt[e] += 1
            tok = (self.sem[e], self.cnt[e])
            self._pending_ok = True
        else:
            tok = (self.sem[e], self.cnt[e] + 1)
        self._mark(tok, reads, writes)
        return tok

    def dma(self, q, out, in_, reads=(), writes=(), **kw):
        slots = self.dma_slots[q]
        i = self.dma_next[q]
        self.dma_next[q] = (i + 1) % len(slots)
        sem, val = slots[i]
        if val > 0:
            self._wait(q, (sem, val))
        self._deps(q, reads, writes)
        ins = self.engs[q].dma_start(out=out, in_=in_, **kw)
        ins.then_inc(sem, 16)
        slots[i][1] = val + 16
        tok = (sem, val + 16)
        self._mark(tok, reads, writes)
        self.ninstr += 1
        return tok

    def barrier(self):
        toks = []
        for e in ("pe", "act", "dve", "pool"):
            if self.cnt[e] > 0:
                assert not self.pend.get(e, False), f"open group on {e} at barrier"
                toks.append((self.sem[e], self.cnt[e]))
        for q in self.dma_slots:
            for sem, val in self.dma_slots[q]:
                if val > 0:
                    toks.append((sem, val))
        for e in self.engs:
            for t in toks:
                self._wait(e, t)

    def wait_all(self, e, ress):
        for r in ress:
            if r.w is not None:
                self._wait(e, r.w)


class Buf:
    def __init__(self, t, name):
        self.t = t
        self.r = Res(name)

    def __getitem__(self, idx):
        return self.t[idx]


class Ring:
    def __init__(self, bufs):
        self.bufs = bufs
        self.i = 0

    def next(self):
        b = self.bufs[self.i]
        self.i = (self.i + 1) % len(self.bufs)
        return b


class Ctx:
    def __init__(self, nc, es):
        self.nc = nc
        self.es = es
        self.S = Sched(nc, es)
        self.n = 0

    def sb(self, shape, dt, name, es=None):
        self.n += 1
        t = (es or self.es).enter_context(self.nc.sbuf_tensor(f"{name}_{self.n}", list(shape), dt))
        return Buf(t, name)

    def ps(self, shape, dt, name, es=None):
        self.n += 1
        t = (es or self.es).enter_context(self.nc.psum_tensor(f"{name}_{self.n}", list(shape), dt))
        return Buf(t, name)

    def sbring(self, n, shape, dt, name, es=None):
        return Ring([self.sb(shape, dt, f"{name}{i}", es) for i in range(n)])


NE1 = int(os.environ.get("NE1", "128"))
NQ = int(os.environ.get("NQ", "4"))
NFOX = int(os.environ.get("NFOX", "8"))
NML = int(os.environ.get("NML", "4"))
GK = 4
NEG = -1.0e30
CORES_PER_LAUNCH = 8


def build_program(stage=99, dbg=False):
    nc = bass.Bass("TRN2", target_bir_lowering=False)

    def din(name, shape, dt=F32):
        return nc.dram_tensor(name, list(shape), dt, kind="ExternalInput").ap()

    x_d = din("x", [T, D])
    c_d = din("c_r", [128, KC])
    wada_d = din("wada_r", [24, 128, KC, 512])
    bada_d = din("bada_r", [128, 96])
    g1_d = din("g1_r", [128, KC])
    g2_d = din("g2_r", [128, KC])
    wfm_d = din("wfm_r", [56, 128, KC, 128])
    wg_d = din("wg_r", [128, KC, 16])
    gb_d = din("gb_r", [16, 1])
    qkg_d = din("qkg_r", [128, 2])
    cw_d = din("convw_r", [128, 16, 4])
    cb_d = din("convb_r", [128, 16])
    hg_d = din("hg_r", [128, 8])
    wout_d = din("wout_r", [4, 128, KC, 512])
    wq_d = din("wq_r", [16, 128, KC, 128])
    k1t_d = din("k1t_r", [128, 128])
    k2t_d = din("k2t_r", [128, 128])
    edt_d = din("edt_r", [128, 128, KC, 128])
    eu_d = din("eu_r", [128, 128, D])
    out_d = nc.dram_tensor("out", [T, D], F32, kind="ExternalOutput").ap()
    mix_d = nc.dram_tensor("mix_scr", [KC, 128, T], BF16, kind="Internal").ap()
    if dbg:
        dbg_d = nc.dram_tensor("dbg", [128, 4096], F32, kind="ExternalOutput").ap()

    with ExitStack() as es:
        C = Ctx(nc, es)
        S = C.S
        V, A, P, G = nc.vector, nc.scalar, nc.tensor, nc.gpsimd

        psb = Ring([C.ps([128, 512], F32, f"ps{i}") for i in range(4)])
        acc_ps = [C.ps([128, 512], F32, f"pacc{i}") for i in range(4)]
        out_res = [Res(f"out{i}") for i in range(NT)]
        mix_res = [Res(f"mix{i}") for i in range(KC)]

        ident_f = C.sb([128, 128], F32, "ident_f")
        ident_b = C.sb([128, 128], BF16, "ident_b")
        ones_f = C.sb([128, 128], F32, "ones_f")
        ones_b = C.sb([128, 128], BF16, "ones_b")
        sel127 = C.sb([128, 128], F32, "sel127")
        tri_b = C.sb([128, 128], BF16, "tri_b")
        zcol = C.sb([128, 1], F32, "zcol")
        lncol = C.sb([128, 1], F32, "lncol")
        S.op("pool", lambda: G.memset(ones_f[:], 1.0), writes=[ones_f.r])
        S.op("pool", lambda: G.memset(ones_b[:], 1.0), writes=[ones_b.r])
        S.op("pool", lambda: G.memset(zcol[:], 0.0), writes=[zcol.r])
        S.op("pool", lambda: G.memset(lncol[:], float(np.log(1.0 / 16.0))), writes=[lncol.r])
        S.op("pool", lambda: G.affine_select(out=ident_f[:], in_=ones_f[:], pattern=[[-1, 128]],
                                             compare_op=ALU.is_equal, fill=0.0, base=0,
                                             channel_multiplier=1),
             reads=[ones_f.r], writes=[ident_f.r])
        S.op("pool", lambda: G.tensor_copy(out=ident_b[:], in_=ident_f[:]),
             reads=[ident_f.r], writes=[ident_b.r])
        S.op("pool", lambda: G.affine_select(out=sel127[:], in_=ones_f[:], pattern=[[0, 128]],
                                             compare_op=ALU.is_equal, fill=0.0, base=-127,
                                             channel_multiplier=1),
             reads=[ones_f.r], writes=[sel127.r])
        S.op("pool", lambda: G.affine_select(out=tri_b[:], in_=ones_b[:], pattern=[[1, 128]],
                                             compare_op=ALU.is_ge, fill=0.0, base=0,
                                             channel_multiplier=-1),
             reads=[ones_b.r], writes=[tri_b.r])

        def load_small(d_ap, shape, name, dt=F32):
            b = C.sb(shape, dt, name)
            S.dma("sp", b[:], d_ap, writes=[b.r])
            return b

        c_sb = load_small(c_d, [128, KC], "c_sb")
        bada_sb = load_small(bada_d, [128, 96], "bada")
        g1_sb = load_small(g1_d, [128, KC], "g1")
        g2_sb = load_small(g2_d, [128, KC], "g2")
        gb_sb = load_small(gb_d, [16, 1], "gb")
        qkg_sb = load_small(qkg_d, [128, 2], "qkg")
        cw_sb = load_small(cw_d, [128, 16, 4], "cw")
        cb_sb = load_small(cb_d, [128, 16], "cb")
        hg_sb = load_small(hg_d, [128, 8], "hg")
        k1t_f = load_small(k1t_d, [128, 128], "k1tf")
        k2t_f = load_small(k2t_d, [128, 128], "k2tf")
        wg_f = load_small(wg_d, [128, KC, 16], "wgf")
        k1t_b = C.sb([128, 128], BF16, "k1tb")
        k2t_b = C.sb([128, 128], BF16, "k2tb")
        wg_b = C.sb([128, KC, 16], BF16, "wgb")
        S.op("pool", lambda: G.tensor_copy(out=k1t_b[:], in_=k1t_f[:]), reads=[k1t_f.r], writes=[k1t_b.r])
        S.op("pool", lambda: G.tensor_copy(out=k2t_b[:], in_=k2t_f[:]), reads=[k2t_f.r], writes=[k2t_b.r])
        S.op("pool", lambda: G.tensor_copy(out=wg_b[:], in_=wg_f[:]), reads=[wg_f.r], writes=[wg_b.r])
        qsc = C.sb([128, 2], F32, "qsc")
        S.op("dve", lambda: V.tensor_scalar(out=qsc[:, 0:1], in0=qkg_sb[:, 0:1], scalar1=128.0 ** -0.5,
                                            scalar2=None, op0=ALU.mult), reads=[qkg_sb.r], writes=[qsc.r])
        S.op("dve", lambda: V.tensor_copy(out=qsc[:, 1:2], in_=qkg_sb[:, 1:2]), reads=[qkg_sb.r, qsc.r], writes=[qsc.r])

        sc_sb = C.sb([128, KC], F32, "sc_sb")
        mod = C.sb([128, 96], F32, "mod")
        S.op("act", lambda: A.activation(out=sc_sb[:], in_=c_sb[:], func=AF.Silu),
             reads=[c_sb.r], writes=[sc_sb.r])
        es_ada = ExitStack()
        wada_ring = C.sbring(2, [128, KC, 512], F32, "wada", es_ada)
        for jb in range(24):
            wb = wada_ring.next()
            S.dma("sp" if jb % 2 == 0 else "act", wb[:], wada_d[jb], writes=[wb.r])
            pm = psb.next()
            for jj in range(4):
                for kc in range(KC):
                    S.op("pe", lambda kc=kc, jj=jj, pm=pm, wb=wb: P.matmul(
                        pm[:, jj:jj + 1], lhsT=wb[:, kc, jj * 128:(jj + 1) * 128],
                        rhs=sc_sb[:, kc:kc + 1], start=(kc == 0), stop=(kc == KC - 1)),
                        reads=[wb.r, sc_sb.r], writes=[pm.r], inc=(kc == KC - 1 and jj == 3))
            S.op("dve", lambda jb=jb, pm=pm: V.tensor_tensor(
                out=mod[:, jb * 4:jb * 4 + 4], in0=pm[:, 0:4],
                in1=bada_sb[:, jb * 4:jb * 4 + 4], op=ALU.add),
                reads=[pm.r, bada_sb.r], writes=[mod.r])
        S.barrier()
        es_ada.close()
        A1 = C.sb([128, KC], F32, "A1")
        A2 = C.sb([128, KC], F32, "A2")
        S.op("dve", lambda: V.scalar_tensor_tensor(out=A1[:], in0=mod[:, 16:32], scalar=1.0, in1=g1_sb[:],
                                                   op0=ALU.add, op1=ALU.mult),
             reads=[mod.r, g1_sb.r], writes=[A1.r])
        S.op("dve", lambda: V.scalar_tensor_tensor(out=A2[:], in0=mod[:, 64:80], scalar=1.0, in1=g2_sb[:],
                                                   op0=ALU.add, op1=ALU.mult),
             reads=[mod.r, g2_sb.r], writes=[A2.r])

        dg = C.sb([128, 128], F32, "diag")

        def build_gate(gt, c0):
            for kc in range(KC):
                S.op("dve", lambda kc=kc: V.tensor_scalar(
                    out=dg[:], in0=ident_f[:], scalar1=mod[:, c0 + kc:c0 + kc + 1], scalar2=None,
                    op0=ALU.mult), reads=[ident_f.r, mod.r], writes=[dg.r])
                pm = psb.next()
                S.op("pe", lambda pm=pm: P.matmul(pm[:, 0:128], lhsT=ones_f[:], rhs=dg[:], start=True, stop=True),
                     reads=[ones_f.r, dg.r], writes=[pm.r])
                S.op("act", lambda pm=pm, kc=kc: A.copy(out=gt[:, kc * 128:(kc + 1) * 128], in_=pm[:, 0:128]),
                     reads=[pm.r], writes=[gt.r])

        def norm_to_T(es_l, src_rows, src_res, Asc, shift_c0, dstT, ntiles, nring):
            xr = C.sbring(nring, [128, D], F32, "xt", es_l)
            xn_r = C.sbring(nring, [128, D], BF16, "xn", es_l)
            ssr = C.sbring(2, [128, 2], F32, "ss", es_l)
            for i in range(ntiles):
                xt = xr.next()
                S.dma("sp", xt[:], src_rows(i), reads=[src_res(i)] if src_res else [], writes=[xt.r])
                ss = ssr.next()
                S.op("dve", lambda ss=ss: V.memset(ss[:], 0.0), writes=[ss.r])
                xn = xn_r.next()
                S.op("act", lambda xt=xt, ss=ss, xn=xn: A.activation(out=xn[:], in_=xt[:], func=AF.Square,
                                                                     accum_out=ss[:, 0:1]),
                     reads=[xt.r, ss.r], writes=[xn.r, ss.r])
                S.op("dve", lambda ss=ss: V.tensor_scalar(out=ss[:, 1:2], in0=ss[:, 0:1], scalar1=1.0 / D,
                                                          scalar2=EPS, op0=ALU.mult, op1=ALU.add),
                     reads=[ss.r], writes=[ss.r])
                S.op("act", lambda ss=ss: A.activation(out=ss[:, 1:2], in_=ss[:, 1:2], func=AF.Sqrt),
                     reads=[ss.r], writes=[ss.r])
                S.op("dve", lambda ss=ss: V.reciprocal(out=ss[:, 1:2], in_=ss[:, 1:2]),
                     reads=[ss.r], writes=[ss.r])
                S.op("act", lambda xt=xt, ss=ss, xn=xn: A.activation(out=xn[:], in_=xt[:], func=AF.Copy,
                                                                     scale=ss[:, 1:2]),
                     reads=[xt.r, ss.r], writes=[xn.r])
                for k4 in range(4):
                    pm = psb.next()
                    pmb = pm.t.bitcast(BF16)
                    for j in range(4):
                        kc = k4 * 4 + j
                        S.op("pe", lambda kc=kc, j=j, pmb=pmb, xn=xn: P.transpose(
                            out=pmb[:, j * 128:(j + 1) * 128], in_=xn[:, kc * 128:(kc + 1) * 128],
                            identity=ident_b[:]), reads=[xn.r, ident_b.r], writes=[pm.r], inc=(j == 3))
                    for j in range(4):
                        kc = k4 * 4 + j
                        S.op("dve", lambda kc=kc, j=j, pmb=pmb, i=i: V.tensor_scalar(
                            out=dstT[:, kc, i * 128:(i + 1) * 128], in0=pmb[:, j * 128:(j + 1) * 128],
                            scalar1=Asc[:, kc:kc + 1], scalar2=mod[:, shift_c0 + kc:shift_c0 + kc + 1],
                            op0=ALU.mult, op1=ALU.add),
                            reads=[pm.r, Asc.r, mod.r], writes=[dstT.r])

        es_m = ExitStack()
        wst = C.sbring(2, [128, KC, 128], F32, "wst", es_m)
        wbf = C.sbring(2, [128, KC, 128], BF16, "wbf", es_m)
        wq_flip = [0]

        def load_wchunk(d_ap):
            st = wst.next()
            q = "sp" if wq_flip[0] % 2 == 0 else "act"
            wq_flip[0] += 1
            S.dma(q, st[:], d_ap, writes=[st.r])
            wb = wbf.next()
            S.op("pool", lambda: G.tensor_copy(out=wb[:], in_=st[:]), reads=[st.r], writes=[wb.r])
            return wb

        h1T = C.sb([128, KC, T], BF16, "h1T", es_m)
        Ltok = C.sb([128, NT, 16], F32, "Ltok", es_m)
        Gtok = C.sb([128, NT, 16], F32, "Gtok", es_m)
        LrefB = C.sb([128, 64], F32, "LrefB", es_m)
        EQ = C.sb([16, T], BF16, "EQ", es_m)
        selm = C.sb([16, 4 * 128], BF16, "selm", es_m)
        qTr = [C.sb([128, T], BF16, f"qT{i}", es_m) for i in range(2)]
        kTr = [C.sb([128, T], BF16, f"kT{i}", es_m) for i in range(2)]
        qpr = [C.sbring(2, [128, 512], BF16, f"qp{i}", es_m) for i in range(2)]
        vtok = C.sb([128, NT, 256], BF16, "vtok", es_m)
        sigo = [C.sb([128, T], BF16, f"sigo{i}", es_m) for i in range(2)]
        rawc = C.sb([128, T + 4], F32, "rawc", es_m)
        cv = C.sb([128, T], F32, "cv", es_m)
        rawf_r = C.sbring(2, [128, 512], F32, "rawf", es_m)
        sqb_r = C.sbring(2, [128, 512], BF16, "sqb", es_m)
        rs_r = C.sbring(2, [128, 512], F32, "rs", es_m)
        pt_r = C.sbring(3, [128, 512], BF16, "pt", es_m)
        kb_r = C.sbring(2, [128, NT], F32, "kb", es_m)
        ktmp = C.sb([128, NT], F32, "ktmp", es_m)
        hT_r = [C.sbring(1, [128, 512], F32, f"hT{i}", es_m) for i in range(2)]
        mixo_r = C.sbring(2, [128, T], BF16, "mixo", es_m)
        es_n1 = ExitStack()
        norm_to_T(es_n1, lambda i: x_d[i * 128:(i + 1) * 128, :], None, A1, 0, h1T, NT, 2)
        S.barrier()
        es_n1.close()

        es_g = ExitStack()
        graw = C.sb([16, T], F32, "graw", es_g)
        lsp = C.sb([16, T], F32, "lsp", es_g)
        Lc = C.sb([16, T], F32, "Lc", es_g)
        for c in range(4):
            pm = psb.next()
            for kc in range(KC):
                S.op("pe", lambda kc=kc, pm=pm, c=c: P.matmul(
                    pm[0:16, :], lhsT=wg_b[:, kc, :], rhs=h1T[:, kc, c * 512:(c + 1) * 512],
                    start=(kc == 0), stop=(kc == KC - 1)),
                    reads=[wg_b.r, h1T.r], writes=[pm.r], inc=(kc == KC - 1))
            S.op("act", lambda pm=pm, c=c: A.activation(out=graw[:, c * 512:(c + 1) * 512], in_=pm[0:16, :],
                                                        func=AF.Identity, bias=gb_sb[:, 0:1]),
                 reads=[pm.r, gb_sb.r], writes=[graw.r])
        S.op("act", lambda: A.activation(out=lsp[:], in_=graw[:], func=AF.Exp, scale=-1.0),
             reads=[graw.r], writes=[lsp.r])
        S.op("act", lambda: A.activation(out=lsp[:], in_=lsp[:], func=AF.Ln, bias=ones_f[0:16, 0:1]),
             reads=[lsp.r, ones_f.r], writes=[lsp.r])
        S.op("dve", lambda: V.tensor_tensor_scan(out=Lc[:], data0=ones_f[0:16, 0:1].broadcast_to([16, T]), data1=lsp[:],
                                                 initial=zcol[0:16, 0:1], op0=ALU.mult, op1=ALU.add),
             reads=[ones_f.r, lsp.r, zcol.r], writes=[Lc.r])
        for (src, dst) in ((Lc, Ltok), (graw, Gtok)):
            for i4 in range(4):
                pm = psb.next()
                for j in range(4):
                    i = i4 * 4 + j
                    S.op("pe", lambda i=i, j=j, pm=pm, src=src: P.transpose(
                        out=pm[:, j * 16:(j + 1) * 16], in_=src[0:16, i * 128:(i + 1) * 128],
                        identity=ident_f[0:16, 0:16]), reads=[src.r, ident_f.r], writes=[pm.r], inc=(j == 3))
                S.op("dve", lambda i4=i4, pm=pm, dst=dst: V.tensor_copy(
                    out=dst[:, i4 * 4:(i4 + 1) * 4, :],
                    in_=pm[:, 0:64].rearrange("p (a b) -> p a b", a=4)),
                    reads=[pm.r], writes=[dst.r])
        pm = psb.next()
        for c in range(4):
            S.op("pe", lambda c=c, pm=pm: P.matmul(pm[:, c * 16:(c + 1) * 16], lhsT=sel127[:],
                                                   rhs=Ltok[:, 4 * c + 3, :], start=True, stop=True),
                 reads=[sel127.r, Ltok.r], writes=[pm.r], inc=(c == 3))
        S.op("dve", lambda pm=pm: V.tensor_copy(out=LrefB[:], in_=pm[:, 0:64]), reads=[pm.r], writes=[LrefB.r])
        for c in range(4):
            S.op("act", lambda c=c: A.activation(out=EQ[0:12, c * 512:(c + 1) * 512], in_=Lc[0:12, c * 512:(c + 1) * 512],
                                                 func=AF.Exp, scale=-1.0,
                                                 bias=Lc[0:12, c * 512 + 511:c * 512 + 512]),
                 reads=[Lc.r], writes=[EQ.r])
        for m in range(4):
            S.op("dve", lambda m=m: V.tensor_scalar(out=selm[:, m * 128:(m + 1) * 128], in0=ones_f[0:16, :],
                                                    scalar1=ident_f[0:16, 8 + m:9 + m], scalar2=None,
                                                    op0=ALU.mult),
                 reads=[ones_f.r, ident_f.r], writes=[selm.r])

        S.barrier()
        es_g.close()
        S.op("pool", lambda: G.memset(rawc[:, 0:4], 0.0), writes=[rawc.r])

        def proj_fm(wb, c, pm):
            for kc in range(KC):
                S.op("pe", lambda kc=kc: P.matmul(pm[:], lhsT=wb[:, kc, :], rhs=h1T[:, kc, c * 512:(c + 1) * 512],
                                                  start=(kc == 0), stop=(kc == KC - 1)),
                     reads=[wb.r, h1T.r], writes=[pm.r], inc=(kc == KC - 1))

        def fox_qk(wb, dst, gcol):
            for c in range(4):
                pm = psb.next()
                proj_fm(wb, c, pm)
                sqb = sqb_r.next()
                rawf = rawf_r.next()
                S.op("act", lambda: A.activation(out=sqb[:], in_=pm[:], func=AF.Square), reads=[pm.r], writes=[sqb.r])
                S.op("act", lambda: A.copy(out=rawf[:], in_=pm[:]), reads=[pm.r], writes=[rawf.r])
                pn = psb.next()
                S.op("pe", lambda: P.matmul(pn[:], lhsT=ones_b[:], rhs=sqb[:], start=True, stop=True),
                     reads=[ones_b.r, sqb.r], writes=[pn.r])
                rs = rs_r.next()
                S.op("dve", lambda: V.tensor_scalar(out=rs[:], in0=pn[:], scalar1=1.0 / 128, scalar2=EPS,
                                                    op0=ALU.mult, op1=ALU.add), reads=[pn.r], writes=[rs.r])
                S.op("act", lambda: A.activation(out=rs[:], in_=rs[:], func=AF.Sqrt), reads=[rs.r], writes=[rs.r])
                S.op("dve", lambda: V.reciprocal(out=rs[:], in_=rs[:]), reads=[rs.r], writes=[rs.r])
                S.op("dve", lambda: V.scalar_tensor_tensor(out=dst[:, c * 512:(c + 1) * 512], in0=rawf[:],
                                                           scalar=qsc[:, gcol:gcol + 1], in1=rs[:],
                                                           op0=ALU.mult, op1=ALU.mult),
                     reads=[rawf.r, qsc.r, rs.r], writes=[dst.r])

        def v_tok(wb, dvc):
            for i4 in range(4):
                pm = psb.next()
                for j in range(4):
                    i = i4 * 4 + j
                    for kc in range(KC):
                        S.op("pe", lambda kc=kc, i=i, j=j: P.matmul(
                            pm[:, j * 128:(j + 1) * 128], lhsT=h1T[:, kc, i * 128:(i + 1) * 128], rhs=wb[:, kc, :],
                            start=(kc == 0), stop=(kc == KC - 1)),
                            reads=[h1T.r, wb.r], writes=[pm.r], inc=(kc == KC - 1 and j == 3))
                S.op("act", lambda: A.copy(out=vtok[:, i4 * 4:(i4 + 1) * 4, dvc * 128:(dvc + 1) * 128],
                                           in_=pm[:].rearrange("p (a b) -> p a b", a=4)),
                     reads=[pm.r], writes=[vtok.r])

        def ml_qk(wb, dst, cch):
            for c in range(4):
                pm = psb.next()
                proj_fm(wb, c, pm)
                S.op("act", lambda: A.copy(out=rawc[:, 4 + c * 512:4 + (c + 1) * 512], in_=pm[:]),
                     reads=[pm.r], writes=[rawc.r])
            S.op("dve", lambda: V.tensor_scalar(out=cv[:], in0=rawc[:, 4:4 + T], scalar1=cw_sb[:, cch, 3:4],
                                                scalar2=cb_sb[:, cch:cch + 1], op0=ALU.mult, op1=ALU.add),
                 reads=[rawc.r, cw_sb.r, cb_sb.r], writes=[cv.r])
            for j in range(3):
                S.op("dve", lambda j=j: V.scalar_tensor_tensor(out=cv[:], in0=rawc[:, 1 + j:1 + j + T],
                                                               scalar=cw_sb[:, cch, j:j + 1], in1=cv[:],
                                                               op0=ALU.mult, op1=ALU.add),
                     reads=[rawc.r, cw_sb.r, cv.r], writes=[cv.r])
            S.op("act", lambda: A.activation(out=dst[:], in_=cv[:], func=AF.Silu), reads=[cv.r], writes=[dst.r])

        def ml_o(wb, dst):
            for c in range(4):
                pm = psb.next()
                proj_fm(wb, c, pm)
                S.op("act", lambda: A.activation(out=dst[:, c * 512:(c + 1) * 512], in_=pm[:], func=AF.Sigmoid),
                     reads=[pm.r], writes=[dst.r])

        def attend(nd, ndv, row, is_fox, mrow, out_chunks, hgc0):
            mixo = [mixo_r.next() for _ in range(ndv)]
            for c in range(4):
                kb = kb_r.next()
                if is_fox:
                    S.op("dve", lambda: V.tensor_scalar(out=kb[:], in0=Ltok[:, :, row],
                                                        scalar1=LrefB[:, c * 16 + row:c * 16 + row + 1],
                                                        scalar2=None, op0=ALU.subtract),
                         reads=[Ltok.r, LrefB.r], writes=[kb.r])
                    qs = [qTr[d][:, c * 512:(c + 1) * 512] for d in range(nd)]
                    qres = [qTr[d].r for d in range(nd)]
                else:
                    S.op("dve", lambda: V.scalar_tensor_tensor(out=ktmp[:, 0:4 * c + 4], in0=Ltok[:, 0:4 * c + 4, row],
                                                               scalar=LrefB[:, c * 16 + row:c * 16 + row + 1],
                                                               in1=Gtok[:, 0:4 * c + 4, 12 + mrow],
                                                               op0=ALU.subtract, op1=ALU.add),
                         reads=[Ltok.r, LrefB.r, Gtok.r], writes=[ktmp.r])
                    S.op("act", lambda: A.activation(out=kb[:, 0:4 * c + 4], in_=ktmp[:, 0:4 * c + 4], func=AF.Exp, bias=lncol[:, 0:1]),
                         reads=[ktmp.r, lncol.r], writes=[kb.r])
                    pe_ = psb.next()
                    S.op("pe", lambda: P.matmul(pe_[:], lhsT=selm[0:12, mrow * 128:(mrow + 1) * 128],
                                                rhs=EQ[0:12, c * 512:(c + 1) * 512], start=True, stop=True),
                         reads=[selm.r, EQ.r], writes=[pe_.r])
                    qs, qres = [], []
                    for d in range(nd):
                        qp = qpr[d].next()
                        S.op("dve", lambda d=d, qp=qp: V.tensor_tensor(out=qp[:], in0=qTr[d][:, c * 512:(c + 1) * 512],
                                                                       in1=pe_[:], op=ALU.mult),
                             reads=[qTr[d].r, pe_.r], writes=[qp.r])
                        qs.append(qp[:])
                        qres.append(qp.r)
                pO = [acc_ps[d] for d in range(ndv)]
                pD = acc_ps[2]
                nj = 4 * c + 4
                for j in range(nj):
                    lo = 128 * (j - 4 * c) if j >= 4 * c else 0
                    pS = psb.next()
                    for d in range(nd):
                        S.op("pe", lambda d=d: P.matmul(pS[:, lo:512], lhsT=kTr[d][:, j * 128:(j + 1) * 128],
                                                        rhs=qs[d][:, lo:512], start=(d == 0), stop=(d == nd - 1)),
                             reads=[kTr[d].r, qres[d]], writes=[pS.r], inc=(d == nd - 1))
                    pt = pt_r.next()
                    if is_fox:
                        S.op("act", lambda: A.activation(out=pt[:, lo:512], in_=pS[:, lo:512], func=AF.Exp,
                                                         bias=kb[:, j:j + 1]),
                             reads=[pS.r, kb.r], writes=[pt.r])
                    else:
                        S.op("dve", lambda: V.tensor_scalar(out=pt[:, lo:512], in0=pS[:, lo:512],
                                                            scalar1=kb[:, j:j + 1], scalar2=None, op0=ALU.mult),
                             reads=[pS.r, kb.r], writes=[pt.r])
                    if j >= 4 * c:
                        S.op("pool", lambda: G.tensor_tensor(out=pt[:, lo:lo + 128], in0=pt[:, lo:lo + 128],
                                                             in1=tri_b[:], op=ALU.mult),
                             reads=[pt.r, tri_b.r], writes=[pt.r])
                    for dv in range(ndv):
                        S.op("pe", lambda dv=dv: P.matmul(pO[dv][:, lo:512], lhsT=vtok[:, j, dv * 128:(dv + 1) * 128],
                                                          rhs=pt[:, lo:512], start=(j == 0), stop=(j == nj - 1)),
                             reads=[vtok.r, pt.r], writes=[pO[dv].r], inc=False)
                    S.op("pe", lambda: P.matmul(pD[:, lo:512], lhsT=ones_b[:], rhs=pt[:, lo:512],
                                                start=(j == 0), stop=(j == nj - 1)),
                         reads=[ones_b.r, pt.r], writes=[pD.r])
                rs = rs_r.next()
                if is_fox:
                    S.op("dve", lambda: V.reciprocal(out=rs[:], in_=pD[:]), reads=[pD.r], writes=[rs.r])
                    S.op("dve", lambda: V.tensor_tensor(out=mixo[0][:, c * 512:(c + 1) * 512], in0=pO[0][:],
                                                        in1=rs[:], op=ALU.mult),
                         reads=[pO[0].r, rs.r], writes=[mixo[0].r])
                else:
                    S.op("dve", lambda: V.tensor_scalar(out=rs[:], in0=pD[:], scalar1=-1.0, scalar2=1.0,
                                                        op0=ALU.mult, op1=ALU.max), reads=[pD.r], writes=[rs.r])
                    S.op("dve", lambda: V.scalar_tensor_tensor(out=rs[:], in0=pD[:], scalar=1.0, in1=rs[:],
                                                               op0=ALU.max, op1=ALU.max),
                         reads=[pD.r, rs.r], writes=[rs.r])
                    S.op("dve", lambda: V.reciprocal(out=rs[:], in_=rs[:]), reads=[rs.r], writes=[rs.r])
                    hTs = []
                    pn = psb.next()
                    for dv in range(ndv):
                        hT = hT_r[dv].next()
                        S.op("dve", lambda dv=dv, hT=hT: V.tensor_tensor(out=hT[:], in0=pO[dv][:], in1=rs[:], op=ALU.mult),
                             reads=[pO[dv].r, rs.r], writes=[hT.r])
                        sqb = sqb_r.next()
                        S.op("act", lambda hT=hT, sqb=sqb: A.activation(out=sqb[:], in_=hT[:], func=AF.Square),
                             reads=[hT.r], writes=[sqb.r])
                        S.op("pe", lambda dv=dv, sqb=sqb: P.matmul(pn[:], lhsT=ones_b[:], rhs=sqb[:],
                                                                    start=(dv == 0), stop=(dv == ndv - 1)),
                             reads=[ones_b.r, sqb.r], writes=[pn.r], inc=(dv == ndv - 1))
                        hTs.append(hT)
                    rs2 = rs_r.next()
                    S.op("dve", lambda: V.tensor_scalar(out=rs2[:], in0=pn[:], scalar1=1.0 / 256, scalar2=EPS,
                                                        op0=ALU.mult, op1=ALU.add), reads=[pn.r], writes=[rs2.r])
                    S.op("act", lambda: A.activation(out=rs2[:], in_=rs2[:], func=AF.Sqrt), reads=[rs2.r], writes=[rs2.r])
                    S.op("dve", lambda: V.reciprocal(out=rs2[:], in_=rs2[:]), reads=[rs2.r], writes=[rs2.r])
                    for dv in range(ndv):
                        S.op("dve", lambda dv=dv: V.scalar_tensor_tensor(
                            out=hTs[dv][:], in0=hTs[dv][:], scalar=hg_sb[:, hgc0 + dv:hgc0 + dv + 1], in1=rs2[:],
                            op0=ALU.mult, op1=ALU.mult), reads=[hTs[dv].r, hg_sb.r, rs2.r], writes=[hTs[dv].r])
                        S.op("dve", lambda dv=dv: V.tensor_tensor(
                            out=mixo[dv][:, c * 512:(c + 1) * 512], in0=hTs[dv][:],
                            in1=sigo[dv][:, c * 512:(c + 1) * 512], op=ALU.mult),
                            reads=[hTs[dv].r, sigo[dv].r], writes=[mixo[dv].r])
            for dv in range(ndv):
                S.dma("sp", mix_d[out_chunks[dv]], mixo[dv][:], reads=[mixo[dv].r],
                      writes=[mix_res[out_chunks[dv]]])

        for h in range(NFOX):
            fox_qk(load_wchunk(wfm_d[3 * h]), qTr[0], 0)
            fox_qk(load_wchunk(wfm_d[3 * h + 1]), kTr[0], 1)
            v_tok(load_wchunk(wfm_d[3 * h + 2]), 0)
            attend(1, 1, h, True, 0, [h], 0)
        for m in range(NML):
            b0 = 24 + 8 * m
            ml_qk(load_wchunk(wfm_d[b0 + 0]), qTr[0], 2 * m)
            ml_qk(load_wchunk(wfm_d[b0 + 1]), qTr[1], 2 * m + 1)
            ml_qk(load_wchunk(wfm_d[b0 + 2]), kTr[0], 8 + 2 * m)
            ml_qk(load_wchunk(wfm_d[b0 + 3]), kTr[1], 8 + 2 * m + 1)
            v_tok(load_wchunk(wfm_d[b0 + 4]), 0)
            v_tok(load_wchunk(wfm_d[b0 + 5]), 1)
            ml_o(load_wchunk(wfm_d[b0 + 6]), sigo[0])
            ml_o(load_wchunk(wfm_d[b0 + 7]), sigo[1])
            attend(2, 2, 8 + m, False, m, [8 + 2 * m, 8 + 2 * m + 1], 2 * m)
        S.barrier()
        es_m.close()

        es_o = ExitStack()
        mixT = C.sb([128, KC, T], BF16, "mixT", es_o)
        written = list(range(NFOX)) + [8 + j for j in range(2 * NML)]
        if len(written) < KC:
            S.op("pool", lambda: G.memset(mixT[:], 0.0), writes=[mixT.r])
        for kc in written:
            S.dma("sp" if kc % 2 == 0 else "act", mixT[:, kc, :], mix_d[kc], reads=[mix_res[kc]], writes=[mixT.r])
        gate1b = C.sb([128, D], F32, "gate1b", es_o)
        build_gate(gate1b, 32)
        wo_st = C.sb([128, KC, 512], F32, "wo_st", es_o)
        wo_bf = C.sbring(2, [128, KC, 512], BF16, "wo_bf", es_o)
        xs_r = C.sbring(3, [128, 512], F32, "xs", es_o)
        t1_r = C.sbring(3, [128, 512], F32, "t1", es_o)
        for cb in range(4):
            S.dma("sp", wo_st[:], wout_d[cb], writes=[wo_st.r])
            wob = wo_bf.next()
            S.op("pool", lambda: G.tensor_copy(out=wob[:], in_=wo_st[:]), reads=[wo_st.r], writes=[wob.r])
            for i in range(NT):
                pm = psb.next()
                for kc in range(KC):
                    S.op("pe", lambda kc=kc: P.matmul(pm[:], lhsT=mixT[:, kc, i * 128:(i + 1) * 128], rhs=wob[:, kc, :],
                                                      start=(kc == 0), stop=(kc == KC - 1)),
                         reads=[mixT.r, wob.r], writes=[pm.r], inc=(kc == KC - 1))
                xs = xs_r.next()
                S.dma("act", xs[:], x_d[i * 128:(i + 1) * 128, cb * 512:(cb + 1) * 512], writes=[xs.r])
                t1 = t1_r.next()
                S.op("dve", lambda: V.tensor_tensor(out=t1[:], in0=pm[:], in1=gate1b[:, cb * 512:(cb + 1) * 512],
                                                    op=ALU.mult), reads=[pm.r, gate1b.r], writes=[t1.r])
                S.op("pool", lambda: G.tensor_tensor(out=t1[:], in0=t1[:], in1=xs[:], op=ALU.add),
                     reads=[t1.r, xs.r], writes=[t1.r])
                S.dma("sp", out_d[i * 128:(i + 1) * 128, cb * 512:(cb + 1) * 512], t1[:], reads=[t1.r],
                      writes=[out_res[i]])
        S.barrier()
        es_o.close()

        es_p = ExitStack()
        psb = Ring(psb.bufs + acc_ps)
        wst = C.sbring(3, [128, KC, 128], F32, "wstp", es_p)
        wbf = C.sbring(2, [128, KC, 128], BF16, "wbfp", es_p)
        gate2b = C.sb([128, D], F32, "gate2b", es_p)
        build_gate(gate2b, 80)
        h2T = C.sb([128, KC, 512], BF16, "h2T", es_p)
        qT = C.sb([128, 16, 512], BF16, "qTp", es_p)
        acc = C.sb([128, 4, D], F32, "acc", es_p)
        Cb = C.sb([128, 8, 512], BF16, "Cb", es_p)
        statsT = C.sb([32, 512], BF16, "statsT", es_p)
        selT = C.sb([32, 8 * 128], BF16, "selT", es_p)
        selg = C.sb([32, 8 * 128], BF16, "selg", es_p)
        ucol = C.sb([32, 8], F32, "ucol", es_p)
        for h in range(8):
            S.op("dve", lambda h=h: V.tensor_tensor(out=ucol[:, h:h + 1], in0=ident_f[0:32, h:h + 1],
                                                    in1=ident_f[0:32, 8 + h:9 + h], op=ALU.add),
                 reads=[ident_f.r, ucol.r], writes=[ucol.r])
            S.op("dve", lambda h=h: V.scalar_tensor_tensor(out=ucol[:, h:h + 1], in0=ucol[:, h:h + 1], scalar=-1.0,
                                                           in1=ident_f[0:32, 16 + h:17 + h],
                                                           op0=ALU.mult, op1=ALU.subtract),
                 reads=[ident_f.r, ucol.r], writes=[ucol.r])
            S.op("dve", lambda h=h: V.tensor_scalar(out=selT[:, h * 128:(h + 1) * 128], in0=ones_f[0:32, :],
                                                    scalar1=ucol[:, h:h + 1], scalar2=None, op0=ALU.mult),
                 reads=[ones_f.r, ucol.r], writes=[selT.r])
            S.op("dve", lambda h=h: V.tensor_scalar(out=selg[:, h * 128:(h + 1) * 128], in0=ones_f[0:32, :],
                                                    scalar1=ident_f[0:32, 24 + h:25 + h], scalar2=None, op0=ALU.mult),
                 reads=[ones_f.r, ident_f.r], writes=[selg.r])
        k1bc_r = C.sbring(2, [128, 128], BF16, "k1bc", es_p)
        eu_st = C.sbring(2, [128, D], F32, "eu_st", es_p)
        eu_bf = C.sbring(GK + 2, [128, D], BF16, "eu_bf", es_p)
        wT_r = C.sbring(2 * GK, [128, 512], BF16, "wT", es_p)
        gA_r = C.sbring(2, [128, 512], BF16, "gA", es_p)
        E_r = C.sbring(2, [128, 512], BF16, "E", es_p)
        Mm2_r = C.sbring(2, [128, 2, 512], BF16, "Mm2", es_p)
        Tt2_r = C.sbring(2, [128, 2, 512], BF16, "Tt2", es_p)
        Ga2_r = C.sbring(2, [128, 2, 512], BF16, "Ga2", es_p)
        sc_r = C.sbring(2, [128, 256], F32, "sc", es_p)
        sc2_r = C.sbring(1, [128, 256], F32, "sc2", es_p)
        v12_r = C.sbring(2, [128, 32], F32, "v12", es_p)
        cand_r = C.sbring(2, [128, 256], F32, "cand", es_p)
        cand2_r = C.sbring(1, [128, 256], F32, "cand2", es_p)
        c16_r = C.sbring(2, [128, 16], F32, "c16", es_p)
        e16_r = C.sbring(2, [128, 16], F32, "e16", es_p)
        sm_r = C.sbring(2, [128, 4], F32, "sm", es_p)
        statf = C.sb([128, 48], F32, "statf", es_p)
        statb = C.sb([128, 32], BF16, "statb", es_p)

        for Q in range(NQ):
            es_n2 = ExitStack()
            norm_to_T(es_n2, lambda i: out_d[(Q * 4 + i) * 128:(Q * 4 + i + 1) * 128, :],
                      lambda i: out_res[Q * 4 + i], A2, 48, h2T, 4, 1)
            S.barrier()
            es_n2.close()
            for cc in range(16):
                st = wst.next()
                S.dma("sp" if cc % 2 == 0 else "act", st[:], wq_d[cc], writes=[st.r])
                wb = wbf.next()
                S.op("pool", lambda: G.tensor_copy(out=wb[:], in_=st[:]), reads=[st.r], writes=[wb.r])
                pm = psb.next()
                for kc in range(KC):
                    S.op("pe", lambda kc=kc: P.matmul(pm[:], lhsT=wb[:, kc, :], rhs=h2T[:, kc, :],
                                                      start=(kc == 0), stop=(kc == KC - 1)),
                         reads=[wb.r, h2T.r], writes=[pm.r], inc=(kc == KC - 1))
                S.op("act", lambda: A.copy(out=qT[:, cc, :], in_=pm[:]), reads=[pm.r], writes=[qT.r])
            for ti in range(4):
                for h in range(8):
                    pm = psb.next()
                    S.op("pe", lambda: P.matmul(pm[:, 0:128], lhsT=qT[:, 2 * h, ti * 128:(ti + 1) * 128],
                                                rhs=k1t_b[:], start=True, stop=True),
                         reads=[qT.r, k1t_b.r], writes=[pm.r], inc=False)
                    S.op("pe", lambda: P.matmul(pm[:, 128:256], lhsT=qT[:, 2 * h + 1, ti * 128:(ti + 1) * 128],
                                                rhs=k2t_b[:], start=True, stop=True),
                         reads=[qT.r, k2t_b.r], writes=[pm.r])
                    sc = sc_r.next()
                    sc2 = sc2_r.next()
                    v12 = v12_r.next()
                    S.op("act", lambda: A.copy(out=sc[:], in_=pm[:, 0:256]), reads=[pm.r], writes=[sc.r])
                    for half in range(2):
                        sl = slice(half * 128, (half + 1) * 128)
                        vo = half * 16
                        S.op("dve", lambda: V.max(out=v12[:, vo:vo + 8], in_=sc[:, sl]), reads=[sc.r, v12.r], writes=[v12.r])
                        S.op("dve", lambda: V.match_replace(out=sc2[:, sl], in_to_replace=v12[:, vo:vo + 8],
                                                            in_values=sc[:, sl], imm_value=NEG),
                             reads=[sc.r, v12.r, sc2.r], writes=[sc2.r])
                        S.op("dve", lambda: V.max(out=v12[:, vo + 8:vo + 16], in_=sc2[:, sl]),
                             reads=[sc2.r, v12.r], writes=[v12.r])
                    cand = cand_r.next()
                    cand2 = cand2_r.next()
                    c16 = c16_r.next()
                    e16 = e16_r.next()
                    sm = sm_r.next()
                    S.op("dve", lambda: V.tensor_tensor(
                        out=cand[:].rearrange("p (a b) -> p a b", a=16),
                        in0=v12[:, 0:16].unsqueeze(2).broadcast_to([128, 16, 16]),
                        in1=v12[:, 16:32].unsqueeze(1).broadcast_to([128, 16, 16]), op=ALU.add),
                        reads=[v12.r], writes=[cand.r])
                    S.op("dve", lambda: V.max(out=c16[:, 0:8], in_=cand[:]), reads=[cand.r, c16.r], writes=[c16.r])
                    S.op("dve", lambda: V.match_replace(out=cand2[:], in_to_replace=c16[:, 0:8], in_values=cand[:],
                                                        imm_value=NEG), reads=[cand.r, c16.r], writes=[cand2.r])
                    S.op("dve", lambda: V.max(out=c16[:, 8:16], in_=cand2[:]), reads=[cand2.r, c16.r], writes=[c16.r])
                    S.op("dve", lambda: V.tensor_scalar(out=sm[:, 0:1], in0=c16[:, 0:1], scalar1=-1.0, scalar2=None,
                                                        op0=ALU.mult), reads=[c16.r, sm.r], writes=[sm.r])
                    S.op("dve", lambda: V.memset(sm[:, 1:2], 0.0), reads=[sm.r], writes=[sm.r])
                    S.op("act", lambda: A.activation(out=e16[:], in_=c16[:], func=AF.Exp, bias=sm[:, 0:1],
                                                     accum_out=sm[:, 1:2]),
                         reads=[c16.r, sm.r], writes=[e16.r, sm.r])
                    S.op("dve", lambda: V.reciprocal(out=sm[:, 2:3], in_=sm[:, 1:2]), reads=[sm.r], writes=[sm.r])
                    S.op("dve", lambda: V.tensor_scalar(out=statf[:, h:h + 1], in0=c16[:, 15:16], scalar1=-3.0e-5,
                                                        scalar2=None, op0=ALU.add),
                         reads=[c16.r, statf.r], writes=[statf.r])
                    S.op("dve", lambda: V.tensor_tensor(out=statf[:, 32 + h:33 + h], in0=e16[:, 15:16], in1=sm[:, 2:3],
                                                        op=ALU.mult), reads=[e16.r, sm.r, statf.r], writes=[statf.r])
                S.op("dve", lambda: V.tensor_copy(out=statb[:, 0:8], in_=statf[:, 0:8]), reads=[statf.r, statb.r], writes=[statb.r])
                S.op("dve", lambda: V.tensor_tensor(out=statf[:, 8:16], in0=statf[:, 0:8], in1=statb[:, 0:8],
                                                    op=ALU.subtract), reads=[statf.r, statb.r], writes=[statf.r])
                S.op("dve", lambda: V.tensor_copy(out=statb[:, 8:16], in_=statf[:, 8:16]), reads=[statf.r, statb.r], writes=[statb.r])
                S.op("dve", lambda: V.tensor_tensor(out=statf[:, 16:24], in0=statf[:, 8:16], in1=statb[:, 8:16],
                                                    op=ALU.subtract), reads=[statf.r, statb.r], writes=[statf.r])
                S.op("dve", lambda: V.tensor_copy(out=statb[:, 16:24], in_=statf[:, 16:24]), reads=[statf.r, statb.r], writes=[statb.r])
                S.op("dve", lambda: V.tensor_copy(out=statb[:, 24:32], in_=statf[:, 32:40]), reads=[statf.r, statb.r], writes=[statb.r])
                pm = psb.next()
                pmb = pm.t.bitcast(BF16)
                S.op("pe", lambda: P.transpose(out=pmb[0:32, 0:128], in_=statb[:, 0:32], identity=ident_b[:]),
                     reads=[statb.r, ident_b.r], writes=[pm.r])
                S.op("act", lambda: A.copy(out=statsT[:, ti * 128:(ti + 1) * 128], in_=pmb[0:32, 0:128]),
                     reads=[pm.r], writes=[statsT.r])
            for h in range(8):
                pm = psb.next()
                S.op("pe", lambda: P.matmul(pm[:], lhsT=selg[:, h * 128:(h + 1) * 128], rhs=statsT[:],
                                            start=True, stop=True), reads=[selg.r, statsT.r], writes=[pm.r])
                S.op("act", lambda: A.copy(out=Cb[:, h, :], in_=pm[:]), reads=[pm.r], writes=[Cb.r])

            grp = []
            ngrp = 0
            pending = []

            def emit_units(n):
                for _ in range(min(n, len(pending))):
                    g_, ti, cbk, first = pending.pop(0)
                    pm = psb.next()
                    for gi, (wT_, eub_) in enumerate(g_):
                        S.op("pe", lambda gi=gi, wT_=wT_, eub_=eub_: P.matmul(
                            pm[:], lhsT=wT_[:, ti * 128:(ti + 1) * 128], rhs=eub_[:, cbk * 512:(cbk + 1) * 512],
                            start=(gi == 0), stop=(gi == len(g_) - 1)),
                            reads=[wT_.r, eub_.r], writes=[pm.r], inc=(gi == len(g_) - 1))
                    if first:
                        S.op("act", lambda: A.copy(out=acc[:, ti, cbk * 512:(cbk + 1) * 512], in_=pm[:]),
                             reads=[pm.r], writes=[acc.r])
                    else:
                        S.op("dve", lambda: V.tensor_tensor(out=acc[:, ti, cbk * 512:(cbk + 1) * 512],
                                                            in0=pm[:], in1=acc[:, ti, cbk * 512:(cbk + 1) * 512],
                                                            op=ALU.add), reads=[pm.r, acc.r], writes=[acc.r])

            for e1 in range(NE1):
                st = wst.next()
                S.dma("sp", st[:], edt_d[e1], writes=[st.r])
                edb = wbf.next()
                S.op("act", lambda: A.copy(out=edb[:], in_=st[:]), reads=[st.r], writes=[edb.r])
                es_ = eu_st.next()
                S.dma("act", es_[:], eu_d[e1], writes=[es_.r])
                eub = eu_bf.next()
                S.op("dve", lambda: V.tensor_copy(out=eub[:], in_=es_[:]), reads=[es_.r], writes=[eub.r])
                k1bc = k1bc_r.next()
                S.op("pool", lambda: G.tensor_copy(out=k1bc[:], in_=k1t_b[:, e1:e1 + 1].broadcast_to([128, 128])),
                     reads=[k1t_b.r], writes=[k1bc.r])
                pA = psb.next()
                for kc in range(KC):
                    S.op("pe", lambda kc=kc: P.matmul(pA[:], lhsT=edb[:, kc, :], rhs=h2T[:, kc, :],
                                                      start=(kc == 0), stop=(kc == KC - 1)),
                         reads=[edb.r, h2T.r], writes=[pA.r], inc=(kc == KC - 1))
                gA = gA_r.next()
                S.op("act", lambda: A.activation(out=gA[:], in_=pA[:], func=AF.Gelu), reads=[pA.r], writes=[gA.r])
                Ga2 = Ga2_r.next()
                for hp in range(4):
                    Mm2 = Mm2_r.next()
                    for hh in range(2):
                        h = 2 * hp + hh
                        pX = psb.next()
                        S.op("pe", lambda: P.matmul(pX[:], lhsT=k2t_b[:], rhs=qT[:, 2 * h + 1, :], start=True, stop=False),
                             reads=[k2t_b.r, qT.r], writes=[pX.r], inc=False)
                        S.op("pe", lambda: P.matmul(pX[:], lhsT=k1bc[:], rhs=qT[:, 2 * h, :], start=False, stop=False),
                             reads=[k1bc.r, qT.r], writes=[pX.r], inc=False)
                        S.op("pe", lambda: P.matmul(pX[:], lhsT=selT[:, h * 128:(h + 1) * 128], rhs=statsT[:],
                                                    start=False, stop=True),
                             reads=[selT.r, statsT.r], writes=[pX.r])
                        E = E_r.next()
                        S.op("act", lambda: A.activation(out=E[:], in_=pX[:], func=AF.Exp), reads=[pX.r], writes=[E.r])
                        S.op("dve", lambda: V.scalar_tensor_tensor(out=Mm2[:, hh, :], in0=pX[:], scalar=0.0, in1=E[:],
                                                                   op0=ALU.is_ge, op1=ALU.mult),
                             reads=[pX.r, E.r, Mm2.r], writes=[Mm2.r])
                    if hp == 0:
                        S.op("dve", lambda: V.tensor_tensor(out=Ga2[:], in0=Mm2[:], in1=Cb[:, 0:2, :], op=ALU.mult),
                             reads=[Mm2.r, Cb.r], writes=[Ga2.r])
                    else:
                        Tt2 = Tt2_r.next()
                        S.op("dve", lambda: V.tensor_tensor(out=Tt2[:], in0=Mm2[:], in1=Cb[:, 2 * hp:2 * hp + 2, :],
                                                            op=ALU.mult), reads=[Mm2.r, Cb.r], writes=[Tt2.r])
                        S.op("pool", lambda: G.tensor_tensor(out=Ga2[:], in0=Ga2[:], in1=Tt2[:], op=ALU.add),
                             reads=[Ga2.r, Tt2.r], writes=[Ga2.r])
                S.op("pool", lambda: G.tensor_tensor(out=Ga2[:, 0, :], in0=Ga2[:, 0, :], in1=Ga2[:, 1, :], op=ALU.add),
                     reads=[Ga2.r], writes=[Ga2.r])
                wT = wT_r.next()
                S.op("dve", lambda: V.tensor_tensor(out=wT[:], in0=Ga2[:, 0, :], in1=gA[:], op=ALU.mult),
                     reads=[Ga2.r, gA.r], writes=[wT.r])
                grp.append((wT, eub))
                emit_units(8)
                if len(grp) == GK or e1 == NE1 - 1:
                    for ti in range(4):
                        for cbk in range(4):
                            pending.append((list(grp), ti, cbk, ngrp == 0))
                    grp = []
                    ngrp += 1
            emit_units(len(pending))
            es_f = ExitStack()
            x1_r = C.sbring(1, [128, D], F32, "x1t", es_f)
            for ti in range(4):
                i = Q * 4 + ti
                x1 = x1_r.next()
                S.dma("sp", x1[:], out_d[i * 128:(i + 1) * 128, :], reads=[out_res[i]], writes=[x1.r])
                S.op("dve", lambda: V.tensor_tensor(out=acc[:, ti, :], in0=acc[:, ti, :], in1=gate2b[:], op=ALU.mult),
                     reads=[acc.r, gate2b.r], writes=[acc.r])
                S.op("pool", lambda: G.tensor_tensor(out=acc[:, ti, :], in0=acc[:, ti, :], in1=x1[:], op=ALU.add),
                     reads=[acc.r, x1.r], writes=[acc.r])
                S.dma("sp", out_d[i * 128:(i + 1) * 128, :], acc[:, ti, :], reads=[acc.r, out_res[i]], writes=[out_res[i]])
            S.barrier()
            es_f.close()
        S.barrier()
        es_p.close()

        for q in ("sp", "pool", "act"):
            for sem, val in S.dma_slots[q]:
                if val > 0:
                    S._wait("sp", (sem, val))
        print("instructions:", S.ninstr)
    return nc


def _host_layouts(inp):
    f = lambda a: np.ascontiguousarray(a, dtype=np.float32)
    L = {}
    w_ada = inp["w_ada"][0]
    L["wada_r"] = f(w_ada.reshape(KC, 128, 24, 512).transpose(2, 1, 0, 3))
    L["bada_r"] = f(inp["b_ada"][0].reshape(96, 128).T)
    L["g1_r"] = f(inp["norm1_gain"][0].reshape(KC, 128).T)
    L["g2_r"] = f(inp["norm2_gain"][0].reshape(KC, 128).T)
    w_in = inp["w_in"][0]
    o_fq, o_fk, o_fv, o_ff, o_mq, o_mk, o_mv, o_mo, o_mi, o_mf = 0, 1024, 2048, 3072, 3080, 4104, 5128, 6152, 7176, 7180
    cols = []
    for h in range(8):
        cols += [o_fq + 128 * h, o_fk + 128 * h, o_fv + 128 * h]
    for m in range(4):
        cols += [o_mq + 256 * m, o_mq + 256 * m + 128, o_mk + 256 * m, o_mk + 256 * m + 128,
                 o_mv + 256 * m, o_mv + 256 * m + 128, o_mo + 256 * m, o_mo + 256 * m + 128]
    wfm = np.empty((56, 128, KC, 128), np.float32)
    for i, c0 in enumerate(cols):
        wfm[i] = w_in[:, c0:c0 + 128].reshape(KC, 128, 128).transpose(1, 0, 2)
    L["wfm_r"] = wfm
    gcols = list(range(o_ff, o_ff + 8)) + list(range(o_mf, o_mf + 4)) + list(range(o_mi, o_mi + 4))
    L["wg_r"] = f(w_in[:, gcols].reshape(KC, 128, 16).transpose(1, 0, 2))
    L["gb_r"] = f(np.concatenate([inp["fox_f_bias"][0], inp["mlstm_f_bias"][0], inp["mlstm_i_bias"][0]]).reshape(16, 1))
    L["qkg_r"] = f(np.stack([inp["fox_q_gain"][0], inp["fox_k_gain"][0]], axis=1))
    L["convw_r"] = f(inp["mlstm_conv_w"][0].reshape(4, 16, 128).transpose(2, 1, 0))
    L["convb_r"] = f(inp["mlstm_conv_b"][0].reshape(16, 128).T)
    L["hg_r"] = f(inp["mlstm_head_gain"][0].reshape(8, 128).T)
    L["wout_r"] = f(inp["w_out"][0].reshape(KC, 128, 4, 512).transpose(2, 1, 0, 3))
    L["wq_r"] = f(inp["peer_w_query"][0].reshape(KC, 128, 16, 128).transpose(2, 1, 0, 3))
    L["k1t_r"] = f(inp["peer_sub_keys_1"][0].T)
    L["k2t_r"] = f(inp["peer_sub_keys_2"][0].T)
    ed = inp["peer_expert_down"][0]
    L["edt_r"] = f(ed.reshape(128, 128, KC, 128).transpose(0, 3, 2, 1))
    L["eu_r"] = f(inp["peer_expert_up"][0].reshape(128, 128, D))
    return L


def _core_inputs(inputs, b):
    return {
        "x": np.ascontiguousarray(inputs["x"][b], dtype=np.float32),
        "c_r": np.ascontiguousarray(inputs["c"][b].reshape(KC, 128).T, dtype=np.float32),
    }


def kernel(**inputs):
    inputs = {k: np.asarray(v) for k, v in inputs.items()}
    L = _host_layouts(inputs)
    nc = build_program()
    outs = []
    for g0 in range(0, 8, CORES_PER_LAUNCH):
        in_maps = []
        for b in range(g0, g0 + CORES_PER_LAUNCH):
            m = dict(L)
            m.update(_core_inputs(inputs, b))
            in_maps.append(m)
        res = run_bass_kernel_spmd(nc, in_maps, core_ids=list(range(CORES_PER_LAUNCH)))
        outs += [np.asarray(r["out"], dtype=np.float32) for r in res.results]
    return np.stack(outs, axis=0)
```

```python
from contextlib import ExitStack
import numpy as np
import concourse.bass as bass
import concourse.mybir as mybir
from concourse.bass_utils import run_bass_kernel_spmd

F32 = mybir.dt.float32
BF16 = mybir.dt.bfloat16
ALU = mybir.AluOpType
AF = mybir.ActivationFunctionType
AX = mybir.AxisListType

import os
NBLK = int(os.environ.get("NBLK", "24"))
ADAQ = os.environ.get("ADAQ", "pool")
D = 2048
T = 2048
KC = 16
NT = 16
EPS = 1e-6


class Res:
    __slots__ = ("name", "w", "r")

    def __init__(self, name=""):
        self.name = name
        self.w = None
        self.r = []


class Sched:
    SEM_LIMIT = 30000

    def __init__(self, nc, es):
        self.nc = nc
        self.es = es
        self.engs = {"pe": nc.tensor, "act": nc.scalar, "dve": nc.vector,
                     "pool": nc.gpsimd, "sp": nc.sync}
        self.sem = {}
        self.cnt = {}
        self.nsem = 0
        self.pe_sems = []
        self.pend = {}
        for e in ("pe", "act", "dve", "pool"):
            self._new_sem(e)
        self.waited = {e: {} for e in self.engs}
        self.dma_slots = {}
        self.dma_next = {}
        for q, n in (("sp", 8), ("pool", 2), ("act", 4)):
            self.dma_slots[q] = [[self._mk(f"d{q}{i}"), 0] for i in range(n)]
            self.dma_next[q] = 0
        self.ninstr = 0

    def _mk(self, name):
        self.nsem += 1
        return self.es.enter_context(self.nc.semaphore(f"{name}_{self.nsem}"))

    def _new_sem(self, e):
        self.sem[e] = self._mk(f"s{e}")
        self.cnt[e] = 0
        if e == "pe":
            self.pe_sems.append(self.sem[e])

    def _wait(self, e, tok):
        sem, val = tok
        if e == "pe" and sem in self.pe_sems:
            return
        key = id(sem)
        if self.waited[e].get(key, 0) >= val:
            return
        self.engs[e].wait_ge(sem, val)
        self.waited[e][key] = val

    def _deps(self, e, reads, writes):
        toks = []
        for r in reads:
            if r.w is not None:
                toks.append(r.w)
        for w in writes:
            if w.w is not None:
                toks.append(w.w)
            toks.extend(w.r)
        for t in toks:
            self._wait(e, t)

    def _mark(self, tok, reads, writes):
        for w in writes:
            w.w = tok
            w.r = []
        for r in reads:
            if r in writes:
                continue
            r.r.append(tok)
            if len(r.r) > 24:
                r.r = r.r[-24:]

    def op(self, e, fn, reads=(), writes=(), inc=True):
        self._deps(e, reads, writes)
        if not self.pend.get(e, False) and self.cnt[e] >= self.SEM_LIMIT:
            self._new_sem(e)
        self.pend[e] = not inc
        ins = fn()
        self.ninstr += 1
        if inc:
            ins.then_inc(self.sem[e], 1)
            self.cnt[e] += 1
            tok = (self.sem[e], self.cnt[e])
            self._pending_ok = True
        else:
            tok = (self.sem[e], self.cnt[e] + 1)
        self._mark(tok, reads, writes)
        return tok

    def dma(self, q, out, in_, reads=(), writes=(), **kw):
        slots = self.dma_slots[q]
        i = self.dma_next[q]
        self.dma_next[q] = (i + 1) % len(slots)
        sem, val = slots[i]
        if val > 0:
            self._wait(q, (sem, val))
        self._deps(q, reads, writes)
        ins = self.engs[q].dma_start(out=out, in_=in_, **kw)
        ins.then_inc(sem, 16)
        slots[i][1] = val + 16
        tok = (sem, val + 16)
        self._mark(tok, reads, writes)
        self.ninstr += 1
        return tok

    def barrier(self):
        toks = []
        for e in ("pe", "act", "dve", "pool"):
            if self.cnt[e] > 0:
                assert not self.pend.get(e, False), f"open group on {e} at barrier"
                toks.append((self.sem[e], self.cnt[e]))
        for q in self.dma_slots:
            for sem, val in self.dma_slots[q]:
                if val > 0:
                    toks.append((sem, val))
        for e in self.engs:
            for t in toks:
                self._wait(e, t)

    def wait_all(self, e, ress):
        for r in ress:
            if r.w is not None:
                self._wait(e, r.w)


class Buf:
    def __init__(self, t, name):
        self.t = t
        self.r = Res(name)

    def __getitem__(self, idx):
        return self.t[idx]


class Ring:
    def __init__(self, bufs):
        self.bufs = bufs
        self.i = 0

    def next(self):
        b = self.bufs[self.i]
        self.i = (self.i + 1) % len(self.bufs)
        return b


class Ctx:
    def __init__(self, nc, es):
        self.nc = nc
        self.es = es
        self.S = Sched(nc, es)
        self.n = 0

    def sb(self, shape, dt, name, es=None):
        self.n += 1
        t = (es or self.es).enter_context(self.nc.sbuf_tensor(f"{name}_{self.n}", list(shape), dt))
        return Buf(t, name)

    def ps(self, shape, dt, name, es=None):
        self.n += 1
        t = (es or self.es).enter_context(self.nc.psum_tensor(f"{name}_{self.n}", list(shape), dt))
        return Buf(t, name)

    def sbring(self, n, shape, dt, name, es=None):
        return Ring([self.sb(shape, dt, f"{name}{i}", es) for i in range(n)])


NE1 = int(os.environ.get("NE1", "128"))
NQ = int(os.environ.get("NQ", "4"))
NFOX = int(os.environ.get("NFOX", "8"))
NML = int(os.environ.get("NML", "4"))
GK = 4
NEG = -1.0e30
CORES_PER_LAUNCH = 8


def build_program(stage=99, dbg=False):
    nc = bass.Bass("TRN2", target_bir_lowering=False)

    def din(name, shape, dt=F32):
        return nc.dram_tensor(name, list(shape), dt, kind="ExternalInput").ap()

    x_d = din("x", [T, D])
    c_d = din("c_r", [128, KC])
    wada_d = din("wada_r", [24, 128, KC, 512])
    bada_d = din("bada_r", [128, 96])
    g1_d = din("g1_r", [128, KC])
    g2_d = din("g2_r", [128, KC])
    wfm_d = din("wfm_r", [56, 128, KC, 128])
    wg_d = din("wg_r", [128, KC, 16])
    gb_d = din("gb_r", [16, 1])
    qkg_d = din("qkg_r", [128, 2])
    cw_d = din("convw_r", [128, 16, 4])
    cb_d = din("convb_r", [128, 16])
    hg_d = din("hg_r", [128, 8])
    wout_d = din("wout_r", [4, 128, KC, 512])
    wq_d = din("wq_r", [16, 128, KC, 128])
    k1t_d = din("k1t_r", [128, 128])
    k2t_d = din("k2t_r", [128, 128])
    edt_d = din("edt_r", [128, 128, KC, 128])
    eu_d = din("eu_r", [128, 128, D])
    out_d = nc.dram_tensor("out", [T, D], F32, kind="ExternalOutput").ap()
    mix_d = nc.dram_tensor("mix_scr", [KC, 128, T], BF16, kind="Internal").ap()
    if dbg:
        dbg_d = nc.dram_tensor("dbg", [128, 4096], F32, kind="ExternalOutput").ap()

    with ExitStack() as es:
        C = Ctx(nc, es)
        S = C.S
        V, A, P, G = nc.vector, nc.scalar, nc.tensor, nc.gpsimd

        psb = Ring([C.ps([128, 512], F32, f"ps{i}") for i in range(4)])
        acc_ps = [C.ps([128, 512], F32, f"pacc{i}") for i in range(4)]
        out_res = [Res(f"out{i}") for i in range(NT)]
        mix_res = [Res(f"mix{i}") for i in range(KC)]

        ident_f = C.sb([128, 128], F32, "ident_f")
        ident_b = C.sb([128, 128], BF16, "ident_b")
        ones_f = C.sb([128, 128], F32, "ones_f")
        ones_b = C.sb([128, 128], BF16, "ones_b")
        sel127 = C.sb([128, 128], F32, "sel127")
        tri_b = C.sb([128, 128], BF16, "tri_b")
        zcol = C.sb([128, 1], F32, "zcol")
        lncol = C.sb([128, 1], F32, "lncol")
        S.op("pool", lambda: G.memset(ones_f[:], 1.0), writes=[ones_f.r])
        S.op("pool", lambda: G.memset(ones_b[:], 1.0), writes=[ones_b.r])
        S.op("pool", lambda: G.memset(zcol[:], 0.0), writes=[zcol.r])
        S.op("pool", lambda: G.memset(lncol[:], float(np.log(1.0 / 16.0))), writes=[lncol.r])
        S.op("pool", lambda: G.affine_select(out=ident_f[:], in_=ones_f[:], pattern=[[-1, 128]],
                                             compare_op=ALU.is_equal, fill=0.0, base=0,
                                             channel_multiplier=1),
             reads=[ones_f.r], writes=[ident_f.r])
        S.op("pool", lambda: G.tensor_copy(out=ident_b[:], in_=ident_f[:]),
             reads=[ident_f.r], writes=[ident_b.r])
        S.op("pool", lambda: G.affine_select(out=sel127[:], in_=ones_f[:], pattern=[[0, 128]],
                                             compare_op=ALU.is_equal, fill=0.0, base=-127,
                                             channel_multiplier=1),
             reads=[ones_f.r], writes=[sel127.r])
        S.op("pool", lambda: G.affine_select(out=tri_b[:], in_=ones_b[:], pattern=[[1, 128]],
                                             compare_op=ALU.is_ge, fill=0.0, base=0,
                                             channel_multiplier=-1),
             reads=[ones_b.r], writes=[tri_b.r])

        def load_small(d_ap, shape, name, dt=F32):
            b = C.sb(shape, dt, name)
            S.dma("sp", b[:], d_ap, writes=[b.r])
            return b

        c_sb = load_small(c_d, [128, KC], "c_sb")
        bada_sb = load_small(bada_d, [128, 96], "bada")
        g1_sb = load_small(g1_d, [128, KC], "g1")
        g2_sb = load_small(g2_d, [128, KC], "g2")
        gb_sb = load_small(gb_d, [16, 1], "gb")
        qkg_sb = load_small(qkg_d, [128, 2], "qkg")
        cw_sb = load_small(cw_d, [128, 16, 4], "cw")
        cb_sb = load_small(cb_d, [128, 16], "cb")
        hg_sb = load_small(hg_d, [128, 8], "hg")
        k1t_f = load_small(k1t_d, [128, 128], "k1tf")
        k2t_f = load_small(k2t_d, [128, 128], "k2tf")
        wg_f = load_small(wg_d, [128, KC, 16], "wgf")
        k1t_b = C.sb([128, 128], BF16, "k1tb")
        k2t_b = C.sb([128, 128], BF16, "k2tb")
        wg_b = C.sb([128, KC, 16], BF16, "wgb")
        S.op("pool", lambda: G.tensor_copy(out=k1t_b[:], in_=k1t_f[:]), reads=[k1t_f.r], writes=[k1t_b.r])
        S.op("pool", lambda: G.tensor_copy(out=k2t_b[:], in_=k2t_f[:]), reads=[k2t_f.r], writes=[k2t_b.r])
        S.op("pool", lambda: G.tensor_copy(out=wg_b[:], in_=wg_f[:]), reads=[wg_f.r], writes=[wg_b.r])
        qsc = C.sb([128, 2], F32, "qsc")
        S.op("dve", lambda: V.tensor_scalar(out=qsc[:, 0:1], in0=qkg_sb[:, 0:1], scalar1=128.0 ** -0.5,
                                            scalar2=None, op0=ALU.mult), reads=[qkg_sb.r], writes=[qsc.r])
        S.op("dve", lambda: V.tensor_copy(out=qsc[:, 1:2], in_=qkg_sb[:, 1:2]), reads=[qkg_sb.r, qsc.r], writes=[qsc.r])

        sc_sb = C.sb([128, KC], F32, "sc_sb")
        mod = C.sb([128, 96], F32, "mod")
        S.op("act", lambda: A.activation(out=sc_sb[:], in_=c_sb[:], func=AF.Silu),
             reads=[c_sb.r], writes=[sc_sb.r])
        es_ada = ExitStack()
        wada_ring = C.sbring(2, [128, KC, 512], F32, "wada", es_ada)
        for jb in range(24):
            wb = wada_ring.next()
            S.dma("sp" if jb % 2 == 0 else "act", wb[:], wada_d[jb], writes=[wb.r])
            pm = psb.next()
            for jj in range(4):
                for kc in range(KC):
                    S.op("pe", lambda kc=kc, jj=jj, pm=pm, wb=wb: P.matmul(
                        pm[:, jj:jj + 1], lhsT=wb[:, kc, jj * 128:(jj + 1) * 128],
                        rhs=sc_sb[:, kc:kc + 1], start=(kc == 0), stop=(kc == KC - 1)),
                        reads=[wb.r, sc_sb.r], writes=[pm.r], inc=(kc == KC - 1 and jj == 3))
            S.op("dve", lambda jb=jb, pm=pm: V.tensor_tensor(
                out=mod[:, jb * 4:jb * 4 + 4], in0=pm[:, 0:4],
                in1=bada_sb[:, jb * 4:jb * 4 + 4], op=ALU.add),
                reads=[pm.r, bada_sb.r], writes=[mod.r])
        S.barrier()
        es_ada.close()
        A1 = C.sb([128, KC], F32, "A1")
        A2 = C.sb([128, KC], F32, "A2")
        S.op("dve", lambda: V.scalar_tensor_tensor(out=A1[:], in0=mod[:, 16:32], scalar=1.0, in1=g1_sb[:],
                                                   op0=ALU.add, op1=ALU.mult),
             reads=[mod.r, g1_sb.r], writes=[A1.r])
        S.op("dve", lambda: V.scalar_tensor_tensor(out=A2[:], in0=mod[:, 64:80], scalar=1.0, in1=g2_sb[:],
                                                   op0=ALU.add, op1=ALU.mult),
             reads=[mod.r, g2_sb.r], writes=[A2.r])

        dg = C.sb([128, 128], F32, "diag")

        def build_gate(gt, c0):
            for kc in range(KC):
                S.op("dve", lambda kc=kc: V.tensor_scalar(
                    out=dg[:], in0=ident_f[:], scalar1=mod[:, c0 + kc:c0 + kc + 1], scalar2=None,
                    op0=ALU.mult), reads=[ident_f.r, mod.r], writes=[dg.r])
                pm = psb.next()
                S.op("pe", lambda pm=pm: P.matmul(pm[:, 0:128], lhsT=ones_f[:], rhs=dg[:], start=True, stop=True),
                     reads=[ones_f.r, dg.r], writes=[pm.r])
                S.op("act", lambda pm=pm, kc=kc: A.copy(out=gt[:, kc * 128:(kc + 1) * 128], in_=pm[:, 0:128]),
                     reads=[pm.r], writes=[gt.r])

        def norm_to_T(es_l, src_rows, src_res, Asc, shift_c0, dstT, ntiles, nring):
            xr = C.sbring(nring, [128, D], F32, "xt", es_l)
            xn_r = C.sbring(nring, [128, D], BF16, "xn", es_l)
            ssr = C.sbring(2, [128, 2], F32, "ss", es_l)
            for i in range(ntiles):
                xt = xr.next()
                S.dma("sp", xt[:], src_rows(i), reads=[src_res(i)] if src_res else [], writes=[xt.r])
                ss = ssr.next()
                S.op("dve", lambda ss=ss: V.memset(ss[:], 0.0), writes=[ss.r])
                xn = xn_r.next()
                S.op("act", lambda xt=xt, ss=ss, xn=xn: A.activation(out=xn[:], in_=xt[:], func=AF.Square,
                                                                     accum_out=ss[:, 0:1]),
                     reads=[xt.r, ss.r], writes=[xn.r, ss.r])
                S.op("dve", lambda ss=ss: V.tensor_scalar(out=ss[:, 1:2], in0=ss[:, 0:1], scalar1=1.0 / D,
                                                          scalar2=EPS, op0=ALU.mult, op1=ALU.add),
                     reads=[ss.r], writes=[ss.r])
                S.op("act", lambda ss=ss: A.activation(out=ss[:, 1:2], in_=ss[:, 1:2], func=AF.Sqrt),
                     reads=[ss.r], writes=[ss.r])
                S.op("dve", lambda ss=ss: V.reciprocal(out=ss[:, 1:2], in_=ss[:, 1:2]),
                     reads=[ss.r], writes=[ss.r])
                S.op("act", lambda xt=xt, ss=ss, xn=xn: A.activation(out=xn[:], in_=xt[:], func=AF.Copy,
                                                                     scale=ss[:, 1:2]),
                     reads=[xt.r, ss.r], writes=[xn.r])
                for k4 in range(4):
                    pm = psb.next()
                    pmb = pm.t.bitcast(BF16)
                    for j in range(4):
                        kc = k4 * 4 + j
                        S.op("pe", lambda kc=kc, j=j, pmb=pmb, xn=xn: P.transpose(
                            out=pmb[:, j * 128:(j + 1) * 128], in_=xn[:, kc * 128:(kc + 1) * 128],
                            identity=ident_b[:]), reads=[xn.r, ident_b.r], writes=[pm.r], inc=(j == 3))
                    for j in range(4):
                        kc = k4 * 4 + j
                        S.op("dve", lambda kc=kc, j=j, pmb=pmb, i=i: V.tensor_scalar(
                            out=dstT[:, kc, i * 128:(i + 1) * 128], in0=pmb[:, j * 128:(j + 1) * 128],
                            scalar1=Asc[:, kc:kc + 1], scalar2=mod[:, shift_c0 + kc:shift_c0 + kc + 1],
                            op0=ALU.mult, op1=ALU.add),
                            reads=[pm.r, Asc.r, mod.r], writes=[dstT.r])

        es_m = ExitStack()
        wst = C.sbring(2, [128, KC, 128], F32, "wst", es_m)
        wbf = C.sbring(2, [128, KC, 128], BF16, "wbf", es_m)
        wq_flip = [0]

        def load_wchunk(d_ap):
            st = wst.next()
            q = "sp" if wq_flip[0] % 2 == 0 else "act"
            wq_flip[0] += 1
            S.dma(q, st[:], d_ap, writes=[st.r])
            wb = wbf.next()
            S.op("pool", lambda: G.tensor_copy(out=wb[:], in_=st[:]), reads=[st.r], writes=[wb.r])
            return wb

        h1T = C.sb([128, KC, T], BF16, "h1T", es_m)
        Ltok = C.sb([128, NT, 16], F32, "Ltok", es_m)
        Gtok = C.sb([128, NT, 16], F32, "Gtok", es_m)
        LrefB = C.sb([128, 64], F32, "LrefB", es_m)
        EQ = C.sb([16, T], BF16, "EQ", es_m)
        selm = C.sb([16, 4 * 128], BF16, "selm", es_m)
        qTr = [C.sb([128, T], BF16, f"qT{i}", es_m) for i in range(2)]
        kTr = [C.sb([128, T], BF16, f"kT{i}", es_m) for i in range(2)]
        qpr = [C.sbring(2, [128, 512], BF16, f"qp{i}", es_m) for i in range(2)]
        vtok = C.sb([128, NT, 256], BF16, "vtok", es_m)
        sigo = [C.sb([128, T], BF16, f"sigo{i}", es_m) for i in range(2)]
        rawc = C.sb([128, T + 4], F32, "rawc", es_m)
        cv = C.sb([128, T], F32, "cv", es_m)
        rawf_r = C.sbring(2, [128, 512], F32, "rawf", es_m)
        sqb_r = C.sbring(2, [128, 512], BF16, "sqb", es_m)
        rs_r = C.sbring(2, [128, 512], F32, "rs", es_m)
        pt_r = C.sbring(3, [128, 512], BF16, "pt", es_m)
        kb_r = C.sbring(2, [128, NT], F32, "kb", es_m)
        ktmp = C.sb([128, NT], F32, "ktmp", es_m)
        hT_r = [C.sbring(1, [128, 512], F32, f"hT{i}", es_m) for i in range(2)]
        mixo_r = C.sbring(2, [128, T], BF16, "mixo", es_m)
        es_n1 = ExitStack()
        norm_to_T(es_n1, lambda i: x_d[i * 128:(i + 1) * 128, :], None, A1, 0, h1T, NT, 2)
        S.barrier()
        es_n1.close()

        es_g = ExitStack()
        graw = C.sb([16, T], F32, "graw", es_g)
        lsp = C.sb([16, T], F32, "lsp", es_g)
        Lc = C.sb([16, T], F32, "Lc", es_g)
        for c in range(4):
            pm = psb.next()
            for kc in range(KC):
                S.op("pe", lambda kc=kc, pm=pm, c=c: P.matmul(
                    pm[0:16, :], lhsT=wg_b[:, kc, :], rhs=h1T[:, kc, c * 512:(c + 1) * 512],
                    start=(kc == 0), stop=(kc == KC - 1)),
                    reads=[wg_b.r, h1T.r], writes=[pm.r], inc=(kc == KC - 1))
            S.op("act", lambda pm=pm, c=c: A.activation(out=graw[:, c * 512:(c + 1) * 512], in_=pm[0:16, :],
                                                        func=AF.Identity, bias=gb_sb[:, 0:1]),
                 reads=[pm.r, gb_sb.r], writes=[graw.r])
        S.op("act", lambda: A.activation(out=lsp[:], in_=graw[:], func=AF.Exp, scale=-1.0),
             reads=[graw.r], writes=[lsp.r])
        S.op("act", lambda: A.activation(out=lsp[:], in_=lsp[:], func=AF.Ln, bias=ones_f[0:16, 0:1]),
             reads=[lsp.r, ones_f.r], writes=[lsp.r])
        S.op("dve", lambda: V.tensor_tensor_scan(out=Lc[:], data0=ones_f[0:16, 0:1].broadcast_to([16, T]), data1=lsp[:],
                                                 initial=zcol[0:16, 0:1], op0=ALU.mult, op1=ALU.add),
             reads=[ones_f.r, lsp.r, zcol.r], writes=[Lc.r])
        for (src, dst) in ((Lc, Ltok), (graw, Gtok)):
            for i4 in range(4):
                pm = psb.next()
                for j in range(4):
                    i = i4 * 4 + j
                    S.op("pe", lambda i=i, j=j, pm=pm, src=src: P.transpose(
                        out=pm[:, j * 16:(j + 1) * 16], in_=src[0:16, i * 128:(i + 1) * 128],
                        identity=ident_f[0:16, 0:16]), reads=[src.r, ident_f.r], writes=[pm.r], inc=(j == 3))
                S.op("dve", lambda i4=i4, pm=pm, dst=dst: V.tensor_copy(
                    out=dst[:, i4 * 4:(i4 + 1) * 4, :],
                    in_=pm[:, 0:64].rearrange("p (a b) -> p a b", a=4)),
                    reads=[pm.r], writes=[dst.r])
        pm = psb.next()
        for c in range(4):
            S.op("pe", lambda c=c, pm=pm: P.matmul(pm[:, c * 16:(c + 1) * 16], lhsT=sel127[:],
                                                   rhs=Ltok[:, 4 * c + 3, :], start=True, stop=True),
                 reads=[sel127.r, Ltok.r], writes=[pm.r], inc=(c == 3))
        S.op("dve", lambda pm=pm: V.tensor_copy(out=LrefB[:], in_=pm[:, 0:64]), reads=[pm.r], writes=[LrefB.r])
        for c in range(4):
            S.op("act", lambda c=c: A.activation(out=EQ[0:12, c * 512:(c + 1) * 512], in_=Lc[0:12, c * 512:(c + 1) * 512],
                                                 func=AF.Exp, scale=-1.0,
                                                 bias=Lc[0:12, c * 512 + 511:c * 512 + 512]),
                 reads=[Lc.r], writes=[EQ.r])
        for m in range(4):
            S.op("dve", lambda m=m: V.tensor_scalar(out=selm[:, m * 128:(m + 1) * 128], in0=ones_f[0:16, :],
                                                    scalar1=ident_f[0:16, 8 + m:9 + m], scalar2=None,
                                                    op0=ALU.mult),
                 reads=[ones_f.r, ident_f.r], writes=[selm.r])

        S.barrier()
        es_g.close()
        S.op("pool", lambda: G.memset(rawc[:, 0:4], 0.0), writes=[rawc.r])

        def proj_fm(wb, c, pm):
            for kc in range(KC):
                S.op("pe", lambda kc=kc: P.matmul(pm[:], lhsT=wb[:, kc, :], rhs=h1T[:, kc, c * 512:(c + 1) * 512],
                                                  start=(kc == 0), stop=(kc == KC - 1)),
                     reads=[wb.r, h1T.r], writes=[pm.r], inc=(kc == KC - 1))

        def fox_qk(wb, dst, gcol):
            def post(c, sqb, rawf):
                pn = psb.next()
                S.op("pe", lambda: P.matmul(pn[:], lhsT=ones_b[:], rhs=sqb[:], start=True, stop=True),
                     reads=[ones_b.r, sqb.r], writes=[pn.r])
                rs = rs_r.next()
                S.op("dve", lambda: V.tensor_scalar(out=rs[:], in0=pn[:], scalar1=1.0 / 128, scalar2=EPS,
                                                    op0=ALU.mult, op1=ALU.add), reads=[pn.r], writes=[rs.r])
                S.op("act", lambda: A.activation(out=rs[:], in_=rs[:], func=AF.Sqrt), reads=[rs.r], writes=[rs.r])
                S.op("dve", lambda: V.reciprocal(out=rs[:], in_=rs[:]), reads=[rs.r], writes=[rs.r])
                S.op("dve", lambda: V.scalar_tensor_tensor(out=dst[:, c * 512:(c + 1) * 512], in0=rawf[:],
                                                           scalar=qsc[:, gcol:gcol + 1], in1=rs[:],
                                                           op0=ALU.mult, op1=ALU.mult),
                     reads=[rawf.r, qsc.r, rs.r], writes=[dst.r])

            prev = None
            for c in range(4):
                pm = psb.next()
                proj_fm(wb, c, pm)
                sqb = sqb_r.next()
                rawf = rawf_r.next()
                S.op("act", lambda: A.activation(out=sqb[:], in_=pm[:], func=AF.Square), reads=[pm.r], writes=[sqb.r])
                S.op("act", lambda: A.copy(out=rawf[:], in_=pm[:]), reads=[pm.r], writes=[rawf.r])
                if prev is not None:
                    post(*prev)
                prev = (c, sqb, rawf)
            post(*prev)

        def v_tok(wb, dvc):
            for i4 in range(4):
                pm = psb.next()
                for j in range(4):
                    i = i4 * 4 + j
                    for kc in range(KC):
                        S.op("pe", lambda kc=kc, i=i, j=j: P.matmul(
                            pm[:, j * 128:(j + 1) * 128], lhsT=h1T[:, kc, i * 128:(i + 1) * 128], rhs=wb[:, kc, :],
                            start=(kc == 0), stop=(kc == KC - 1)),
                            reads=[h1T.r, wb.r], writes=[pm.r], inc=(kc == KC - 1 and j == 3))
                S.op("act", lambda: A.copy(out=vtok[:, i4 * 4:(i4 + 1) * 4, dvc * 128:(dvc + 1) * 128],
                                           in_=pm[:].rearrange("p (a b) -> p a b", a=4)),
                     reads=[pm.r], writes=[vtok.r])

        def ml_qk(wb, dst, cch):
            for c in range(4):
                pm = psb.next()
                proj_fm(wb, c, pm)
                S.op("act", lambda: A.copy(out=rawc[:, 4 + c * 512:4 + (c + 1) * 512], in_=pm[:]),
                     reads=[pm.r], writes=[rawc.r])
            S.op("dve", lambda: V.tensor_scalar(out=cv[:], in0=rawc[:, 4:4 + T], scalar1=cw_sb[:, cch, 3:4],
                                                scalar2=cb_sb[:, cch:cch + 1], op0=ALU.mult, op1=ALU.add),
                 reads=[rawc.r, cw_sb.r, cb_sb.r], writes=[cv.r])
            for j in range(3):
                S.op("dve", lambda j=j: V.scalar_tensor_tensor(out=cv[:], in0=rawc[:, 1 + j:1 + j + T],
                                                               scalar=cw_sb[:, cch, j:j + 1], in1=cv[:],
                                                               op0=ALU.mult, op1=ALU.add),
                     reads=[rawc.r, cw_sb.r, cv.r], writes=[cv.r])
            S.op("act", lambda: A.activation(out=dst[:], in_=cv[:], func=AF.Silu), reads=[cv.r], writes=[dst.r])

        def ml_o(wb, dst):
            for c in range(4):
                pm = psb.next()
                proj_fm(wb, c, pm)
                S.op("act", lambda: A.activation(out=dst[:, c * 512:(c + 1) * 512], in_=pm[:], func=AF.Sigmoid),
                     reads=[pm.r], writes=[dst.r])

        def attend(nd, ndv, row, is_fox, mrow, out_chunks, hgc0):
            mixo = [mixo_r.next() for _ in range(ndv)]
            for c in range(4):
                kb = kb_r.next()
                if is_fox:
                    S.op("dve", lambda: V.tensor_scalar(out=kb[:], in0=Ltok[:, :, row],
                                                        scalar1=LrefB[:, c * 16 + row:c * 16 + row + 1],
                                                        scalar2=None, op0=ALU.subtract),
                         reads=[Ltok.r, LrefB.r], writes=[kb.r])
                    qs = [qTr[d][:, c * 512:(c + 1) * 512] for d in range(nd)]
                    qres = [qTr[d].r for d in range(nd)]
                else:
                    S.op("dve", lambda: V.scalar_tensor_tensor(out=ktmp[:, 0:4 * c + 4], in0=Ltok[:, 0:4 * c + 4, row],
                                                               scalar=LrefB[:, c * 16 + row:c * 16 + row + 1],
                                                               in1=Gtok[:, 0:4 * c + 4, 12 + mrow],
                                                               op0=ALU.subtract, op1=ALU.add),
                         reads=[Ltok.r, LrefB.r, Gtok.r], writes=[ktmp.r])
                    S.op("act", lambda: A.activation(out=kb[:, 0:4 * c + 4], in_=ktmp[:, 0:4 * c + 4], func=AF.Exp, bias=lncol[:, 0:1]),
                         reads=[ktmp.r, lncol.r], writes=[kb.r])
                    pe_ = psb.next()
                    S.op("pe", lambda: P.matmul(pe_[:], lhsT=selm[0:12, mrow * 128:(mrow + 1) * 128],
                                                rhs=EQ[0:12, c * 512:(c + 1) * 512], start=True, stop=True),
                         reads=[selm.r, EQ.r], writes=[pe_.r])
                    qs, qres = [], []
                    for d in range(nd):
                        qp = qpr[d].next()
                        S.op("dve", lambda d=d, qp=qp: V.tensor_tensor(out=qp[:], in0=qTr[d][:, c * 512:(c + 1) * 512],
                                                                       in1=pe_[:], op=ALU.mult),
                             reads=[qTr[d].r, pe_.r], writes=[qp.r])
                        qs.append(qp[:])
                        qres.append(qp.r)
                pO = [acc_ps[d] for d in range(ndv)]
                pD = acc_ps[2]
                nj = 4 * c + 4

                def emit_pv(j, lo, pt):
                    for dv in range(ndv):
                        S.op("pe", lambda dv=dv: P.matmul(pO[dv][:, lo:512], lhsT=vtok[:, j, dv * 128:(dv + 1) * 128],
                                                          rhs=pt[:, lo:512], start=(j == 0), stop=(j == nj - 1)),
                             reads=[vtok.r, pt.r], writes=[pO[dv].r], inc=False)
                    S.op("pe", lambda: P.matmul(pD[:, lo:512], lhsT=ones_b[:], rhs=pt[:, lo:512],
                                                start=(j == 0), stop=(j == nj - 1)),
                         reads=[ones_b.r, pt.r], writes=[pD.r])

                prevpv = None
                for j in range(nj):
                    lo = 128 * (j - 4 * c) if j >= 4 * c else 0
                    pS = psb.next()
                    for d in range(nd):
                        S.op("pe", lambda d=d: P.matmul(pS[:, lo:512], lhsT=kTr[d][:, j * 128:(j + 1) * 128],
                                                        rhs=qs[d][:, lo:512], start=(d == 0), stop=(d == nd - 1)),
                             reads=[kTr[d].r, qres[d]], writes=[pS.r], inc=(d == nd - 1))
                    pt = pt_r.next()
                    if is_fox:
                        S.op("act", lambda: A.activation(out=pt[:, lo:512], in_=pS[:, lo:512], func=AF.Exp,
                                                         bias=kb[:, j:j + 1]),
                             reads=[pS.r, kb.r], writes=[pt.r])
                    else:
                        S.op("dve", lambda: V.tensor_scalar(out=pt[:, lo:512], in0=pS[:, lo:512],
                                                            scalar1=kb[:, j:j + 1], scalar2=None, op0=ALU.mult),
                             reads=[pS.r, kb.r], writes=[pt.r])
                    if j >= 4 * c:
                        S.op("pool", lambda: G.tensor_tensor(out=pt[:, lo:lo + 128], in0=pt[:, lo:lo + 128],
                                                             in1=tri_b[:], op=ALU.mult),
                             reads=[pt.r, tri_b.r], writes=[pt.r])
                    if prevpv is not None:
                        emit_pv(*prevpv)
                    prevpv = (j, lo, pt)
                emit_pv(*prevpv)
                rs = rs_r.next()
                if is_fox:
                    S.op("dve", lambda: V.reciprocal(out=rs[:], in_=pD[:]), reads=[pD.r], writes=[rs.r])
                    S.op("dve", lambda: V.tensor_tensor(out=mixo[0][:, c * 512:(c + 1) * 512], in0=pO[0][:],
                                                        in1=rs[:], op=ALU.mult),
                         reads=[pO[0].r, rs.r], writes=[mixo[0].r])
                else:
                    S.op("dve", lambda: V.tensor_scalar(out=rs[:], in0=pD[:], scalar1=-1.0, scalar2=1.0,
                                                        op0=ALU.mult, op1=ALU.max), reads=[pD.r], writes=[rs.r])
                    S.op("dve", lambda: V.scalar_tensor_tensor(out=rs[:], in0=pD[:], scalar=1.0, in1=rs[:],
                                                               op0=ALU.max, op1=ALU.max),
                         reads=[pD.r, rs.r], writes=[rs.r])
                    S.op("dve", lambda: V.reciprocal(out=rs[:], in_=rs[:]), reads=[rs.r], writes=[rs.r])
                    hTs = []
                    pn = psb.next()
                    for dv in range(ndv):
                        hT = hT_r[dv].next()
                        S.op("dve", lambda dv=dv, hT=hT: V.tensor_tensor(out=hT[:], in0=pO[dv][:], in1=rs[:], op=ALU.mult),
                             reads=[pO[dv].r, rs.r], writes=[hT.r])
                        sqb = sqb_r.next()
                        S.op("act", lambda hT=hT, sqb=sqb: A.activation(out=sqb[:], in_=hT[:], func=AF.Square),
                             reads=[hT.r], writes=[sqb.r])
                        S.op("pe", lambda dv=dv, sqb=sqb: P.matmul(pn[:], lhsT=ones_b[:], rhs=sqb[:],
                                                                    start=(dv == 0), stop=(dv == ndv - 1)),
                             reads=[ones_b.r, sqb.r], writes=[pn.r], inc=(dv == ndv - 1))
                        hTs.append(hT)
                    rs2 = rs_r.next()
                    S.op("dve", lambda: V.tensor_scalar(out=rs2[:], in0=pn[:], scalar1=1.0 / 256, scalar2=EPS,
                                                        op0=ALU.mult, op1=ALU.add), reads=[pn.r], writes=[rs2.r])
                    S.op("act", lambda: A.activation(out=rs2[:], in_=rs2[:], func=AF.Sqrt), reads=[rs2.r], writes=[rs2.r])
                    S.op("dve", lambda: V.reciprocal(out=rs2[:], in_=rs2[:]), reads=[rs2.r], writes=[rs2.r])
                    for dv in range(ndv):
                        S.op("dve", lambda dv=dv: V.scalar_tensor_tensor(
                            out=hTs[dv][:], in0=hTs[dv][:], scalar=hg_sb[:, hgc0 + dv:hgc0 + dv + 1], in1=rs2[:],
                            op0=ALU.mult, op1=ALU.mult), reads=[hTs[dv].r, hg_sb.r, rs2.r], writes=[hTs[dv].r])
                        S.op("dve", lambda dv=dv: V.tensor_tensor(
                            out=mixo[dv][:, c * 512:(c + 1) * 512], in0=hTs[dv][:],
                            in1=sigo[dv][:, c * 512:(c + 1) * 512], op=ALU.mult),
                            reads=[hTs[dv].r, sigo[dv].r], writes=[mixo[dv].r])
            for dv in range(ndv):
                S.dma("sp", mix_d[out_chunks[dv]], mixo[dv][:], reads=[mixo[dv].r],
                      writes=[mix_res[out_chunks[dv]]])

        for h in range(NFOX):
            fox_qk(load_wchunk(wfm_d[3 * h]), qTr[0], 0)
            fox_qk(load_wchunk(wfm_d[3 * h + 1]), kTr[0], 1)
            v_tok(load_wchunk(wfm_d[3 * h + 2]), 0)
            attend(1, 1, h, True, 0, [h], 0)
        for m in range(NML):
            b0 = 24 + 8 * m
            ml_qk(load_wchunk(wfm_d[b0 + 0]), qTr[0], 2 * m)
            ml_qk(load_wchunk(wfm_d[b0 + 1]), qTr[1], 2 * m + 1)
            ml_qk(load_wchunk(wfm_d[b0 + 2]), kTr[0], 8 + 2 * m)
            ml_qk(load_wchunk(wfm_d[b0 + 3]), kTr[1], 8 + 2 * m + 1)
            v_tok(load_wchunk(wfm_d[b0 + 4]), 0)
            v_tok(load_wchunk(wfm_d[b0 + 5]), 1)
            ml_o(load_wchunk(wfm_d[b0 + 6]), sigo[0])
            ml_o(load_wchunk(wfm_d[b0 + 7]), sigo[1])
            attend(2, 2, 8 + m, False, m, [8 + 2 * m, 8 + 2 * m + 1], 2 * m)
        S.barrier()
        es_m.close()

        es_o = ExitStack()
        mixT = C.sb([128, KC, T], BF16, "mixT", es_o)
        written = list(range(NFOX)) + [8 + j for j in range(2 * NML)]
        if len(written) < KC:
            S.op("pool", lambda: G.memset(mixT[:], 0.0), writes=[mixT.r])
        for kc in written:
            S.dma("sp" if kc % 2 == 0 else "act", mixT[:, kc, :], mix_d[kc], reads=[mix_res[kc]], writes=[mixT.r])
        gate1b = C.sb([128, D], F32, "gate1b", es_o)
        build_gate(gate1b, 32)
        wo_st = C.sb([128, KC, 512], F32, "wo_st", es_o)
        wo_bf = C.sbring(2, [128, KC, 512], BF16, "wo_bf", es_o)
        xs_r = C.sbring(3, [128, 512], F32, "xs", es_o)
        t1_r = C.sbring(3, [128, 512], F32, "t1", es_o)
        for cb in range(4):
            S.dma("sp", wo_st[:], wout_d[cb], writes=[wo_st.r])
            wob = wo_bf.next()
            S.op("pool", lambda: G.tensor_copy(out=wob[:], in_=wo_st[:]), reads=[wo_st.r], writes=[wob.r])
            for i in range(NT):
                pm = psb.next()
                for kc in range(KC):
                    S.op("pe", lambda kc=kc: P.matmul(pm[:], lhsT=mixT[:, kc, i * 128:(i + 1) * 128], rhs=wob[:, kc, :],
                                                      start=(kc == 0), stop=(kc == KC - 1)),
                         reads=[mixT.r, wob.r], writes=[pm.r], inc=(kc == KC - 1))
                xs = xs_r.next()
                S.dma("act", xs[:], x_d[i * 128:(i + 1) * 128, cb * 512:(cb + 1) * 512], writes=[xs.r])
                t1 = t1_r.next()
                S.op("dve", lambda: V.tensor_tensor(out=t1[:], in0=pm[:], in1=gate1b[:, cb * 512:(cb + 1) * 512],
                                                    op=ALU.mult), reads=[pm.r, gate1b.r], writes=[t1.r])
                S.op("pool", lambda: G.tensor_tensor(out=t1[:], in0=t1[:], in1=xs[:], op=ALU.add),
                     reads=[t1.r, xs.r], writes=[t1.r])
                S.dma("sp", out_d[i * 128:(i + 1) * 128, cb * 512:(cb + 1) * 512], t1[:], reads=[t1.r],
                      writes=[out_res[i]])
        S.barrier()
        es_o.close()

        es_p = ExitStack()
        psb = Ring(psb.bufs + acc_ps)
        wst = C.sbring(3, [128, KC, 128], F32, "wstp", es_p)
        wbf = C.sbring(2, [128, KC, 128], BF16, "wbfp", es_p)
        gate2b = C.sb([128, D], F32, "gate2b", es_p)
        build_gate(gate2b, 80)
        h2T = C.sb([128, KC, 512], BF16, "h2T", es_p)
        qT = C.sb([128, 16, 512], BF16, "qTp", es_p)
        acc = C.sb([128, 4, D], F32, "acc", es_p)
        Cb = C.sb([128, 8, 512], BF16, "Cb", es_p)
        statsT = C.sb([32, 512], BF16, "statsT", es_p)
        selT = C.sb([32, 8 * 128], BF16, "selT", es_p)
        selg = C.sb([32, 8 * 128], BF16, "selg", es_p)
        ucol = C.sb([32, 8], F32, "ucol", es_p)
        for h in range(8):
            S.op("dve", lambda h=h: V.tensor_tensor(out=ucol[:, h:h + 1], in0=ident_f[0:32, h:h + 1],
                                                    in1=ident_f[0:32, 8 + h:9 + h], op=ALU.add),
                 reads=[ident_f.r, ucol.r], writes=[ucol.r])
            S.op("dve", lambda h=h: V.scalar_tensor_tensor(out=ucol[:, h:h + 1], in0=ucol[:, h:h + 1], scalar=-1.0,
                                                           in1=ident_f[0:32, 16 + h:17 + h],
                                                           op0=ALU.mult, op1=ALU.subtract),
                 reads=[ident_f.r, ucol.r], writes=[ucol.r])
            S.op("dve", lambda h=h: V.tensor_scalar(out=selT[:, h * 128:(h + 1) * 128], in0=ones_f[0:32, :],
                                                    scalar1=ucol[:, h:h + 1], scalar2=None, op0=ALU.mult),
                 reads=[ones_f.r, ucol.r], writes=[selT.r])
            S.op("dve", lambda h=h: V.tensor_scalar(out=selg[:, h * 128:(h + 1) * 128], in0=ones_f[0:32, :],
                                                    scalar1=ident_f[0:32, 24 + h:25 + h], scalar2=None, op0=ALU.mult),
                 reads=[ones_f.r, ident_f.r], writes=[selg.r])
        k1bc_r = C.sbring(2, [128, 128], BF16, "k1bc", es_p)
        eu_st = C.sbring(2, [128, D], F32, "eu_st", es_p)
        eu_bf = C.sbring(GK + 2, [128, D], BF16, "eu_bf", es_p)
        wT_r = C.sbring(2 * GK, [128, 512], BF16, "wT", es_p)
        gA_r = C.sbring(2, [128, 512], BF16, "gA", es_p)
        E_r = C.sbring(2, [128, 512], BF16, "E", es_p)
        Mm2_r = C.sbring(2, [128, 2, 512], BF16, "Mm2", es_p)
        Tt2_r = C.sbring(2, [128, 2, 512], BF16, "Tt2", es_p)
        Ga2_r = C.sbring(2, [128, 2, 512], BF16, "Ga2", es_p)
        sc_r = C.sbring(2, [128, 256], F32, "sc", es_p)
        sc2_r = C.sbring(1, [128, 256], F32, "sc2", es_p)
        v12_r = C.sbring(2, [128, 32], F32, "v12", es_p)
        cand_r = C.sbring(2, [128, 256], F32, "cand", es_p)
        cand2_r = C.sbring(1, [128, 256], F32, "cand2", es_p)
        c16_r = C.sbring(2, [128, 16], F32, "c16", es_p)
        e16_r = C.sbring(2, [128, 16], F32, "e16", es_p)
        sm_r = C.sbring(2, [128, 4], F32, "sm", es_p)
        statf = C.sb([128, 48], F32, "statf", es_p)
        statb = C.sb([128, 32], BF16, "statb", es_p)

        for Q in range(NQ):
            es_n2 = ExitStack()
            norm_to_T(es_n2, lambda i: out_d[(Q * 4 + i) * 128:(Q * 4 + i + 1) * 128, :],
                      lambda i: out_res[Q * 4 + i], A2, 48, h2T, 4, 1)
            S.barrier()
            es_n2.close()
            for cc in range(16):
                st = wst.next()
                S.dma("sp" if cc % 2 == 0 else "act", st[:], wq_d[cc], writes=[st.r])
                wb = wbf.next()
                S.op("pool", lambda: G.tensor_copy(out=wb[:], in_=st[:]), reads=[st.r], writes=[wb.r])
                pm = psb.next()
                for kc in range(KC):
                    S.op("pe", lambda kc=kc: P.matmul(pm[:], lhsT=wb[:, kc, :], rhs=h2T[:, kc, :],
                                                      start=(kc == 0), stop=(kc == KC - 1)),
                         reads=[wb.r, h2T.r], writes=[pm.r], inc=(kc == KC - 1))
                S.op("act", lambda: A.copy(out=qT[:, cc, :], in_=pm[:]), reads=[pm.r], writes=[qT.r])
            for ti in range(4):
                for h in range(8):
                    pm = psb.next()
                    S.op("pe", lambda: P.matmul(pm[:, 0:128], lhsT=qT[:, 2 * h, ti * 128:(ti + 1) * 128],
                                                rhs=k1t_b[:], start=True, stop=True),
                         reads=[qT.r, k1t_b.r], writes=[pm.r], inc=False)
                    S.op("pe", lambda: P.matmul(pm[:, 128:256], lhsT=qT[:, 2 * h + 1, ti * 128:(ti + 1) * 128],
                                                rhs=k2t_b[:], start=True, stop=True),
                         reads=[qT.r, k2t_b.r], writes=[pm.r])
                    sc = sc_r.next()
                    sc2 = sc2_r.next()
                    v12 = v12_r.next()
                    S.op("act", lambda: A.copy(out=sc[:], in_=pm[:, 0:256]), reads=[pm.r], writes=[sc.r])
                    for half in range(2):
                        sl = slice(half * 128, (half + 1) * 128)
                        vo = half * 16
                        S.op("dve", lambda: V.max(out=v12[:, vo:vo + 8], in_=sc[:, sl]), reads=[sc.r, v12.r], writes=[v12.r])
                        S.op("dve", lambda: V.match_replace(out=sc2[:, sl], in_to_replace=v12[:, vo:vo + 8],
                                                            in_values=sc[:, sl], imm_value=NEG),
                             reads=[sc.r, v12.r, sc2.r], writes=[sc2.r])
                        S.op("dve", lambda: V.max(out=v12[:, vo + 8:vo + 16], in_=sc2[:, sl]),
                             reads=[sc2.r, v12.r], writes=[v12.r])
                    cand = cand_r.next()
                    cand2 = cand2_r.next()
                    c16 = c16_r.next()
                    e16 = e16_r.next()
                    sm = sm_r.next()
                    S.op("dve", lambda: V.tensor_tensor(
                        out=cand[:].rearrange("p (a b) -> p a b", a=16),
                        in0=v12[:, 0:16].unsqueeze(2).broadcast_to([128, 16, 16]),
                        in1=v12[:, 16:32].unsqueeze(1).broadcast_to([128, 16, 16]), op=ALU.add),
                        reads=[v12.r], writes=[cand.r])
                    S.op("dve", lambda: V.max(out=c16[:, 0:8], in_=cand[:]), reads=[cand.r, c16.r], writes=[c16.r])
                    S.op("dve", lambda: V.match_replace(out=cand2[:], in_to_replace=c16[:, 0:8], in_values=cand[:],
                                                        imm_value=NEG), reads=[cand.r, c16.r], writes=[cand2.r])
                    S.op("dve", lambda: V.max(out=c16[:, 8:16], in_=cand2[:]), reads=[cand2.r, c16.r], writes=[c16.r])
                    S.op("dve", lambda: V.tensor_scalar(out=sm[:, 0:1], in0=c16[:, 0:1], scalar1=-1.0, scalar2=None,
                                                        op0=ALU.mult), reads=[c16.r, sm.r], writes=[sm.r])
                    S.op("dve", lambda: V.memset(sm[:, 1:2], 0.0), reads=[sm.r], writes=[sm.r])
                    S.op("act", lambda: A.activation(out=e16[:], in_=c16[:], func=AF.Exp, bias=sm[:, 0:1],
                                                     accum_out=sm[:, 1:2]),
                         reads=[c16.r, sm.r], writes=[e16.r, sm.r])
                    S.op("dve", lambda: V.reciprocal(out=sm[:, 2:3], in_=sm[:, 1:2]), reads=[sm.r], writes=[sm.r])
                    S.op("dve", lambda: V.tensor_scalar(out=statf[:, h:h + 1], in0=c16[:, 15:16], scalar1=-3.0e-5,
                                                        scalar2=None, op0=ALU.add),
                         reads=[c16.r, statf.r], writes=[statf.r])
                    S.op("dve", lambda: V.tensor_tensor(out=statf[:, 32 + h:33 + h], in0=e16[:, 15:16], in1=sm[:, 2:3],
                                                        op=ALU.mult), reads=[e16.r, sm.r, statf.r], writes=[statf.r])
                S.op("dve", lambda: V.tensor_copy(out=statb[:, 0:8], in_=statf[:, 0:8]), reads=[statf.r, statb.r], writes=[statb.r])
                S.op("dve", lambda: V.tensor_tensor(out=statf[:, 8:16], in0=statf[:, 0:8], in1=statb[:, 0:8],
                                                    op=ALU.subtract), reads=[statf.r, statb.r], writes=[statf.r])
                S.op("dve", lambda: V.tensor_copy(out=statb[:, 8:16], in_=statf[:, 8:16]), reads=[statf.r, statb.r], writes=[statb.r])
                S.op("dve", lambda: V.tensor_tensor(out=statf[:, 16:24], in0=statf[:, 8:16], in1=statb[:, 8:16],
                                                    op=ALU.subtract), reads=[statf.r, statb.r], writes=[statf.r])
                S.op("dve", lambda: V.tensor_copy(out=statb[:, 16:24], in_=statf[:, 16:24]), reads=[statf.r, statb.r], writes=[statb.r])
                S.op("dve", lambda: V.tensor_copy(out=statb[:, 24:32], in_=statf[:, 32:40]), reads=[statf.r, statb.r], writes=[statb.r])
                pm = psb.next()
                pmb = pm.t.bitcast(BF16)
                S.op("pe", lambda: P.transpose(out=pmb[0:32, 0:128], in_=statb[:, 0:32], identity=ident_b[:]),
                     reads=[statb.r, ident_b.r], writes=[pm.r])
                S.op("act", lambda: A.copy(out=statsT[:, ti * 128:(ti + 1) * 128], in_=pmb[0:32, 0:128]),
                     reads=[pm.r], writes=[statsT.r])
            for h in range(8):
                pm = psb.next()
                S.op("pe", lambda: P.matmul(pm[:], lhsT=selg[:, h * 128:(h + 1) * 128], rhs=statsT[:],
                                            start=True, stop=True), reads=[selg.r, statsT.r], writes=[pm.r])
                S.op("act", lambda: A.copy(out=Cb[:, h, :], in_=pm[:]), reads=[pm.r], writes=[Cb.r])

            grp = []
            ngrp = 0
            pending = []

            def emit_units(n):
                for _ in range(min(n, len(pending))):
                    g_, ti, cbk, first = pending.pop(0)
                    pm = psb.next()
                    for gi, (wT_, eub_) in enumerate(g_):
                        S.op("pe", lambda gi=gi, wT_=wT_, eub_=eub_: P.matmul(
                            pm[:], lhsT=wT_[:, ti * 128:(ti + 1) * 128], rhs=eub_[:, cbk * 512:(cbk + 1) * 512],
                            start=(gi == 0), stop=(gi == len(g_) - 1)),
                            reads=[wT_.r, eub_.r], writes=[pm.r], inc=(gi == len(g_) - 1))
                    if first:
                        S.op("act", lambda: A.copy(out=acc[:, ti, cbk * 512:(cbk + 1) * 512], in_=pm[:]),
                             reads=[pm.r], writes=[acc.r])
                    else:
                        S.op("dve", lambda: V.tensor_tensor(out=acc[:, ti, cbk * 512:(cbk + 1) * 512],
                                                            in0=pm[:], in1=acc[:, ti, cbk * 512:(cbk + 1) * 512],
                                                            op=ALU.add), reads=[pm.r, acc.r], writes=[acc.r])

            for e1 in range(NE1):
                st = wst.next()
                S.dma("sp", st[:], edt_d[e1], writes=[st.r])
                edb = wbf.next()
                S.op("act", lambda: A.copy(out=edb[:], in_=st[:]), reads=[st.r], writes=[edb.r])
                es_ = eu_st.next()
                S.dma("act", es_[:], eu_d[e1], writes=[es_.r])
                eub = eu_bf.next()
                S.op("act", lambda: A.copy(out=eub[:], in_=es_[:]), reads=[es_.r], writes=[eub.r])
                k1bc = k1bc_r.next()
                S.op("pool", lambda: G.tensor_copy(out=k1bc[:], in_=k1t_b[:, e1:e1 + 1].broadcast_to([128, 128])),
                     reads=[k1t_b.r], writes=[k1bc.r])
                pA = psb.next()
                for kc in range(KC):
                    S.op("pe", lambda kc=kc: P.matmul(pA[:], lhsT=edb[:, kc, :], rhs=h2T[:, kc, :],
                                                      start=(kc == 0), stop=(kc == KC - 1)),
                         reads=[edb.r, h2T.r], writes=[pA.r], inc=(kc == KC - 1))
                gA = gA_r.next()
                S.op("act", lambda: A.activation(out=gA[:], in_=pA[:], func=AF.Gelu), reads=[pA.r], writes=[gA.r])
                Ga2 = Ga2_r.next()
                for hp in range(4):
                    Mm2 = Mm2_r.next()
                    for hh in range(2):
                        h = 2 * hp + hh
                        pX = psb.next()
                        S.op("pe", lambda: P.matmul(pX[:], lhsT=k2t_b[:], rhs=qT[:, 2 * h + 1, :], start=True, stop=False),
                             reads=[k2t_b.r, qT.r], writes=[pX.r], inc=False)
                        S.op("pe", lambda: P.matmul(pX[:], lhsT=k1bc[:], rhs=qT[:, 2 * h, :], start=False, stop=False),
                             reads=[k1bc.r, qT.r], writes=[pX.r], inc=False)
                        S.op("pe", lambda: P.matmul(pX[:], lhsT=selT[:, h * 128:(h + 1) * 128], rhs=statsT[:],
                                                    start=False, stop=True),
                             reads=[selT.r, statsT.r], writes=[pX.r])
                        E = E_r.next()
                        S.op("act", lambda: A.activation(out=E[:], in_=pX[:], func=AF.Exp), reads=[pX.r], writes=[E.r])
                        S.op("dve", lambda: V.scalar_tensor_tensor(out=Mm2[:, hh, :], in0=pX[:], scalar=0.0, in1=E[:],
                                                                   op0=ALU.is_ge, op1=ALU.mult),
                             reads=[pX.r, E.r, Mm2.r], writes=[Mm2.r])
                    if hp == 0:
                        S.op("dve", lambda: V.tensor_tensor(out=Ga2[:], in0=Mm2[:], in1=Cb[:, 0:2, :], op=ALU.mult),
                             reads=[Mm2.r, Cb.r], writes=[Ga2.r])
                    else:
                        Tt2 = Tt2_r.next()
                        S.op("dve", lambda: V.tensor_tensor(out=Tt2[:], in0=Mm2[:], in1=Cb[:, 2 * hp:2 * hp + 2, :],
                                                            op=ALU.mult), reads=[Mm2.r, Cb.r], writes=[Tt2.r])
                        S.op("pool", lambda: G.tensor_tensor(out=Ga2[:], in0=Ga2[:], in1=Tt2[:], op=ALU.add),
                             reads=[Ga2.r, Tt2.r], writes=[Ga2.r])
                S.op("pool", lambda: G.tensor_tensor(out=Ga2[:, 0, :], in0=Ga2[:, 0, :], in1=Ga2[:, 1, :], op=ALU.add),
                     reads=[Ga2.r], writes=[Ga2.r])
                wT = wT_r.next()
                S.op("dve", lambda: V.tensor_tensor(out=wT[:], in0=Ga2[:, 0, :], in1=gA[:], op=ALU.mult),
                     reads=[Ga2.r, gA.r], writes=[wT.r])
                grp.append((wT, eub))
                emit_units(8)
                if len(grp) == GK or e1 == NE1 - 1:
                    for ti in range(4):
                        for cbk in range(4):
                            pending.append((list(grp), ti, cbk, ngrp == 0))
                    grp = []
                    ngrp += 1
            emit_units(len(pending))
            es_f = ExitStack()
            x1_r = C.sbring(1, [128, D], F32, "x1t", es_f)
            for ti in range(4):
                i = Q * 4 + ti
                x1 = x1_r.next()
                S.dma("sp", x1[:], out_d[i * 128:(i + 1) * 128, :], reads=[out_res[i]], writes=[x1.r])
                S.op("dve", lambda: V.tensor_tensor(out=acc[:, ti, :], in0=acc[:, ti, :], in1=gate2b[:], op=ALU.mult),
                     reads=[acc.r, gate2b.r], writes=[acc.r])
                S.op("pool", lambda: G.tensor_tensor(out=acc[:, ti, :], in0=acc[:, ti, :], in1=x1[:], op=ALU.add),
                     reads=[acc.r, x1.r], writes=[acc.r])
                S.dma("sp", out_d[i * 128:(i + 1) * 128, :], acc[:, ti, :], reads=[acc.r, out_res[i]], writes=[out_res[i]])
            S.barrier()
            es_f.close()
        S.barrier()
        es_p.close()

        for q in ("sp", "pool", "act"):
            for sem, val in S.dma_slots[q]:
                if val > 0:
                    S._wait("sp", (sem, val))
        print("instructions:", S.ninstr)
    return nc


def _host_layouts(inp):
    f = lambda a: np.ascontiguousarray(a, dtype=np.float32)
    L = {}
    w_ada = inp["w_ada"][0]
    L["wada_r"] = f(w_ada.reshape(KC, 128, 24, 512).transpose(2, 1, 0, 3))
    L["bada_r"] = f(inp["b_ada"][0].reshape(96, 128).T)
    L["g1_r"] = f(inp["norm1_gain"][0].reshape(KC, 128).T)
    L["g2_r"] = f(inp["norm2_gain"][0].reshape(KC, 128).T)
    w_in = inp["w_in"][0]
    o_fq, o_fk, o_fv, o_ff, o_mq, o_mk, o_mv, o_mo, o_mi, o_mf = 0, 1024, 2048, 3072, 3080, 4104, 5128, 6152, 7176, 7180
    cols = []
    for h in range(8):
        cols += [o_fq + 128 * h, o_fk + 128 * h, o_fv + 128 * h]
    for m in range(4):
        cols += [o_mq + 256 * m, o_mq + 256 * m + 128, o_mk + 256 * m, o_mk + 256 * m + 128,
                 o_mv + 256 * m, o_mv + 256 * m + 128, o_mo + 256 * m, o_mo + 256 * m + 128]
    wfm = np.empty((56, 128, KC, 128), np.float32)
    for i, c0 in enumerate(cols):
        wfm[i] = w_in[:, c0:c0 + 128].reshape(KC, 128, 128).transpose(1, 0, 2)
    L["wfm_r"] = wfm
    gcols = list(range(o_ff, o_ff + 8)) + list(range(o_mf, o_mf + 4)) + list(range(o_mi, o_mi + 4))
    L["wg_r"] = f(w_in[:, gcols].reshape(KC, 128, 16).transpose(1, 0, 2))
    L["gb_r"] = f(np.concatenate([inp["fox_f_bias"][0], inp["mlstm_f_bias"][0], inp["mlstm_i_bias"][0]]).reshape(16, 1))
    L["qkg_r"] = f(np.stack([inp["fox_q_gain"][0], inp["fox_k_gain"][0]], axis=1))
    L["convw_r"] = f(inp["mlstm_conv_w"][0].reshape(4, 16, 128).transpose(2, 1, 0))
    L["convb_r"] = f(inp["mlstm_conv_b"][0].reshape(16, 128).T)
    L["hg_r"] = f(inp["mlstm_head_gain"][0].reshape(8, 128).T)
    L["wout_r"] = f(inp["w_out"][0].reshape(KC, 128, 4, 512).transpose(2, 1, 0, 3))
    L["wq_r"] = f(inp["peer_w_query"][0].reshape(KC, 128, 16, 128).transpose(2, 1, 0, 3))
    L["k1t_r"] = f(inp["peer_sub_keys_1"][0].T)
    L["k2t_r"] = f(inp["peer_sub_keys_2"][0].T)
    ed = inp["peer_expert_down"][0]
    L["edt_r"] = f(ed.reshape(128, 128, KC, 128).transpose(0, 3, 2, 1))
    L["eu_r"] = f(inp["peer_expert_up"][0].reshape(128, 128, D))
    return L


def _core_inputs(inputs, b):
    return {
        "x": np.ascontiguousarray(inputs["x"][b], dtype=np.float32),
        "c_r": np.ascontiguousarray(inputs["c"][b].reshape(KC, 128).T, dtype=np.float32),
    }


def kernel(**inputs):
    inputs = {k: np.asarray(v) for k, v in inputs.items()}
    L = _host_layouts(inputs)
    nc = build_program()
    outs = []
    for g0 in range(0, 8, CORES_PER_LAUNCH):
        in_maps = []
        for b in range(g0, g0 + CORES_PER_LAUNCH):
            m = dict(L)
            m.update(_core_inputs(inputs, b))
            in_maps.append(m)
        res = run_bass_kernel_spmd(nc, in_maps, core_ids=list(range(CORES_PER_LAUNCH)))
        outs += [np.asarray(r["out"], dtype=np.float32) for r in res.results]
    return np.stack(outs, axis=0)
```

```python
from contextlib import ExitStack
import numpy as np
import concourse.bass as bass
import concourse.mybir as mybir
from concourse.bass_utils import run_bass_kernel_spmd

F32 = mybir.dt.float32
BF16 = mybir.dt.bfloat16
ALU = mybir.AluOpType
AF = mybir.ActivationFunctionType
AX = mybir.AxisListType

import os
NBLK = int(os.environ.get("NBLK", "24"))
ADAQ = os.environ.get("ADAQ", "pool")
D = 2048
T = 2048
KC = 16
NT = 16
EPS = 1e-6


class Res:
    __slots__ = ("name", "w", "r")

    def __init__(self, name=""):
        self.name = name
        self.w = None
        self.r = []


class Sched:
    SEM_LIMIT = 30000

    def __init__(self, nc, es):
        self.nc = nc
        self.es = es
        self.engs = {"pe": nc.tensor, "act": nc.scalar, "dve": nc.vector,
                     "pool": nc.gpsimd, "sp": nc.sync}
        self.sem = {}
        self.cnt = {}
        self.nsem = 0
        self.pe_sems = []
        self.pend = {}
        for e in ("pe", "act", "dve", "pool"):
            self._new_sem(e)
        self.waited = {e: {} for e in self.engs}
        self.dma_slots = {}
        self.dma_next = {}
        for q, n in (("sp", 8), ("pool", 2), ("act", 4)):
            self.dma_slots[q] = [[self._mk(f"d{q}{i}"), 0] for i in range(n)]
            self.dma_next[q] = 0
        self.ninstr = 0

    def _mk(self, name):
        self.nsem += 1
        return self.es.enter_context(self.nc.semaphore(f"{name}_{self.nsem}"))

    def _new_sem(self, e):
        self.sem[e] = self._mk(f"s{e}")
        self.cnt[e] = 0
        if e == "pe":
            self.pe_sems.append(self.sem[e])

    def _wait(self, e, tok):
        sem, val = tok
        if e == "pe" and sem in self.pe_sems:
            return
        key = id(sem)
        if self.waited[e].get(key, 0) >= val:
            return
        self.engs[e].wait_ge(sem, val)
        self.waited[e][key] = val

    def _deps(self, e, reads, writes):
        toks = []
        for r in reads:
            if r.w is not None:
                toks.append(r.w)
        for w in writes:
            if w.w is not None:
                toks.append(w.w)
            toks.extend(w.r)
        for t in toks:
            self._wait(e, t)

    def _mark(self, tok, reads, writes):
        for w in writes:
            w.w = tok
            w.r = []
        for r in reads:
            if r in writes:
                continue
            r.r.append(tok)
            if len(r.r) > 24:
                r.r = r.r[-24:]

    def op(self, e, fn, reads=(), writes=(), inc=True):
        self._deps(e, reads, writes)
        if not self.pend.get(e, False) and self.cnt[e] >= self.SEM_LIMIT:
            self._new_sem(e)
        self.pend[e] = not inc
        ins = fn()
        self.ninstr += 1
        if inc:
            ins.then_inc(self.sem[e], 1)
            self.cnt[e] += 1
            tok = (self.sem[e], self.cnt[e])
            self._pending_ok = True
        else:
            tok = (self.sem[e], self.cnt[e] + 1)
        self._mark(tok, reads, writes)
        return tok

    def dma(self, q, out, in_, reads=(), writes=(), **kw):
        slots = self.dma_slots[q]
        i = self.dma_next[q]
        self.dma_next[q] = (i + 1) % len(slots)
        sem, val = slots[i]
        if val > 0:
            self._wait(q, (sem, val))
        self._deps(q, reads, writes)
        ins = self.engs[q].dma_start(out=out, in_=in_, **kw)
        ins.then_inc(sem, 16)
        slots[i][1] = val + 16
        tok = (sem, val + 16)
        self._mark(tok, reads, writes)
        self.ninstr += 1
        return tok

    def barrier(self):
        toks = []
        for e in ("pe", "act", "dve", "pool"):
            if self.cnt[e] > 0:
                assert not self.pend.get(e, False), f"open group on {e} at barrier"
                toks.append((self.sem[e], self.cnt[e]))
        for q in self.dma_slots:
            for sem, val in self.dma_slots[q]:
                if val > 0:
                    toks.append((sem, val))
        for e in self.engs:
            for t in toks:
                self._wait(e, t)

    def wait_all(self, e, ress):
        for r in ress:
            if r.w is not None:
                self._wait(e, r.w)


class Buf:
    def __init__(self, t, name):
        self.t = t
        self.r = Res(name)

    def __getitem__(self, idx):
        return self.t[idx]


class Ring:
    def __init__(self, bufs):
        self.bufs = bufs
        self.i = 0

    def next(self):
        b = self.bufs[self.i]
        self.i = (self.i + 1) % len(self.bufs)
        return b


class Ctx:
    def __init__(self, nc, es):
        self.nc = nc
        self.es = es
        self.S = Sched(nc, es)
        self.n = 0

    def sb(self, shape, dt, name, es=None):
        self.n += 1
        t = (es or self.es).enter_context(self.nc.sbuf_tensor(f"{name}_{self.n}", list(shape), dt))
        return Buf(t, name)

    def ps(self, shape, dt, name, es=None):
        self.n += 1
        t = (es or self.es).enter_context(self.nc.psum_tensor(f"{name}_{self.n}", list(shape), dt))
        return Buf(t, name)

    def sbring(self, n, shape, dt, name, es=None):
        return Ring([self.sb(shape, dt, f"{name}{i}", es) for i in range(n)])


NE1 = int(os.environ.get("NE1", "128"))
NQ = int(os.environ.get("NQ", "4"))
NFOX = int(os.environ.get("NFOX", "8"))
NML = int(os.environ.get("NML", "4"))
GK = 4
NEG = -1.0e30
CORES_PER_LAUNCH = 8


def build_program(stage=99, dbg=False):
    nc = bass.Bass("TRN2", target_bir_lowering=False)

    def din(name, shape, dt=F32):
        return nc.dram_tensor(name, list(shape), dt, kind="ExternalInput").ap()

    x_d = din("x", [T, D])
    c_d = din("c_r", [128, KC])
    wada_d = din("wada_r", [24, 128, KC, 512])
    bada_d = din("bada_r", [128, 96])
    g1_d = din("g1_r", [128, KC])
    g2_d = din("g2_r", [128, KC])
    wfm_d = din("wfm_r", [56, 128, KC, 128])
    wg_d = din("wg_r", [128, KC, 16])
    gb_d = din("gb_r", [16, 1])
    qkg_d = din("qkg_r", [128, 2])
    cw_d = din("convw_r", [128, 16, 4])
    cb_d = din("convb_r", [128, 16])
    hg_d = din("hg_r", [128, 8])
    wout_d = din("wout_r", [4, 128, KC, 512])
    wq_d = din("wq_r", [16, 128, KC, 128])
    k1t_d = din("k1t_r", [128, 128])
    k2t_d = din("k2t_r", [128, 128])
    edt_d = din("edt_r", [128, 128, KC, 128])
    eu_d = din("eu_r", [128, 128, D])
    out_d = nc.dram_tensor("out", [T, D], F32, kind="ExternalOutput").ap()
    mix_d = nc.dram_tensor("mix_scr", [KC, 128, T], BF16, kind="Internal").ap()
    if dbg:
        dbg_d = nc.dram_tensor("dbg", [128, 4096], F32, kind="ExternalOutput").ap()

    with ExitStack() as es:
        C = Ctx(nc, es)
        S = C.S
        V, A, P, G = nc.vector, nc.scalar, nc.tensor, nc.gpsimd

        psb = Ring([C.ps([128, 512], F32, f"ps{i}") for i in range(4)])
        acc_ps = [C.ps([128, 512], F32, f"pacc{i}") for i in range(4)]
        out_res = [Res(f"out{i}") for i in range(NT)]
        mix_res = [Res(f"mix{i}") for i in range(KC)]

        ident_f = C.sb([128, 128], F32, "ident_f")
        ident_b = C.sb([128, 128], BF16, "ident_b")
        ones_f = C.sb([128, 128], F32, "ones_f")
        ones_b = C.sb([128, 128], BF16, "ones_b")
        sel127 = C.sb([128, 128], F32, "sel127")
        tri_b = C.sb([128, 128], BF16, "tri_b")
        zcol = C.sb([128, 1], F32, "zcol")
        lncol = C.sb([128, 1], F32, "lncol")
        S.op("pool", lambda: G.memset(ones_f[:], 1.0), writes=[ones_f.r])
        S.op("pool", lambda: G.memset(ones_b[:], 1.0), writes=[ones_b.r])
        S.op("pool", lambda: G.memset(zcol[:], 0.0), writes=[zcol.r])
        S.op("pool", lambda: G.memset(lncol[:], float(np.log(1.0 / 16.0))), writes=[lncol.r])
        S.op("pool", lambda: G.affine_select(out=ident_f[:], in_=ones_f[:], pattern=[[-1, 128]],
                                             compare_op=ALU.is_equal, fill=0.0, base=0,
                                             channel_multiplier=1),
             reads=[ones_f.r], writes=[ident_f.r])
        S.op("pool", lambda: G.tensor_copy(out=ident_b[:], in_=ident_f[:]),
             reads=[ident_f.r], writes=[ident_b.r])
        S.op("pool", lambda: G.affine_select(out=sel127[:], in_=ones_f[:], pattern=[[0, 128]],
                                             compare_op=ALU.is_equal, fill=0.0, base=-127,
                                             channel_multiplier=1),
             reads=[ones_f.r], writes=[sel127.r])
        S.op("pool", lambda: G.affine_select(out=tri_b[:], in_=ones_b[:], pattern=[[1, 128]],
                                             compare_op=ALU.is_ge, fill=0.0, base=0,
                                             channel_multiplier=-1),
             reads=[ones_b.r], writes=[tri_b.r])

        def load_small(d_ap, shape, name, dt=F32):
            b = C.sb(shape, dt, name)
            S.dma("sp", b[:], d_ap, writes=[b.r])
            return b

        c_sb = load_small(c_d, [128, KC], "c_sb")
        bada_sb = load_small(bada_d, [128, 96], "bada")
        g1_sb = load_small(g1_d, [128, KC], "g1")
        g2_sb = load_small(g2_d, [128, KC], "g2")
        gb_sb = load_small(gb_d, [16, 1], "gb")
        qkg_sb = load_small(qkg_d, [128, 2], "qkg")
        cw_sb = load_small(cw_d, [128, 16, 4], "cw")
        cb_sb = load_small(cb_d, [128, 16], "cb")
        hg_sb = load_small(hg_d, [128, 8], "hg")
        k1t_f = load_small(k1t_d, [128, 128], "k1tf")
        k2t_f = load_small(k2t_d, [128, 128], "k2tf")
        wg_f = load_small(wg_d, [128, KC, 16], "wgf")
        k1t_b = C.sb([128, 128], BF16, "k1tb")
        k2t_b = C.sb([128, 128], BF16, "k2tb")
        wg_b = C.sb([128, KC, 16], BF16, "wgb")
        S.op("pool", lambda: G.tensor_copy(out=k1t_b[:], in_=k1t_f[:]), reads=[k1t_f.r], writes=[k1t_b.r])
        S.op("pool", lambda: G.tensor_copy(out=k2t_b[:], in_=k2t_f[:]), reads=[k2t_f.r], writes=[k2t_b.r])
        S.op("pool", lambda: G.tensor_copy(out=wg_b[:], in_=wg_f[:]), reads=[wg_f.r], writes=[wg_b.r])
        qsc = C.sb([128, 2], F32, "qsc")
        S.op("dve", lambda: V.tensor_scalar(out=qsc[:, 0:1], in0=qkg_sb[:, 0:1], scalar1=128.0 ** -0.5,
                                            scalar2=None, op0=ALU.mult), reads=[qkg_sb.r], writes=[qsc.r])
        S.op("dve", lambda: V.tensor_copy(out=qsc[:, 1:2], in_=qkg_sb[:, 1:2]), reads=[qkg_sb.r, qsc.r], writes=[qsc.r])

        sc_sb = C.sb([128, KC], F32, "sc_sb")
        mod = C.sb([128, 96], F32, "mod")
        S.op("act", lambda: A.activation(out=sc_sb[:], in_=c_sb[:], func=AF.Silu),
             reads=[c_sb.r], writes=[sc_sb.r])
        es_ada = ExitStack()
        wada_ring = C.sbring(2, [128, KC, 512], F32, "wada", es_ada)
        for jb in range(24):
            wb = wada_ring.next()
            S.dma("sp" if jb % 2 == 0 else "act", wb[:], wada_d[jb], writes=[wb.r])
            pm = psb.next()
            for jj in range(4):
                for kc in range(KC):
                    S.op("pe", lambda kc=kc, jj=jj, pm=pm, wb=wb: P.matmul(
                        pm[:, jj:jj + 1], lhsT=wb[:, kc, jj * 128:(jj + 1) * 128],
                        rhs=sc_sb[:, kc:kc + 1], start=(kc == 0), stop=(kc == KC - 1)),
                        reads=[wb.r, sc_sb.r], writes=[pm.r], inc=(kc == KC - 1 and jj == 3))
            S.op("dve", lambda jb=jb, pm=pm: V.tensor_tensor(
                out=mod[:, jb * 4:jb * 4 + 4], in0=pm[:, 0:4],
                in1=bada_sb[:, jb * 4:jb * 4 + 4], op=ALU.add),
                reads=[pm.r, bada_sb.r], writes=[mod.r])
        S.barrier()
        es_ada.close()
        A1 = C.sb([128, KC], F32, "A1")
        A2 = C.sb([128, KC], F32, "A2")
        S.op("dve", lambda: V.scalar_tensor_tensor(out=A1[:], in0=mod[:, 16:32], scalar=1.0, in1=g1_sb[:],
                                                   op0=ALU.add, op1=ALU.mult),
             reads=[mod.r, g1_sb.r], writes=[A1.r])
        S.op("dve", lambda: V.scalar_tensor_tensor(out=A2[:], in0=mod[:, 64:80], scalar=1.0, in1=g2_sb[:],
                                                   op0=ALU.add, op1=ALU.mult),
             reads=[mod.r, g2_sb.r], writes=[A2.r])

        dg = C.sb([128, 128], F32, "diag")

        def build_gate(gt, c0):
            for kc in range(KC):
                S.op("dve", lambda kc=kc: V.tensor_scalar(
                    out=dg[:], in0=ident_f[:], scalar1=mod[:, c0 + kc:c0 + kc + 1], scalar2=None,
                    op0=ALU.mult), reads=[ident_f.r, mod.r], writes=[dg.r])
                pm = psb.next()
                S.op("pe", lambda pm=pm: P.matmul(pm[:, 0:128], lhsT=ones_f[:], rhs=dg[:], start=True, stop=True),
                     reads=[ones_f.r, dg.r], writes=[pm.r])
                S.op("act", lambda pm=pm, kc=kc: A.copy(out=gt[:, kc * 128:(kc + 1) * 128], in_=pm[:, 0:128]),
                     reads=[pm.r], writes=[gt.r])

        def norm_to_T(es_l, src_rows, src_res, Asc, shift_c0, dstT, ntiles, nring):
            xr = C.sbring(nring, [128, D], F32, "xt", es_l)
            xn_r = C.sbring(nring, [128, D], BF16, "xn", es_l)
            ssr = C.sbring(2, [128, 2], F32, "ss", es_l)
            for i in range(ntiles):
                xt = xr.next()
                S.dma("sp", xt[:], src_rows(i), reads=[src_res(i)] if src_res else [], writes=[xt.r])
                ss = ssr.next()
                S.op("dve", lambda ss=ss: V.memset(ss[:], 0.0), writes=[ss.r])
                xn = xn_r.next()
                S.op("act", lambda xt=xt, ss=ss, xn=xn: A.activation(out=xn[:], in_=xt[:], func=AF.Square,
                                                                     accum_out=ss[:, 0:1]),
                     reads=[xt.r, ss.r], writes=[xn.r, ss.r])
                S.op("dve", lambda ss=ss: V.tensor_scalar(out=ss[:, 1:2], in0=ss[:, 0:1], scalar1=1.0 / D,
                                                          scalar2=EPS, op0=ALU.mult, op1=ALU.add),
                     reads=[ss.r], writes=[ss.r])
                S.op("act", lambda ss=ss: A.activation(out=ss[:, 1:2], in_=ss[:, 1:2], func=AF.Sqrt),
                     reads=[ss.r], writes=[ss.r])
                S.op("dve", lambda ss=ss: V.reciprocal(out=ss[:, 1:2], in_=ss[:, 1:2]),
                     reads=[ss.r], writes=[ss.r])
                S.op("act", lambda xt=xt, ss=ss, xn=xn: A.activation(out=xn[:], in_=xt[:], func=AF.Copy,
                                                                     scale=ss[:, 1:2]),
                     reads=[xt.r, ss.r], writes=[xn.r])
                for k4 in range(4):
                    pm = psb.next()
                    pmb = pm.t.bitcast(BF16)
                    for j in range(4):
                        kc = k4 * 4 + j
                        S.op("pe", lambda kc=kc, j=j, pmb=pmb, xn=xn: P.transpose(
                            out=pmb[:, j * 128:(j + 1) * 128], in_=xn[:, kc * 128:(kc + 1) * 128],
                            identity=ident_b[:]), reads=[xn.r, ident_b.r], writes=[pm.r], inc=(j == 3))
                    for j in range(4):
                        kc = k4 * 4 + j
                        S.op("dve", lambda kc=kc, j=j, pmb=pmb, i=i: V.tensor_scalar(
                            out=dstT[:, kc, i * 128:(i + 1) * 128], in0=pmb[:, j * 128:(j + 1) * 128],
                            scalar1=Asc[:, kc:kc + 1], scalar2=mod[:, shift_c0 + kc:shift_c0 + kc + 1],
                            op0=ALU.mult, op1=ALU.add),
                            reads=[pm.r, Asc.r, mod.r], writes=[dstT.r])

        es_m = ExitStack()
        wst = C.sbring(2, [128, KC, 128], F32, "wst", es_m)
        wbf = C.sbring(2, [128, KC, 128], BF16, "wbf", es_m)
        wq_flip = [0]

        def load_wchunk(d_ap):
            st = wst.next()
            q = "sp" if wq_flip[0] % 2 == 0 else "act"
            wq_flip[0] += 1
            S.dma(q, st[:], d_ap, writes=[st.r])
            wb = wbf.next()
            S.op("pool", lambda: G.tensor_copy(out=wb[:], in_=st[:]), reads=[st.r], writes=[wb.r])
            return wb

        h1T = C.sb([128, KC, T], BF16, "h1T", es_m)
        Ltok = C.sb([128, NT, 16], F32, "Ltok", es_m)
        Gtok = C.sb([128, NT, 16], F32, "Gtok", es_m)
        LrefB = C.sb([128, 64], F32, "LrefB", es_m)
        EQ = C.sb([16, T], BF16, "EQ", es_m)
        selm = C.sb([16, 4 * 128], BF16, "selm", es_m)
        qTr = [C.sb([128, T], BF16, f"qT{i}", es_m) for i in range(2)]
        kTr = [C.sb([128, T], BF16, f"kT{i}", es_m) for i in range(2)]
        qpr = [C.sbring(2, [128, 512], BF16, f"qp{i}", es_m) for i in range(2)]
        vtok = C.sb([128, NT, 256], BF16, "vtok", es_m)
        sigo = [C.sb([128, T], BF16, f"sigo{i}", es_m) for i in range(2)]
        rawc = C.sb([128, T + 4], F32, "rawc", es_m)
        cv = C.sb([128, T], F32, "cv", es_m)
        rawf_r = C.sbring(2, [128, 512], F32, "rawf", es_m)
        sqb_r = C.sbring(2, [128, 512], BF16, "sqb", es_m)
        rs_r = C.sbring(2, [128, 512], F32, "rs", es_m)
        pt_r = C.sbring(3, [128, 512], BF16, "pt", es_m)
        kb_r = C.sbring(2, [128, NT], F32, "kb", es_m)
        ktmp = C.sb([128, NT], F32, "ktmp", es_m)
        hT_r = [C.sbring(1, [128, 512], F32, f"hT{i}", es_m) for i in range(2)]
        mixo_r = C.sbring(2, [128, T], BF16, "mixo", es_m)
        es_n1 = ExitStack()
        norm_to_T(es_n1, lambda i: x_d[i * 128:(i + 1) * 128, :], None, A1, 0, h1T, NT, 2)
        S.barrier()
        es_n1.close()

        es_g = ExitStack()
        graw = C.sb([16, T], F32, "graw", es_g)
        lsp = C.sb([16, T], F32, "lsp", es_g)
        Lc = C.sb([16, T], F32, "Lc", es_g)
        for c in range(4):
            pm = psb.next()
            for kc in range(KC):
                S.op("pe", lambda kc=kc, pm=pm, c=c: P.matmul(
                    pm[0:16, :], lhsT=wg_b[:, kc, :], rhs=h1T[:, kc, c * 512:(c + 1) * 512],
                    start=(kc == 0), stop=(kc == KC - 1)),
                    reads=[wg_b.r, h1T.r], writes=[pm.r], inc=(kc == KC - 1))
            S.op("act", lambda pm=pm, c=c: A.activation(out=graw[:, c * 512:(c + 1) * 512], in_=pm[0:16, :],
                                                        func=AF.Identity, bias=gb_sb[:, 0:1]),
                 reads=[pm.r, gb_sb.r], writes=[graw.r])
        S.op("act", lambda: A.activation(out=lsp[:], in_=graw[:], func=AF.Exp, scale=-1.0),
             reads=[graw.r], writes=[lsp.r])
        S.op("act", lambda: A.activation(out=lsp[:], in_=lsp[:], func=AF.Ln, bias=ones_f[0:16, 0:1]),
             reads=[lsp.r, ones_f.r], writes=[lsp.r])
        S.op("dve", lambda: V.tensor_tensor_scan(out=Lc[:], data0=ones_f[0:16, 0:1].broadcast_to([16, T]), data1=lsp[:],
                                                 initial=zcol[0:16, 0:1], op0=ALU.mult, op1=ALU.add),
             reads=[ones_f.r, lsp.r, zcol.r], writes=[Lc.r])
        for (src, dst) in ((Lc, Ltok), (graw, Gtok)):
            for i4 in range(4):
                pm = psb.next()
                for j in range(4):
                    i = i4 * 4 + j
                    S.op("pe", lambda i=i, j=j, pm=pm, src=src: P.transpose(
                        out=pm[:, j * 16:(j + 1) * 16], in_=src[0:16, i * 128:(i + 1) * 128],
                        identity=ident_f[0:16, 0:16]), reads=[src.r, ident_f.r], writes=[pm.r], inc=(j == 3))
                S.op("dve", lambda i4=i4, pm=pm, dst=dst: V.tensor_copy(
                    out=dst[:, i4 * 4:(i4 + 1) * 4, :],
                    in_=pm[:, 0:64].rearrange("p (a b) -> p a b", a=4)),
                    reads=[pm.r], writes=[dst.r])
        pm = psb.next()
        for c in range(4):
            S.op("pe", lambda c=c, pm=pm: P.matmul(pm[:, c * 16:(c + 1) * 16], lhsT=sel127[:],
                                                   rhs=Ltok[:, 4 * c + 3, :], start=True, stop=True),
                 reads=[sel127.r, Ltok.r], writes=[pm.r], inc=(c == 3))
        S.op("dve", lambda pm=pm: V.tensor_copy(out=LrefB[:], in_=pm[:, 0:64]), reads=[pm.r], writes=[LrefB.r])
        for c in range(4):
            S.op("act", lambda c=c: A.activation(out=EQ[0:12, c * 512:(c + 1) * 512], in_=Lc[0:12, c * 512:(c + 1) * 512],
                                                 func=AF.Exp, scale=-1.0,
                                                 bias=Lc[0:12, c * 512 + 511:c * 512 + 512]),
                 reads=[Lc.r], writes=[EQ.r])
        for m in range(4):
            S.op("dve", lambda m=m: V.tensor_scalar(out=selm[:, m * 128:(m + 1) * 128], in0=ones_f[0:16, :],
                                                    scalar1=ident_f[0:16, 8 + m:9 + m], scalar2=None,
                                                    op0=ALU.mult),
                 reads=[ones_f.r, ident_f.r], writes=[selm.r])

        S.barrier()
        es_g.close()
        S.op("pool", lambda: G.memset(rawc[:, 0:4], 0.0), writes=[rawc.r])

        def proj_fm(wb, c, pm):
            for kc in range(KC):
                S.op("pe", lambda kc=kc: P.matmul(pm[:], lhsT=wb[:, kc, :], rhs=h1T[:, kc, c * 512:(c + 1) * 512],
                                                  start=(kc == 0), stop=(kc == KC - 1)),
                     reads=[wb.r, h1T.r], writes=[pm.r], inc=(kc == KC - 1))

        def fox_qk(wb, dst, gcol):
            def post(c, sqb, rawf):
                pn = psb.next()
                S.op("pe", lambda: P.matmul(pn[:], lhsT=ones_b[:], rhs=sqb[:], start=True, stop=True),
                     reads=[ones_b.r, sqb.r], writes=[pn.r])
                rs = rs_r.next()
                S.op("dve", lambda: V.tensor_scalar(out=rs[:], in0=pn[:], scalar1=1.0 / 128, scalar2=EPS,
                                                    op0=ALU.mult, op1=ALU.add), reads=[pn.r], writes=[rs.r])
                S.op("act", lambda: A.activation(out=rs[:], in_=rs[:], func=AF.Sqrt), reads=[rs.r], writes=[rs.r])
                S.op("dve", lambda: V.reciprocal(out=rs[:], in_=rs[:]), reads=[rs.r], writes=[rs.r])
                S.op("dve", lambda: V.scalar_tensor_tensor(out=dst[:, c * 512:(c + 1) * 512], in0=rawf[:],
                                                           scalar=qsc[:, gcol:gcol + 1], in1=rs[:],
                                                           op0=ALU.mult, op1=ALU.mult),
                     reads=[rawf.r, qsc.r, rs.r], writes=[dst.r])

            prev = None
            for c in range(4):
                pm = psb.next()
                proj_fm(wb, c, pm)
                sqb = sqb_r.next()
                rawf = rawf_r.next()
                S.op("act", lambda: A.activation(out=sqb[:], in_=pm[:], func=AF.Square), reads=[pm.r], writes=[sqb.r])
                S.op("act", lambda: A.copy(out=rawf[:], in_=pm[:]), reads=[pm.r], writes=[rawf.r])
                if prev is not None:
                    post(*prev)
                prev = (c, sqb, rawf)
            post(*prev)

        def v_tok(wb, dvc):
            for i4 in range(4):
                pm = psb.next()
                for j in range(4):
                    i = i4 * 4 + j
                    for kc in range(KC):
                        S.op("pe", lambda kc=kc, i=i, j=j: P.matmul(
                            pm[:, j * 128:(j + 1) * 128], lhsT=h1T[:, kc, i * 128:(i + 1) * 128], rhs=wb[:, kc, :],
                            start=(kc == 0), stop=(kc == KC - 1)),
                            reads=[h1T.r, wb.r], writes=[pm.r], inc=(kc == KC - 1 and j == 3))
                S.op("act", lambda: A.copy(out=vtok[:, i4 * 4:(i4 + 1) * 4, dvc * 128:(dvc + 1) * 128],
                                           in_=pm[:].rearrange("p (a b) -> p a b", a=4)),
                     reads=[pm.r], writes=[vtok.r])

        def ml_qk(wb, dst, cch):
            for c in range(4):
                pm = psb.next()
                proj_fm(wb, c, pm)
                S.op("act", lambda: A.copy(out=rawc[:, 4 + c * 512:4 + (c + 1) * 512], in_=pm[:]),
                     reads=[pm.r], writes=[rawc.r])
            S.op("dve", lambda: V.tensor_scalar(out=cv[:], in0=rawc[:, 4:4 + T], scalar1=cw_sb[:, cch, 3:4],
                                                scalar2=cb_sb[:, cch:cch + 1], op0=ALU.mult, op1=ALU.add),
                 reads=[rawc.r, cw_sb.r, cb_sb.r], writes=[cv.r])
            for j in range(3):
                S.op("dve", lambda j=j: V.scalar_tensor_tensor(out=cv[:], in0=rawc[:, 1 + j:1 + j + T],
                                                               scalar=cw_sb[:, cch, j:j + 1], in1=cv[:],
                                                               op0=ALU.mult, op1=ALU.add),
                     reads=[rawc.r, cw_sb.r, cv.r], writes=[cv.r])
            S.op("act", lambda: A.activation(out=dst[:], in_=cv[:], func=AF.Silu), reads=[cv.r], writes=[dst.r])

        def ml_o(wb, dst):
            for c in range(4):
                pm = psb.next()
                proj_fm(wb, c, pm)
                S.op("act", lambda: A.activation(out=dst[:, c * 512:(c + 1) * 512], in_=pm[:], func=AF.Sigmoid),
                     reads=[pm.r], writes=[dst.r])

        def attend(nd, ndv, row, is_fox, mrow, out_chunks, hgc0):
            mixo = [mixo_r.next() for _ in range(ndv)]
            for c in range(4):
                kb = kb_r.next()
                if is_fox:
                    S.op("dve", lambda: V.tensor_scalar(out=kb[:], in0=Ltok[:, :, row],
                                                        scalar1=LrefB[:, c * 16 + row:c * 16 + row + 1],
                                                        scalar2=None, op0=ALU.subtract),
                         reads=[Ltok.r, LrefB.r], writes=[kb.r])
                    qs = [qTr[d][:, c * 512:(c + 1) * 512] for d in range(nd)]
                    qres = [qTr[d].r for d in range(nd)]
                else:
                    S.op("dve", lambda: V.scalar_tensor_tensor(out=ktmp[:, 0:4 * c + 4], in0=Ltok[:, 0:4 * c + 4, row],
                                                               scalar=LrefB[:, c * 16 + row:c * 16 + row + 1],
                                                               in1=Gtok[:, 0:4 * c + 4, 12 + mrow],
                                                               op0=ALU.subtract, op1=ALU.add),
                         reads=[Ltok.r, LrefB.r, Gtok.r], writes=[ktmp.r])
                    S.op("act", lambda: A.activation(out=kb[:, 0:4 * c + 4], in_=ktmp[:, 0:4 * c + 4], func=AF.Exp, bias=lncol[:, 0:1]),
                         reads=[ktmp.r, lncol.r], writes=[kb.r])
                    pe_ = psb.next()
                    S.op("pe", lambda: P.matmul(pe_[:], lhsT=selm[0:12, mrow * 128:(mrow + 1) * 128],
                                                rhs=EQ[0:12, c * 512:(c + 1) * 512], start=True, stop=True),
                         reads=[selm.r, EQ.r], writes=[pe_.r])
                    qs, qres = [], []
                    for d in range(nd):
                        qp = qpr[d].next()
                        S.op("dve", lambda d=d, qp=qp: V.tensor_tensor(out=qp[:], in0=qTr[d][:, c * 512:(c + 1) * 512],
                                                                       in1=pe_[:], op=ALU.mult),
                             reads=[qTr[d].r, pe_.r], writes=[qp.r])
                        qs.append(qp[:])
                        qres.append(qp.r)
                pO = [acc_ps[d] for d in range(ndv)]
                pD = acc_ps[2]
                nj = 4 * c + 4

                def emit_pv(j, lo, pt):
                    for dv in range(ndv):
                        S.op("pe", lambda dv=dv: P.matmul(pO[dv][:, lo:512], lhsT=vtok[:, j, dv * 128:(dv + 1) * 128],
                                                          rhs=pt[:, lo:512], start=(j == 0), stop=(j == nj - 1)),
                             reads=[vtok.r, pt.r], writes=[pO[dv].r], inc=False)
                    S.op("pe", lambda: P.matmul(pD[:, lo:512], lhsT=ones_b[:], rhs=pt[:, lo:512],
                                                start=(j == 0), stop=(j == nj - 1)),
                         reads=[ones_b.r, pt.r], writes=[pD.r])

                prevpv = None
                for j in range(nj):
                    lo = 128 * (j - 4 * c) if j >= 4 * c else 0
                    pS = psb.next()
                    for d in range(nd):
                        S.op("pe", lambda d=d: P.matmul(pS[:, lo:512], lhsT=kTr[d][:, j * 128:(j + 1) * 128],
                                                        rhs=qs[d][:, lo:512], start=(d == 0), stop=(d == nd - 1)),
                             reads=[kTr[d].r, qres[d]], writes=[pS.r], inc=(d == nd - 1))
                    pt = pt_r.next()
                    if is_fox:
                        S.op("act", lambda: A.activation(out=pt[:, lo:512], in_=pS[:, lo:512], func=AF.Exp,
                                                         bias=kb[:, j:j + 1]),
                             reads=[pS.r, kb.r], writes=[pt.r])
                    else:
                        S.op("dve", lambda: V.tensor_scalar(out=pt[:, lo:512], in0=pS[:, lo:512],
                                                            scalar1=kb[:, j:j + 1], scalar2=None, op0=ALU.mult),
                             reads=[pS.r, kb.r], writes=[pt.r])
                    if j >= 4 * c:
                        S.op("pool", lambda: G.tensor_tensor(out=pt[:, lo:lo + 128], in0=pt[:, lo:lo + 128],
                                                             in1=tri_b[:], op=ALU.mult),
                             reads=[pt.r, tri_b.r], writes=[pt.r])
                    if prevpv is not None:
                        emit_pv(*prevpv)
                    prevpv = (j, lo, pt)
                emit_pv(*prevpv)
                rs = rs_r.next()
                if is_fox:
                    S.op("dve", lambda: V.reciprocal(out=rs[:], in_=pD[:]), reads=[pD.r], writes=[rs.r])
                    S.op("dve", lambda: V.tensor_tensor(out=mixo[0][:, c * 512:(c + 1) * 512], in0=pO[0][:],
                                                        in1=rs[:], op=ALU.mult),
                         reads=[pO[0].r, rs.r], writes=[mixo[0].r])
                else:
                    S.op("dve", lambda: V.tensor_scalar(out=rs[:], in0=pD[:], scalar1=-1.0, scalar2=1.0,
                                                        op0=ALU.mult, op1=ALU.max), reads=[pD.r], writes=[rs.r])
                    S.op("dve", lambda: V.scalar_tensor_tensor(out=rs[:], in0=pD[:], scalar=1.0, in1=rs[:],
                                                               op0=ALU.max, op1=ALU.max),
                         reads=[pD.r, rs.r], writes=[rs.r])
                    S.op("dve", lambda: V.reciprocal(out=rs[:], in_=rs[:]), reads=[rs.r], writes=[rs.r])
                    hTs = []
                    pn = psb.next()
                    for dv in range(ndv):
                        hT = hT_r[dv].next()
                        S.op("dve", lambda dv=dv, hT=hT: V.tensor_tensor(out=hT[:], in0=pO[dv][:], in1=rs[:], op=ALU.mult),
                             reads=[pO[dv].r, rs.r], writes=[hT.r])
                        sqb = sqb_r.next()
                        S.op("act", lambda hT=hT, sqb=sqb: A.activation(out=sqb[:], in_=hT[:], func=AF.Square),
                             reads=[hT.r], writes=[sqb.r])
                        S.op("pe", lambda dv=dv, sqb=sqb: P.matmul(pn[:], lhsT=ones_b[:], rhs=sqb[:],
                                                                    start=(dv == 0), stop=(dv == ndv - 1)),
                             reads=[ones_b.r, sqb.r], writes=[pn.r], inc=(dv == ndv - 1))
                        hTs.append(hT)
                    rs2 = rs_r.next()
                    S.op("dve", lambda: V.tensor_scalar(out=rs2[:], in0=pn[:], scalar1=1.0 / 256, scalar2=EPS,
                                                        op0=ALU.mult, op1=ALU.add), reads=[pn.r], writes=[rs2.r])
                    S.op("act", lambda: A.activation(out=rs2[:], in_=rs2[:], func=AF.Sqrt), reads=[rs2.r], writes=[rs2.r])
                    S.op("dve", lambda: V.reciprocal(out=rs2[:], in_=rs2[:]), reads=[rs2.r], writes=[rs2.r])
                    for dv in range(ndv):
                        S.op("dve", lambda dv=dv: V.scalar_tensor_tensor(
                            out=hTs[dv][:], in0=hTs[dv][:], scalar=hg_sb[:, hgc0 + dv:hgc0 + dv + 1], in1=rs2[:],
                            op0=ALU.mult, op1=ALU.mult), reads=[hTs[dv].r, hg_sb.r, rs2.r], writes=[hTs[dv].r])
                        S.op("dve", lambda dv=dv: V.tensor_tensor(
                            out=mixo[dv][:, c * 512:(c + 1) * 512], in0=hTs[dv][:],
                            in1=sigo[dv][:, c * 512:(c + 1) * 512], op=ALU.mult),
                            reads=[hTs[dv].r, sigo[dv].r], writes=[mixo[dv].r])
            for dv in range(ndv):
                S.dma("sp", mix_d[out_chunks[dv]], mixo[dv][:], reads=[mixo[dv].r],
                      writes=[mix_res[out_chunks[dv]]])

        for h in range(NFOX):
            fox_qk(load_wchunk(wfm_d[3 * h]), qTr[0], 0)
            fox_qk(load_wchunk(wfm_d[3 * h + 1]), kTr[0], 1)
            v_tok(load_wchunk(wfm_d[3 * h + 2]), 0)
            attend(1, 1, h, True, 0, [h], 0)
        for m in range(NML):
            b0 = 24 + 8 * m
            ml_qk(load_wchunk(wfm_d[b0 + 0]), qTr[0], 2 * m)
            ml_qk(load_wchunk(wfm_d[b0 + 1]), qTr[1], 2 * m + 1)
            ml_qk(load_wchunk(wfm_d[b0 + 2]), kTr[0], 8 + 2 * m)
            ml_qk(load_wchunk(wfm_d[b0 + 3]), kTr[1], 8 + 2 * m + 1)
            v_tok(load_wchunk(wfm_d[b0 + 4]), 0)
            v_tok(load_wchunk(wfm_d[b0 + 5]), 1)
            ml_o(load_wchunk(wfm_d[b0 + 6]), sigo[0])
            ml_o(load_wchunk(wfm_d[b0 + 7]), sigo[1])
            attend(2, 2, 8 + m, False, m, [8 + 2 * m, 8 + 2 * m + 1], 2 * m)
        S.barrier()
        es_m.close()

        es_o = ExitStack()
        mixT = C.sb([128, KC, T], BF16, "mixT", es_o)
        written = list(range(NFOX)) + [8 + j for j in range(2 * NML)]
        if len(written) < KC:
            S.op("pool", lambda: G.memset(mixT[:], 0.0), writes=[mixT.r])
        for kc in written:
            S.dma("sp" if kc % 2 == 0 else "act", mixT[:, kc, :], mix_d[kc], reads=[mix_res[kc]], writes=[mixT.r])
        gate1b = C.sb([128, D], F32, "gate1b", es_o)
        build_gate(gate1b, 32)
        wo_st = C.sb([128, KC, 512], F32, "wo_st", es_o)
        wo_bf = C.sbring(2, [128, KC, 512], BF16, "wo_bf", es_o)
        xs_r = C.sbring(3, [128, 512], F32, "xs", es_o)
        t1_r = C.sbring(3, [128, 512], F32, "t1", es_o)
        for cb in range(4):
            S.dma("sp", wo_st[:], wout_d[cb], writes=[wo_st.r])
            wob = wo_bf.next()
            S.op("pool", lambda: G.tensor_copy(out=wob[:], in_=wo_st[:]), reads=[wo_st.r], writes=[wob.r])
            for i in range(NT):
                pm = psb.next()
                for kc in range(KC):
                    S.op("pe", lambda kc=kc: P.matmul(pm[:], lhsT=mixT[:, kc, i * 128:(i + 1) * 128], rhs=wob[:, kc, :],
                                                      start=(kc == 0), stop=(kc == KC - 1)),
                         reads=[mixT.r, wob.r], writes=[pm.r], inc=(kc == KC - 1))
                xs = xs_r.next()
                S.dma("act", xs[:], x_d[i * 128:(i + 1) * 128, cb * 512:(cb + 1) * 512], writes=[xs.r])
                t1 = t1_r.next()
                S.op("dve", lambda: V.tensor_tensor(out=t1[:], in0=pm[:], in1=gate1b[:, cb * 512:(cb + 1) * 512],
                                                    op=ALU.mult), reads=[pm.r, gate1b.r], writes=[t1.r])
                S.op("pool", lambda: G.tensor_tensor(out=t1[:], in0=t1[:], in1=xs[:], op=ALU.add),
                     reads=[t1.r, xs.r], writes=[t1.r])
                S.dma("sp", out_d[i * 128:(i + 1) * 128, cb * 512:(cb + 1) * 512], t1[:], reads=[t1.r],
                      writes=[out_res[i]])
        S.barrier()
        es_o.close()

        es_p = ExitStack()
        psb = Ring(psb.bufs + acc_ps)
        wst = C.sbring(3, [128, KC, 128], F32, "wstp", es_p)
        wbf = C.sbring(2, [128, KC, 128], BF16, "wbfp", es_p)
        gate2b = C.sb([128, D], F32, "gate2b", es_p)
        build_gate(gate2b, 80)
        h2T = C.sb([128, KC, 512], BF16, "h2T", es_p)
        qT = C.sb([128, 16, 512], BF16, "qTp", es_p)
        acc = C.sb([128, 4, D], F32, "acc", es_p)
        Cb = C.sb([128, 8, 512], BF16, "Cb", es_p)
        statsT = C.sb([32, 512], BF16, "statsT", es_p)
        selT = C.sb([32, 8 * 128], BF16, "selT", es_p)
        selg = C.sb([32, 8 * 128], BF16, "selg", es_p)
        ucol = C.sb([32, 8], F32, "ucol", es_p)
        for h in range(8):
            S.op("dve", lambda h=h: V.tensor_tensor(out=ucol[:, h:h + 1], in0=ident_f[0:32, h:h + 1],
                                                    in1=ident_f[0:32, 8 + h:9 + h], op=ALU.add),
                 reads=[ident_f.r, ucol.r], writes=[ucol.r])
            S.op("dve", lambda h=h: V.scalar_tensor_tensor(out=ucol[:, h:h + 1], in0=ucol[:, h:h + 1], scalar=-1.0,
                                                           in1=ident_f[0:32, 16 + h:17 + h],
                                                           op0=ALU.mult, op1=ALU.subtract),
                 reads=[ident_f.r, ucol.r], writes=[ucol.r])
            S.op("dve", lambda h=h: V.tensor_scalar(out=selT[:, h * 128:(h + 1) * 128], in0=ones_f[0:32, :],
                                                    scalar1=ucol[:, h:h + 1], scalar2=None, op0=ALU.mult),
                 reads=[ones_f.r, ucol.r], writes=[selT.r])
            S.op("dve", lambda h=h: V.tensor_scalar(out=selg[:, h * 128:(h + 1) * 128], in0=ones_f[0:32, :],
                                                    scalar1=ident_f[0:32, 24 + h:25 + h], scalar2=None, op0=ALU.mult),
                 reads=[ones_f.r, ident_f.r], writes=[selg.r])
        k1bc_r = C.sbring(2, [128, 128], BF16, "k1bc", es_p)
        eu_st = C.sbring(2, [128, D], F32, "eu_st", es_p)
        eu_bf = C.sbring(GK + 2, [128, D], BF16, "eu_bf", es_p)
        wT_r = C.sbring(2 * GK, [128, 512], BF16, "wT", es_p)
        gA_r = C.sbring(2, [128, 512], BF16, "gA", es_p)
        E_r = C.sbring(2, [128, 512], BF16, "E", es_p)
        Mm2_r = C.sbring(2, [128, 2, 512], BF16, "Mm2", es_p)
        Tt2_r = C.sbring(2, [128, 2, 512], BF16, "Tt2", es_p)
        Ga2_r = C.sbring(2, [128, 2, 512], BF16, "Ga2", es_p)
        sc_r = C.sbring(2, [128, 256], F32, "sc", es_p)
        sc2_r = C.sbring(1, [128, 256], F32, "sc2", es_p)
        v12_r = C.sbring(2, [128, 32], F32, "v12", es_p)
        cand_r = C.sbring(2, [128, 256], F32, "cand", es_p)
        cand2_r = C.sbring(1, [128, 256], F32, "cand2", es_p)
        c16_r = C.sbring(2, [128, 16], F32, "c16", es_p)
        e16_r = C.sbring(2, [128, 16], F32, "e16", es_p)
        sm_r = C.sbring(2, [128, 4], F32, "sm", es_p)
        statf = C.sb([128, 48], F32, "statf", es_p)
        statb = C.sb([128, 32], BF16, "statb", es_p)

        for Q in range(NQ):
            es_n2 = ExitStack()
            norm_to_T(es_n2, lambda i: out_d[(Q * 4 + i) * 128:(Q * 4 + i + 1) * 128, :],
                      lambda i: out_res[Q * 4 + i], A2, 48, h2T, 4, 1)
            S.barrier()
            es_n2.close()
            for cc in range(16):
                st = wst.next()
                S.dma("sp" if cc % 2 == 0 else "act", st[:], wq_d[cc], writes=[st.r])
                wb = wbf.next()
                S.op("pool", lambda: G.tensor_copy(out=wb[:], in_=st[:]), reads=[st.r], writes=[wb.r])
                pm = psb.next()
                for kc in range(KC):
                    S.op("pe", lambda kc=kc: P.matmul(pm[:], lhsT=wb[:, kc, :], rhs=h2T[:, kc, :],
                                                      start=(kc == 0), stop=(kc == KC - 1)),
                         reads=[wb.r, h2T.r], writes=[pm.r], inc=(kc == KC - 1))
                S.op("act", lambda: A.copy(out=qT[:, cc, :], in_=pm[:]), reads=[pm.r], writes=[qT.r])
            for ti in range(4):
                for h in range(8):
                    pm = psb.next()
                    S.op("pe", lambda: P.matmul(pm[:, 0:128], lhsT=qT[:, 2 * h, ti * 128:(ti + 1) * 128],
                                                rhs=k1t_b[:], start=True, stop=True),
                         reads=[qT.r, k1t_b.r], writes=[pm.r], inc=False)
                    S.op("pe", lambda: P.matmul(pm[:, 128:256], lhsT=qT[:, 2 * h + 1, ti * 128:(ti + 1) * 128],
                                                rhs=k2t_b[:], start=True, stop=True),
                         reads=[qT.r, k2t_b.r], writes=[pm.r])
                    sc = sc_r.next()
                    sc2 = sc2_r.next()
                    v12 = v12_r.next()
                    S.op("act", lambda: A.copy(out=sc[:], in_=pm[:, 0:256]), reads=[pm.r], writes=[sc.r])
                    for half in range(2):
                        sl = slice(half * 128, (half + 1) * 128)
                        vo = half * 16
                        S.op("dve", lambda: V.max(out=v12[:, vo:vo + 8], in_=sc[:, sl]), reads=[sc.r, v12.r], writes=[v12.r])
                        S.op("dve", lambda: V.match_replace(out=sc2[:, sl], in_to_replace=v12[:, vo:vo + 8],
                                                            in_values=sc[:, sl], imm_value=NEG),
                             reads=[sc.r, v12.r, sc2.r], writes=[sc2.r])
                        S.op("dve", lambda: V.max(out=v12[:, vo + 8:vo + 16], in_=sc2[:, sl]),
                             reads=[sc2.r, v12.r], writes=[v12.r])
                    cand = cand_r.next()
                    cand2 = cand2_r.next()
                    c16 = c16_r.next()
                    e16 = e16_r.next()
                    sm = sm_r.next()
                    S.op("dve", lambda: V.tensor_tensor(
                        out=cand[:].rearrange("p (a b) -> p a b", a=16),
                        in0=v12[:, 0:16].unsqueeze(2).broadcast_to([128, 16, 16]),
                        in1=v12[:, 16:32].unsqueeze(1).broadcast_to([128, 16, 16]), op=ALU.add),
                        reads=[v12.r], writes=[cand.r])
                    S.op("dve", lambda: V.max(out=c16[:, 0:8], in_=cand[:]), reads=[cand.r, c16.r], writes=[c16.r])
                    S.op("dve", lambda: V.match_replace(out=cand2[:], in_to_replace=c16[:, 0:8], in_values=cand[:],
                                                        imm_value=NEG), reads=[cand.r, c16.r], writes=[cand2.r])
                    S.op("dve", lambda: V.max(out=c16[:, 8:16], in_=cand2[:]), reads=[cand2.r, c16.r], writes=[c16.r])
                    S.op("dve", lambda: V.tensor_scalar(out=sm[:, 0:1], in0=c16[:, 0:1], scalar1=-1.0, scalar2=None,
                                                        op0=ALU.mult), reads=[c16.r, sm.r], writes=[sm.r])
                    S.op("dve", lambda: V.memset(sm[:, 1:2], 0.0), reads=[sm.r], writes=[sm.r])
                    S.op("act", lambda: A.activation(out=e16[:], in_=c16[:], func=AF.Exp, bias=sm[:, 0:1],
                                                     accum_out=sm[:, 1:2]),
                         reads=[c16.r, sm.r], writes=[e16.r, sm.r])
                    S.op("dve", lambda: V.reciprocal(out=sm[:, 2:3], in_=sm[:, 1:2]), reads=[sm.r], writes=[sm.r])
                    S.op("dve", lambda: V.tensor_scalar(out=statf[:, h:h + 1], in0=c16[:, 15:16], scalar1=-3.0e-5,
                                                        scalar2=None, op0=ALU.add),
                         reads=[c16.r, statf.r], writes=[statf.r])
                    S.op("dve", lambda: V.tensor_tensor(out=statf[:, 32 + h:33 + h], in0=e16[:, 15:16], in1=sm[:, 2:3],
                                                        op=ALU.mult), reads=[e16.r, sm.r, statf.r], writes=[statf.r])
                S.op("dve", lambda: V.tensor_copy(out=statb[:, 0:8], in_=statf[:, 0:8]), reads=[statf.r, statb.r], writes=[statb.r])
                S.op("dve", lambda: V.tensor_tensor(out=statf[:, 8:16], in0=statf[:, 0:8], in1=statb[:, 0:8],
                                                    op=ALU.subtract), reads=[statf.r, statb.r], writes=[statf.r])
                S.op("dve", lambda: V.tensor_copy(out=statb[:, 8:16], in_=statf[:, 8:16]), reads=[statf.r, statb.r], writes=[statb.r])
                S.op("dve", lambda: V.tensor_tensor(out=statf[:, 16:24], in0=statf[:, 8:16], in1=statb[:, 8:16],
                                                    op=ALU.subtract), reads=[statf.r, statb.r], writes=[statf.r])
                S.op("dve", lambda: V.tensor_copy(out=statb[:, 16:24], in_=statf[:, 16:24]), reads=[statf.r, statb.r], writes=[statb.r])
                S.op("dve", lambda: V.tensor_copy(out=statb[:, 24:32], in_=statf[:, 32:40]), reads=[statf.r, statb.r], writes=[statb.r])
                pm = psb.next()
                pmb = pm.t.bitcast(BF16)
                S.op("pe", lambda: P.transpose(out=pmb[0:32, 0:128], in_=statb[:, 0:32], identity=ident_b[:]),
                     reads=[statb.r, ident_b.r], writes=[pm.r])
                S.op("act", lambda: A.copy(out=statsT[:, ti * 128:(ti + 1) * 128], in_=pmb[0:32, 0:128]),
                     reads=[pm.r], writes=[statsT.r])
            for h in range(8):
                pm = psb.next()
                S.op("pe", lambda: P.matmul(pm[:], lhsT=selg[:, h * 128:(h + 1) * 128], rhs=statsT[:],
                                            start=True, stop=True), reads=[selg.r, statsT.r], writes=[pm.r])
                S.op("act", lambda: A.copy(out=Cb[:, h, :], in_=pm[:]), reads=[pm.r], writes=[Cb.r])

            grp = []
            ngrp = 0
            pending = []

            def emit_units(n):
                for _ in range(min(n, len(pending))):
                    g_, ti, cbk, first = pending.pop(0)
                    pm = psb.next()
                    for gi, (wT_, eub_) in enumerate(g_):
                        S.op("pe", lambda gi=gi, wT_=wT_, eub_=eub_: P.matmul(
                            pm[:], lhsT=wT_[:, ti * 128:(ti + 1) * 128], rhs=eub_[:, cbk * 512:(cbk + 1) * 512],
                            start=(gi == 0), stop=(gi == len(g_) - 1)),
                            reads=[wT_.r, eub_.r], writes=[pm.r], inc=(gi == len(g_) - 1))
                    if first:
                        S.op("act", lambda: A.copy(out=acc[:, ti, cbk * 512:(cbk + 1) * 512], in_=pm[:]),
                             reads=[pm.r], writes=[acc.r])
                    else:
                        S.op("dve", lambda: V.tensor_tensor(out=acc[:, ti, cbk * 512:(cbk + 1) * 512],
                                                            in0=pm[:], in1=acc[:, ti, cbk * 512:(cbk + 1) * 512],
                                                            op=ALU.add), reads=[pm.r, acc.r], writes=[acc.r])

            prepped = {}

            def prep(e1):
                st = wst.next()
                S.dma("sp", st[:], edt_d[e1], writes=[st.r])
                edb = wbf.next()
                S.op("act", lambda: A.copy(out=edb[:], in_=st[:]), reads=[st.r], writes=[edb.r])
                es_ = eu_st.next()
                S.dma("act", es_[:], eu_d[e1], writes=[es_.r])
                eub = eu_bf.next()
                S.op("act", lambda: A.copy(out=eub[:], in_=es_[:]), reads=[es_.r], writes=[eub.r])
                k1bc = k1bc_r.next()
                S.op("pool", lambda: G.tensor_copy(out=k1bc[:], in_=k1t_b[:, e1:e1 + 1].broadcast_to([128, 128])),
                     reads=[k1t_b.r], writes=[k1bc.r])
                prepped[e1] = (edb, eub, k1bc)

            prep(0)
            for e1 in range(NE1):
                if e1 + 1 < NE1:
                    prep(e1 + 1)
                edb, eub, k1bc = prepped.pop(e1)
                pA = psb.next()
                for kc in range(KC):
                    S.op("pe", lambda kc=kc: P.matmul(pA[:], lhsT=edb[:, kc, :], rhs=h2T[:, kc, :],
                                                      start=(kc == 0), stop=(kc == KC - 1)),
                         reads=[edb.r, h2T.r], writes=[pA.r], inc=(kc == KC - 1))
                gA = gA_r.next()
                S.op("act", lambda: A.activation(out=gA[:], in_=pA[:], func=AF.Gelu), reads=[pA.r], writes=[gA.r])
                Ga2 = Ga2_r.next()
                for hp in range(4):
                    Mm2 = Mm2_r.next()
                    for hh in range(2):
                        h = 2 * hp + hh
                        pX = psb.next()
                        S.op("pe", lambda: P.matmul(pX[:], lhsT=k2t_b[:], rhs=qT[:, 2 * h + 1, :], start=True, stop=False),
                             reads=[k2t_b.r, qT.r], writes=[pX.r], inc=False)
                        S.op("pe", lambda: P.matmul(pX[:], lhsT=k1bc[:], rhs=qT[:, 2 * h, :], start=False, stop=False),
                             reads=[k1bc.r, qT.r], writes=[pX.r], inc=False)
                        S.op("pe", lambda: P.matmul(pX[:], lhsT=selT[:, h * 128:(h + 1) * 128], rhs=statsT[:],
                                                    start=False, stop=True),
                             reads=[selT.r, statsT.r], writes=[pX.r])
                        E = E_r.next()
                        S.op("act", lambda: A.activation(out=E[:], in_=pX[:], func=AF.Exp), reads=[pX.r], writes=[E.r])
                        S.op("dve", lambda: V.scalar_tensor_tensor(out=Mm2[:, hh, :], in0=pX[:], scalar=0.0, in1=E[:],
                                                                   op0=ALU.is_ge, op1=ALU.mult),
                             reads=[pX.r, E.r, Mm2.r], writes=[Mm2.r])
                    if hp == 0:
                        S.op("dve", lambda: V.tensor_tensor(out=Ga2[:], in0=Mm2[:], in1=Cb[:, 0:2, :], op=ALU.mult),
                             reads=[Mm2.r, Cb.r], writes=[Ga2.r])
                    else:
                        Tt2 = Tt2_r.next()
                        S.op("dve", lambda: V.tensor_tensor(out=Tt2[:], in0=Mm2[:], in1=Cb[:, 2 * hp:2 * hp + 2, :],
                                                            op=ALU.mult), reads=[Mm2.r, Cb.r], writes=[Tt2.r])
                        S.op("pool", lambda: G.tensor_tensor(out=Ga2[:], in0=Ga2[:], in1=Tt2[:], op=ALU.add),
                             reads=[Ga2.r, Tt2.r], writes=[Ga2.r])
                S.op("pool", lambda: G.tensor_tensor(out=Ga2[:, 0, :], in0=Ga2[:, 0, :], in1=Ga2[:, 1, :], op=ALU.add),
                     reads=[Ga2.r], writes=[Ga2.r])
                wT = wT_r.next()
                S.op("dve", lambda: V.tensor_tensor(out=wT[:], in0=Ga2[:, 0, :], in1=gA[:], op=ALU.mult),
                     reads=[Ga2.r, gA.r], writes=[wT.r])
                grp.append((wT, eub))
                emit_units(16)
                if len(grp) == GK or e1 == NE1 - 1:
                    for ti in range(4):
                        for cbk in range(4):
                            pending.append((list(grp), ti, cbk, ngrp == 0))
                    grp = []
                    ngrp += 1
            emit_units(len(pending))
            es_f = ExitStack()
            x1_r = C.sbring(1, [128, D], F32, "x1t", es_f)
            for ti in range(4):
                i = Q * 4 + ti
                x1 = x1_r.next()
                S.dma("sp", x1[:], out_d[i * 128:(i + 1) * 128, :], reads=[out_res[i]], writes=[x1.r])
                S.op("dve", lambda: V.tensor_tensor(out=acc[:, ti, :], in0=acc[:, ti, :], in1=gate2b[:], op=ALU.mult),
                     reads=[acc.r, gate2b.r], writes=[acc.r])
                S.op("pool", lambda: G.tensor_tensor(out=acc[:, ti, :], in0=acc[:, ti, :], in1=x1[:], op=ALU.add),
                     reads=[acc.r, x1.r], writes=[acc.r])
                S.dma("sp", out_d[i * 128:(i + 1) * 128, :], acc[:, ti, :], reads=[acc.r, out_res[i]], writes=[out_res[i]])
            S.barrier()
            es_f.close()
        S.barrier()
        es_p.close()

        for q in ("sp", "pool", "act"):
            for sem, val in S.dma_slots[q]:
                if val > 0:
                    S._wait("sp", (sem, val))
        print("instructions:", S.ninstr)
    return nc


def _host_layouts(inp):
    f = lambda a: np.ascontiguousarray(a, dtype=np.float32)
    L = {}
    w_ada = inp["w_ada"][0]
    L["wada_r"] = f(w_ada.reshape(KC, 128, 24, 512).transpose(2, 1, 0, 3))
    L["bada_r"] = f(inp["b_ada"][0].reshape(96, 128).T)
    L["g1_r"] = f(inp["norm1_gain"][0].reshape(KC, 128).T)
    L["g2_r"] = f(inp["norm2_gain"][0].reshape(KC, 128).T)
    w_in = inp["w_in"][0]
    o_fq, o_fk, o_fv, o_ff, o_mq, o_mk, o_mv, o_mo, o_mi, o_mf = 0, 1024, 2048, 3072, 3080, 4104, 5128, 6152, 7176, 7180
    cols = []
    for h in range(8):
        cols += [o_fq + 128 * h, o_fk + 128 * h, o_fv + 128 * h]
    for m in range(4):
        cols += [o_mq + 256 * m, o_mq + 256 * m + 128, o_mk + 256 * m, o_mk + 256 * m + 128,
                 o_mv + 256 * m, o_mv + 256 * m + 128, o_mo + 256 * m, o_mo + 256 * m + 128]
    wfm = np.empty((56, 128, KC, 128), np.float32)
    for i, c0 in enumerate(cols):
        wfm[i] = w_in[:, c0:c0 + 128].reshape(KC, 128, 128).transpose(1, 0, 2)
    L["wfm_r"] = wfm
    gcols = list(range(o_ff, o_ff + 8)) + list(range(o_mf, o_mf + 4)) + list(range(o_mi, o_mi + 4))
    L["wg_r"] = f(w_in[:, gcols].reshape(KC, 128, 16).transpose(1, 0, 2))
    L["gb_r"] = f(np.concatenate([inp["fox_f_bias"][0], inp["mlstm_f_bias"][0], inp["mlstm_i_bias"][0]]).reshape(16, 1))
    L["qkg_r"] = f(np.stack([inp["fox_q_gain"][0], inp["fox_k_gain"][0]], axis=1))
    L["convw_r"] = f(inp["mlstm_conv_w"][0].reshape(4, 16, 128).transpose(2, 1, 0))
    L["convb_r"] = f(inp["mlstm_conv_b"][0].reshape(16, 128).T)
    L["hg_r"] = f(inp["mlstm_head_gain"][0].reshape(8, 128).T)
    L["wout_r"] = f(inp["w_out"][0].reshape(KC, 128, 4, 512).transpose(2, 1, 0, 3))
    L["wq_r"] = f(inp["peer_w_query"][0].reshape(KC, 128, 16, 128).transpose(2, 1, 0, 3))
    L["k1t_r"] = f(inp["peer_sub_keys_1"][0].T)
    L["k2t_r"] = f(inp["peer_sub_keys_2"][0].T)
    ed = inp["peer_expert_down"][0]
    L["edt_r"] = f(ed.reshape(128, 128, KC, 128).transpose(0, 3, 2, 1))
    L["eu_r"] = f(inp["peer_expert_up"][0].reshape(128, 128, D))
    return L


def _core_inputs(inputs, b):
    return {
        "x": np.ascontiguousarray(inputs["x"][b], dtype=np.float32),
        "c_r": np.ascontiguousarray(inputs["c"][b].reshape(KC, 128).T, dtype=np.float32),
    }


def kernel(**inputs):
    inputs = {k: np.asarray(v) for k, v in inputs.items()}
    L = _host_layouts(inputs)
    nc = build_program()
    outs = []
    for g0 in range(0, 8, CORES_PER_LAUNCH):
        in_maps = []
        for b in range(g0, g0 + CORES_PER_LAUNCH):
            m = dict(L)
            m.update(_core_inputs(inputs, b))
            in_maps.append(m)
        res = run_bass_kernel_spmd(nc, in_maps, core_ids=list(range(CORES_PER_LAUNCH)))
        outs += [np.asarray(r["out"], dtype=np.float32) for r in res.results]
    return np.stack(outs, axis=0)
```

```python
from contextlib import ExitStack
import numpy as np
import concourse.bass as bass
import concourse.mybir as mybir
from concourse.bass_utils import run_bass_kernel_spmd

F32 = mybir.dt.float32
BF16 = mybir.dt.bfloat16
ALU = mybir.AluOpType
AF = mybir.ActivationFunctionType
AX = mybir.AxisListType

import os
NBLK = int(os.environ.get("NBLK", "24"))
ADAQ = os.environ.get("ADAQ", "pool")
D = 2048
T = 2048
KC = 16
NT = 16
EPS = 1e-6


class Res:
    __slots__ = ("name", "w", "r")

    def __init__(self, name=""):
        self.name = name
        self.w = None
        self.r = []


class Sched:
    SEM_LIMIT = 30000

    def __init__(self, nc, es):
        self.nc = nc
        self.es = es
        self.engs = {"pe": nc.tensor, "act": nc.scalar, "dve": nc.vector,
                     "pool": nc.gpsimd, "sp": nc.sync}
        self.sem = {}
        self.cnt = {}
        self.nsem = 0
        self.pe_sems = []
        self.pend = {}
        for e in ("pe", "act", "dve", "pool"):
            self._new_sem(e)
        self.waited = {e: {} for e in self.engs}
        self.dma_slots = {}
        self.dma_next = {}
        for q, n in (("sp", 8), ("pool", 2), ("act", 4)):
            self.dma_slots[q] = [[self._mk(f"d{q}{i}"), 0] for i in range(n)]
            self.dma_next[q] = 0
        self.ninstr = 0

    def _mk(self, name):
        self.nsem += 1
        return self.es.enter_context(self.nc.semaphore(f"{name}_{self.nsem}"))

    def _new_sem(self, e):
        self.sem[e] = self._mk(f"s{e}")
        self.cnt[e] = 0
        if e == "pe":
            self.pe_sems.append(self.sem[e])

    def _wait(self, e, tok):
        sem, val = tok
        if e == "pe" and sem in self.pe_sems:
            return
        key = id(sem)
        if self.waited[e].get(key, 0) >= val:
            return
        self.engs[e].wait_ge(sem, val)
        self.waited[e][key] = val

    def _deps(self, e, reads, writes):
        toks = []
        for r in reads:
            if r.w is not None:
                toks.append(r.w)
        for w in writes:
            if w.w is not None:
                toks.append(w.w)
            toks.extend(w.r)
        for t in toks:
            self._wait(e, t)

    def _mark(self, tok, reads, writes):
        for w in writes:
            w.w = tok
            w.r = []
        for r in reads:
            if r in writes:
                continue
            r.r.append(tok)
            if len(r.r) > 24:
                r.r = r.r[-24:]

    def op(self, e, fn, reads=(), writes=(), inc=True):
        self._deps(e, reads, writes)
        if not self.pend.get(e, False) and self.cnt[e] >= self.SEM_LIMIT:
            self._new_sem(e)
        self.pend[e] = not inc
        ins = fn()
        self.ninstr += 1
        if inc:
            ins.then_inc(self.sem[e], 1)
            self.cnt[e] += 1
            tok = (self.sem[e], self.cnt[e])
            self._pending_ok = True
        else:
            tok = (self.sem[e], self.cnt[e] + 1)
        self._mark(tok, reads, writes)
        return tok

    def dma(self, q, out, in_, reads=(), writes=(), **kw):
        slots = self.dma_slots[q]
        i = self.dma_next[q]
        self.dma_next[q] = (i + 1) % len(slots)
        sem, val = slots[i]
        if val > 0:
            self._wait(q, (sem, val))
        self._deps(q, reads, writes)
        ins = self.engs[q].dma_start(out=out, in_=in_, **kw)
        ins.then_inc(sem, 16)
        slots[i][1] = val + 16
        tok = (sem, val + 16)
        self._mark(tok, reads, writes)
        self.ninstr += 1
        return tok

    def barrier(self):
        toks = []
        for e in ("pe", "act", "dve", "pool"):
            if self.cnt[e] > 0:
                assert not self.pend.get(e, False), f"open group on {e} at barrier"
                toks.append((self.sem[e], self.cnt[e]))
        for q in self.dma_slots:
            for sem, val in self.dma_slots[q]:
                if val > 0:
                    toks.append((sem, val))
        for e in self.engs:
            for t in toks:
                self._wait(e, t)

    def wait_all(self, e, ress):
        for r in ress:
            if r.w is not None:
                self._wait(e, r.w)


class Buf:
    def __init__(self, t, name):
        self.t = t
        self.r = Res(name)

    def __getitem__(self, idx):
        return self.t[idx]


class Ring:
    def __init__(self, bufs):
        self.bufs = bufs
        self.i = 0

    def next(self):
        b = self.bufs[self.i]
        self.i = (self.i + 1) % len(self.bufs)
        return b


class Ctx:
    def __init__(self, nc, es):
        self.nc = nc
        self.es = es
        self.S = Sched(nc, es)
        self.n = 0

    def sb(self, shape, dt, name, es=None):
        self.n += 1
        t = (es or self.es).enter_context(self.nc.sbuf_tensor(f"{name}_{self.n}", list(shape), dt))
        return Buf(t, name)

    def ps(self, shape, dt, name, es=None):
        self.n += 1
        t = (es or self.es).enter_context(self.nc.psum_tensor(f"{name}_{self.n}", list(shape), dt))
        return Buf(t, name)

    def sbring(self, n, shape, dt, name, es=None):
        return Ring([self.sb(shape, dt, f"{name}{i}", es) for i in range(n)])


NE1 = int(os.environ.get("NE1", "128"))
NQ = int(os.environ.get("NQ", "4"))
NFOX = int(os.environ.get("NFOX", "8"))
NML = int(os.environ.get("NML", "4"))
GK = 4
NEG = -1.0e30
CORES_PER_LAUNCH = 8


def build_program(stage=99, dbg=False):
    nc = bass.Bass("TRN2", target_bir_lowering=False)

    def din(name, shape, dt=F32):
        return nc.dram_tensor(name, list(shape), dt, kind="ExternalInput").ap()

    x_d = din("x", [T, D])
    c_d = din("c_r", [128, KC])
    wada_d = din("wada_r", [24, 128, KC, 512])
    bada_d = din("bada_r", [128, 96])
    g1_d = din("g1_r", [128, KC])
    g2_d = din("g2_r", [128, KC])
    wfm_d = din("wfm_r", [56, 128, KC, 128])
    wg_d = din("wg_r", [128, KC, 16])
    gb_d = din("gb_r", [16, 1])
    qkg_d = din("qkg_r", [128, 2])
    cw_d = din("convw_r", [128, 16, 4])
    cb_d = din("convb_r", [128, 16])
    hg_d = din("hg_r", [128, 8])
    wout_d = din("wout_r", [4, 128, KC, 512])
    wq_d = din("wq_r", [16, 128, KC, 128])
    k1t_d = din("k1t_r", [128, 128])
    k2t_d = din("k2t_r", [128, 128])
    edt_d = din("edt_r", [128, 128, KC, 128])
    eu_d = din("eu_r", [128, 128, D])
    out_d = nc.dram_tensor("out", [T, D], F32, kind="ExternalOutput").ap()
    mix_d = nc.dram_tensor("mix_scr", [KC, 128, T], BF16, kind="Internal").ap()
    if dbg:
        dbg_d = nc.dram_tensor("dbg", [128, 4096], F32, kind="ExternalOutput").ap()

    with ExitStack() as es:
        C = Ctx(nc, es)
        S = C.S
        V, A, P, G = nc.vector, nc.scalar, nc.tensor, nc.gpsimd

        psb = Ring([C.ps([128, 512], F32, f"ps{i}") for i in range(4)])
        acc_ps = [C.ps([128, 512], F32, f"pacc{i}") for i in range(4)]
        out_res = [Res(f"out{i}") for i in range(NT)]
        mix_res = [Res(f"mix{i}") for i in range(KC)]

        ident_f = C.sb([128, 128], F32, "ident_f")
        ident_b = C.sb([128, 128], BF16, "ident_b")
        ones_f = C.sb([128, 128], F32, "ones_f")
        ones_b = C.sb([128, 128], BF16, "ones_b")
        sel127 = C.sb([128, 128], F32, "sel127")
        tri_b = C.sb([128, 128], BF16, "tri_b")
        zcol = C.sb([128, 1], F32, "zcol")
        lncol = C.sb([128, 1], F32, "lncol")
        S.op("pool", lambda: G.memset(ones_f[:], 1.0), writes=[ones_f.r])
        S.op("pool", lambda: G.memset(ones_b[:], 1.0), writes=[ones_b.r])
        S.op("pool", lambda: G.memset(zcol[:], 0.0), writes=[zcol.r])
        S.op("pool", lambda: G.memset(lncol[:], float(np.log(1.0 / 16.0))), writes=[lncol.r])
        S.op("pool", lambda: G.affine_select(out=ident_f[:], in_=ones_f[:], pattern=[[-1, 128]],
                                             compare_op=ALU.is_equal, fill=0.0, base=0,
                                             channel_multiplier=1),
             reads=[ones_f.r], writes=[ident_f.r])
        S.op("pool", lambda: G.tensor_copy(out=ident_b[:], in_=ident_f[:]),
             reads=[ident_f.r], writes=[ident_b.r])
        S.op("pool", lambda: G.affine_select(out=sel127[:], in_=ones_f[:], pattern=[[0, 128]],
                                             compare_op=ALU.is_equal, fill=0.0, base=-127,
                                             channel_multiplier=1),
             reads=[ones_f.r], writes=[sel127.r])
        S.op("pool", lambda: G.affine_select(out=tri_b[:], in_=ones_b[:], pattern=[[1, 128]],
                                             compare_op=ALU.is_ge, fill=0.0, base=0,
                                             channel_multiplier=-1),
             reads=[ones_b.r], writes=[tri_b.r])

        def load_small(d_ap, shape, name, dt=F32):
            b = C.sb(shape, dt, name)
            S.dma("sp", b[:], d_ap, writes=[b.r])
            return b

        c_sb = load_small(c_d, [128, KC], "c_sb")
        bada_sb = load_small(bada_d, [128, 96], "bada")
        g1_sb = load_small(g1_d, [128, KC], "g1")
        g2_sb = load_small(g2_d, [128, KC], "g2")
        gb_sb = load_small(gb_d, [16, 1], "gb")
        qkg_sb = load_small(qkg_d, [128, 2], "qkg")
        cw_sb = load_small(cw_d, [128, 16, 4], "cw")
        cb_sb = load_small(cb_d, [128, 16], "cb")
        hg_sb = load_small(hg_d, [128, 8], "hg")
        k1t_f = load_small(k1t_d, [128, 128], "k1tf")
        k2t_f = load_small(k2t_d, [128, 128], "k2tf")
        wg_f = load_small(wg_d, [128, KC, 16], "wgf")
        k1t_b = C.sb([128, 128], BF16, "k1tb")
        k2t_b = C.sb([128, 128], BF16, "k2tb")
        wg_b = C.sb([128, KC, 16], BF16, "wgb")
        S.op("pool", lambda: G.tensor_copy(out=k1t_b[:], in_=k1t_f[:]), reads=[k1t_f.r], writes=[k1t_b.r])
        S.op("pool", lambda: G.tensor_copy(out=k2t_b[:], in_=k2t_f[:]), reads=[k2t_f.r], writes=[k2t_b.r])
        S.op("pool", lambda: G.tensor_copy(out=wg_b[:], in_=wg_f[:]), reads=[wg_f.r], writes=[wg_b.r])
        qsc = C.sb([128, 2], F32, "qsc")
        S.op("dve", lambda: V.tensor_scalar(out=qsc[:, 0:1], in0=qkg_sb[:, 0:1], scalar1=128.0 ** -0.5,
                                            scalar2=None, op0=ALU.mult), reads=[qkg_sb.r], writes=[qsc.r])
        S.op("dve", lambda: V.tensor_copy(out=qsc[:, 1:2], in_=qkg_sb[:, 1:2]), reads=[qkg_sb.r, qsc.r], writes=[qsc.r])

        sc_sb = C.sb([128, KC], F32, "sc_sb")
        mod = C.sb([128, 96], F32, "mod")
        S.op("act", lambda: A.activation(out=sc_sb[:], in_=c_sb[:], func=AF.Silu),
             reads=[c_sb.r], writes=[sc_sb.r])
        es_ada = ExitStack()
        wada_ring = C.sbring(2, [128, KC, 512], F32, "wada", es_ada)
        for jb in range(24):
            wb = wada_ring.next()
            S.dma("sp" if jb % 2 == 0 else "act", wb[:], wada_d[jb], writes=[wb.r])
            pm = psb.next()
            for jj in range(4):
                for kc in range(KC):
                    S.op("pe", lambda kc=kc, jj=jj, pm=pm, wb=wb: P.matmul(
                        pm[:, jj:jj + 1], lhsT=wb[:, kc, jj * 128:(jj + 1) * 128],
                        rhs=sc_sb[:, kc:kc + 1], start=(kc == 0), stop=(kc == KC - 1)),
                        reads=[wb.r, sc_sb.r], writes=[pm.r], inc=(kc == KC - 1 and jj == 3))
            S.op("dve", lambda jb=jb, pm=pm: V.tensor_tensor(
                out=mod[:, jb * 4:jb * 4 + 4], in0=pm[:, 0:4],
                in1=bada_sb[:, jb * 4:jb * 4 + 4], op=ALU.add),
                reads=[pm.r, bada_sb.r], writes=[mod.r])
        S.barrier()
        es_ada.close()
        A1 = C.sb([128, KC], F32, "A1")
        A2 = C.sb([128, KC], F32, "A2")
        S.op("dve", lambda: V.scalar_tensor_tensor(out=A1[:], in0=mod[:, 16:32], scalar=1.0, in1=g1_sb[:],
                                                   op0=ALU.add, op1=ALU.mult),
             reads=[mod.r, g1_sb.r], writes=[A1.r])
        S.op("dve", lambda: V.scalar_tensor_tensor(out=A2[:], in0=mod[:, 64:80], scalar=1.0, in1=g2_sb[:],
                                                   op0=ALU.add, op1=ALU.mult),
             reads=[mod.r, g2_sb.r], writes=[A2.r])

        dg = C.sb([128, 128], F32, "diag")

        def build_gate(gt, c0):
            for kc in range(KC):
                S.op("dve", lambda kc=kc: V.tensor_scalar(
                    out=dg[:], in0=ident_f[:], scalar1=mod[:, c0 + kc:c0 + kc + 1], scalar2=None,
                    op0=ALU.mult), reads=[ident_f.r, mod.r], writes=[dg.r])
                pm = psb.next()
                S.op("pe", lambda pm=pm: P.matmul(pm[:, 0:128], lhsT=ones_f[:], rhs=dg[:], start=True, stop=True),
                     reads=[ones_f.r, dg.r], writes=[pm.r])
                S.op("act", lambda pm=pm, kc=kc: A.copy(out=gt[:, kc * 128:(kc + 1) * 128], in_=pm[:, 0:128]),
                     reads=[pm.r], writes=[gt.r])

        def norm_to_T(es_l, src_rows, src_res, Asc, shift_c0, dstT, ntiles, nring):
            xr = C.sbring(nring, [128, D], F32, "xt", es_l)
            xn_r = C.sbring(nring, [128, D], BF16, "xn", es_l)
            ssr = C.sbring(2, [128, 2], F32, "ss", es_l)
            for i in range(ntiles):
                xt = xr.next()
                S.dma("sp", xt[:], src_rows(i), reads=[src_res(i)] if src_res else [], writes=[xt.r])
                ss = ssr.next()
                S.op("dve", lambda ss=ss: V.memset(ss[:], 0.0), writes=[ss.r])
                xn = xn_r.next()
                S.op("act", lambda xt=xt, ss=ss, xn=xn: A.activation(out=xn[:], in_=xt[:], func=AF.Square,
                                                                     accum_out=ss[:, 0:1]),
                     reads=[xt.r, ss.r], writes=[xn.r, ss.r])
                S.op("dve", lambda ss=ss: V.tensor_scalar(out=ss[:, 1:2], in0=ss[:, 0:1], scalar1=1.0 / D,
                                                          scalar2=EPS, op0=ALU.mult, op1=ALU.add),
                     reads=[ss.r], writes=[ss.r])
                S.op("act", lambda ss=ss: A.activation(out=ss[:, 1:2], in_=ss[:, 1:2], func=AF.Sqrt),
                     reads=[ss.r], writes=[ss.r])
                S.op("dve", lambda ss=ss: V.reciprocal(out=ss[:, 1:2], in_=ss[:, 1:2]),
                     reads=[ss.r], writes=[ss.r])
                S.op("act", lambda xt=xt, ss=ss, xn=xn: A.activation(out=xn[:], in_=xt[:], func=AF.Copy,
                                                                     scale=ss[:, 1:2]),
                     reads=[xt.r, ss.r], writes=[xn.r])
                for k4 in range(4):
                    pm = psb.next()
                    pmb = pm.t.bitcast(BF16)
                    for j in range(4):
                        kc = k4 * 4 + j
                        S.op("pe", lambda kc=kc, j=j, pmb=pmb, xn=xn: P.transpose(
                            out=pmb[:, j * 128:(j + 1) * 128], in_=xn[:, kc * 128:(kc + 1) * 128],
                            identity=ident_b[:]), reads=[xn.r, ident_b.r], writes=[pm.r], inc=(j == 3))
                    for j in range(4):
                        kc = k4 * 4 + j
                        S.op("dve", lambda kc=kc, j=j, pmb=pmb, i=i: V.tensor_scalar(
                            out=dstT[:, kc, i * 128:(i + 1) * 128], in0=pmb[:, j * 128:(j + 1) * 128],
                            scalar1=Asc[:, kc:kc + 1], scalar2=mod[:, shift_c0 + kc:shift_c0 + kc + 1],
                            op0=ALU.mult, op1=ALU.add),
                            reads=[pm.r, Asc.r, mod.r], writes=[dstT.r])

        es_m = ExitStack()
        wst = C.sbring(2, [128, KC, 128], F32, "wst", es_m)
        wbf = C.sbring(2, [128, KC, 128], BF16, "wbf", es_m)
        wq_flip = [0]

        def load_wchunk(d_ap):
            st = wst.next()
            q = "sp" if wq_flip[0] % 2 == 0 else "act"
            wq_flip[0] += 1
            S.dma(q, st[:], d_ap, writes=[st.r])
            wb = wbf.next()
            S.op("pool", lambda: G.tensor_copy(out=wb[:], in_=st[:]), reads=[st.r], writes=[wb.r])
            return wb

        h1T = C.sb([128, KC, T], BF16, "h1T", es_m)
        Ltok = C.sb([128, NT, 16], F32, "Ltok", es_m)
        Gtok = C.sb([128, NT, 16], F32, "Gtok", es_m)
        LrefB = C.sb([128, 64], F32, "LrefB", es_m)
        EQ = C.sb([16, T], BF16, "EQ", es_m)
        selm = C.sb([16, 4 * 128], BF16, "selm", es_m)
        qTr = [C.sb([128, T], BF16, f"qT{i}", es_m) for i in range(2)]
        kTr = [C.sb([128, T], BF16, f"kT{i}", es_m) for i in range(2)]
        qpr = [C.sbring(2, [128, 512], BF16, f"qp{i}", es_m) for i in range(2)]
        vtok = C.sb([128, NT, 256], BF16, "vtok", es_m)
        sigo = [C.sb([128, T], BF16, f"sigo{i}", es_m) for i in range(2)]
        rawc = C.sb([128, T + 4], F32, "rawc", es_m)
        cv = C.sb([128, T], F32, "cv", es_m)
        rawf_r = C.sbring(2, [128, 512], F32, "rawf", es_m)
        sqb_r = C.sbring(2, [128, 512], BF16, "sqb", es_m)
        rs_r = C.sbring(2, [128, 512], F32, "rs", es_m)
        pt_r = C.sbring(3, [128, 512], BF16, "pt", es_m)
        kb_r = C.sbring(2, [128, NT], F32, "kb", es_m)
        ktmp = C.sb([128, NT], F32, "ktmp", es_m)
        hT_r = [C.sbring(1, [128, 512], F32, f"hT{i}", es_m) for i in range(2)]
        mixo_r = C.sbring(2, [128, T], BF16, "mixo", es_m)
        es_n1 = ExitStack()
        norm_to_T(es_n1, lambda i: x_d[i * 128:(i + 1) * 128, :], None, A1, 0, h1T, NT, 2)
        S.barrier()
        es_n1.close()

        es_g = ExitStack()
        graw = C.sb([16, T], F32, "graw", es_g)
        lsp = C.sb([16, T], F32, "lsp", es_g)
        Lc = C.sb([16, T], F32, "Lc", es_g)
        for c in range(4):
            pm = psb.next()
            for kc in range(KC):
                S.op("pe", lambda kc=kc, pm=pm, c=c: P.matmul(
                    pm[0:16, :], lhsT=wg_b[:, kc, :], rhs=h1T[:, kc, c * 512:(c + 1) * 512],
                    start=(kc == 0), stop=(kc == KC - 1)),
                    reads=[wg_b.r, h1T.r], writes=[pm.r], inc=(kc == KC - 1))
            S.op("act", lambda pm=pm, c=c: A.activation(out=graw[:, c * 512:(c + 1) * 512], in_=pm[0:16, :],
                                                        func=AF.Identity, bias=gb_sb[:, 0:1]),
                 reads=[pm.r, gb_sb.r], writes=[graw.r])
        S.op("act", lambda: A.activation(out=lsp[:], in_=graw[:], func=AF.Exp, scale=-1.0),
             reads=[graw.r], writes=[lsp.r])
        S.op("act", lambda: A.activation(out=lsp[:], in_=lsp[:], func=AF.Ln, bias=ones_f[0:16, 0:1]),
             reads=[lsp.r, ones_f.r], writes=[lsp.r])
        S.op("dve", lambda: V.tensor_tensor_scan(out=Lc[:], data0=ones_f[0:16, 0:1].broadcast_to([16, T]), data1=lsp[:],
                                                 initial=zcol[0:16, 0:1], op0=ALU.mult, op1=ALU.add),
             reads=[ones_f.r, lsp.r, zcol.r], writes=[Lc.r])
        for (src, dst) in ((Lc, Ltok), (graw, Gtok)):
            for i4 in range(4):
                pm = psb.next()
                for j in range(4):
                    i = i4 * 4 + j
                    S.op("pe", lambda i=i, j=j, pm=pm, src=src: P.transpose(
                        out=pm[:, j * 16:(j + 1) * 16], in_=src[0:16, i * 128:(i + 1) * 128],
                        identity=ident_f[0:16, 0:16]), reads=[src.r, ident_f.r], writes=[pm.r], inc=(j == 3))
                S.op("dve", lambda i4=i4, pm=pm, dst=dst: V.tensor_copy(
                    out=dst[:, i4 * 4:(i4 + 1) * 4, :],
                    in_=pm[:, 0:64].rearrange("p (a b) -> p a b", a=4)),
                    reads=[pm.r], writes=[dst.r])
        pm = psb.next()
        for c in range(4):
            S.op("pe", lambda c=c, pm=pm: P.matmul(pm[:, c * 16:(c + 1) * 16], lhsT=sel127[:],
                                                   rhs=Ltok[:, 4 * c + 3, :], start=True, stop=True),
                 reads=[sel127.r, Ltok.r], writes=[pm.r], inc=(c == 3))
        S.op("dve", lambda pm=pm: V.tensor_copy(out=LrefB[:], in_=pm[:, 0:64]), reads=[pm.r], writes=[LrefB.r])
        for c in range(4):
            S.op("act", lambda c=c: A.activation(out=EQ[0:12, c * 512:(c + 1) * 512], in_=Lc[0:12, c * 512:(c + 1) * 512],
                                                 func=AF.Exp, scale=-1.0,
                                                 bias=Lc[0:12, c * 512 + 511:c * 512 + 512]),
                 reads=[Lc.r], writes=[EQ.r])
        for m in range(4):
            S.op("dve", lambda m=m: V.tensor_scalar(out=selm[:, m * 128:(m + 1) * 128], in0=ones_f[0:16, :],
                                                    scalar1=ident_f[0:16, 8 + m:9 + m], scalar2=None,
                                                    op0=ALU.mult),
                 reads=[ones_f.r, ident_f.r], writes=[selm.r])

        S.barrier()
        es_g.close()
        S.op("pool", lambda: G.memset(rawc[:, 0:4], 0.0), writes=[rawc.r])

        def proj_fm(wb, c, pm):
            for kc in range(KC):
                S.op("pe", lambda kc=kc: P.matmul(pm[:], lhsT=wb[:, kc, :], rhs=h1T[:, kc, c * 512:(c + 1) * 512],
                                                  start=(kc == 0), stop=(kc == KC - 1)),
                     reads=[wb.r, h1T.r], writes=[pm.r], inc=(kc == KC - 1))

        def fox_qk(wb, dst, gcol):
            def post(c, sqb, rawf):
                pn = psb.next()
                S.op("pe", lambda: P.matmul(pn[:], lhsT=ones_b[:], rhs=sqb[:], start=True, stop=True),
                     reads=[ones_b.r, sqb.r], writes=[pn.r])
                rs = rs_r.next()
                S.op("dve", lambda: V.tensor_scalar(out=rs[:], in0=pn[:], scalar1=1.0 / 128, scalar2=EPS,
                                                    op0=ALU.mult, op1=ALU.add), reads=[pn.r], writes=[rs.r])
                S.op("act", lambda: A.activation(out=rs[:], in_=rs[:], func=AF.Sqrt), reads=[rs.r], writes=[rs.r])
                S.op("dve", lambda: V.reciprocal(out=rs[:], in_=rs[:]), reads=[rs.r], writes=[rs.r])
                S.op("dve", lambda: V.scalar_tensor_tensor(out=dst[:, c * 512:(c + 1) * 512], in0=rawf[:],
                                                           scalar=qsc[:, gcol:gcol + 1], in1=rs[:],
                                                           op0=ALU.mult, op1=ALU.mult),
                     reads=[rawf.r, qsc.r, rs.r], writes=[dst.r])

            prev = None
            for c in range(4):
                pm = psb.next()
                proj_fm(wb, c, pm)
                sqb = sqb_r.next()
                rawf = rawf_r.next()
                S.op("act", lambda: A.activation(out=sqb[:], in_=pm[:], func=AF.Square), reads=[pm.r], writes=[sqb.r])
                S.op("act", lambda: A.copy(out=rawf[:], in_=pm[:]), reads=[pm.r], writes=[rawf.r])
                if prev is not None:
                    post(*prev)
                prev = (c, sqb, rawf)
            post(*prev)

        def v_tok(wb, dvc):
            for i4 in range(4):
                pm = psb.next()
                for j in range(4):
                    i = i4 * 4 + j
                    for kc in range(KC):
                        S.op("pe", lambda kc=kc, i=i, j=j: P.matmul(
                            pm[:, j * 128:(j + 1) * 128], lhsT=h1T[:, kc, i * 128:(i + 1) * 128], rhs=wb[:, kc, :],
                            start=(kc == 0), stop=(kc == KC - 1)),
                            reads=[h1T.r, wb.r], writes=[pm.r], inc=(kc == KC - 1 and j == 3))
                S.op("act", lambda: A.copy(out=vtok[:, i4 * 4:(i4 + 1) * 4, dvc * 128:(dvc + 1) * 128],
                                           in_=pm[:].rearrange("p (a b) -> p a b", a=4)),
                     reads=[pm.r], writes=[vtok.r])

        def ml_qk(wb, dst, cch):
            for c in range(4):
                pm = psb.next()
                proj_fm(wb, c, pm)
                S.op("act", lambda: A.copy(out=rawc[:, 4 + c * 512:4 + (c + 1) * 512], in_=pm[:]),
                     reads=[pm.r], writes=[rawc.r])
            S.op("dve", lambda: V.tensor_scalar(out=cv[:], in0=rawc[:, 4:4 + T], scalar1=cw_sb[:, cch, 3:4],
                                                scalar2=cb_sb[:, cch:cch + 1], op0=ALU.mult, op1=ALU.add),
                 reads=[rawc.r, cw_sb.r, cb_sb.r], writes=[cv.r])
            for j in range(3):
                S.op("dve", lambda j=j: V.scalar_tensor_tensor(out=cv[:], in0=rawc[:, 1 + j:1 + j + T],
                                                               scalar=cw_sb[:, cch, j:j + 1], in1=cv[:],
                                                               op0=ALU.mult, op1=ALU.add),
                     reads=[rawc.r, cw_sb.r, cv.r], writes=[cv.r])
            S.op("act", lambda: A.activation(out=dst[:], in_=cv[:], func=AF.Silu), reads=[cv.r], writes=[dst.r])

        def ml_o(wb, dst):
            for c in range(4):
                pm = psb.next()
                proj_fm(wb, c, pm)
                S.op("act", lambda: A.activation(out=dst[:, c * 512:(c + 1) * 512], in_=pm[:], func=AF.Sigmoid),
                     reads=[pm.r], writes=[dst.r])

        def attend(nd, ndv, row, is_fox, mrow, out_chunks, hgc0):
            mixo = [mixo_r.next() for _ in range(ndv)]
            for c in range(4):
                kb = kb_r.next()
                if is_fox:
                    S.op("dve", lambda: V.tensor_scalar(out=kb[:], in0=Ltok[:, :, row],
                                                        scalar1=LrefB[:, c * 16 + row:c * 16 + row + 1],
                                                        scalar2=None, op0=ALU.subtract),
                         reads=[Ltok.r, LrefB.r], writes=[kb.r])
                    qs = [qTr[d][:, c * 512:(c + 1) * 512] for d in range(nd)]
                    qres = [qTr[d].r for d in range(nd)]
                else:
                    S.op("dve", lambda: V.scalar_tensor_tensor(out=ktmp[:, 0:4 * c + 4], in0=Ltok[:, 0:4 * c + 4, row],
                                                               scalar=LrefB[:, c * 16 + row:c * 16 + row + 1],
                                                               in1=Gtok[:, 0:4 * c + 4, 12 + mrow],
                                                               op0=ALU.subtract, op1=ALU.add),
                         reads=[Ltok.r, LrefB.r, Gtok.r], writes=[ktmp.r])
                    S.op("act", lambda: A.activation(out=kb[:, 0:4 * c + 4], in_=ktmp[:, 0:4 * c + 4], func=AF.Exp, bias=lncol[:, 0:1]),
                         reads=[ktmp.r, lncol.r], writes=[kb.r])
                    pe_ = psb.next()
                    S.op("pe", lambda: P.matmul(pe_[:], lhsT=selm[0:12, mrow * 128:(mrow + 1) * 128],
                                                rhs=EQ[0:12, c * 512:(c + 1) * 512], start=True, stop=True),
                         reads=[selm.r, EQ.r], writes=[pe_.r])
                    qs, qres = [], []
                    for d in range(nd):
                        qp = qpr[d].next()
                        S.op("dve", lambda d=d, qp=qp: V.tensor_tensor(out=qp[:], in0=qTr[d][:, c * 512:(c + 1) * 512],
                                                                       in1=pe_[:], op=ALU.mult),
                             reads=[qTr[d].r, pe_.r], writes=[qp.r])
                        qs.append(qp[:])
                        qres.append(qp.r)
                pO = [acc_ps[d] for d in range(ndv)]
                pD = acc_ps[2]
                nj = 4 * c + 4

                def emit_pv(j, lo, pt):
                    for dv in range(ndv):
                        S.op("pe", lambda dv=dv: P.matmul(pO[dv][:, lo:512], lhsT=vtok[:, j, dv * 128:(dv + 1) * 128],
                                                          rhs=pt[:, lo:512], start=(j == 0), stop=(j == nj - 1)),
                             reads=[vtok.r, pt.r], writes=[pO[dv].r], inc=False)
                    S.op("pe", lambda: P.matmul(pD[:, lo:512], lhsT=ones_b[:], rhs=pt[:, lo:512],
                                                start=(j == 0), stop=(j == nj - 1)),
                         reads=[ones_b.r, pt.r], writes=[pD.r])

                prevpv = None
                for j in range(nj):
                    lo = 128 * (j - 4 * c) if j >= 4 * c else 0
                    pS = psb.next()
                    for d in range(nd):
                        S.op("pe", lambda d=d: P.matmul(pS[:, lo:512], lhsT=kTr[d][:, j * 128:(j + 1) * 128],
                                                        rhs=qs[d][:, lo:512], start=(d == 0), stop=(d == nd - 1)),
                             reads=[kTr[d].r, qres[d]], writes=[pS.r], inc=(d == nd - 1))
                    pt = pt_r.next()
                    if is_fox:
                        S.op("act", lambda: A.activation(out=pt[:, lo:512], in_=pS[:, lo:512], func=AF.Exp,
                                                         bias=kb[:, j:j + 1]),
                             reads=[pS.r, kb.r], writes=[pt.r])
                    else:
                        S.op("dve", lambda: V.tensor_scalar(out=pt[:, lo:512], in0=pS[:, lo:512],
                                                            scalar1=kb[:, j:j + 1], scalar2=None, op0=ALU.mult),
                             reads=[pS.r, kb.r], writes=[pt.r])
                    if j >= 4 * c:
                        S.op("pool", lambda: G.tensor_tensor(out=pt[:, lo:lo + 128], in0=pt[:, lo:lo + 128],
                                                             in1=tri_b[:], op=ALU.mult),
                             reads=[pt.r, tri_b.r], writes=[pt.r])
                    if prevpv is not None:
                        emit_pv(*prevpv)
                    prevpv = (j, lo, pt)
                emit_pv(*prevpv)
                rs = rs_r.next()
                if is_fox:
                    S.op("dve", lambda: V.reciprocal(out=rs[:], in_=pD[:]), reads=[pD.r], writes=[rs.r])
                    S.op("dve", lambda: V.tensor_tensor(out=mixo[0][:, c * 512:(c + 1) * 512], in0=pO[0][:],
                                                        in1=rs[:], op=ALU.mult),
                         reads=[pO[0].r, rs.r], writes=[mixo[0].r])
                else:
                    S.op("dve", lambda: V.tensor_scalar(out=rs[:], in0=pD[:], scalar1=-1.0, scalar2=1.0,
                                                        op0=ALU.mult, op1=ALU.max), reads=[pD.r], writes=[rs.r])
                    S.op("dve", lambda: V.scalar_tensor_tensor(out=rs[:], in0=pD[:], scalar=1.0, in1=rs[:],
                                                               op0=ALU.max, op1=ALU.max),
                         reads=[pD.r, rs.r], writes=[rs.r])
                    S.op("dve", lambda: V.reciprocal(out=rs[:], in_=rs[:]), reads=[rs.r], writes=[rs.r])
                    hTs = []
                    pn = psb.next()
                    for dv in range(ndv):
                        hT = hT_r[dv].next()
                        S.op("dve", lambda dv=dv, hT=hT: V.tensor_tensor(out=hT[:], in0=pO[dv][:], in1=rs[:], op=ALU.mult),
                             reads=[pO[dv].r, rs.r], writes=[hT.r])
                        sqb = sqb_r.next()
                        S.op("act", lambda hT=hT, sqb=sqb: A.activation(out=sqb[:], in_=hT[:], func=AF.Square),
                             reads=[hT.r], writes=[sqb.r])
                        S.op("pe", lambda dv=dv, sqb=sqb: P.matmul(pn[:], lhsT=ones_b[:], rhs=sqb[:],
                                                                    start=(dv == 0), stop=(dv == ndv - 1)),
                             reads=[ones_b.r, sqb.r], writes=[pn.r], inc=(dv == ndv - 1))
                        hTs.append(hT)
                    rs2 = rs_r.next()
                    S.op("dve", lambda: V.tensor_scalar(out=rs2[:], in0=pn[:], scalar1=1.0 / 256, scalar2=EPS,
                                                        op0=ALU.mult, op1=ALU.add), reads=[pn.r], writes=[rs2.r])
                    S.op("act", lambda: A.activation(out=rs2[:], in_=rs2[:], func=AF.Sqrt), reads=[rs2.r], writes=[rs2.r])
                    S.op("dve", lambda: V.reciprocal(out=rs2[:], in_=rs2[:]), reads=[rs2.r], writes=[rs2.r])
                    for dv in range(ndv):
                        S.op("dve", lambda dv=dv: V.scalar_tensor_tensor(
                            out=hTs[dv][:], in0=hTs[dv][:], scalar=hg_sb[:, hgc0 + dv:hgc0 + dv + 1], in1=rs2[:],
                            op0=ALU.mult, op1=ALU.mult), reads=[hTs[dv].r, hg_sb.r, rs2.r], writes=[hTs[dv].r])
                        S.op("dve", lambda dv=dv: V.tensor_tensor(
                            out=mixo[dv][:, c * 512:(c + 1) * 512], in0=hTs[dv][:],
                            in1=sigo[dv][:, c * 512:(c + 1) * 512], op=ALU.mult),
                            reads=[hTs[dv].r, sigo[dv].r], writes=[mixo[dv].r])
            for dv in range(ndv):
                S.dma("sp", mix_d[out_chunks[dv]], mixo[dv][:], reads=[mixo[dv].r],
                      writes=[mix_res[out_chunks[dv]]])

        for h in range(NFOX):
            fox_qk(load_wchunk(wfm_d[3 * h]), qTr[0], 0)
            fox_qk(load_wchunk(wfm_d[3 * h + 1]), kTr[0], 1)
            v_tok(load_wchunk(wfm_d[3 * h + 2]), 0)
            attend(1, 1, h, True, 0, [h], 0)
        for m in range(NML):
            b0 = 24 + 8 * m
            ml_qk(load_wchunk(wfm_d[b0 + 0]), qTr[0], 2 * m)
            ml_qk(load_wchunk(wfm_d[b0 + 1]), qTr[1], 2 * m + 1)
            ml_qk(load_wchunk(wfm_d[b0 + 2]), kTr[0], 8 + 2 * m)
            ml_qk(load_wchunk(wfm_d[b0 + 3]), kTr[1], 8 + 2 * m + 1)
            v_tok(load_wchunk(wfm_d[b0 + 4]), 0)
            v_tok(load_wchunk(wfm_d[b0 + 5]), 1)
            ml_o(load_wchunk(wfm_d[b0 + 6]), sigo[0])
            ml_o(load_wchunk(wfm_d[b0 + 7]), sigo[1])
            attend(2, 2, 8 + m, False, m, [8 + 2 * m, 8 + 2 * m + 1], 2 * m)
        S.barrier()
        es_m.close()

        es_o = ExitStack()
        mixT = C.sb([128, KC, T], BF16, "mixT", es_o)
        written = list(range(NFOX)) + [8 + j for j in range(2 * NML)]
        if len(written) < KC:
            S.op("pool", lambda: G.memset(mixT[:], 0.0), writes=[mixT.r])
        for kc in written:
            S.dma("sp" if kc % 2 == 0 else "act", mixT[:, kc, :], mix_d[kc], reads=[mix_res[kc]], writes=[mixT.r])
        gate1b = C.sb([128, D], F32, "gate1b", es_o)
        build_gate(gate1b, 32)
        wo_st = C.sb([128, KC, 512], F32, "wo_st", es_o)
        wo_bf = C.sbring(2, [128, KC, 512], BF16, "wo_bf", es_o)
        xs_r = C.sbring(3, [128, 512], F32, "xs", es_o)
        t1_r = C.sbring(3, [128, 512], F32, "t1", es_o)
        for cb in range(4):
            S.dma("sp", wo_st[:], wout_d[cb], writes=[wo_st.r])
            wob = wo_bf.next()
            S.op("pool", lambda: G.tensor_copy(out=wob[:], in_=wo_st[:]), reads=[wo_st.r], writes=[wob.r])
            for i in range(NT):
                pm = psb.next()
                for kc in range(KC):
                    S.op("pe", lambda kc=kc: P.matmul(pm[:], lhsT=mixT[:, kc, i * 128:(i + 1) * 128], rhs=wob[:, kc, :],
                                                      start=(kc == 0), stop=(kc == KC - 1)),
                         reads=[mixT.r, wob.r], writes=[pm.r], inc=(kc == KC - 1))
                xs = xs_r.next()
                S.dma("act", xs[:], x_d[i * 128:(i + 1) * 128, cb * 512:(cb + 1) * 512], writes=[xs.r])
                t1 = t1_r.next()
                S.op("dve", lambda: V.tensor_tensor(out=t1[:], in0=pm[:], in1=gate1b[:, cb * 512:(cb + 1) * 512],
                                                    op=ALU.mult), reads=[pm.r, gate1b.r], writes=[t1.r])
                S.op("pool", lambda: G.tensor_tensor(out=t1[:], in0=t1[:], in1=xs[:], op=ALU.add),
                     reads=[t1.r, xs.r], writes=[t1.r])
                S.dma("sp", out_d[i * 128:(i + 1) * 128, cb * 512:(cb + 1) * 512], t1[:], reads=[t1.r],
                      writes=[out_res[i]])
        S.barrier()
        es_o.close()

        es_p = ExitStack()
        psb = Ring(psb.bufs + acc_ps)
        wst = C.sbring(3, [128, KC, 128], F32, "wstp", es_p)
        wbf = C.sbring(2, [128, KC, 128], BF16, "wbfp", es_p)
        gate2b = C.sb([128, D], F32, "gate2b", es_p)
        build_gate(gate2b, 80)
        h2T = C.sb([128, KC, 512], BF16, "h2T", es_p)
        qT = C.sb([128, 16, 512], BF16, "qTp", es_p)
        acc = C.sb([128, 4, D], F32, "acc", es_p)
        Cb = C.sb([128, 8, 512], BF16, "Cb", es_p)
        statsT = C.sb([32, 512], BF16, "statsT", es_p)
        selT = C.sb([32, 8 * 128], BF16, "selT", es_p)
        selg = C.sb([32, 8 * 128], BF16, "selg", es_p)
        ucol = C.sb([32, 8], F32, "ucol", es_p)
        for h in range(8):
            S.op("dve", lambda h=h: V.tensor_tensor(out=ucol[:, h:h + 1], in0=ident_f[0:32, h:h + 1],
                                                    in1=ident_f[0:32, 8 + h:9 + h], op=ALU.add),
                 reads=[ident_f.r, ucol.r], writes=[ucol.r])
            S.op("dve", lambda h=h: V.scalar_tensor_tensor(out=ucol[:, h:h + 1], in0=ucol[:, h:h + 1], scalar=-1.0,
                                                           in1=ident_f[0:32, 16 + h:17 + h],
                                                           op0=ALU.mult, op1=ALU.subtract),
                 reads=[ident_f.r, ucol.r], writes=[ucol.r])
            S.op("dve", lambda h=h: V.tensor_scalar(out=selT[:, h * 128:(h + 1) * 128], in0=ones_f[0:32, :],
                                                    scalar1=ucol[:, h:h + 1], scalar2=None, op0=ALU.mult),
                 reads=[ones_f.r, ucol.r], writes=[selT.r])
            S.op("dve", lambda h=h: V.tensor_scalar(out=selg[:, h * 128:(h + 1) * 128], in0=ones_f[0:32, :],
                                                    scalar1=ident_f[0:32, 24 + h:25 + h], scalar2=None, op0=ALU.mult),
                 reads=[ones_f.r, ident_f.r], writes=[selg.r])
        k1bc_r = C.sbring(2, [128, 128], BF16, "k1bc", es_p)
        eu_st = C.sbring(2, [128, D], F32, "eu_st", es_p)
        eu_bf = C.sbring(GK + 2, [128, D], BF16, "eu_bf", es_p)
        wT_r = C.sbring(2 * GK, [128, 512], BF16, "wT", es_p)
        gA_r = C.sbring(2, [128, 512], BF16, "gA", es_p)
        E_r = C.sbring(4, [128, 512], BF16, "E", es_p)
        Mm2_r = C.sbring(2, [128, 2, 512], BF16, "Mm2", es_p)
        Tt2_r = C.sbring(2, [128, 2, 512], BF16, "Tt2", es_p)
        Ga2_r = C.sbring(2, [128, 2, 512], BF16, "Ga2", es_p)
        sc_r = C.sbring(1, [128, 256], F32, "sc", es_p)
        sc2_r = C.sbring(1, [128, 256], F32, "sc2", es_p)
        v12_r = C.sbring(2, [128, 32], F32, "v12", es_p)
        cand_r = C.sbring(1, [128, 256], F32, "cand", es_p)
        cand2_r = C.sbring(1, [128, 256], F32, "cand2", es_p)
        c16_r = C.sbring(2, [128, 16], F32, "c16", es_p)
        e16_r = C.sbring(2, [128, 16], F32, "e16", es_p)
        sm_r = C.sbring(2, [128, 4], F32, "sm", es_p)
        statf = C.sb([128, 48], F32, "statf", es_p)
        statb = C.sb([128, 32], BF16, "statb", es_p)

        for Q in range(NQ):
            es_n2 = ExitStack()
            norm_to_T(es_n2, lambda i: out_d[(Q * 4 + i) * 128:(Q * 4 + i + 1) * 128, :],
                      lambda i: out_res[Q * 4 + i], A2, 48, h2T, 4, 1)
            S.barrier()
            es_n2.close()
            for cc in range(16):
                st = wst.next()
                S.dma("sp" if cc % 2 == 0 else "act", st[:], wq_d[cc], writes=[st.r])
                wb = wbf.next()
                S.op("pool", lambda: G.tensor_copy(out=wb[:], in_=st[:]), reads=[st.r], writes=[wb.r])
                pm = psb.next()
                for kc in range(KC):
                    S.op("pe", lambda kc=kc: P.matmul(pm[:], lhsT=wb[:, kc, :], rhs=h2T[:, kc, :],
                                                      start=(kc == 0), stop=(kc == KC - 1)),
                         reads=[wb.r, h2T.r], writes=[pm.r], inc=(kc == KC - 1))
                S.op("act", lambda: A.copy(out=qT[:, cc, :], in_=pm[:]), reads=[pm.r], writes=[qT.r])
            for ti in range(4):
                for h in range(8):
                    pm = psb.next()
                    S.op("pe", lambda: P.matmul(pm[:, 0:128], lhsT=qT[:, 2 * h, ti * 128:(ti + 1) * 128],
                                                rhs=k1t_b[:], start=True, stop=True),
                         reads=[qT.r, k1t_b.r], writes=[pm.r], inc=False)
                    S.op("pe", lambda: P.matmul(pm[:, 128:256], lhsT=qT[:, 2 * h + 1, ti * 128:(ti + 1) * 128],
                                                rhs=k2t_b[:], start=True, stop=True),
                         reads=[qT.r, k2t_b.r], writes=[pm.r])
                    sc = sc_r.next()
                    sc2 = sc2_r.next()
                    v12 = v12_r.next()
                    S.op("act", lambda: A.copy(out=sc[:], in_=pm[:, 0:256]), reads=[pm.r], writes=[sc.r])
                    for half in range(2):
                        sl = slice(half * 128, (half + 1) * 128)
                        vo = half * 16
                        S.op("dve", lambda: V.max(out=v12[:, vo:vo + 8], in_=sc[:, sl]), reads=[sc.r, v12.r], writes=[v12.r])
                        S.op("dve", lambda: V.match_replace(out=sc2[:, sl], in_to_replace=v12[:, vo:vo + 8],
                                                            in_values=sc[:, sl], imm_value=NEG),
                             reads=[sc.r, v12.r, sc2.r], writes=[sc2.r])
                        S.op("dve", lambda: V.max(out=v12[:, vo + 8:vo + 16], in_=sc2[:, sl]),
                             reads=[sc2.r, v12.r], writes=[v12.r])
                    cand = cand_r.next()
                    cand2 = cand2_r.next()
                    c16 = c16_r.next()
                    e16 = e16_r.next()
                    sm = sm_r.next()
                    S.op("dve", lambda: V.tensor_tensor(
                        out=cand[:].rearrange("p (a b) -> p a b", a=16),
                        in0=v12[:, 0:16].unsqueeze(2).broadcast_to([128, 16, 16]),
                        in1=v12[:, 16:32].unsqueeze(1).broadcast_to([128, 16, 16]), op=ALU.add),
                        reads=[v12.r], writes=[cand.r])
                    S.op("dve", lambda: V.max(out=c16[:, 0:8], in_=cand[:]), reads=[cand.r, c16.r], writes=[c16.r])
                    S.op("dve", lambda: V.match_replace(out=cand2[:], in_to_replace=c16[:, 0:8], in_values=cand[:],
                                                        imm_value=NEG), reads=[cand.r, c16.r], writes=[cand2.r])
                    S.op("dve", lambda: V.max(out=c16[:, 8:16], in_=cand2[:]), reads=[cand2.r, c16.r], writes=[c16.r])
                    S.op("dve", lambda: V.tensor_scalar(out=sm[:, 0:1], in0=c16[:, 0:1], scalar1=-1.0, scalar2=None,
                                                        op0=ALU.mult), reads=[c16.r, sm.r], writes=[sm.r])
                    S.op("dve", lambda: V.memset(sm[:, 1:2], 0.0), reads=[sm.r], writes=[sm.r])
                    S.op("act", lambda: A.activation(out=e16[:], in_=c16[:], func=AF.Exp, bias=sm[:, 0:1],
                                                     accum_out=sm[:, 1:2]),
                         reads=[c16.r, sm.r], writes=[e16.r, sm.r])
                    S.op("dve", lambda: V.reciprocal(out=sm[:, 2:3], in_=sm[:, 1:2]), reads=[sm.r], writes=[sm.r])
                    S.op("dve", lambda: V.tensor_scalar(out=statf[:, h:h + 1], in0=c16[:, 15:16], scalar1=-3.0e-5,
                                                        scalar2=None, op0=ALU.add),
                         reads=[c16.r, statf.r], writes=[statf.r])
                    S.op("dve", lambda: V.tensor_tensor(out=statf[:, 32 + h:33 + h], in0=e16[:, 15:16], in1=sm[:, 2:3],
                                                        op=ALU.mult), reads=[e16.r, sm.r, statf.r], writes=[statf.r])
                S.op("dve", lambda: V.tensor_copy(out=statb[:, 0:8], in_=statf[:, 0:8]), reads=[statf.r, statb.r], writes=[statb.r])
                S.op("dve", lambda: V.tensor_tensor(out=statf[:, 8:16], in0=statf[:, 0:8], in1=statb[:, 0:8],
                                                    op=ALU.subtract), reads=[statf.r, statb.r], writes=[statf.r])
                S.op("dve", lambda: V.tensor_copy(out=statb[:, 8:16], in_=statf[:, 8:16]), reads=[statf.r, statb.r], writes=[statb.r])
                S.op("dve", lambda: V.tensor_tensor(out=statf[:, 16:24], in0=statf[:, 8:16], in1=statb[:, 8:16],
                                                    op=ALU.subtract), reads=[statf.r, statb.r], writes=[statf.r])
                S.op("dve", lambda: V.tensor_copy(out=statb[:, 16:24], in_=statf[:, 16:24]), reads=[statf.r, statb.r], writes=[statb.r])
                S.op("dve", lambda: V.tensor_copy(out=statb[:, 24:32], in_=statf[:, 32:40]), reads=[statf.r, statb.r], writes=[statb.r])
                pm = psb.next()
                pmb = pm.t.bitcast(BF16)
                S.op("pe", lambda: P.transpose(out=pmb[0:32, 0:128], in_=statb[:, 0:32], identity=ident_b[:]),
                     reads=[statb.r, ident_b.r], writes=[pm.r])
                S.op("act", lambda: A.copy(out=statsT[:, ti * 128:(ti + 1) * 128], in_=pmb[0:32, 0:128]),
                     reads=[pm.r], writes=[statsT.r])
            for h in range(8):
                pm = psb.next()
                S.op("pe", lambda: P.matmul(pm[:], lhsT=selg[:, h * 128:(h + 1) * 128], rhs=statsT[:],
                                            start=True, stop=True), reads=[selg.r, statsT.r], writes=[pm.r])
                S.op("act", lambda: A.copy(out=Cb[:, h, :], in_=pm[:]), reads=[pm.r], writes=[Cb.r])

            grp = []
            ngrp = 0
            pending = []

            def emit_units(n):
                for _ in range(min(n, len(pending))):
                    g_, ti, cbk, first = pending.pop(0)
                    pm = psb.next()
                    for gi, (wT_, eub_) in enumerate(g_):
                        S.op("pe", lambda gi=gi, wT_=wT_, eub_=eub_: P.matmul(
                            pm[:], lhsT=wT_[:, ti * 128:(ti + 1) * 128], rhs=eub_[:, cbk * 512:(cbk + 1) * 512],
                            start=(gi == 0), stop=(gi == len(g_) - 1)),
                            reads=[wT_.r, eub_.r], writes=[pm.r], inc=(gi == len(g_) - 1))
                    if first:
                        S.op("act", lambda: A.copy(out=acc[:, ti, cbk * 512:(cbk + 1) * 512], in_=pm[:]),
                             reads=[pm.r], writes=[acc.r])
                    else:
                        S.op("dve", lambda: V.tensor_tensor(out=acc[:, ti, cbk * 512:(cbk + 1) * 512],
                                                            in0=pm[:], in1=acc[:, ti, cbk * 512:(cbk + 1) * 512],
                                                            op=ALU.add), reads=[pm.r, acc.r], writes=[acc.r])

            prepped = {}

            def prep(e1):
                st = wst.next()
                S.dma("sp", st[:], edt_d[e1], writes=[st.r])
                edb = wbf.next()
                S.op("act", lambda: A.copy(out=edb[:], in_=st[:]), reads=[st.r], writes=[edb.r])
                es_ = eu_st.next()
                S.dma("act", es_[:], eu_d[e1], writes=[es_.r])
                eub = eu_bf.next()
                S.op("act", lambda: A.copy(out=eub[:], in_=es_[:]), reads=[es_.r], writes=[eub.r])
                k1bc = k1bc_r.next()
                S.op("pool", lambda: G.tensor_copy(out=k1bc[:], in_=k1t_b[:, e1:e1 + 1].broadcast_to([128, 128])),
                     reads=[k1t_b.r], writes=[k1bc.r])
                prepped[e1] = (edb, eub, k1bc)

            prep(0)
            for e1 in range(NE1):
                if e1 + 1 < NE1:
                    prep(e1 + 1)
                edb, eub, k1bc = prepped.pop(e1)
                pA = psb.next()
                for kc in range(KC):
                    S.op("pe", lambda kc=kc: P.matmul(pA[:], lhsT=edb[:, kc, :], rhs=h2T[:, kc, :],
                                                      start=(kc == 0), stop=(kc == KC - 1)),
                         reads=[edb.r, h2T.r], writes=[pA.r], inc=(kc == KC - 1))
                gA = gA_r.next()
                S.op("act", lambda: A.activation(out=gA[:], in_=pA[:], func=AF.Gelu), reads=[pA.r], writes=[gA.r])
                Ga2 = Ga2_r.next()
                for hp in range(4):
                    Mm2 = Mm2_r.next()
                    for hh in range(2):
                        h = 2 * hp + hh
                        pX = psb.next()
                        S.op("pe", lambda: P.matmul(pX[:], lhsT=k2t_b[:], rhs=qT[:, 2 * h + 1, :], start=True, stop=False),
                             reads=[k2t_b.r, qT.r], writes=[pX.r], inc=False)
                        S.op("pe", lambda: P.matmul(pX[:], lhsT=k1bc[:], rhs=qT[:, 2 * h, :], start=False, stop=False),
                             reads=[k1bc.r, qT.r], writes=[pX.r], inc=False)
                        S.op("pe", lambda: P.matmul(pX[:], lhsT=selT[:, h * 128:(h + 1) * 128], rhs=statsT[:],
                                                    start=False, stop=True),
                             reads=[selT.r, statsT.r], writes=[pX.r])
                        E = E_r.next()
                        S.op("act", lambda: A.activation(out=E[:], in_=pX[:], func=AF.Exp), reads=[pX.r], writes=[E.r])
                        S.op("dve", lambda: V.scalar_tensor_tensor(out=Mm2[:, hh, :], in0=pX[:], scalar=0.0, in1=E[:],
                                                                   op0=ALU.is_ge, op1=ALU.mult),
                             reads=[pX.r, E.r, Mm2.r], writes=[Mm2.r])
                    if hp == 0:
                        S.op("dve", lambda: V.tensor_tensor(out=Ga2[:], in0=Mm2[:], in1=Cb[:, 0:2, :], op=ALU.mult),
                             reads=[Mm2.r, Cb.r], writes=[Ga2.r])
                    else:
                        Tt2 = Tt2_r.next()
                        S.op("dve", lambda: V.tensor_tensor(out=Tt2[:], in0=Mm2[:], in1=Cb[:, 2 * hp:2 * hp + 2, :],
                                                            op=ALU.mult), reads=[Mm2.r, Cb.r], writes=[Tt2.r])
                        S.op("pool", lambda: G.tensor_tensor(out=Ga2[:], in0=Ga2[:], in1=Tt2[:], op=ALU.add),
                             reads=[Ga2.r, Tt2.r], writes=[Ga2.r])
                S.op("pool", lambda: G.tensor_tensor(out=Ga2[:, 0, :], in0=Ga2[:, 0, :], in1=Ga2[:, 1, :], op=ALU.add),
                     reads=[Ga2.r], writes=[Ga2.r])
                wT = wT_r.next()
                S.op("dve", lambda: V.tensor_tensor(out=wT[:], in0=Ga2[:, 0, :], in1=gA[:], op=ALU.mult),
                     reads=[Ga2.r, gA.r], writes=[wT.r])
                grp.append((wT, eub))
                emit_units(16)
                if len(grp) == GK or e1 == NE1 - 1:
                    for ti in range(4):
                        for cbk in range(4):
                            pending.append((list(grp), ti, cbk, ngrp == 0))
                    grp = []
                    ngrp += 1
            emit_units(len(pending))
            es_f = ExitStack()
            x1_r = C.sbring(1, [128, D], F32, "x1t", es_f)
            for ti in range(4):
                i = Q * 4 + ti
                x1 = x1_r.next()
                S.dma("sp", x1[:], out_d[i * 128:(i + 1) * 128, :], reads=[out_res[i]], writes=[x1.r])
                S.op("dve", lambda: V.tensor_tensor(out=acc[:, ti, :], in0=acc[:, ti, :], in1=gate2b[:], op=ALU.mult),
                     reads=[acc.r, gate2b.r], writes=[acc.r])
                S.op("pool", lambda: G.tensor_tensor(out=acc[:, ti, :], in0=acc[:, ti, :], in1=x1[:], op=ALU.add),
                     reads=[acc.r, x1.r], writes=[acc.r])
                S.dma("sp", out_d[i * 128:(i + 1) * 128, :], acc[:, ti, :], reads=[acc.r, out_res[i]], writes=[out_res[i]])
            S.barrier()
            es_f.close()
        S.barrier()
        es_p.close()

        for q in ("sp", "pool", "act"):
            for sem, val in S.dma_slots[q]:
                if val > 0:
                    S._wait("sp", (sem, val))
        print("instructions:", S.ninstr)
    return nc


def _host_layouts(inp):
    f = lambda a: np.ascontiguousarray(a, dtype=np.float32)
    L = {}
    w_ada = inp["w_ada"][0]
    L["wada_r"] = f(w_ada.reshape(KC, 128, 24, 512).transpose(2, 1, 0, 3))
    L["bada_r"] = f(inp["b_ada"][0].reshape(96, 128).T)
    L["g1_r"] = f(inp["norm1_gain"][0].reshape(KC, 128).T)
    L["g2_r"] = f(inp["norm2_gain"][0].reshape(KC, 128).T)
    w_in = inp["w_in"][0]
    o_fq, o_fk, o_fv, o_ff, o_mq, o_mk, o_mv, o_mo, o_mi, o_mf = 0, 1024, 2048, 3072, 3080, 4104, 5128, 6152, 7176, 7180
    cols = []
    for h in range(8):
        cols += [o_fq + 128 * h, o_fk + 128 * h, o_fv + 128 * h]
    for m in range(4):
        cols += [o_mq + 256 * m, o_mq + 256 * m + 128, o_mk + 256 * m, o_mk + 256 * m + 128,
                 o_mv + 256 * m, o_mv + 256 * m + 128, o_mo + 256 * m, o_mo + 256 * m + 128]
    wfm = np.empty((56, 128, KC, 128), np.float32)
    for i, c0 in enumerate(cols):
        wfm[i] = w_in[:, c0:c0 + 128].reshape(KC, 128, 128).transpose(1, 0, 2)
    L["wfm_r"] = wfm
    gcols = list(range(o_ff, o_ff + 8)) + list(range(o_mf, o_mf + 4)) + list(range(o_mi, o_mi + 4))
    L["wg_r"] = f(w_in[:, gcols].reshape(KC, 128, 16).transpose(1, 0, 2))
    L["gb_r"] = f(np.concatenate([inp["fox_f_bias"][0], inp["mlstm_f_bias"][0], inp["mlstm_i_bias"][0]]).reshape(16, 1))
    L["qkg_r"] = f(np.stack([inp["fox_q_gain"][0], inp["fox_k_gain"][0]], axis=1))
    L["convw_r"] = f(inp["mlstm_conv_w"][0].reshape(4, 16, 128).transpose(2, 1, 0))
    L["convb_r"] = f(inp["mlstm_conv_b"][0].reshape(16, 128).T)
    L["hg_r"] = f(inp["mlstm_head_gain"][0].reshape(8, 128).T)
    L["wout_r"] = f(inp["w_out"][0].reshape(KC, 128, 4, 512).transpose(2, 1, 0, 3))
    L["wq_r"] = f(inp["peer_w_query"][0].reshape(KC, 128, 16, 128).transpose(2, 1, 0, 3))
    L["k1t_r"] = f(inp["peer_sub_keys_1"][0].T)
    L["k2t_r"] = f(inp["peer_sub_keys_2"][0].T)
    ed = inp["peer_expert_down"][0]
    L["edt_r"] = f(ed.reshape(128, 128, KC, 128).transpose(0, 3, 2, 1))
    L["eu_r"] = f(inp["peer_expert_up"][0].reshape(128, 128, D))
    return L


def _core_inputs(inputs, b):
    return {
        "x": np.ascontiguousarray(inputs["x"][b], dtype=np.float32),
        "c_r": np.ascontiguousarray(inputs["c"][b].reshape(KC, 128).T, dtype=np.float32),
    }


def kernel(**inputs):
    inputs = {k: np.asarray(v) for k, v in inputs.items()}
    L = _host_layouts(inputs)
    nc = build_program()
    outs = []
    for g0 in range(0, 8, CORES_PER_LAUNCH):
        in_maps = []
        for b in range(g0, g0 + CORES_PER_LAUNCH):
            m = dict(L)
            m.update(_core_inputs(inputs, b))
            in_maps.append(m)
        res = run_bass_kernel_spmd(nc, in_maps, core_ids=list(range(CORES_PER_LAUNCH)))
        outs += [np.asarray(r["out"], dtype=np.float32) for r in res.results]
    return np.stack(outs, axis=0)
```

```python
from contextlib import ExitStack
import numpy as np
import concourse.bass as bass
import concourse.mybir as mybir
from concourse.bass_utils import run_bass_kernel_spmd

F32 = mybir.dt.float32
BF16 = mybir.dt.bfloat16
ALU = mybir.AluOpType
AF = mybir.ActivationFunctionType
AX = mybir.AxisListType

import os
NBLK = int(os.environ.get("NBLK", "24"))
ADAQ = os.environ.get("ADAQ", "pool")
D = 2048
T = 2048
KC = 16
NT = 16
EPS = 1e-6


class Res:
    __slots__ = ("name", "w", "r")

    def __init__(self, name=""):
        self.name = name
        self.w = None
        self.r = []


class Sched:
    SEM_LIMIT = 30000

    def __init__(self, nc, es):
        self.nc = nc
        self.es = es
        self.engs = {"pe": nc.tensor, "act": nc.scalar, "dve": nc.vector,
                     "pool": nc.gpsimd, "sp": nc.sync}
        self.sem = {}
        self.cnt = {}
        self.nsem = 0
        self.pe_sems = []
        self.pend = {}
        for e in ("pe", "act", "dve", "pool"):
            self._new_sem(e)
        self.waited = {e: {} for e in self.engs}
        self.dma_slots = {}
        self.dma_next = {}
        for q, n in (("sp", 8), ("pool", 2), ("act", 4)):
            self.dma_slots[q] = [[self._mk(f"d{q}{i}"), 0] for i in range(n)]
            self.dma_next[q] = 0
        self.ninstr = 0

    def _mk(self, name):
        self.nsem += 1
        return self.es.enter_context(self.nc.semaphore(f"{name}_{self.nsem}"))

    def _new_sem(self, e):
        self.sem[e] = self._mk(f"s{e}")
        self.cnt[e] = 0
        if e == "pe":
            self.pe_sems.append(self.sem[e])

    def _wait(self, e, tok):
        sem, val = tok
        if e == "pe" and sem in self.pe_sems:
            return
        key = id(sem)
        if self.waited[e].get(key, 0) >= val:
            return
        self.engs[e].wait_ge(sem, val)
        self.waited[e][key] = val

    def _deps(self, e, reads, writes):
        toks = []
        for r in reads:
            if r.w is not None:
                toks.append(r.w)
        for w in writes:
            if w.w is not None:
                toks.append(w.w)
            toks.extend(w.r)
        for t in toks:
            self._wait(e, t)

    def _mark(self, tok, reads, writes):
        for w in writes:
            w.w = tok
            w.r = []
        for r in reads:
            if r in writes:
                continue
            r.r.append(tok)
            if len(r.r) > 24:
                r.r = r.r[-24:]

    def op(self, e, fn, reads=(), writes=(), inc=True):
        self._deps(e, reads, writes)
        if not self.pend.get(e, False) and self.cnt[e] >= self.SEM_LIMIT:
            self._new_sem(e)
        self.pend[e] = not inc
        ins = fn()
        self.ninstr += 1
        if inc:
            ins.then_inc(self.sem[e], 1)
            self.cnt[e] += 1
            tok = (self.sem[e], self.cnt[e])
            self._pending_ok = True
        else:
            tok = (self.sem[e], self.cnt[e] + 1)
        self._mark(tok, reads, writes)
        return tok

    def dma(self, q, out, in_, reads=(), writes=(), **kw):
        slots = self.dma_slots[q]
        i = self.dma_next[q]
        self.dma_next[q] = (i + 1) % len(slots)
        sem, val = slots[i]
        if val > 0:
            self._wait(q, (sem, val))
        self._deps(q, reads, writes)
        ins = self.engs[q].dma_start(out=out, in_=in_, **kw)
        ins.then_inc(sem, 16)
        slots[i][1] = val + 16
        tok = (sem, val + 16)
        self._mark(tok, reads, writes)
        self.ninstr += 1
        return tok

    def barrier(self):
        toks = []
        for e in ("pe", "act", "dve", "pool"):
            if self.cnt[e] > 0:
                assert not self.pend.get(e, False), f"open group on {e} at barrier"
                toks.append((self.sem[e], self.cnt[e]))
        for q in self.dma_slots:
            for sem, val in self.dma_slots[q]:
                if val > 0:
                    toks.append((sem, val))
        for e in self.engs:
            for t in toks:
                self._wait(e, t)

    def wait_all(self, e, ress):
        for r in ress:
            if r.w is not None:
                self._wait(e, r.w)


class Buf:
    def __init__(self, t, name):
        self.t = t
        self.r = Res(name)

    def __getitem__(self, idx):
        return self.t[idx]


class Ring:
    def __init__(self, bufs):
        self.bufs = bufs
        self.i = 0

    def next(self):
        b = self.bufs[self.i]
        self.i = (self.i + 1) % len(self.bufs)
        return b


class Ctx:
    def __init__(self, nc, es):
        self.nc = nc
        self.es = es
        self.S = Sched(nc, es)
        self.n = 0

    def sb(self, shape, dt, name, es=None):
        self.n += 1
        t = (es or self.es).enter_context(self.nc.sbuf_tensor(f"{name}_{self.n}", list(shape), dt))
        return Buf(t, name)

    def ps(self, shape, dt, name, es=None):
        self.n += 1
        t = (es or self.es).enter_context(self.nc.psum_tensor(f"{name}_{self.n}", list(shape), dt))
        return Buf(t, name)

    def sbring(self, n, shape, dt, name, es=None):
        return Ring([self.sb(shape, dt, f"{name}{i}", es) for i in range(n)])


NE1 = int(os.environ.get("NE1", "128"))
NQ = int(os.environ.get("NQ", "4"))
NFOX = int(os.environ.get("NFOX", "8"))
NML = int(os.environ.get("NML", "4"))
GK = 4
NEG = -1.0e30
CORES_PER_LAUNCH = 8


def build_program(stage=99, dbg=False):
    nc = bass.Bass("TRN2", target_bir_lowering=False)

    def din(name, shape, dt=F32):
        return nc.dram_tensor(name, list(shape), dt, kind="ExternalInput").ap()

    x_d = din("x", [T, D])
    c_d = din("c_r", [128, KC])
    wada_d = din("wada_r", [24, 128, KC, 512])
    bada_d = din("bada_r", [128, 96])
    g1_d = din("g1_r", [128, KC])
    g2_d = din("g2_r", [128, KC])
    wfm_d = din("wfm_r", [56, 128, KC, 128])
    wg_d = din("wg_r", [128, KC, 16])
    gb_d = din("gb_r", [16, 1])
    qkg_d = din("qkg_r", [128, 2])
    cw_d = din("convw_r", [128, 16, 4])
    cb_d = din("convb_r", [128, 16])
    hg_d = din("hg_r", [128, 8])
    wout_d = din("wout_r", [4, 128, KC, 512])
    wq_d = din("wq_r", [16, 128, KC, 128])
    k1t_d = din("k1t_r", [128, 128])
    k2t_d = din("k2t_r", [128, 128])
    edt_d = din("edt_r", [128, 128, KC, 128])
    eu_d = din("eu_r", [128, 128, D])
    out_d = nc.dram_tensor("out", [T, D], F32, kind="ExternalOutput").ap()
    mix_d = nc.dram_tensor("mix_scr", [KC, 128, T], BF16, kind="Internal").ap()
    if dbg:
        dbg_d = nc.dram_tensor("dbg", [128, 4096], F32, kind="ExternalOutput").ap()

    with ExitStack() as es:
        C = Ctx(nc, es)
        S = C.S
        V, A, P, G = nc.vector, nc.scalar, nc.tensor, nc.gpsimd

        psb = Ring([C.ps([128, 512], F32, f"ps{i}") for i in range(4)])
        acc_ps = [C.ps([128, 512], F32, f"pacc{i}") for i in range(4)]
        out_res = [Res(f"out{i}") for i in range(NT)]
        mix_res = [Res(f"mix{i}") for i in range(KC)]

        ident_f = C.sb([128, 128], F32, "ident_f")
        ident_b = C.sb([128, 128], BF16, "ident_b")
        ones_f = C.sb([128, 128], F32, "ones_f")
        ones_b = C.sb([128, 128], BF16, "ones_b")
        sel127 = C.sb([128, 128], F32, "sel127")
        tri_b = C.sb([128, 128], BF16, "tri_b")
        zcol = C.sb([128, 1], F32, "zcol")
        lncol = C.sb([128, 1], F32, "lncol")
        S.op("pool", lambda: G.memset(ones_f[:], 1.0), writes=[ones_f.r])
        S.op("pool", lambda: G.memset(ones_b[:], 1.0), writes=[ones_b.r])
        S.op("pool", lambda: G.memset(zcol[:], 0.0), writes=[zcol.r])
        S.op("pool", lambda: G.memset(lncol[:], float(np.log(1.0 / 16.0))), writes=[lncol.r])
        S.op("pool", lambda: G.affine_select(out=ident_f[:], in_=ones_f[:], pattern=[[-1, 128]],
                                             compare_op=ALU.is_equal, fill=0.0, base=0,
                                             channel_multiplier=1),
             reads=[ones_f.r], writes=[ident_f.r])
        S.op("pool", lambda: G.tensor_copy(out=ident_b[:], in_=ident_f[:]),
             reads=[ident_f.r], writes=[ident_b.r])
        S.op("pool", lambda: G.affine_select(out=sel127[:], in_=ones_f[:], pattern=[[0, 128]],
                                             compare_op=ALU.is_equal, fill=0.0, base=-127,
                                             channel_multiplier=1),
             reads=[ones_f.r], writes=[sel127.r])
        S.op("pool", lambda: G.affine_select(out=tri_b[:], in_=ones_b[:], pattern=[[1, 128]],
                                             compare_op=ALU.is_ge, fill=0.0, base=0,
                                             channel_multiplier=-1),
             reads=[ones_b.r], writes=[tri_b.r])

        def load_small(d_ap, shape, name, dt=F32):
            b = C.sb(shape, dt, name)
            S.dma("sp", b[:], d_ap, writes=[b.r])
            return b

        c_sb = load_small(c_d, [128, KC], "c_sb")
        bada_sb = load_small(bada_d, [128, 96], "bada")
        g1_sb = load_small(g1_d, [128, KC], "g1")
        g2_sb = load_small(g2_d, [128, KC], "g2")
        gb_sb = load_small(gb_d, [16, 1], "gb")
        qkg_sb = load_small(qkg_d, [128, 2], "qkg")
        cw_sb = load_small(cw_d, [128, 16, 4], "cw")
        cb_sb = load_small(cb_d, [128, 16], "cb")
        hg_sb = load_small(hg_d, [128, 8], "hg")
        k1t_f = load_small(k1t_d, [128, 128], "k1tf")
        k2t_f = load_small(k2t_d, [128, 128], "k2tf")
        wg_f = load_small(wg_d, [128, KC, 16], "wgf")
        k1t_b = C.sb([128, 128], BF16, "k1tb")
        k2t_b = C.sb([128, 128], BF16, "k2tb")
        wg_b = C.sb([128, KC, 16], BF16, "wgb")
        S.op("pool", lambda: G.tensor_copy(out=k1t_b[:], in_=k1t_f[:]), reads=[k1t_f.r], writes=[k1t_b.r])
        S.op("pool", lambda: G.tensor_copy(out=k2t_b[:], in_=k2t_f[:]), reads=[k2t_f.r], writes=[k2t_b.r])
        S.op("pool", lambda: G.tensor_copy(out=wg_b[:], in_=wg_f[:]), reads=[wg_f.r], writes=[wg_b.r])
        qsc = C.sb([128, 2], F32, "qsc")
        S.op("dve", lambda: V.tensor_scalar(out=qsc[:, 0:1], in0=qkg_sb[:, 0:1], scalar1=128.0 ** -0.5,
                                            scalar2=None, op0=ALU.mult), reads=[qkg_sb.r], writes=[qsc.r])
        S.op("dve", lambda: V.tensor_copy(out=qsc[:, 1:2], in_=qkg_sb[:, 1:2]), reads=[qkg_sb.r, qsc.r], writes=[qsc.r])

        sc_sb = C.sb([128, KC], F32, "sc_sb")
        mod = C.sb([128, 96], F32, "mod")
        S.op("act", lambda: A.activation(out=sc_sb[:], in_=c_sb[:], func=AF.Silu),
             reads=[c_sb.r], writes=[sc_sb.r])
        es_ada = ExitStack()
        wada_ring = C.sbring(2, [128, KC, 512], F32, "wada", es_ada)
        for jb in range(24):
            wb = wada_ring.next()
            S.dma("sp" if jb % 2 == 0 else "act", wb[:], wada_d[jb], writes=[wb.r])
            pm = psb.next()
            for jj in range(4):
                for kc in range(KC):
                    S.op("pe", lambda kc=kc, jj=jj, pm=pm, wb=wb: P.matmul(
                        pm[:, jj:jj + 1], lhsT=wb[:, kc, jj * 128:(jj + 1) * 128],
                        rhs=sc_sb[:, kc:kc + 1], start=(kc == 0), stop=(kc == KC - 1)),
                        reads=[wb.r, sc_sb.r], writes=[pm.r], inc=(kc == KC - 1 and jj == 3))
            S.op("dve", lambda jb=jb, pm=pm: V.tensor_tensor(
                out=mod[:, jb * 4:jb * 4 + 4], in0=pm[:, 0:4],
                in1=bada_sb[:, jb * 4:jb * 4 + 4], op=ALU.add),
                reads=[pm.r, bada_sb.r], writes=[mod.r])
        S.barrier()
        es_ada.close()
        A1 = C.sb([128, KC], F32, "A1")
        A2 = C.sb([128, KC], F32, "A2")
        S.op("dve", lambda: V.scalar_tensor_tensor(out=A1[:], in0=mod[:, 16:32], scalar=1.0, in1=g1_sb[:],
                                                   op0=ALU.add, op1=ALU.mult),
             reads=[mod.r, g1_sb.r], writes=[A1.r])
        S.op("dve", lambda: V.scalar_tensor_tensor(out=A2[:], in0=mod[:, 64:80], scalar=1.0, in1=g2_sb[:],
                                                   op0=ALU.add, op1=ALU.mult),
             reads=[mod.r, g2_sb.r], writes=[A2.r])

        dg = C.sb([128, 128], F32, "diag")

        def build_gate(gt, c0):
            for kc in range(KC):
                S.op("dve", lambda kc=kc: V.tensor_scalar(
                    out=dg[:], in0=ident_f[:], scalar1=mod[:, c0 + kc:c0 + kc + 1], scalar2=None,
                    op0=ALU.mult), reads=[ident_f.r, mod.r], writes=[dg.r])
                pm = psb.next()
                S.op("pe", lambda pm=pm: P.matmul(pm[:, 0:128], lhsT=ones_f[:], rhs=dg[:], start=True, stop=True),
                     reads=[ones_f.r, dg.r], writes=[pm.r])
                S.op("act", lambda pm=pm, kc=kc: A.copy(out=gt[:, kc * 128:(kc + 1) * 128], in_=pm[:, 0:128]),
                     reads=[pm.r], writes=[gt.r])

        def norm_to_T(es_l, src_rows, src_res, Asc, shift_c0, dstT, ntiles, nring):
            xr = C.sbring(nring, [128, D], F32, "xt", es_l)
            xn_r = C.sbring(nring, [128, D], BF16, "xn", es_l)
            ssr = C.sbring(2, [128, 2], F32, "ss", es_l)
            for i in range(ntiles):
                xt = xr.next()
                S.dma("sp", xt[:], src_rows(i), reads=[src_res(i)] if src_res else [], writes=[xt.r])
                ss = ssr.next()
                S.op("dve", lambda ss=ss: V.memset(ss[:], 0.0), writes=[ss.r])
                xn = xn_r.next()
                S.op("act", lambda xt=xt, ss=ss, xn=xn: A.activation(out=xn[:], in_=xt[:], func=AF.Square,
                                                                     accum_out=ss[:, 0:1]),
                     reads=[xt.r, ss.r], writes=[xn.r, ss.r])
                S.op("dve", lambda ss=ss: V.tensor_scalar(out=ss[:, 1:2], in0=ss[:, 0:1], scalar1=1.0 / D,
                                                          scalar2=EPS, op0=ALU.mult, op1=ALU.add),
                     reads=[ss.r], writes=[ss.r])
                S.op("act", lambda ss=ss: A.activation(out=ss[:, 1:2], in_=ss[:, 1:2], func=AF.Sqrt),
                     reads=[ss.r], writes=[ss.r])
                S.op("dve", lambda ss=ss: V.reciprocal(out=ss[:, 1:2], in_=ss[:, 1:2]),
                     reads=[ss.r], writes=[ss.r])
                S.op("act", lambda xt=xt, ss=ss, xn=xn: A.activation(out=xn[:], in_=xt[:], func=AF.Copy,
                                                                     scale=ss[:, 1:2]),
                     reads=[xt.r, ss.r], writes=[xn.r])
                for k4 in range(4):
                    pm = psb.next()
                    pmb = pm.t.bitcast(BF16)
                    for j in range(4):
                        kc = k4 * 4 + j
                        S.op("pe", lambda kc=kc, j=j, pmb=pmb, xn=xn: P.transpose(
                            out=pmb[:, j * 128:(j + 1) * 128], in_=xn[:, kc * 128:(kc + 1) * 128],
                            identity=ident_b[:]), reads=[xn.r, ident_b.r], writes=[pm.r], inc=(j == 3))
                    for j in range(4):
                        kc = k4 * 4 + j
                        S.op("dve", lambda kc=kc, j=j, pmb=pmb, i=i: V.tensor_scalar(
                            out=dstT[:, kc, i * 128:(i + 1) * 128], in0=pmb[:, j * 128:(j + 1) * 128],
                            scalar1=Asc[:, kc:kc + 1], scalar2=mod[:, shift_c0 + kc:shift_c0 + kc + 1],
                            op0=ALU.mult, op1=ALU.add),
                            reads=[pm.r, Asc.r, mod.r], writes=[dstT.r])

        es_m = ExitStack()
        wst = C.sbring(2, [128, KC, 128], F32, "wst", es_m)
        wbf = C.sbring(2, [128, KC, 128], BF16, "wbf", es_m)
        wq_flip = [0]

        def load_wchunk(d_ap):
            st = wst.next()
            q = "sp" if wq_flip[0] % 2 == 0 else "act"
            wq_flip[0] += 1
            S.dma(q, st[:], d_ap, writes=[st.r])
            wb = wbf.next()
            S.op("pool", lambda: G.tensor_copy(out=wb[:], in_=st[:]), reads=[st.r], writes=[wb.r])
            return wb

        h1T = C.sb([128, KC, T], BF16, "h1T", es_m)
        Ltok = C.sb([128, NT, 16], F32, "Ltok", es_m)
        Gtok = C.sb([128, NT, 16], F32, "Gtok", es_m)
        LrefB = C.sb([128, 64], F32, "LrefB", es_m)
        EQ = C.sb([16, T], BF16, "EQ", es_m)
        selm = C.sb([16, 4 * 128], BF16, "selm", es_m)
        qTr = [C.sb([128, T], BF16, f"qT{i}", es_m) for i in range(2)]
        kTr = [C.sb([128, T], BF16, f"kT{i}", es_m) for i in range(2)]
        qpr = [C.sbring(2, [128, 512], BF16, f"qp{i}", es_m) for i in range(2)]
        vtok = C.sb([128, NT, 256], BF16, "vtok", es_m)
        sigo = [C.sb([128, T], BF16, f"sigo{i}", es_m) for i in range(2)]
        rawc = C.sb([128, T + 4], F32, "rawc", es_m)
        cv = C.sb([128, T], F32, "cv", es_m)
        rawf_r = C.sbring(2, [128, 512], F32, "rawf", es_m)
        sqb_r = C.sbring(2, [128, 512], BF16, "sqb", es_m)
        rs_r = C.sbring(2, [128, 512], F32, "rs", es_m)
        pt_r = C.sbring(3, [128, 512], BF16, "pt", es_m)
        kb_r = C.sbring(2, [128, NT], F32, "kb", es_m)
        ktmp = C.sb([128, NT], F32, "ktmp", es_m)
        hT_r = [C.sbring(1, [128, 512], F32, f"hT{i}", es_m) for i in range(2)]
        mixo_r = C.sbring(2, [128, T], BF16, "mixo", es_m)
        es_n1 = ExitStack()
        norm_to_T(es_n1, lambda i: x_d[i * 128:(i + 1) * 128, :], None, A1, 0, h1T, NT, 2)
        S.barrier()
        es_n1.close()

        es_g = ExitStack()
        graw = C.sb([16, T], F32, "graw", es_g)
        lsp = C.sb([16, T], F32, "lsp", es_g)
        Lc = C.sb([16, T], F32, "Lc", es_g)
        for c in range(4):
            pm = psb.next()
            for kc in range(KC):
                S.op("pe", lambda kc=kc, pm=pm, c=c: P.matmul(
                    pm[0:16, :], lhsT=wg_b[:, kc, :], rhs=h1T[:, kc, c * 512:(c + 1) * 512],
                    start=(kc == 0), stop=(kc == KC - 1)),
                    reads=[wg_b.r, h1T.r], writes=[pm.r], inc=(kc == KC - 1))
            S.op("act", lambda pm=pm, c=c: A.activation(out=graw[:, c * 512:(c + 1) * 512], in_=pm[0:16, :],
                                                        func=AF.Identity, bias=gb_sb[:, 0:1]),
                 reads=[pm.r, gb_sb.r], writes=[graw.r])
        S.op("act", lambda: A.activation(out=lsp[:], in_=graw[:], func=AF.Exp, scale=-1.0),
             reads=[graw.r], writes=[lsp.r])
        S.op("act", lambda: A.activation(out=lsp[:], in_=lsp[:], func=AF.Ln, bias=ones_f[0:16, 0:1]),
             reads=[lsp.r, ones_f.r], writes=[lsp.r])
        S.op("dve", lambda: V.tensor_tensor_scan(out=Lc[:], data0=ones_f[0:16, 0:1].broadcast_to([16, T]), data1=lsp[:],
                                                 initial=zcol[0:16, 0:1], op0=ALU.mult, op1=ALU.add),
             reads=[ones_f.r, lsp.r, zcol.r], writes=[Lc.r])
        for (src, dst) in ((Lc, Ltok), (graw, Gtok)):
            for i4 in range(4):
                pm = psb.next()
                for j in range(4):
                    i = i4 * 4 + j
                    S.op("pe", lambda i=i, j=j, pm=pm, src=src: P.transpose(
                        out=pm[:, j * 16:(j + 1) * 16], in_=src[0:16, i * 128:(i + 1) * 128],
                        identity=ident_f[0:16, 0:16]), reads=[src.r, ident_f.r], writes=[pm.r], inc=(j == 3))
                S.op("dve", lambda i4=i4, pm=pm, dst=dst: V.tensor_copy(
                    out=dst[:, i4 * 4:(i4 + 1) * 4, :],
                    in_=pm[:, 0:64].rearrange("p (a b) -> p a b", a=4)),
                    reads=[pm.r], writes=[dst.r])
        pm = psb.next()
        for c in range(4):
            S.op("pe", lambda c=c, pm=pm: P.matmul(pm[:, c * 16:(c + 1) * 16], lhsT=sel127[:],
                                                   rhs=Ltok[:, 4 * c + 3, :], start=True, stop=True),
                 reads=[sel127.r, Ltok.r], writes=[pm.r], inc=(c == 3))
        S.op("dve", lambda pm=pm: V.tensor_copy(out=LrefB[:], in_=pm[:, 0:64]), reads=[pm.r], writes=[LrefB.r])
        for c in range(4):
            S.op("act", lambda c=c: A.activation(out=EQ[0:12, c * 512:(c + 1) * 512], in_=Lc[0:12, c * 512:(c + 1) * 512],
                                                 func=AF.Exp, scale=-1.0,
                                                 bias=Lc[0:12, c * 512 + 511:c * 512 + 512]),
                 reads=[Lc.r], writes=[EQ.r])
        for m in range(4):
            S.op("dve", lambda m=m: V.tensor_scalar(out=selm[:, m * 128:(m + 1) * 128], in0=ones_f[0:16, :],
                                                    scalar1=ident_f[0:16, 8 + m:9 + m], scalar2=None,
                                                    op0=ALU.mult),
                 reads=[ones_f.r, ident_f.r], writes=[selm.r])

        S.barrier()
        es_g.close()
        S.op("pool", lambda: G.memset(rawc[:, 0:4], 0.0), writes=[rawc.r])

        def proj_fm(wb, c, pm):
            for kc in range(KC):
                S.op("pe", lambda kc=kc: P.matmul(pm[:], lhsT=wb[:, kc, :], rhs=h1T[:, kc, c * 512:(c + 1) * 512],
                                                  start=(kc == 0), stop=(kc == KC - 1)),
                     reads=[wb.r, h1T.r], writes=[pm.r], inc=(kc == KC - 1))

        def fox_qk(wb, dst, gcol):
            def post(c, sqb, rawf):
                pn = psb.next()
                S.op("pe", lambda: P.matmul(pn[:], lhsT=ones_b[:], rhs=sqb[:], start=True, stop=True),
                     reads=[ones_b.r, sqb.r], writes=[pn.r])
                rs = rs_r.next()
                S.op("dve", lambda: V.tensor_scalar(out=rs[:], in0=pn[:], scalar1=1.0 / 128, scalar2=EPS,
                                                    op0=ALU.mult, op1=ALU.add), reads=[pn.r], writes=[rs.r])
                S.op("act", lambda: A.activation(out=rs[:], in_=rs[:], func=AF.Sqrt), reads=[rs.r], writes=[rs.r])
                S.op("dve", lambda: V.reciprocal(out=rs[:], in_=rs[:]), reads=[rs.r], writes=[rs.r])
                S.op("dve", lambda: V.scalar_tensor_tensor(out=dst[:, c * 512:(c + 1) * 512], in0=rawf[:],
                                                           scalar=qsc[:, gcol:gcol + 1], in1=rs[:],
                                                           op0=ALU.mult, op1=ALU.mult),
                     reads=[rawf.r, qsc.r, rs.r], writes=[dst.r])

            prev = None
            for c in range(4):
                pm = psb.next()
                proj_fm(wb, c, pm)
                sqb = sqb_r.next()
                rawf = rawf_r.next()
                S.op("act", lambda: A.activation(out=sqb[:], in_=pm[:], func=AF.Square), reads=[pm.r], writes=[sqb.r])
                S.op("act", lambda: A.copy(out=rawf[:], in_=pm[:]), reads=[pm.r], writes=[rawf.r])
                if prev is not None:
                    post(*prev)
                prev = (c, sqb, rawf)
            post(*prev)

        def v_tok(wb, dvc):
            for i4 in range(4):
                pm = psb.next()
                for j in range(4):
                    i = i4 * 4 + j
                    for kc in range(KC):
                        S.op("pe", lambda kc=kc, i=i, j=j: P.matmul(
                            pm[:, j * 128:(j + 1) * 128], lhsT=h1T[:, kc, i * 128:(i + 1) * 128], rhs=wb[:, kc, :],
                            start=(kc == 0), stop=(kc == KC - 1)),
                            reads=[h1T.r, wb.r], writes=[pm.r], inc=(kc == KC - 1 and j == 3))
                S.op("act", lambda: A.copy(out=vtok[:, i4 * 4:(i4 + 1) * 4, dvc * 128:(dvc + 1) * 128],
                                           in_=pm[:].rearrange("p (a b) -> p a b", a=4)),
                     reads=[pm.r], writes=[vtok.r])

        def ml_qk(wb, dst, cch):
            for c in range(4):
                pm = psb.next()
                proj_fm(wb, c, pm)
                S.op("act", lambda: A.copy(out=rawc[:, 4 + c * 512:4 + (c + 1) * 512], in_=pm[:]),
                     reads=[pm.r], writes=[rawc.r])
            S.op("dve", lambda: V.tensor_scalar(out=cv[:], in0=rawc[:, 4:4 + T], scalar1=cw_sb[:, cch, 3:4],
                                                scalar2=cb_sb[:, cch:cch + 1], op0=ALU.mult, op1=ALU.add),
                 reads=[rawc.r, cw_sb.r, cb_sb.r], writes=[cv.r])
            for j in range(3):
                S.op("dve", lambda j=j: V.scalar_tensor_tensor(out=cv[:], in0=rawc[:, 1 + j:1 + j + T],
                                                               scalar=cw_sb[:, cch, j:j + 1], in1=cv[:],
                                                               op0=ALU.mult, op1=ALU.add),
                     reads=[rawc.r, cw_sb.r, cv.r], writes=[cv.r])
            S.op("act", lambda: A.activation(out=dst[:], in_=cv[:], func=AF.Silu), reads=[cv.r], writes=[dst.r])

        def ml_o(wb, dst):
            for c in range(4):
                pm = psb.next()
                proj_fm(wb, c, pm)
                S.op("act", lambda: A.activation(out=dst[:, c * 512:(c + 1) * 512], in_=pm[:], func=AF.Sigmoid),
                     reads=[pm.r], writes=[dst.r])

        def attend(nd, ndv, row, is_fox, mrow, out_chunks, hgc0):
            mixo = [mixo_r.next() for _ in range(ndv)]
            for c in range(4):
                kb = kb_r.next()
                if is_fox:
                    S.op("dve", lambda: V.tensor_scalar(out=kb[:], in0=Ltok[:, :, row],
                                                        scalar1=LrefB[:, c * 16 + row:c * 16 + row + 1],
                                                        scalar2=None, op0=ALU.subtract),
                         reads=[Ltok.r, LrefB.r], writes=[kb.r])
                    qs = [qTr[d][:, c * 512:(c + 1) * 512] for d in range(nd)]
                    qres = [qTr[d].r for d in range(nd)]
                else:
                    S.op("dve", lambda: V.scalar_tensor_tensor(out=ktmp[:, 0:4 * c + 4], in0=Ltok[:, 0:4 * c + 4, row],
                                                               scalar=LrefB[:, c * 16 + row:c * 16 + row + 1],
                                                               in1=Gtok[:, 0:4 * c + 4, 12 + mrow],
                                                               op0=ALU.subtract, op1=ALU.add),
                         reads=[Ltok.r, LrefB.r, Gtok.r], writes=[ktmp.r])
                    S.op("act", lambda: A.activation(out=kb[:, 0:4 * c + 4], in_=ktmp[:, 0:4 * c + 4], func=AF.Exp, bias=lncol[:, 0:1]),
                         reads=[ktmp.r, lncol.r], writes=[kb.r])
                    pe_ = psb.next()
                    S.op("pe", lambda: P.matmul(pe_[:], lhsT=selm[0:12, mrow * 128:(mrow + 1) * 128],
                                                rhs=EQ[0:12, c * 512:(c + 1) * 512], start=True, stop=True),
                         reads=[selm.r, EQ.r], writes=[pe_.r])
                    qs, qres = [], []
                    for d in range(nd):
                        qp = qpr[d].next()
                        S.op("dve", lambda d=d, qp=qp: V.tensor_tensor(out=qp[:], in0=qTr[d][:, c * 512:(c + 1) * 512],
                                                                       in1=pe_[:], op=ALU.mult),
                             reads=[qTr[d].r, pe_.r], writes=[qp.r])
                        qs.append(qp[:])
                        qres.append(qp.r)
                pO = [acc_ps[d] for d in range(ndv)]
                pD = acc_ps[2]
                nj = 4 * c + 4

                def emit_pv(j, lo, pt):
                    for dv in range(ndv):
                        S.op("pe", lambda dv=dv: P.matmul(pO[dv][:, lo:512], lhsT=vtok[:, j, dv * 128:(dv + 1) * 128],
                                                          rhs=pt[:, lo:512], start=(j == 0), stop=(j == nj - 1)),
                             reads=[vtok.r, pt.r], writes=[pO[dv].r], inc=False)
                    S.op("pe", lambda: P.matmul(pD[:, lo:512], lhsT=ones_b[:], rhs=pt[:, lo:512],
                                                start=(j == 0), stop=(j == nj - 1)),
                         reads=[ones_b.r, pt.r], writes=[pD.r])

                prevpv = None
                for j in range(nj):
                    lo = 128 * (j - 4 * c) if j >= 4 * c else 0
                    pS = psb.next()
                    for d in range(nd):
                        S.op("pe", lambda d=d: P.matmul(pS[:, lo:512], lhsT=kTr[d][:, j * 128:(j + 1) * 128],
                                                        rhs=qs[d][:, lo:512], start=(d == 0), stop=(d == nd - 1)),
                             reads=[kTr[d].r, qres[d]], writes=[pS.r], inc=(d == nd - 1))
                    pt = pt_r.next()
                    if is_fox:
                        S.op("act", lambda: A.activation(out=pt[:, lo:512], in_=pS[:, lo:512], func=AF.Exp,
                                                         bias=kb[:, j:j + 1]),
                             reads=[pS.r, kb.r], writes=[pt.r])
                    else:
                        S.op("dve", lambda: V.tensor_scalar(out=pt[:, lo:512], in0=pS[:, lo:512],
                                                            scalar1=kb[:, j:j + 1], scalar2=None, op0=ALU.mult),
                             reads=[pS.r, kb.r], writes=[pt.r])
                    if j >= 4 * c:
                        S.op("pool", lambda: G.tensor_tensor(out=pt[:, lo:lo + 128], in0=pt[:, lo:lo + 128],
                                                             in1=tri_b[:], op=ALU.mult),
                             reads=[pt.r, tri_b.r], writes=[pt.r])
                    if prevpv is not None:
                        emit_pv(*prevpv)
                    prevpv = (j, lo, pt)
                emit_pv(*prevpv)
                rs = rs_r.next()
                if is_fox:
                    S.op("dve", lambda: V.reciprocal(out=rs[:], in_=pD[:]), reads=[pD.r], writes=[rs.r])
                    S.op("dve", lambda: V.tensor_tensor(out=mixo[0][:, c * 512:(c + 1) * 512], in0=pO[0][:],
                                                        in1=rs[:], op=ALU.mult),
                         reads=[pO[0].r, rs.r], writes=[mixo[0].r])
                else:
                    S.op("dve", lambda: V.tensor_scalar(out=rs[:], in0=pD[:], scalar1=-1.0, scalar2=1.0,
                                                        op0=ALU.mult, op1=ALU.max), reads=[pD.r], writes=[rs.r])
                    S.op("dve", lambda: V.scalar_tensor_tensor(out=rs[:], in0=pD[:], scalar=1.0, in1=rs[:],
                                                               op0=ALU.max, op1=ALU.max),
                         reads=[pD.r, rs.r], writes=[rs.r])
                    S.op("dve", lambda: V.reciprocal(out=rs[:], in_=rs[:]), reads=[rs.r], writes=[rs.r])
                    hTs = []
                    pn = psb.next()
                    for dv in range(ndv):
                        hT = hT_r[dv].next()
                        S.op("dve", lambda dv=dv, hT=hT: V.tensor_tensor(out=hT[:], in0=pO[dv][:], in1=rs[:], op=ALU.mult),
                             reads=[pO[dv].r, rs.r], writes=[hT.r])
                        sqb = sqb_r.next()
                        S.op("act", lambda hT=hT, sqb=sqb: A.activation(out=sqb[:], in_=hT[:], func=AF.Square),
                             reads=[hT.r], writes=[sqb.r])
                        S.op("pe", lambda dv=dv, sqb=sqb: P.matmul(pn[:], lhsT=ones_b[:], rhs=sqb[:],
                                                                    start=(dv == 0), stop=(dv == ndv - 1)),
                             reads=[ones_b.r, sqb.r], writes=[pn.r], inc=(dv == ndv - 1))
                        hTs.append(hT)
                    rs2 = rs_r.next()
                    S.op("dve", lambda: V.tensor_scalar(out=rs2[:], in0=pn[:], scalar1=1.0 / 256, scalar2=EPS,
                                                        op0=ALU.mult, op1=ALU.add), reads=[pn.r], writes=[rs2.r])
                    S.op("act", lambda: A.activation(out=rs2[:], in_=rs2[:], func=AF.Sqrt), reads=[rs2.r], writes=[rs2.r])
                    S.op("dve", lambda: V.reciprocal(out=rs2[:], in_=rs2[:]), reads=[rs2.r], writes=[rs2.r])
                    for dv in range(ndv):
                        S.op("dve", lambda dv=dv: V.scalar_tensor_tensor(
                            out=hTs[dv][:], in0=hTs[dv][:], scalar=hg_sb[:, hgc0 + dv:hgc0 + dv + 1], in1=rs2[:],
                            op0=ALU.mult, op1=ALU.mult), reads=[hTs[dv].r, hg_sb.r, rs2.r], writes=[hTs[dv].r])
                        S.op("dve", lambda dv=dv: V.tensor_tensor(
                            out=mixo[dv][:, c * 512:(c + 1) * 512], in0=hTs[dv][:],
                            in1=sigo[dv][:, c * 512:(c + 1) * 512], op=ALU.mult),
                            reads=[hTs[dv].r, sigo[dv].r], writes=[mixo[dv].r])
            for dv in range(ndv):
                S.dma("sp", mix_d[out_chunks[dv]], mixo[dv][:], reads=[mixo[dv].r],
                      writes=[mix_res[out_chunks[dv]]])

        for h in range(NFOX):
            fox_qk(load_wchunk(wfm_d[3 * h]), qTr[0], 0)
            fox_qk(load_wchunk(wfm_d[3 * h + 1]), kTr[0], 1)
            v_tok(load_wchunk(wfm_d[3 * h + 2]), 0)
            attend(1, 1, h, True, 0, [h], 0)
        for m in range(NML):
            b0 = 24 + 8 * m
            ml_qk(load_wchunk(wfm_d[b0 + 0]), qTr[0], 2 * m)
            ml_qk(load_wchunk(wfm_d[b0 + 1]), qTr[1], 2 * m + 1)
            ml_qk(load_wchunk(wfm_d[b0 + 2]), kTr[0], 8 + 2 * m)
            ml_qk(load_wchunk(wfm_d[b0 + 3]), kTr[1], 8 + 2 * m + 1)
            v_tok(load_wchunk(wfm_d[b0 + 4]), 0)
            v_tok(load_wchunk(wfm_d[b0 + 5]), 1)
            ml_o(load_wchunk(wfm_d[b0 + 6]), sigo[0])
            ml_o(load_wchunk(wfm_d[b0 + 7]), sigo[1])
            attend(2, 2, 8 + m, False, m, [8 + 2 * m, 8 + 2 * m + 1], 2 * m)
        S.barrier()
        es_m.close()

        es_o = ExitStack()
        mixT = C.sb([128, KC, T], BF16, "mixT", es_o)
        written = list(range(NFOX)) + [8 + j for j in range(2 * NML)]
        if len(written) < KC:
            S.op("pool", lambda: G.memset(mixT[:], 0.0), writes=[mixT.r])
        for kc in written:
            S.dma("sp" if kc % 2 == 0 else "act", mixT[:, kc, :], mix_d[kc], reads=[mix_res[kc]], writes=[mixT.r])
        gate1b = C.sb([128, D], F32, "gate1b", es_o)
        build_gate(gate1b, 32)
        wo_st = C.sb([128, KC, 512], F32, "wo_st", es_o)
        wo_bf = C.sbring(2, [128, KC, 512], BF16, "wo_bf", es_o)
        xs_r = C.sbring(3, [128, 512], F32, "xs", es_o)
        t1_r = C.sbring(3, [128, 512], F32, "t1", es_o)
        for cb in range(4):
            S.dma("sp", wo_st[:], wout_d[cb], writes=[wo_st.r])
            wob = wo_bf.next()
            S.op("pool", lambda: G.tensor_copy(out=wob[:], in_=wo_st[:]), reads=[wo_st.r], writes=[wob.r])
            for i in range(NT):
                pm = psb.next()
                for kc in range(KC):
                    S.op("pe", lambda kc=kc: P.matmul(pm[:], lhsT=mixT[:, kc, i * 128:(i + 1) * 128], rhs=wob[:, kc, :],
                                                      start=(kc == 0), stop=(kc == KC - 1)),
                         reads=[mixT.r, wob.r], writes=[pm.r], inc=(kc == KC - 1))
                xs = xs_r.next()
                S.dma("act", xs[:], x_d[i * 128:(i + 1) * 128, cb * 512:(cb + 1) * 512], writes=[xs.r])
                t1 = t1_r.next()
                S.op("dve", lambda: V.tensor_tensor(out=t1[:], in0=pm[:], in1=gate1b[:, cb * 512:(cb + 1) * 512],
                                                    op=ALU.mult), reads=[pm.r, gate1b.r], writes=[t1.r])
                S.op("pool", lambda: G.tensor_tensor(out=t1[:], in0=t1[:], in1=xs[:], op=ALU.add),
                     reads=[t1.r, xs.r], writes=[t1.r])
                S.dma("sp", out_d[i * 128:(i + 1) * 128, cb * 512:(cb + 1) * 512], t1[:], reads=[t1.r],
                      writes=[out_res[i]])
        S.barrier()
        es_o.close()

        es_p = ExitStack()
        psb = Ring(psb.bufs + acc_ps)
        wst = C.sbring(2, [128, KC, 128], F32, "wstp", es_p)
        wbf = C.sbring(2, [128, KC, 128], BF16, "wbfp", es_p)
        gate2b = C.sb([128, D], F32, "gate2b", es_p)
        build_gate(gate2b, 80)
        h2T = C.sb([128, KC, 512], BF16, "h2T", es_p)
        qT = C.sb([128, 16, 512], BF16, "qTp", es_p)
        acc = C.sb([128, 4, D], F32, "acc", es_p)
        Cb = C.sb([128, 8, 512], BF16, "Cb", es_p)
        statsT = C.sb([32, 512], BF16, "statsT", es_p)
        selT = C.sb([32, 8 * 128], BF16, "selT", es_p)
        selg = C.sb([32, 8 * 128], BF16, "selg", es_p)
        ucol = C.sb([32, 8], F32, "ucol", es_p)
        for h in range(8):
            S.op("dve", lambda h=h: V.tensor_tensor(out=ucol[:, h:h + 1], in0=ident_f[0:32, h:h + 1],
                                                    in1=ident_f[0:32, 8 + h:9 + h], op=ALU.add),
                 reads=[ident_f.r, ucol.r], writes=[ucol.r])
            S.op("dve", lambda h=h: V.scalar_tensor_tensor(out=ucol[:, h:h + 1], in0=ucol[:, h:h + 1], scalar=-1.0,
                                                           in1=ident_f[0:32, 16 + h:17 + h],
                                                           op0=ALU.mult, op1=ALU.subtract),
                 reads=[ident_f.r, ucol.r], writes=[ucol.r])
            S.op("dve", lambda h=h: V.tensor_scalar(out=selT[:, h * 128:(h + 1) * 128], in0=ones_f[0:32, :],
                                                    scalar1=ucol[:, h:h + 1], scalar2=None, op0=ALU.mult),
                 reads=[ones_f.r, ucol.r], writes=[selT.r])
            S.op("dve", lambda h=h: V.tensor_scalar(out=selg[:, h * 128:(h + 1) * 128], in0=ones_f[0:32, :],
                                                    scalar1=ident_f[0:32, 24 + h:25 + h], scalar2=None, op0=ALU.mult),
                 reads=[ones_f.r, ident_f.r], writes=[selg.r])
        k1bc_r = C.sbring(2, [128, 128], BF16, "k1bc", es_p)
        eu_st = C.sbring(2, [128, D], F32, "eu_st", es_p)
        eu_bf = C.sbring(GK + 2, [128, D], BF16, "eu_bf", es_p)
        wT_r = C.sbring(2 * GK, [128, 512], BF16, "wT", es_p)
        gA_r = C.sbring(2, [128, 512], BF16, "gA", es_p)
        E_r = C.sbring(6, [128, 512], BF16, "E", es_p)
        Mm2_r = C.sbring(3, [128, 2, 512], BF16, "Mm2", es_p)
        Tt2_r = C.sbring(4, [128, 2, 512], BF16, "Tt2", es_p)
        Ga2_r = C.sbring(2, [128, 2, 512], BF16, "Ga2", es_p)
        sc_r = C.sbring(1, [128, 256], F32, "sc", es_p)
        sc2_r = C.sbring(1, [128, 256], F32, "sc2", es_p)
        v12_r = C.sbring(2, [128, 32], F32, "v12", es_p)
        cand_r = C.sbring(1, [128, 256], F32, "cand", es_p)
        cand2_r = C.sbring(1, [128, 256], F32, "cand2", es_p)
        c16_r = C.sbring(2, [128, 16], F32, "c16", es_p)
        e16_r = C.sbring(2, [128, 16], F32, "e16", es_p)
        sm_r = C.sbring(2, [128, 4], F32, "sm", es_p)
        statf = C.sb([128, 48], F32, "statf", es_p)
        statb = C.sb([128, 32], BF16, "statb", es_p)

        for Q in range(NQ):
            es_n2 = ExitStack()
            norm_to_T(es_n2, lambda i: out_d[(Q * 4 + i) * 128:(Q * 4 + i + 1) * 128, :],
                      lambda i: out_res[Q * 4 + i], A2, 48, h2T, 4, 1)
            S.barrier()
            es_n2.close()
            for cc in range(16):
                st = wst.next()
                S.dma("sp" if cc % 2 == 0 else "act", st[:], wq_d[cc], writes=[st.r])
                wb = wbf.next()
                S.op("pool", lambda: G.tensor_copy(out=wb[:], in_=st[:]), reads=[st.r], writes=[wb.r])
                pm = psb.next()
                for kc in range(KC):
                    S.op("pe", lambda kc=kc: P.matmul(pm[:], lhsT=wb[:, kc, :], rhs=h2T[:, kc, :],
                                                      start=(kc == 0), stop=(kc == KC - 1)),
                         reads=[wb.r, h2T.r], writes=[pm.r], inc=(kc == KC - 1))
                S.op("act", lambda: A.copy(out=qT[:, cc, :], in_=pm[:]), reads=[pm.r], writes=[qT.r])
            for ti in range(4):
                for h in range(8):
                    pm = psb.next()
                    S.op("pe", lambda: P.matmul(pm[:, 0:128], lhsT=qT[:, 2 * h, ti * 128:(ti + 1) * 128],
                                                rhs=k1t_b[:], start=True, stop=True),
                         reads=[qT.r, k1t_b.r], writes=[pm.r], inc=False)
                    S.op("pe", lambda: P.matmul(pm[:, 128:256], lhsT=qT[:, 2 * h + 1, ti * 128:(ti + 1) * 128],
                                                rhs=k2t_b[:], start=True, stop=True),
                         reads=[qT.r, k2t_b.r], writes=[pm.r])
                    sc = sc_r.next()
                    sc2 = sc2_r.next()
                    v12 = v12_r.next()
                    S.op("act", lambda: A.copy(out=sc[:], in_=pm[:, 0:256]), reads=[pm.r], writes=[sc.r])
                    for half in range(2):
                        sl = slice(half * 128, (half + 1) * 128)
                        vo = half * 16
                        S.op("dve", lambda: V.max(out=v12[:, vo:vo + 8], in_=sc[:, sl]), reads=[sc.r, v12.r], writes=[v12.r])
                        S.op("dve", lambda: V.match_replace(out=sc2[:, sl], in_to_replace=v12[:, vo:vo + 8],
                                                            in_values=sc[:, sl], imm_value=NEG),
                             reads=[sc.r, v12.r, sc2.r], writes=[sc2.r])
                        S.op("dve", lambda: V.max(out=v12[:, vo + 8:vo + 16], in_=sc2[:, sl]),
                             reads=[sc2.r, v12.r], writes=[v12.r])
                    cand = cand_r.next()
                    cand2 = cand2_r.next()
                    c16 = c16_r.next()
                    e16 = e16_r.next()
                    sm = sm_r.next()
                    S.op("dve", lambda: V.tensor_tensor(
                        out=cand[:].rearrange("p (a b) -> p a b", a=16),
                        in0=v12[:, 0:16].unsqueeze(2).broadcast_to([128, 16, 16]),
                        in1=v12[:, 16:32].unsqueeze(1).broadcast_to([128, 16, 16]), op=ALU.add),
                        reads=[v12.r], writes=[cand.r])
                    S.op("dve", lambda: V.max(out=c16[:, 0:8], in_=cand[:]), reads=[cand.r, c16.r], writes=[c16.r])
                    S.op("dve", lambda: V.match_replace(out=cand2[:], in_to_replace=c16[:, 0:8], in_values=cand[:],
                                                        imm_value=NEG), reads=[cand.r, c16.r], writes=[cand2.r])
                    S.op("dve", lambda: V.max(out=c16[:, 8:16], in_=cand2[:]), reads=[cand2.r, c16.r], writes=[c16.r])
                    S.op("dve", lambda: V.tensor_scalar(out=sm[:, 0:1], in0=c16[:, 0:1], scalar1=-1.0, scalar2=None,
                                                        op0=ALU.mult), reads=[c16.r, sm.r], writes=[sm.r])
                    S.op("dve", lambda: V.memset(sm[:, 1:2], 0.0), reads=[sm.r], writes=[sm.r])
                    S.op("act", lambda: A.activation(out=e16[:], in_=c16[:], func=AF.Exp, bias=sm[:, 0:1],
                                                     accum_out=sm[:, 1:2]),
                         reads=[c16.r, sm.r], writes=[e16.r, sm.r])
                    S.op("dve", lambda: V.reciprocal(out=sm[:, 2:3], in_=sm[:, 1:2]), reads=[sm.r], writes=[sm.r])
                    S.op("dve", lambda: V.tensor_scalar(out=statf[:, h:h + 1], in0=c16[:, 15:16], scalar1=-3.0e-5,
                                                        scalar2=None, op0=ALU.add),
                         reads=[c16.r, statf.r], writes=[statf.r])
                    S.op("dve", lambda: V.tensor_tensor(out=statf[:, 32 + h:33 + h], in0=e16[:, 15:16], in1=sm[:, 2:3],
                                                        op=ALU.mult), reads=[e16.r, sm.r, statf.r], writes=[statf.r])
                S.op("dve", lambda: V.tensor_copy(out=statb[:, 0:8], in_=statf[:, 0:8]), reads=[statf.r, statb.r], writes=[statb.r])
                S.op("dve", lambda: V.tensor_tensor(out=statf[:, 8:16], in0=statf[:, 0:8], in1=statb[:, 0:8],
                                                    op=ALU.subtract), reads=[statf.r, statb.r], writes=[statf.r])
                S.op("dve", lambda: V.tensor_copy(out=statb[:, 8:16], in_=statf[:, 8:16]), reads=[statf.r, statb.r], writes=[statb.r])
                S.op("dve", lambda: V.tensor_tensor(out=statf[:, 16:24], in0=statf[:, 8:16], in1=statb[:, 8:16],
                                                    op=ALU.subtract), reads=[statf.r, statb.r], writes=[statf.r])
                S.op("dve", lambda: V.tensor_copy(out=statb[:, 16:24], in_=statf[:, 16:24]), reads=[statf.r, statb.r], writes=[statb.r])
                S.op("dve", lambda: V.tensor_copy(out=statb[:, 24:32], in_=statf[:, 32:40]), reads=[statf.r, statb.r], writes=[statb.r])
                pm = psb.next()
                pmb = pm.t.bitcast(BF16)
                S.op("pe", lambda: P.transpose(out=pmb[0:32, 0:128], in_=statb[:, 0:32], identity=ident_b[:]),
                     reads=[statb.r, ident_b.r], writes=[pm.r])
                S.op("act", lambda: A.copy(out=statsT[:, ti * 128:(ti + 1) * 128], in_=pmb[0:32, 0:128]),
                     reads=[pm.r], writes=[statsT.r])
            for h in range(8):
                pm = psb.next()
                S.op("pe", lambda: P.matmul(pm[:], lhsT=selg[:, h * 128:(h + 1) * 128], rhs=statsT[:],
                                            start=True, stop=True), reads=[selg.r, statsT.r], writes=[pm.r])
                S.op("act", lambda: A.copy(out=Cb[:, h, :], in_=pm[:]), reads=[pm.r], writes=[Cb.r])

            grp = []
            ngrp = 0
            pending = []

            def emit_units(n):
                for _ in range(min(n, len(pending))):
                    g_, ti, cbk, first = pending.pop(0)
                    pm = psb.next()
                    for gi, (wT_, eub_) in enumerate(g_):
                        S.op("pe", lambda gi=gi, wT_=wT_, eub_=eub_: P.matmul(
                            pm[:], lhsT=wT_[:, ti * 128:(ti + 1) * 128], rhs=eub_[:, cbk * 512:(cbk + 1) * 512],
                            start=(gi == 0), stop=(gi == len(g_) - 1)),
                            reads=[wT_.r, eub_.r], writes=[pm.r], inc=(gi == len(g_) - 1))
                    if first:
                        S.op("act", lambda: A.copy(out=acc[:, ti, cbk * 512:(cbk + 1) * 512], in_=pm[:]),
                             reads=[pm.r], writes=[acc.r])
                    else:
                        S.op("dve", lambda: V.tensor_tensor(out=acc[:, ti, cbk * 512:(cbk + 1) * 512],
                                                            in0=pm[:], in1=acc[:, ti, cbk * 512:(cbk + 1) * 512],
                                                            op=ALU.add), reads=[pm.r, acc.r], writes=[acc.r])

            prepped = {}

            def prep(e1):
                st = wst.next()
                S.dma("sp", st[:], edt_d[e1], writes=[st.r])
                edb = wbf.next()
                S.op("act", lambda: A.copy(out=edb[:], in_=st[:]), reads=[st.r], writes=[edb.r])
                es_ = eu_st.next()
                S.dma("act", es_[:], eu_d[e1], writes=[es_.r])
                eub = eu_bf.next()
                S.op("act", lambda: A.copy(out=eub[:], in_=es_[:]), reads=[es_.r], writes=[eub.r])
                k1bc = k1bc_r.next()
                S.op("pool", lambda: G.tensor_copy(out=k1bc[:], in_=k1t_b[:, e1:e1 + 1].broadcast_to([128, 128])),
                     reads=[k1t_b.r], writes=[k1bc.r])
                prepped[e1] = (edb, eub, k1bc)

            prep(0)
            for e1 in range(NE1):
                if e1 + 1 < NE1:
                    prep(e1 + 1)
                edb, eub, k1bc = prepped.pop(e1)
                pA = psb.next()
                for kc in range(KC):
                    S.op("pe", lambda kc=kc: P.matmul(pA[:], lhsT=edb[:, kc, :], rhs=h2T[:, kc, :],
                                                      start=(kc == 0), stop=(kc == KC - 1)),
                         reads=[edb.r, h2T.r], writes=[pA.r], inc=(kc == KC - 1))
                gA = gA_r.next()
                S.op("act", lambda: A.activation(out=gA[:], in_=pA[:], func=AF.Gelu), reads=[pA.r], writes=[gA.r])
                Ga2 = Ga2_r.next()
                for hp in range(4):
                    Mm2 = Mm2_r.next()
                    for hh in range(2):
                        h = 2 * hp + hh
                        pX = psb.next()
                        S.op("pe", lambda: P.matmul(pX[:], lhsT=k2t_b[:], rhs=qT[:, 2 * h + 1, :], start=True, stop=False),
                             reads=[k2t_b.r, qT.r], writes=[pX.r], inc=False)
                        S.op("pe", lambda: P.matmul(pX[:], lhsT=k1bc[:], rhs=qT[:, 2 * h, :], start=False, stop=False),
                             reads=[k1bc.r, qT.r], writes=[pX.r], inc=False)
                        S.op("pe", lambda: P.matmul(pX[:], lhsT=selT[:, h * 128:(h + 1) * 128], rhs=statsT[:],
                                                    start=False, stop=True),
                             reads=[selT.r, statsT.r], writes=[pX.r])
                        E = E_r.next()
                        S.op("act", lambda: A.activation(out=E[:], in_=pX[:], func=AF.Exp), reads=[pX.r], writes=[E.r])
                        S.op("dve", lambda: V.scalar_tensor_tensor(out=Mm2[:, hh, :], in0=pX[:], scalar=0.0, in1=E[:],
                                                                   op0=ALU.is_ge, op1=ALU.mult),
                             reads=[pX.r, E.r, Mm2.r], writes=[Mm2.r])
                    if hp == 0:
                        S.op("dve", lambda: V.tensor_tensor(out=Ga2[:], in0=Mm2[:], in1=Cb[:, 0:2, :], op=ALU.mult),
                             reads=[Mm2.r, Cb.r], writes=[Ga2.r])
                    else:
                        Tt2 = Tt2_r.next()
                        S.op("dve", lambda: V.tensor_tensor(out=Tt2[:], in0=Mm2[:], in1=Cb[:, 2 * hp:2 * hp + 2, :],
                                                            op=ALU.mult), reads=[Mm2.r, Cb.r], writes=[Tt2.r])
                        S.op("pool", lambda: G.tensor_tensor(out=Ga2[:], in0=Ga2[:], in1=Tt2[:], op=ALU.add),
                             reads=[Ga2.r, Tt2.r], writes=[Ga2.r])
                S.op("pool", lambda: G.tensor_tensor(out=Ga2[:, 0, :], in0=Ga2[:, 0, :], in1=Ga2[:, 1, :], op=ALU.add),
                     reads=[Ga2.r], writes=[Ga2.r])
                wT = wT_r.next()
                S.op("dve", lambda: V.tensor_tensor(out=wT[:], in0=Ga2[:, 0, :], in1=gA[:], op=ALU.mult),
                     reads=[Ga2.r, gA.r], writes=[wT.r])
                grp.append((wT, eub))
                emit_units(16)
                if len(grp) == GK or e1 == NE1 - 1:
                    for ti in range(4):
                        for cbk in range(4):
                            pending.append((list(grp), ti, cbk, ngrp == 0))
                    grp = []
                    ngrp += 1
            emit_units(len(pending))
            es_f = ExitStack()
            x1_r = C.sbring(1, [128, D], F32, "x1t", es_f)
            for ti in range(4):
                i = Q * 4 + ti
                x1 = x1_r.next()
                S.dma("sp", x1[:], out_d[i * 128:(i + 1) * 128, :], reads=[out_res[i]], writes=[x1.r])
                S.op("dve", lambda: V.tensor_tensor(out=acc[:, ti, :], in0=acc[:, ti, :], in1=gate2b[:], op=ALU.mult),
                     reads=[acc.r, gate2b.r], writes=[acc.r])
                S.op("pool", lambda: G.tensor_tensor(out=acc[:, ti, :], in0=acc[:, ti, :], in1=x1[:], op=ALU.add),
                     reads=[acc.r, x1.r], writes=[acc.r])
                S.dma("sp", out_d[i * 128:(i + 1) * 128, :], acc[:, ti, :], reads=[acc.r, out_res[i]], writes=[out_res[i]])
            S.barrier()
            es_f.close()
        S.barrier()
        es_p.close()

        for q in ("sp", "pool", "act"):
            for sem, val in S.dma_slots[q]:
                if val > 0:
                    S._wait("sp", (sem, val))
        print("instructions:", S.ninstr)
    return nc


def _host_layouts(inp):
    f = lambda a: np.ascontiguousarray(a, dtype=np.float32)
    L = {}
    w_ada = inp["w_ada"][0]
    L["wada_r"] = f(w_ada.reshape(KC, 128, 24, 512).transpose(2, 1, 0, 3))
    L["bada_r"] = f(inp["b_ada"][0].reshape(96, 128).T)
    L["g1_r"] = f(inp["norm1_gain"][0].reshape(KC, 128).T)
    L["g2_r"] = f(inp["norm2_gain"][0].reshape(KC, 128).T)
    w_in = inp["w_in"][0]
    o_fq, o_fk, o_fv, o_ff, o_mq, o_mk, o_mv, o_mo, o_mi, o_mf = 0, 1024, 2048, 3072, 3080, 4104, 5128, 6152, 7176, 7180
    cols = []
    for h in range(8):
        cols += [o_fq + 128 * h, o_fk + 128 * h, o_fv + 128 * h]
    for m in range(4):
        cols += [o_mq + 256 * m, o_mq + 256 * m + 128, o_mk + 256 * m, o_mk + 256 * m + 128,
                 o_mv + 256 * m, o_mv + 256 * m + 128, o_mo + 256 * m, o_mo + 256 * m + 128]
    wfm = np.empty((56, 128, KC, 128), np.float32)
    for i, c0 in enumerate(cols):
        wfm[i] = w_in[:, c0:c0 + 128].reshape(KC, 128, 128).transpose(1, 0, 2)
    L["wfm_r"] = wfm
    gcols = list(range(o_ff, o_ff + 8)) + list(range(o_mf, o_mf + 4)) + list(range(o_mi, o_mi + 4))
    L["wg_r"] = f(w_in[:, gcols].reshape(KC, 128, 16).transpose(1, 0, 2))
    L["gb_r"] = f(np.concatenate([inp["fox_f_bias"][0], inp["mlstm_f_bias"][0], inp["mlstm_i_bias"][0]]).reshape(16, 1))
    L["qkg_r"] = f(np.stack([inp["fox_q_gain"][0], inp["fox_k_gain"][0]], axis=1))
    L["convw_r"] = f(inp["mlstm_conv_w"][0].reshape(4, 16, 128).transpose(2, 1, 0))
    L["convb_r"] = f(inp["mlstm_conv_b"][0].reshape(16, 128).T)
    L["hg_r"] = f(inp["mlstm_head_gain"][0].reshape(8, 128).T)
    L["wout_r"] = f(inp["w_out"][0].reshape(KC, 128, 4, 512).transpose(2, 1, 0, 3))
    L["wq_r"] = f(inp["peer_w_query"][0].reshape(KC, 128, 16, 128).transpose(2, 1, 0, 3))
    L["k1t_r"] = f(inp["peer_sub_keys_1"][0].T)
    L["k2t_r"] = f(inp["peer_sub_keys_2"][0].T)
    ed = inp["peer_expert_down"][0]
    L["edt_r"] = f(ed.reshape(128, 128, KC, 128).transpose(0, 3, 2, 1))
    L["eu_r"] = f(inp["peer_expert_up"][0].reshape(128, 128, D))
    return L


def _core_inputs(inputs, b):
    return {
        "x": np.ascontiguousarray(inputs["x"][b], dtype=np.float32),
        "c_r": np.ascontiguousarray(inputs["c"][b].reshape(KC, 128).T, dtype=np.float32),
    }


def kernel(**inputs):
    inputs = {k: np.asarray(v) for k, v in inputs.items()}
    L = _host_layouts(inputs)
    nc = build_program()
    outs = []
    for g0 in range(0, 8, CORES_PER_LAUNCH):
        in_maps = []
        for b in range(g0, g0 + CORES_PER_LAUNCH):
            m = dict(L)
            m.update(_core_inputs(inputs, b))
            in_maps.append(m)
        res = run_bass_kernel_spmd(nc, in_maps, core_ids=list(range(CORES_PER_LAUNCH)))
        outs += [np.asarray(r["out"], dtype=np.float32) for r in res.results]
    return np.stack(outs, axis=0)
```

```python
from contextlib import ExitStack
import numpy as np
import concourse.bass as bass
import concourse.mybir as mybir
from concourse.bass_utils import run_bass_kernel_spmd

F32 = mybir.dt.float32
BF16 = mybir.dt.bfloat16
ALU = mybir.AluOpType
AF = mybir.ActivationFunctionType
AX = mybir.AxisListType

import os
NBLK = int(os.environ.get("NBLK", "24"))
ADAQ = os.environ.get("ADAQ", "pool")
D = 2048
T = 2048
KC = 16
NT = 16
EPS = 1e-6


class Res:
    __slots__ = ("name", "w", "r")

    def __init__(self, name=""):
        self.name = name
        self.w = None
        self.r = []


class Sched:
    SEM_LIMIT = 30000

    def __init__(self, nc, es):
        self.nc = nc
        self.es = es
        self.engs = {"pe": nc.tensor, "act": nc.scalar, "dve": nc.vector,
                     "pool": nc.gpsimd, "sp": nc.sync}
        self.sem = {}
        self.cnt = {}
        self.nsem = 0
        self.pe_sems = []
        self.pend = {}
        for e in ("pe", "act", "dve", "pool"):
            self._new_sem(e)
        self.waited = {e: {} for e in self.engs}
        self.dma_slots = {}
        self.dma_next = {}
        for q, n in (("sp", 8), ("pool", 2), ("act", 4)):
            self.dma_slots[q] = [[self._mk(f"d{q}{i}"), 0] for i in range(n)]
            self.dma_next[q] = 0
        self.ninstr = 0

    def _mk(self, name):
        self.nsem += 1
        return self.es.enter_context(self.nc.semaphore(f"{name}_{self.nsem}"))

    def _new_sem(self, e):
        self.sem[e] = self._mk(f"s{e}")
        self.cnt[e] = 0
        if e == "pe":
            self.pe_sems.append(self.sem[e])

    def _wait(self, e, tok):
        sem, val = tok
        if e == "pe" and sem in self.pe_sems:
            return
        key = id(sem)
        if self.waited[e].get(key, 0) >= val:
            return
        self.engs[e].wait_ge(sem, val)
        self.waited[e][key] = val

    def _deps(self, e, reads, writes):
        toks = []
        for r in reads:
            if r.w is not None:
                toks.append(r.w)
        for w in writes:
            if w.w is not None:
                toks.append(w.w)
            toks.extend(w.r)
        for t in toks:
            self._wait(e, t)

    def _mark(self, tok, reads, writes):
        for w in writes:
            w.w = tok
            w.r = []
        for r in reads:
            if r in writes:
                continue
            r.r.append(tok)
            if len(r.r) > 24:
                r.r = r.r[-24:]

    def op(self, e, fn, reads=(), writes=(), inc=True):
        self._deps(e, reads, writes)
        if not self.pend.get(e, False) and self.cnt[e] >= self.SEM_LIMIT:
            self._new_sem(e)
        self.pend[e] = not inc
        ins = fn()
        self.ninstr += 1
        if inc:
            ins.then_inc(self.sem[e], 1)
            self.cnt[e] += 1
            tok = (self.sem[e], self.cnt[e])
            self._pending_ok = True
        else:
            tok = (self.sem[e], self.cnt[e] + 1)
        self._mark(tok, reads, writes)
        return tok

    def dma(self, q, out, in_, reads=(), writes=(), **kw):
        slots = self.dma_slots[q]
        i = self.dma_next[q]
        self.dma_next[q] = (i + 1) % len(slots)
        sem, val = slots[i]
        if val > 0:
            self._wait(q, (sem, val))
        self._deps(q, reads, writes)
        ins = self.engs[q].dma_start(out=out, in_=in_, **kw)
        ins.then_inc(sem, 16)
        slots[i][1] = val + 16
        tok = (sem, val + 16)
        self._mark(tok, reads, writes)
        self.ninstr += 1
        return tok

    def barrier(self):
        toks = []
        for e in ("pe", "act", "dve", "pool"):
            if self.cnt[e] > 0:
                assert not self.pend.get(e, False), f"open group on {e} at barrier"
                toks.append((self.sem[e], self.cnt[e]))
        for q in self.dma_slots:
            for sem, val in self.dma_slots[q]:
                if val > 0:
                    toks.append((sem, val))
        for e in self.engs:
            for t in toks:
                self._wait(e, t)

    def wait_all(self, e, ress):
        for r in ress:
            if r.w is not None:
                self._wait(e, r.w)


class Buf:
    def __init__(self, t, name):
        self.t = t
        self.r = Res(name)

    def __getitem__(self, idx):
        return self.t[idx]


class Ring:
    def __init__(self, bufs):
        self.bufs = bufs
        self.i = 0

    def next(self):
        b = self.bufs[self.i]
        self.i = (self.i + 1) % len(self.bufs)
        return b


class Ctx:
    def __init__(self, nc, es):
        self.nc = nc
        self.es = es
        self.S = Sched(nc, es)
        self.n = 0

    def sb(self, shape, dt, name, es=None):
        self.n += 1
        t = (es or self.es).enter_context(self.nc.sbuf_tensor(f"{name}_{self.n}", list(shape), dt))
        return Buf(t, name)

    def ps(self, shape, dt, name, es=None):
        self.n += 1
        t = (es or self.es).enter_context(self.nc.psum_tensor(f"{name}_{self.n}", list(shape), dt))
        return Buf(t, name)

    def sbring(self, n, shape, dt, name, es=None):
        return Ring([self.sb(shape, dt, f"{name}{i}", es) for i in range(n)])


NE1 = int(os.environ.get("NE1", "128"))
NQ = int(os.environ.get("NQ", "4"))
NFOX = int(os.environ.get("NFOX", "8"))
NML = int(os.environ.get("NML", "4"))
GK = 4
NEG = -1.0e30
CORES_PER_LAUNCH = 8


def build_program(stage=99, dbg=False):
    nc = bass.Bass("TRN2", target_bir_lowering=False)

    def din(name, shape, dt=F32):
        return nc.dram_tensor(name, list(shape), dt, kind="ExternalInput").ap()

    x_d = din("x", [T, D])
    c_d = din("c_r", [128, KC])
    wada_d = din("wada_r", [24, 128, KC, 512])
    bada_d = din("bada_r", [128, 96])
    g1_d = din("g1_r", [128, KC])
    g2_d = din("g2_r", [128, KC])
    wfm_d = din("wfm_r", [56, 128, KC, 128])
    wg_d = din("wg_r", [128, KC, 16])
    gb_d = din("gb_r", [16, 1])
    qkg_d = din("qkg_r", [128, 2])
    cw_d = din("convw_r", [128, 16, 4])
    cb_d = din("convb_r", [128, 16])
    hg_d = din("hg_r", [128, 8])
    wout_d = din("wout_r", [4, 128, KC, 512])
    wq_d = din("wq_r", [16, 128, KC, 128])
    k1t_d = din("k1t_r", [128, 128])
    k2t_d = din("k2t_r", [128, 128])
    edt_d = din("edt_r", [128, 128, KC, 128])
    eu_d = din("eu_r", [128, 128, D])
    out_d = nc.dram_tensor("out", [T, D], F32, kind="ExternalOutput").ap()
    mix_d = nc.dram_tensor("mix_scr", [KC, 128, T], BF16, kind="Internal").ap()
    if dbg:
        dbg_d = nc.dram_tensor("dbg", [128, 4096], F32, kind="ExternalOutput").ap()

    with ExitStack() as es:
        C = Ctx(nc, es)
        S = C.S
        V, A, P, G = nc.vector, nc.scalar, nc.tensor, nc.gpsimd

        psb = Ring([C.ps([128, 512], F32, f"ps{i}") for i in range(4)])
        acc_ps = [C.ps([128, 512], F32, f"pacc{i}") for i in range(4)]
        out_res = [Res(f"out{i}") for i in range(NT)]
        mix_res = [Res(f"mix{i}") for i in range(KC)]

        ident_f = C.sb([128, 128], F32, "ident_f")
        ident_b = C.sb([128, 128], BF16, "ident_b")
        ones_f = C.sb([128, 128], F32, "ones_f")
        ones_b = C.sb([128, 128], BF16, "ones_b")
        sel127 = C.sb([128, 128], F32, "sel127")
        tri_b = C.sb([128, 128], BF16, "tri_b")
        zcol = C.sb([128, 1], F32, "zcol")
        lncol = C.sb([128, 1], F32, "lncol")
        S.op("pool", lambda: G.memset(ones_f[:], 1.0), writes=[ones_f.r])
        S.op("pool", lambda: G.memset(ones_b[:], 1.0), writes=[ones_b.r])
        S.op("pool", lambda: G.memset(zcol[:], 0.0), writes=[zcol.r])
        S.op("pool", lambda: G.memset(lncol[:], float(np.log(1.0 / 16.0))), writes=[lncol.r])
        S.op("pool", lambda: G.affine_select(out=ident_f[:], in_=ones_f[:], pattern=[[-1, 128]],
                                             compare_op=ALU.is_equal, fill=0.0, base=0,
                                             channel_multiplier=1),
             reads=[ones_f.r], writes=[ident_f.r])
        S.op("pool", lambda: G.tensor_copy(out=ident_b[:], in_=ident_f[:]),
             reads=[ident_f.r], writes=[ident_b.r])
        S.op("pool", lambda: G.affine_select(out=sel127[:], in_=ones_f[:], pattern=[[0, 128]],
                                             compare_op=ALU.is_equal, fill=0.0, base=-127,
                                             channel_multiplier=1),
             reads=[ones_f.r], writes=[sel127.r])
        S.op("pool", lambda: G.affine_select(out=tri_b[:], in_=ones_b[:], pattern=[[1, 128]],
                                             compare_op=ALU.is_ge, fill=0.0, base=0,
                                             channel_multiplier=-1),
             reads=[ones_b.r], writes=[tri_b.r])

        def load_small(d_ap, shape, name, dt=F32):
            b = C.sb(shape, dt, name)
            S.dma("sp", b[:], d_ap, writes=[b.r])
            return b

        c_sb = load_small(c_d, [128, KC], "c_sb")
        bada_sb = load_small(bada_d, [128, 96], "bada")
        g1_sb = load_small(g1_d, [128, KC], "g1")
        g2_sb = load_small(g2_d, [128, KC], "g2")
        gb_sb = load_small(gb_d, [16, 1], "gb")
        qkg_sb = load_small(qkg_d, [128, 2], "qkg")
        cw_sb = load_small(cw_d, [128, 16, 4], "cw")
        cb_sb = load_small(cb_d, [128, 16], "cb")
        hg_sb = load_small(hg_d, [128, 8], "hg")
        k1t_f = load_small(k1t_d, [128, 128], "k1tf")
        k2t_f = load_small(k2t_d, [128, 128], "k2tf")
        wg_f = load_small(wg_d, [128, KC, 16], "wgf")
        k1t_b = C.sb([128, 128], BF16, "k1tb")
        k2t_b = C.sb([128, 128], BF16, "k2tb")
        wg_b = C.sb([128, KC, 16], BF16, "wgb")
        S.op("pool", lambda: G.tensor_copy(out=k1t_b[:], in_=k1t_f[:]), reads=[k1t_f.r], writes=[k1t_b.r])
        S.op("pool", lambda: G.tensor_copy(out=k2t_b[:], in_=k2t_f[:]), reads=[k2t_f.r], writes=[k2t_b.r])
        S.op("pool", lambda: G.tensor_copy(out=wg_b[:], in_=wg_f[:]), reads=[wg_f.r], writes=[wg_b.r])
        qsc = C.sb([128, 2], F32, "qsc")
        S.op("dve", lambda: V.tensor_scalar(out=qsc[:, 0:1], in0=qkg_sb[:, 0:1], scalar1=128.0 ** -0.5,
                                            scalar2=None, op0=ALU.mult), reads=[qkg_sb.r], writes=[qsc.r])
        S.op("dve", lambda: V.tensor_copy(out=qsc[:, 1:2], in_=qkg_sb[:, 1:2]), reads=[qkg_sb.r, qsc.r], writes=[qsc.r])

        sc_sb = C.sb([128, KC], F32, "sc_sb")
        mod = C.sb([128, 96], F32, "mod")
        S.op("act", lambda: A.activation(out=sc_sb[:], in_=c_sb[:], func=AF.Silu),
             reads=[c_sb.r], writes=[sc_sb.r])
        es_ada = ExitStack()
        wada_ring = C.sbring(2, [128, KC, 512], F32, "wada", es_ada)
        for jb in range(24):
            wb = wada_ring.next()
            S.dma("sp" if jb % 2 == 0 else "act", wb[:], wada_d[jb], writes=[wb.r])
            pm = psb.next()
            for jj in range(4):
                for kc in range(KC):
                    S.op("pe", lambda kc=kc, jj=jj, pm=pm, wb=wb: P.matmul(
                        pm[:, jj:jj + 1], lhsT=wb[:, kc, jj * 128:(jj + 1) * 128],
                        rhs=sc_sb[:, kc:kc + 1], start=(kc == 0), stop=(kc == KC - 1)),
                        reads=[wb.r, sc_sb.r], writes=[pm.r], inc=(kc == KC - 1 and jj == 3))
            S.op("dve", lambda jb=jb, pm=pm: V.tensor_tensor(
                out=mod[:, jb * 4:jb * 4 + 4], in0=pm[:, 0:4],
                in1=bada_sb[:, jb * 4:jb * 4 + 4], op=ALU.add),
                reads=[pm.r, bada_sb.r], writes=[mod.r])
        S.barrier()
        es_ada.close()
        A1 = C.sb([128, KC], F32, "A1")
        A2 = C.sb([128, KC], F32, "A2")
        S.op("dve", lambda: V.scalar_tensor_tensor(out=A1[:], in0=mod[:, 16:32], scalar=1.0, in1=g1_sb[:],
                                                   op0=ALU.add, op1=ALU.mult),
             reads=[mod.r, g1_sb.r], writes=[A1.r])
        S.op("dve", lambda: V.scalar_tensor_tensor(out=A2[:], in0=mod[:, 64:80], scalar=1.0, in1=g2_sb[:],
                                                   op0=ALU.add, op1=ALU.mult),
             reads=[mod.r, g2_sb.r], writes=[A2.r])

        dg = C.sb([128, 128], F32, "diag")

        def build_gate(gt, c0):
            for kc in range(KC):
                S.op("dve", lambda kc=kc: V.tensor_scalar(
                    out=dg[:], in0=ident_f[:], scalar1=mod[:, c0 + kc:c0 + kc + 1], scalar2=None,
                    op0=ALU.mult), reads=[ident_f.r, mod.r], writes=[dg.r])
                pm = psb.next()
                S.op("pe", lambda pm=pm: P.matmul(pm[:, 0:128], lhsT=ones_f[:], rhs=dg[:], start=True, stop=True),
                     reads=[ones_f.r, dg.r], writes=[pm.r])
                S.op("act", lambda pm=pm, kc=kc: A.copy(out=gt[:, kc * 128:(kc + 1) * 128], in_=pm[:, 0:128]),
                     reads=[pm.r], writes=[gt.r])

        def norm_to_T(es_l, src_rows, src_res, Asc, shift_c0, dstT, ntiles, nring):
            xr = C.sbring(nring, [128, D], F32, "xt", es_l)
            xn_r = C.sbring(nring, [128, D], BF16, "xn", es_l)
            ssr = C.sbring(2, [128, 2], F32, "ss", es_l)
            for i in range(ntiles):
                xt = xr.next()
                S.dma("sp", xt[:], src_rows(i), reads=[src_res(i)] if src_res else [], writes=[xt.r])
                ss = ssr.next()
                S.op("dve", lambda ss=ss: V.memset(ss[:], 0.0), writes=[ss.r])
                xn = xn_r.next()
                S.op("act", lambda xt=xt, ss=ss, xn=xn: A.activation(out=xn[:], in_=xt[:], func=AF.Square,
                                                                     accum_out=ss[:, 0:1]),
                     reads=[xt.r, ss.r], writes=[xn.r, ss.r])
                S.op("dve", lambda ss=ss: V.tensor_scalar(out=ss[:, 1:2], in0=ss[:, 0:1], scalar1=1.0 / D,
                                                          scalar2=EPS, op0=ALU.mult, op1=ALU.add),
                     reads=[ss.r], writes=[ss.r])
                S.op("act", lambda ss=ss: A.activation(out=ss[:, 1:2], in_=ss[:, 1:2], func=AF.Sqrt),
                     reads=[ss.r], writes=[ss.r])
                S.op("dve", lambda ss=ss: V.reciprocal(out=ss[:, 1:2], in_=ss[:, 1:2]),
                     reads=[ss.r], writes=[ss.r])
                S.op("act", lambda xt=xt, ss=ss, xn=xn: A.activation(out=xn[:], in_=xt[:], func=AF.Copy,
                                                                     scale=ss[:, 1:2]),
                     reads=[xt.r, ss.r], writes=[xn.r])
                for k4 in range(4):
                    pm = psb.next()
                    pmb = pm.t.bitcast(BF16)
                    for j in range(4):
                        kc = k4 * 4 + j
                        S.op("pe", lambda kc=kc, j=j, pmb=pmb, xn=xn: P.transpose(
                            out=pmb[:, j * 128:(j + 1) * 128], in_=xn[:, kc * 128:(kc + 1) * 128],
                            identity=ident_b[:]), reads=[xn.r, ident_b.r], writes=[pm.r], inc=(j == 3))
                    for j in range(4):
                        kc = k4 * 4 + j
                        S.op("dve", lambda kc=kc, j=j, pmb=pmb, i=i: V.tensor_scalar(
                            out=dstT[:, kc, i * 128:(i + 1) * 128], in0=pmb[:, j * 128:(j + 1) * 128],
                            scalar1=Asc[:, kc:kc + 1], scalar2=mod[:, shift_c0 + kc:shift_c0 + kc + 1],
                            op0=ALU.mult, op1=ALU.add),
                            reads=[pm.r, Asc.r, mod.r], writes=[dstT.r])

        es_m = ExitStack()
        wst = C.sbring(2, [128, KC, 128], F32, "wst", es_m)
        wbf = C.sbring(2, [128, KC, 128], BF16, "wbf", es_m)
        wq_flip = [0]

        def load_wchunk(d_ap):
            st = wst.next()
            q = "sp" if wq_flip[0] % 2 == 0 else "act"
            wq_flip[0] += 1
            S.dma(q, st[:], d_ap, writes=[st.r])
            wb = wbf.next()
            S.op("pool", lambda: G.tensor_copy(out=wb[:], in_=st[:]), reads=[st.r], writes=[wb.r])
            return wb

        h1T = C.sb([128, KC, T], BF16, "h1T", es_m)
        Ltok = C.sb([128, NT, 16], F32, "Ltok", es_m)
        Gtok = C.sb([128, NT, 16], F32, "Gtok", es_m)
        LrefB = C.sb([128, 64], F32, "LrefB", es_m)
        EQ = C.sb([16, T], BF16, "EQ", es_m)
        selm = C.sb([16, 4 * 128], BF16, "selm", es_m)
        qTr = [C.sb([128, T], BF16, f"qT{i}", es_m) for i in range(2)]
        kTr = [C.sb([128, T], BF16, f"kT{i}", es_m) for i in range(2)]
        qpr = [C.sbring(2, [128, 512], BF16, f"qp{i}", es_m) for i in range(2)]
        vtok = C.sb([128, NT, 256], BF16, "vtok", es_m)
        sigo = [C.sb([128, T], BF16, f"sigo{i}", es_m) for i in range(2)]
        rawc = C.sb([128, T + 4], F32, "rawc", es_m)
        cv = C.sb([128, T], F32, "cv", es_m)
        rawf_r = C.sbring(2, [128, 512], F32, "rawf", es_m)
        sqb_r = C.sbring(2, [128, 512], BF16, "sqb", es_m)
        rs_r = C.sbring(2, [128, 512], F32, "rs", es_m)
        pt_r = C.sbring(3, [128, 512], BF16, "pt", es_m)
        kb_r = C.sbring(2, [128, NT], F32, "kb", es_m)
        ktmp = C.sb([128, NT], F32, "ktmp", es_m)
        hT_r = [C.sbring(1, [128, 512], F32, f"hT{i}", es_m) for i in range(2)]
        mixo_r = C.sbring(2, [128, T], BF16, "mixo", es_m)
        es_n1 = ExitStack()
        norm_to_T(es_n1, lambda i: x_d[i * 128:(i + 1) * 128, :], None, A1, 0, h1T, NT, 2)
        S.barrier()
        es_n1.close()

        es_g = ExitStack()
        graw = C.sb([16, T], F32, "graw", es_g)
        lsp = C.sb([16, T], F32, "lsp", es_g)
        Lc = C.sb([16, T], F32, "Lc", es_g)
        for c in range(4):
            pm = psb.next()
            for kc in range(KC):
                S.op("pe", lambda kc=kc, pm=pm, c=c: P.matmul(
                    pm[0:16, :], lhsT=wg_b[:, kc, :], rhs=h1T[:, kc, c * 512:(c + 1) * 512],
                    start=(kc == 0), stop=(kc == KC - 1)),
                    reads=[wg_b.r, h1T.r], writes=[pm.r], inc=(kc == KC - 1))
            S.op("act", lambda pm=pm, c=c: A.activation(out=graw[:, c * 512:(c + 1) * 512], in_=pm[0:16, :],
                                                        func=AF.Identity, bias=gb_sb[:, 0:1]),
                 reads=[pm.r, gb_sb.r], writes=[graw.r])
        S.op("act", lambda: A.activation(out=lsp[:], in_=graw[:], func=AF.Exp, scale=-1.0),
             reads=[graw.r], writes=[lsp.r])
        S.op("act", lambda: A.activation(out=lsp[:], in_=lsp[:], func=AF.Ln, bias=ones_f[0:16, 0:1]),
             reads=[lsp.r, ones_f.r], writes=[lsp.r])
        S.op("dve", lambda: V.tensor_tensor_scan(out=Lc[:], data0=ones_f[0:16, 0:1].broadcast_to([16, T]), data1=lsp[:],
                                                 initial=zcol[0:16, 0:1], op0=ALU.mult, op1=ALU.add),
             reads=[ones_f.r, lsp.r, zcol.r], writes=[Lc.r])
        for (src, dst) in ((Lc, Ltok), (graw, Gtok)):
            for i4 in range(4):
                pm = psb.next()
                for j in range(4):
                    i = i4 * 4 + j
                    S.op("pe", lambda i=i, j=j, pm=pm, src=src: P.transpose(
                        out=pm[:, j * 16:(j + 1) * 16], in_=src[0:16, i * 128:(i + 1) * 128],
                        identity=ident_f[0:16, 0:16]), reads=[src.r, ident_f.r], writes=[pm.r], inc=(j == 3))
                S.op("dve", lambda i4=i4, pm=pm, dst=dst: V.tensor_copy(
                    out=dst[:, i4 * 4:(i4 + 1) * 4, :],
                    in_=pm[:, 0:64].rearrange("p (a b) -> p a b", a=4)),
                    reads=[pm.r], writes=[dst.r])
        pm = psb.next()
        for c in range(4):
            S.op("pe", lambda c=c, pm=pm: P.matmul(pm[:, c * 16:(c + 1) * 16], lhsT=sel127[:],
                                                   rhs=Ltok[:, 4 * c + 3, :], start=True, stop=True),
                 reads=[sel127.r, Ltok.r], writes=[pm.r], inc=(c == 3))
        S.op("dve", lambda pm=pm: V.tensor_copy(out=LrefB[:], in_=pm[:, 0:64]), reads=[pm.r], writes=[LrefB.r])
        for c in range(4):
            S.op("act", lambda c=c: A.activation(out=EQ[0:12, c * 512:(c + 1) * 512], in_=Lc[0:12, c * 512:(c + 1) * 512],
                                                 func=AF.Exp, scale=-1.0,
                                                 bias=Lc[0:12, c * 512 + 511:c * 512 + 512]),
                 reads=[Lc.r], writes=[EQ.r])
        for m in range(4):
            S.op("dve", lambda m=m: V.tensor_scalar(out=selm[:, m * 128:(m + 1) * 128], in0=ones_f[0:16, :],
                                                    scalar1=ident_f[0:16, 8 + m:9 + m], scalar2=None,
                                                    op0=ALU.mult),
                 reads=[ones_f.r, ident_f.r], writes=[selm.r])

        S.barrier()
        es_g.close()
        S.op("pool", lambda: G.memset(rawc[:, 0:4], 0.0), writes=[rawc.r])

        def proj_fm(wb, c, pm):
            for kc in range(KC):
                S.op("pe", lambda kc=kc: P.matmul(pm[:], lhsT=wb[:, kc, :], rhs=h1T[:, kc, c * 512:(c + 1) * 512],
                                                  start=(kc == 0), stop=(kc == KC - 1)),
                     reads=[wb.r, h1T.r], writes=[pm.r], inc=(kc == KC - 1))

        def fox_qk(wb, dst, gcol):
            def post(c, sqb, rawf):
                pn = psb.next()
                S.op("pe", lambda: P.matmul(pn[:], lhsT=ones_b[:], rhs=sqb[:], start=True, stop=True),
                     reads=[ones_b.r, sqb.r], writes=[pn.r])
                rs = rs_r.next()
                S.op("dve", lambda: V.tensor_scalar(out=rs[:], in0=pn[:], scalar1=1.0 / 128, scalar2=EPS,
                                                    op0=ALU.mult, op1=ALU.add), reads=[pn.r], writes=[rs.r])
                S.op("act", lambda: A.activation(out=rs[:], in_=rs[:], func=AF.Sqrt), reads=[rs.r], writes=[rs.r])
                S.op("dve", lambda: V.reciprocal(out=rs[:], in_=rs[:]), reads=[rs.r], writes=[rs.r])
                S.op("dve", lambda: V.scalar_tensor_tensor(out=dst[:, c * 512:(c + 1) * 512], in0=rawf[:],
                                                           scalar=qsc[:, gcol:gcol + 1], in1=rs[:],
                                                           op0=ALU.mult, op1=ALU.mult),
                     reads=[rawf.r, qsc.r, rs.r], writes=[dst.r])

            prev = None
            for c in range(4):
                pm = psb.next()
                proj_fm(wb, c, pm)
                sqb = sqb_r.next()
                rawf = rawf_r.next()
                S.op("act", lambda: A.activation(out=sqb[:], in_=pm[:], func=AF.Square), reads=[pm.r], writes=[sqb.r])
                S.op("act", lambda: A.copy(out=rawf[:], in_=pm[:]), reads=[pm.r], writes=[rawf.r])
                if prev is not None:
                    post(*prev)
                prev = (c, sqb, rawf)
            post(*prev)

        def v_tok(wb, dvc):
            for i4 in range(4):
                pm = psb.next()
                for j in range(4):
                    i = i4 * 4 + j
                    for kc in range(KC):
                        S.op("pe", lambda kc=kc, i=i, j=j: P.matmul(
                            pm[:, j * 128:(j + 1) * 128], lhsT=h1T[:, kc, i * 128:(i + 1) * 128], rhs=wb[:, kc, :],
                            start=(kc == 0), stop=(kc == KC - 1)),
                            reads=[h1T.r, wb.r], writes=[pm.r], inc=(kc == KC - 1 and j == 3))
                S.op("act", lambda: A.copy(out=vtok[:, i4 * 4:(i4 + 1) * 4, dvc * 128:(dvc + 1) * 128],
                                           in_=pm[:].rearrange("p (a b) -> p a b", a=4)),
                     reads=[pm.r], writes=[vtok.r])

        def ml_qk(wb, dst, cch):
            for c in range(4):
                pm = psb.next()
                proj_fm(wb, c, pm)
                S.op("act", lambda: A.copy(out=rawc[:, 4 + c * 512:4 + (c + 1) * 512], in_=pm[:]),
                     reads=[pm.r], writes=[rawc.r])
            S.op("dve", lambda: V.tensor_scalar(out=cv[:], in0=rawc[:, 4:4 + T], scalar1=cw_sb[:, cch, 3:4],
                                                scalar2=cb_sb[:, cch:cch + 1], op0=ALU.mult, op1=ALU.add),
                 reads=[rawc.r, cw_sb.r, cb_sb.r], writes=[cv.r])
            for j in range(3):
                S.op("dve", lambda j=j: V.scalar_tensor_tensor(out=cv[:], in0=rawc[:, 1 + j:1 + j + T],
                                                               scalar=cw_sb[:, cch, j:j + 1], in1=cv[:],
                                                               op0=ALU.mult, op1=ALU.add),
                     reads=[rawc.r, cw_sb.r, cv.r], writes=[cv.r])
            S.op("act", lambda: A.activation(out=dst[:], in_=cv[:], func=AF.Silu), reads=[cv.r], writes=[dst.r])

        def ml_o(wb, dst):
            for c in range(4):
                pm = psb.next()
                proj_fm(wb, c, pm)
                S.op("act", lambda: A.activation(out=dst[:, c * 512:(c + 1) * 512], in_=pm[:], func=AF.Sigmoid),
                     reads=[pm.r], writes=[dst.r])

        def attend(nd, ndv, row, is_fox, mrow, out_chunks, hgc0):
            mixo = [mixo_r.next() for _ in range(ndv)]
            for c in range(4):
                kb = kb_r.next()
                if is_fox:
                    S.op("dve", lambda: V.tensor_scalar(out=kb[:], in0=Ltok[:, :, row],
                                                        scalar1=LrefB[:, c * 16 + row:c * 16 + row + 1],
                                                        scalar2=None, op0=ALU.subtract),
                         reads=[Ltok.r, LrefB.r], writes=[kb.r])
                    qs = [qTr[d][:, c * 512:(c + 1) * 512] for d in range(nd)]
                    qres = [qTr[d].r for d in range(nd)]
                else:
                    S.op("dve", lambda: V.scalar_tensor_tensor(out=ktmp[:, 0:4 * c + 4], in0=Ltok[:, 0:4 * c + 4, row],
                                                               scalar=LrefB[:, c * 16 + row:c * 16 + row + 1],
                                                               in1=Gtok[:, 0:4 * c + 4, 12 + mrow],
                                                               op0=ALU.subtract, op1=ALU.add),
                         reads=[Ltok.r, LrefB.r, Gtok.r], writes=[ktmp.r])
                    S.op("act", lambda: A.activation(out=kb[:, 0:4 * c + 4], in_=ktmp[:, 0:4 * c + 4], func=AF.Exp, bias=lncol[:, 0:1]),
                         reads=[ktmp.r, lncol.r], writes=[kb.r])
                    pe_ = psb.next()
                    S.op("pe", lambda: P.matmul(pe_[:], lhsT=selm[0:12, mrow * 128:(mrow + 1) * 128],
                                                rhs=EQ[0:12, c * 512:(c + 1) * 512], start=True, stop=True),
                         reads=[selm.r, EQ.r], writes=[pe_.r])
                    qs, qres = [], []
                    for d in range(nd):
                        qp = qpr[d].next()
                        S.op("dve", lambda d=d, qp=qp: V.tensor_tensor(out=qp[:], in0=qTr[d][:, c * 512:(c + 1) * 512],
                                                                       in1=pe_[:], op=ALU.mult),
                             reads=[qTr[d].r, pe_.r], writes=[qp.r])
                        qs.append(qp[:])
                        qres.append(qp.r)
                pO = [acc_ps[d] for d in range(ndv)]
                pD = acc_ps[2]
                nj = 4 * c + 4

                def emit_pv(j, lo, pt):
                    for dv in range(ndv):
                        S.op("pe", lambda dv=dv: P.matmul(pO[dv][:, lo:512], lhsT=vtok[:, j, dv * 128:(dv + 1) * 128],
                                                          rhs=pt[:, lo:512], start=(j == 0), stop=(j == nj - 1)),
                             reads=[vtok.r, pt.r], writes=[pO[dv].r], inc=False)
                    S.op("pe", lambda: P.matmul(pD[:, lo:512], lhsT=ones_b[:], rhs=pt[:, lo:512],
                                                start=(j == 0), stop=(j == nj - 1)),
                         reads=[ones_b.r, pt.r], writes=[pD.r])

                prevpv = None
                for j in range(nj):
                    lo = 128 * (j - 4 * c) if j >= 4 * c else 0
                    pS = psb.next()
                    for d in range(nd):
                        S.op("pe", lambda d=d: P.matmul(pS[:, lo:512], lhsT=kTr[d][:, j * 128:(j + 1) * 128],
                                                        rhs=qs[d][:, lo:512], start=(d == 0), stop=(d == nd - 1)),
                             reads=[kTr[d].r, qres[d]], writes=[pS.r], inc=(d == nd - 1))
                    pt = pt_r.next()
                    if is_fox:
                        S.op("act", lambda: A.activation(out=pt[:, lo:512], in_=pS[:, lo:512], func=AF.Exp,
                                                         bias=kb[:, j:j + 1]),
                             reads=[pS.r, kb.r], writes=[pt.r])
                    else:
                        S.op("dve", lambda: V.tensor_scalar(out=pt[:, lo:512], in0=pS[:, lo:512],
                                                            scalar1=kb[:, j:j + 1], scalar2=None, op0=ALU.mult),
                             reads=[pS.r, kb.r], writes=[pt.r])
                    if j >= 4 * c:
                        S.op("pool", lambda: G.tensor_tensor(out=pt[:, lo:lo + 128], in0=pt[:, lo:lo + 128],
                                                             in1=tri_b[:], op=ALU.mult),
                             reads=[pt.r, tri_b.r], writes=[pt.r])
                    if prevpv is not None:
                        emit_pv(*prevpv)
                    prevpv = (j, lo, pt)
                emit_pv(*prevpv)
                rs = rs_r.next()
                if is_fox:
                    S.op("dve", lambda: V.reciprocal(out=rs[:], in_=pD[:]), reads=[pD.r], writes=[rs.r])
                    S.op("dve", lambda: V.tensor_tensor(out=mixo[0][:, c * 512:(c + 1) * 512], in0=pO[0][:],
                                                        in1=rs[:], op=ALU.mult),
                         reads=[pO[0].r, rs.r], writes=[mixo[0].r])
                else:
                    S.op("dve", lambda: V.tensor_scalar(out=rs[:], in0=pD[:], scalar1=-1.0, scalar2=1.0,
                                                        op0=ALU.mult, op1=ALU.max), reads=[pD.r], writes=[rs.r])
                    S.op("dve", lambda: V.scalar_tensor_tensor(out=rs[:], in0=pD[:], scalar=1.0, in1=rs[:],
                                                               op0=ALU.max, op1=ALU.max),
                         reads=[pD.r, rs.r], writes=[rs.r])
                    S.op("dve", lambda: V.reciprocal(out=rs[:], in_=rs[:]), reads=[rs.r], writes=[rs.r])
                    hTs = []
                    pn = psb.next()
                    for dv in range(ndv):
                        hT = hT_r[dv].next()
                        S.op("dve", lambda dv=dv, hT=hT: V.tensor_tensor(out=hT[:], in0=pO[dv][:], in1=rs[:], op=ALU.mult),
                             reads=[pO[dv].r, rs.r], writes=[hT.r])
                        sqb = sqb_r.next()
                        S.op("act", lambda hT=hT, sqb=sqb: A.activation(out=sqb[:], in_=hT[:], func=AF.Square),
                             reads=[hT.r], writes=[sqb.r])
                        S.op("pe", lambda dv=dv, sqb=sqb: P.matmul(pn[:], lhsT=ones_b[:], rhs=sqb[:],
                                                                    start=(dv == 0), stop=(dv == ndv - 1)),
                             reads=[ones_b.r, sqb.r], writes=[pn.r], inc=(dv == ndv - 1))
                        hTs.append(hT)
                    rs2 = rs_r.next()
                    S.op("dve", lambda: V.tensor_scalar(out=rs2[:], in0=pn[:], scalar1=1.0 / 256, scalar2=EPS,
                                                        op0=ALU.mult, op1=ALU.add), reads=[pn.r], writes=[rs2.r])
                    S.op("act", lambda: A.activation(out=rs2[:], in_=rs2[:], func=AF.Sqrt), reads=[rs2.r], writes=[rs2.r])
                    S.op("dve", lambda: V.reciprocal(out=rs2[:], in_=rs2[:]), reads=[rs2.r], writes=[rs2.r])
                    for dv in range(ndv):
                        S.op("dve", lambda dv=dv: V.scalar_tensor_tensor(
                            out=hTs[dv][:], in0=hTs[dv][:], scalar=hg_sb[:, hgc0 + dv:hgc0 + dv + 1], in1=rs2[:],
                            op0=ALU.mult, op1=ALU.mult), reads=[hTs[dv].r, hg_sb.r, rs2.r], writes=[hTs[dv].r])
                        S.op("dve", lambda dv=dv: V.tensor_tensor(
                            out=mixo[dv][:, c * 512:(c + 1) * 512], in0=hTs[dv][:],
                            in1=sigo[dv][:, c * 512:(c + 1) * 512], op=ALU.mult),
                            reads=[hTs[dv].r, sigo[dv].r], writes=[mixo[dv].r])
            for dv in range(ndv):
                S.dma("sp", mix_d[out_chunks[dv]], mixo[dv][:], reads=[mixo[dv].r],
                      writes=[mix_res[out_chunks[dv]]])

        for h in range(NFOX):
            fox_qk(load_wchunk(wfm_d[3 * h]), qTr[0], 0)
            fox_qk(load_wchunk(wfm_d[3 * h + 1]), kTr[0], 1)
            v_tok(load_wchunk(wfm_d[3 * h + 2]), 0)
            attend(1, 1, h, True, 0, [h], 0)
        for m in range(NML):
            b0 = 24 + 8 * m
            ml_qk(load_wchunk(wfm_d[b0 + 0]), qTr[0], 2 * m)
            ml_qk(load_wchunk(wfm_d[b0 + 1]), qTr[1], 2 * m + 1)
            ml_qk(load_wchunk(wfm_d[b0 + 2]), kTr[0], 8 + 2 * m)
            ml_qk(load_wchunk(wfm_d[b0 + 3]), kTr[1], 8 + 2 * m + 1)
            v_tok(load_wchunk(wfm_d[b0 + 4]), 0)
            v_tok(load_wchunk(wfm_d[b0 + 5]), 1)
            ml_o(load_wchunk(wfm_d[b0 + 6]), sigo[0])
            ml_o(load_wchunk(wfm_d[b0 + 7]), sigo[1])
            attend(2, 2, 8 + m, False, m, [8 + 2 * m, 8 + 2 * m + 1], 2 * m)
        S.barrier()
        es_m.close()

        es_o = ExitStack()
        mixT = C.sb([128, KC, T], BF16, "mixT", es_o)
        written = list(range(NFOX)) + [8 + j for j in range(2 * NML)]
        if len(written) < KC:
            S.op("pool", lambda: G.memset(mixT[:], 0.0), writes=[mixT.r])
        for kc in written:
            S.dma("sp" if kc % 2 == 0 else "act", mixT[:, kc, :], mix_d[kc], reads=[mix_res[kc]], writes=[mixT.r])
        gate1b = C.sb([128, D], F32, "gate1b", es_o)
        build_gate(gate1b, 32)
        wo_st = C.sb([128, KC, 512], F32, "wo_st", es_o)
        wo_bf = C.sbring(2, [128, KC, 512], BF16, "wo_bf", es_o)
        xs_r = C.sbring(3, [128, 512], F32, "xs", es_o)
        t1_r = C.sbring(3, [128, 512], F32, "t1", es_o)
        for cb in range(4):
            S.dma("sp", wo_st[:], wout_d[cb], writes=[wo_st.r])
            wob = wo_bf.next()
            S.op("pool", lambda: G.tensor_copy(out=wob[:], in_=wo_st[:]), reads=[wo_st.r], writes=[wob.r])
            for i in range(NT):
                pm = psb.next()
                for kc in range(KC):
                    S.op("pe", lambda kc=kc: P.matmul(pm[:], lhsT=mixT[:, kc, i * 128:(i + 1) * 128], rhs=wob[:, kc, :],
                                                      start=(kc == 0), stop=(kc == KC - 1)),
                         reads=[mixT.r, wob.r], writes=[pm.r], inc=(kc == KC - 1))
                xs = xs_r.next()
                S.dma("act", xs[:], x_d[i * 128:(i + 1) * 128, cb * 512:(cb + 1) * 512], writes=[xs.r])
                t1 = t1_r.next()
                S.op("dve", lambda: V.tensor_tensor(out=t1[:], in0=pm[:], in1=gate1b[:, cb * 512:(cb + 1) * 512],
                                                    op=ALU.mult), reads=[pm.r, gate1b.r], writes=[t1.r])
                S.op("pool", lambda: G.tensor_tensor(out=t1[:], in0=t1[:], in1=xs[:], op=ALU.add),
                     reads=[t1.r, xs.r], writes=[t1.r])
                S.dma("sp", out_d[i * 128:(i + 1) * 128, cb * 512:(cb + 1) * 512], t1[:], reads=[t1.r],
                      writes=[out_res[i]])
        S.barrier()
        es_o.close()

        es_p = ExitStack()
        psb = Ring(psb.bufs + acc_ps)
        wst = C.sbring(2, [128, KC, 128], F32, "wstp", es_p)
        wbf = C.sbring(2, [128, KC, 128], BF16, "wbfp", es_p)
        gate2b = C.sb([128, D], F32, "gate2b", es_p)
        build_gate(gate2b, 80)
        h2T = C.sb([128, KC, 512], BF16, "h2T", es_p)
        qT = C.sb([128, 16, 512], BF16, "qTp", es_p)
        acc = C.sb([128, 4, D], F32, "acc", es_p)
        Cb = C.sb([128, 8, 512], BF16, "Cb", es_p)
        statsT = C.sb([32, 512], BF16, "statsT", es_p)
        selT = C.sb([32, 8 * 128], BF16, "selT", es_p)
        selg = C.sb([32, 8 * 128], BF16, "selg", es_p)
        ucol = C.sb([32, 8], F32, "ucol", es_p)
        for h in range(8):
            S.op("dve", lambda h=h: V.tensor_tensor(out=ucol[:, h:h + 1], in0=ident_f[0:32, h:h + 1],
                                                    in1=ident_f[0:32, 8 + h:9 + h], op=ALU.add),
                 reads=[ident_f.r, ucol.r], writes=[ucol.r])
            S.op("dve", lambda h=h: V.scalar_tensor_tensor(out=ucol[:, h:h + 1], in0=ucol[:, h:h + 1], scalar=-1.0,
                                                           in1=ident_f[0:32, 16 + h:17 + h],
                                                           op0=ALU.mult, op1=ALU.subtract),
                 reads=[ident_f.r, ucol.r], writes=[ucol.r])
            S.op("dve", lambda h=h: V.tensor_scalar(out=selT[:, h * 128:(h + 1) * 128], in0=ones_f[0:32, :],
                                                    scalar1=ucol[:, h:h + 1], scalar2=None, op0=ALU.mult),
                 reads=[ones_f.r, ucol.r], writes=[selT.r])
            S.op("dve", lambda h=h: V.tensor_scalar(out=selg[:, h * 128:(h + 1) * 128], in0=ones_f[0:32, :],
                                                    scalar1=ident_f[0:32, 24 + h:25 + h], scalar2=None, op0=ALU.mult),
                 reads=[ones_f.r, ident_f.r], writes=[selg.r])
        k1bc_r = C.sbring(2, [128, 128], BF16, "k1bc", es_p)
        eu_st = C.sbring(2, [128, D], F32, "eu_st", es_p)
        eu_bf = C.sbring(GK + 2, [128, D], BF16, "eu_bf", es_p)
        wT_r = C.sbring(2 * GK, [128, 512], BF16, "wT", es_p)
        gA_r = C.sbring(2, [128, 512], BF16, "gA", es_p)
        E_r = C.sbring(6, [128, 512], BF16, "E", es_p)
        Mm2_r = C.sbring(3, [128, 2, 512], BF16, "Mm2", es_p)
        Tt2_r = C.sbring(4, [128, 2, 512], BF16, "Tt2", es_p)
        Ga2_r = C.sbring(2, [128, 2, 512], BF16, "Ga2", es_p)
        sc_r = C.sbring(1, [128, 256], F32, "sc", es_p)
        sc2_r = C.sbring(1, [128, 256], F32, "sc2", es_p)
        v12_r = C.sbring(2, [128, 32], F32, "v12", es_p)
        cand_r = C.sbring(1, [128, 256], F32, "cand", es_p)
        cand2_r = C.sbring(1, [128, 256], F32, "cand2", es_p)
        c16_r = C.sbring(2, [128, 16], F32, "c16", es_p)
        e16_r = C.sbring(2, [128, 16], F32, "e16", es_p)
        sm_r = C.sbring(2, [128, 4], F32, "sm", es_p)
        statf = C.sb([128, 48], F32, "statf", es_p)
        statb = C.sb([128, 32], BF16, "statb", es_p)

        for Q in range(NQ):
            es_n2 = ExitStack()
            norm_to_T(es_n2, lambda i: out_d[(Q * 4 + i) * 128:(Q * 4 + i + 1) * 128, :],
                      lambda i: out_res[Q * 4 + i], A2, 48, h2T, 4, 1)
            S.barrier()
            es_n2.close()
            for cc in range(16):
                st = wst.next()
                S.dma("sp" if cc % 2 == 0 else "act", st[:], wq_d[cc], writes=[st.r])
                wb = wbf.next()
                S.op("pool", lambda: G.tensor_copy(out=wb[:], in_=st[:]), reads=[st.r], writes=[wb.r])
                pm = psb.next()
                for kc in range(KC):
                    S.op("pe", lambda kc=kc: P.matmul(pm[:], lhsT=wb[:, kc, :], rhs=h2T[:, kc, :],
                                                      start=(kc == 0), stop=(kc == KC - 1)),
                         reads=[wb.r, h2T.r], writes=[pm.r], inc=(kc == KC - 1))
                S.op("act", lambda: A.copy(out=qT[:, cc, :], in_=pm[:]), reads=[pm.r], writes=[qT.r])
            for ti in range(4):
                for h in range(8):
                    pm = psb.next()
                    S.op("pe", lambda: P.matmul(pm[:, 0:128], lhsT=qT[:, 2 * h, ti * 128:(ti + 1) * 128],
                                                rhs=k1t_b[:], start=True, stop=True),
                         reads=[qT.r, k1t_b.r], writes=[pm.r], inc=False)
                    S.op("pe", lambda: P.matmul(pm[:, 128:256], lhsT=qT[:, 2 * h + 1, ti * 128:(ti + 1) * 128],
                                                rhs=k2t_b[:], start=True, stop=True),
                         reads=[qT.r, k2t_b.r], writes=[pm.r])
                    sc = sc_r.next()
                    sc2 = sc2_r.next()
                    v12 = v12_r.next()
                    S.op("act", lambda: A.copy(out=sc[:], in_=pm[:, 0:256]), reads=[pm.r], writes=[sc.r])
                    for half in range(2):
                        sl = slice(half * 128, (half + 1) * 128)
                        vo = half * 16
                        S.op("dve", lambda: V.max(out=v12[:, vo:vo + 8], in_=sc[:, sl]), reads=[sc.r, v12.r], writes=[v12.r])
                        S.op("dve", lambda: V.match_replace(out=sc2[:, sl], in_to_replace=v12[:, vo:vo + 8],
                                                            in_values=sc[:, sl], imm_value=NEG),
                             reads=[sc.r, v12.r, sc2.r], writes=[sc2.r])
                        S.op("dve", lambda: V.max(out=v12[:, vo + 8:vo + 16], in_=sc2[:, sl]),
                             reads=[sc2.r, v12.r], writes=[v12.r])
                    cand = cand_r.next()
                    cand2 = cand2_r.next()
                    c16 = c16_r.next()
                    e16 = e16_r.next()
                    sm = sm_r.next()
                    S.op("dve", lambda: V.tensor_tensor(
                        out=cand[:].rearrange("p (a b) -> p a b", a=16),
                        in0=v12[:, 0:16].unsqueeze(2).broadcast_to([128, 16, 16]),
                        in1=v12[:, 16:32].unsqueeze(1).broadcast_to([128, 16, 16]), op=ALU.add),
                        reads=[v12.r], writes=[cand.r])
                    S.op("dve", lambda: V.max(out=c16[:, 0:8], in_=cand[:]), reads=[cand.r, c16.r], writes=[c16.r])
                    S.op("dve", lambda: V.match_replace(out=cand2[:], in_to_replace=c16[:, 0:8], in_values=cand[:],
                                                        imm_value=NEG), reads=[cand.r, c16.r], writes=[cand2.r])
                    S.op("dve", lambda: V.max(out=c16[:, 8:16], in_=cand2[:]), reads=[cand2.r, c16.r], writes=[c16.r])
                    S.op("dve", lambda: V.tensor_scalar(out=sm[:, 0:1], in0=c16[:, 0:1], scalar1=-1.0, scalar2=None,
                                                        op0=ALU.mult), reads=[c16.r, sm.r], writes=[sm.r])
                    S.op("dve", lambda: V.memset(sm[:, 1:2], 0.0), reads=[sm.r], writes=[sm.r])
                    S.op("act", lambda: A.activation(out=e16[:], in_=c16[:], func=AF.Exp, bias=sm[:, 0:1],
                                                     accum_out=sm[:, 1:2]),
                         reads=[c16.r, sm.r], writes=[e16.r, sm.r])
                    S.op("dve", lambda: V.reciprocal(out=sm[:, 2:3], in_=sm[:, 1:2]), reads=[sm.r], writes=[sm.r])
                    S.op("dve", lambda: V.tensor_scalar(out=statf[:, h:h + 1], in0=c16[:, 15:16], scalar1=-3.0e-5,
                                                        scalar2=None, op0=ALU.add),
                         reads=[c16.r, statf.r], writes=[statf.r])
                    S.op("dve", lambda: V.tensor_tensor(out=statf[:, 32 + h:33 + h], in0=e16[:, 15:16], in1=sm[:, 2:3],
                                                        op=ALU.mult), reads=[e16.r, sm.r, statf.r], writes=[statf.r])
                S.op("dve", lambda: V.tensor_copy(out=statb[:, 0:8], in_=statf[:, 0:8]), reads=[statf.r, statb.r], writes=[statb.r])
                S.op("dve", lambda: V.tensor_tensor(out=statf[:, 8:16], in0=statf[:, 0:8], in1=statb[:, 0:8],
                                                    op=ALU.subtract), reads=[statf.r, statb.r], writes=[statf.r])
                S.op("dve", lambda: V.tensor_copy(out=statb[:, 8:16], in_=statf[:, 8:16]), reads=[statf.r, statb.r], writes=[statb.r])
                S.op("dve", lambda: V.tensor_tensor(out=statf[:, 16:24], in0=statf[:, 8:16], in1=statb[:, 8:16],
                                                    op=ALU.subtract), reads=[statf.r, statb.r], writes=[statf.r])
                S.op("dve", lambda: V.tensor_copy(out=statb[:, 16:24], in_=statf[:, 16:24]), reads=[statf.r, statb.r], writes=[statb.r])
                S.op("dve", lambda: V.tensor_copy(out=statb[:, 24:32], in_=statf[:, 32:40]), reads=[statf.r, statb.r], writes=[statb.r])
                pm = psb.next()
                pmb = pm.t.bitcast(BF16)
                S.op("pe", lambda: P.transpose(out=pmb[0:32, 0:128], in_=statb[:, 0:32], identity=ident_b[:]),
                     reads=[statb.r, ident_b.r], writes=[pm.r])
                S.op("act", lambda: A.copy(out=statsT[:, ti * 128:(ti + 1) * 128], in_=pmb[0:32, 0:128]),
                     reads=[pm.r], writes=[statsT.r])
            for h in range(8):
                pm = psb.next()
                S.op("pe", lambda: P.matmul(pm[:], lhsT=selg[:, h * 128:(h + 1) * 128], rhs=statsT[:],
                                            start=True, stop=True), reads=[selg.r, statsT.r], writes=[pm.r])
                S.op("act", lambda: A.copy(out=Cb[:, h, :], in_=pm[:]), reads=[pm.r], writes=[Cb.r])

            grp = []
            ngrp = 0
            pending = []

            def emit_units(n):
                for _ in range(min(n, len(pending))):
                    g_, ti, cbk, first = pending.pop(0)
                    pm = psb.next()
                    for gi, (wT_, eub_) in enumerate(g_):
                        S.op("pe", lambda gi=gi, wT_=wT_, eub_=eub_: P.matmul(
                            pm[:], lhsT=wT_[:, ti * 128:(ti + 1) * 128], rhs=eub_[:, cbk * 512:(cbk + 1) * 512],
                            start=(gi == 0), stop=(gi == len(g_) - 1)),
                            reads=[wT_.r, eub_.r], writes=[pm.r], inc=(gi == len(g_) - 1))
                    if first:
                        S.op("act", lambda: A.copy(out=acc[:, ti, cbk * 512:(cbk + 1) * 512], in_=pm[:]),
                             reads=[pm.r], writes=[acc.r])
                    else:
                        S.op("dve", lambda: V.tensor_tensor(out=acc[:, ti, cbk * 512:(cbk + 1) * 512],
                                                            in0=pm[:], in1=acc[:, ti, cbk * 512:(cbk + 1) * 512],
                                                            op=ALU.add), reads=[pm.r, acc.r], writes=[acc.r])

            prepped = {}

            def prep(e1):
                st = wst.next()
                S.dma("sp", st[:], edt_d[e1], writes=[st.r])
                edb = wbf.next()
                S.op("act", lambda: A.copy(out=edb[:], in_=st[:]), reads=[st.r], writes=[edb.r])
                es_ = eu_st.next()
                S.dma("sp", es_[:], eu_d[e1], writes=[es_.r])
                eub = eu_bf.next()
                S.op("act", lambda: A.copy(out=eub[:], in_=es_[:]), reads=[es_.r], writes=[eub.r])
                k1bc = k1bc_r.next()
                S.op("pool", lambda: G.tensor_copy(out=k1bc[:], in_=k1t_b[:, e1:e1 + 1].broadcast_to([128, 128])),
                     reads=[k1t_b.r], writes=[k1bc.r])
                prepped[e1] = (edb, eub, k1bc)

            prep(0)
            for e1 in range(NE1):
                if e1 + 1 < NE1:
                    prep(e1 + 1)
                edb, eub, k1bc = prepped.pop(e1)
                pA = psb.next()
                for kc in range(KC):
                    S.op("pe", lambda kc=kc: P.matmul(pA[:], lhsT=edb[:, kc, :], rhs=h2T[:, kc, :],
                                                      start=(kc == 0), stop=(kc == KC - 1)),
                         reads=[edb.r, h2T.r], writes=[pA.r], inc=(kc == KC - 1))
                gA = gA_r.next()
                S.op("act", lambda: A.activation(out=gA[:], in_=pA[:], func=AF.Gelu), reads=[pA.r], writes=[gA.r])
                Ga2 = Ga2_r.next()
                for hp in range(4):
                    Mm2 = Mm2_r.next()
                    for hh in range(2):
                        h = 2 * hp + hh
                        pX = psb.next()
                        S.op("pe", lambda: P.matmul(pX[:], lhsT=k2t_b[:], rhs=qT[:, 2 * h + 1, :], start=True, stop=False),
                             reads=[k2t_b.r, qT.r], writes=[pX.r], inc=False)
                        S.op("pe", lambda: P.matmul(pX[:], lhsT=k1bc[:], rhs=qT[:, 2 * h, :], start=False, stop=False),
                             reads=[k1bc.r, qT.r], writes=[pX.r], inc=False)
                        S.op("pe", lambda: P.matmul(pX[:], lhsT=selT[:, h * 128:(h + 1) * 128], rhs=statsT[:],
                                                    start=False, stop=True),
                             reads=[selT.r, statsT.r], writes=[pX.r])
                        E = E_r.next()
                        S.op("act", lambda: A.activation(out=E[:], in_=pX[:], func=AF.Exp), reads=[pX.r], writes=[E.r])
                        S.op("dve", lambda: V.scalar_tensor_tensor(out=Mm2[:, hh, :], in0=pX[:], scalar=0.0, in1=E[:],
                                                                   op0=ALU.is_ge, op1=ALU.mult),
                             reads=[pX.r, E.r, Mm2.r], writes=[Mm2.r])
                    if hp == 0:
                        S.op("dve", lambda: V.tensor_tensor(out=Ga2[:], in0=Mm2[:], in1=Cb[:, 0:2, :], op=ALU.mult),
                             reads=[Mm2.r, Cb.r], writes=[Ga2.r])
                    else:
                        Tt2 = Tt2_r.next()
                        S.op("dve", lambda: V.tensor_tensor(out=Tt2[:], in0=Mm2[:], in1=Cb[:, 2 * hp:2 * hp + 2, :],
                                                            op=ALU.mult), reads=[Mm2.r, Cb.r], writes=[Tt2.r])
                        S.op("pool", lambda: G.tensor_tensor(out=Ga2[:], in0=Ga2[:], in1=Tt2[:], op=ALU.add),
                             reads=[Ga2.r, Tt2.r], writes=[Ga2.r])
                S.op("pool", lambda: G.tensor_tensor(out=Ga2[:, 0, :], in0=Ga2[:, 0, :], in1=Ga2[:, 1, :], op=ALU.add),
                     reads=[Ga2.r], writes=[Ga2.r])
                wT = wT_r.next()
                S.op("dve", lambda: V.tensor_tensor(out=wT[:], in0=Ga2[:, 0, :], in1=gA[:], op=ALU.mult),
                     reads=[Ga2.r, gA.r], writes=[wT.r])
                grp.append((wT, eub))
                emit_units(16)
                if len(grp) == GK or e1 == NE1 - 1:
                    for ti in range(4):
                        for cbk in range(4):
                            pending.append((list(grp), ti, cbk, ngrp == 0))
                    grp = []
                    ngrp += 1
            emit_units(len(pending))
            es_f = ExitStack()
            x1_r = C.sbring(1, [128, D], F32, "x1t", es_f)
            for ti in range(4):
                i = Q * 4 + ti
                x1 = x1_r.next()
                S.dma("sp", x1[:], out_d[i * 128:(i + 1) * 128, :], reads=[out_res[i]], writes=[x1.r])
                S.op("dve", lambda: V.tensor_tensor(out=acc[:, ti, :], in0=acc[:, ti, :], in1=gate2b[:], op=ALU.mult),
                     reads=[acc.r, gate2b.r], writes=[acc.r])
                S.op("pool", lambda: G.tensor_tensor(out=acc[:, ti, :], in0=acc[:, ti, :], in1=x1[:], op=ALU.add),
                     reads=[acc.r, x1.r], writes=[acc.r])
                S.dma("sp", out_d[i * 128:(i + 1) * 128, :], acc[:, ti, :], reads=[acc.r, out_res[i]], writes=[out_res[i]])
            S.barrier()
            es_f.close()
        S.barrier()
        es_p.close()

        for q in ("sp", "pool", "act"):
            for sem, val in S.dma_slots[q]:
                if val > 0:
                    S._wait("sp", (sem, val))
        print("instructions:", S.ninstr)
    return nc


def _host_layouts(inp):
    f = lambda a: np.ascontiguousarray(a, dtype=np.float32)
    L = {}
    w_ada = inp["w_ada"][0]
    L["wada_r"] = f(w_ada.reshape(KC, 128, 24, 512).transpose(2, 1, 0, 3))
    L["bada_r"] = f(inp["b_ada"][0].reshape(96, 128).T)
    L["g1_r"] = f(inp["norm1_gain"][0].reshape(KC, 128).T)
    L["g2_r"] = f(inp["norm2_gain"][0].reshape(KC, 128).T)
    w_in = inp["w_in"][0]
    o_fq, o_fk, o_fv, o_ff, o_mq, o_mk, o_mv, o_mo, o_mi, o_mf = 0, 1024, 2048, 3072, 3080, 4104, 5128, 6152, 7176, 7180
    cols = []
    for h in range(8):
        cols += [o_fq + 128 * h, o_fk + 128 * h, o_fv + 128 * h]
    for m in range(4):
        cols += [o_mq + 256 * m, o_mq + 256 * m + 128, o_mk + 256 * m, o_mk + 256 * m + 128,
                 o_mv + 256 * m, o_mv + 256 * m + 128, o_mo + 256 * m, o_mo + 256 * m + 128]
    wfm = np.empty((56, 128, KC, 128), np.float32)
    for i, c0 in enumerate(cols):
        wfm[i] = w_in[:, c0:c0 + 128].reshape(KC, 128, 128).transpose(1, 0, 2)
    L["wfm_r"] = wfm
    gcols = list(range(o_ff, o_ff + 8)) + list(range(o_mf, o_mf + 4)) + list(range(o_mi, o_mi + 4))
    L["wg_r"] = f(w_in[:, gcols].reshape(KC, 128, 16).transpose(1, 0, 2))
    L["gb_r"] = f(np.concatenate([inp["fox_f_bias"][0], inp["mlstm_f_bias"][0], inp["mlstm_i_bias"][0]]).reshape(16, 1))
    L["qkg_r"] = f(np.stack([inp["fox_q_gain"][0], inp["fox_k_gain"][0]], axis=1))
    L["convw_r"] = f(inp["mlstm_conv_w"][0].reshape(4, 16, 128).transpose(2, 1, 0))
    L["convb_r"] = f(inp["mlstm_conv_b"][0].reshape(16, 128).T)
    L["hg_r"] = f(inp["mlstm_head_gain"][0].reshape(8, 128).T)
    L["wout_r"] = f(inp["w_out"][0].reshape(KC, 128, 4, 512).transpose(2, 1, 0, 3))
    L["wq_r"] = f(inp["peer_w_query"][0].reshape(KC, 128, 16, 128).transpose(2, 1, 0, 3))
    L["k1t_r"] = f(inp["peer_sub_keys_1"][0].T)
    L["k2t_r"] = f(inp["peer_sub_keys_2"][0].T)
    ed = inp["peer_expert_down"][0]
    L["edt_r"] = f(ed.reshape(128, 128, KC, 128).transpose(0, 3, 2, 1))
    L["eu_r"] = f(inp["peer_expert_up"][0].reshape(128, 128, D))
    return L


def _core_inputs(inputs, b):
    return {
        "x": np.ascontiguousarray(inputs["x"][b], dtype=np.float32),
        "c_r": np.ascontiguousarray(inputs["c"][b].reshape(KC, 128).T, dtype=np.float32),
    }


def kernel(**inputs):
    inputs = {k: np.asarray(v) for k, v in inputs.items()}
    L = _host_layouts(inputs)
    nc = build_program()
    outs = []
    for g0 in range(0, 8, CORES_PER_LAUNCH):
        in_maps = []
        for b in range(g0, g0 + CORES_PER_LAUNCH):
            m = dict(L)
            m.update(_core_inputs(inputs, b))
            in_maps.append(m)
        res = run_bass_kernel_spmd(nc, in_maps, core_ids=list(range(CORES_PER_LAUNCH)))
        outs += [np.asarray(r["out"], dtype=np.float32) for r in res.results]
    return np.stack(outs, axis=0)
```

```python
from contextlib import ExitStack
import numpy as np
import concourse.bass as bass
import concourse.mybir as mybir
from concourse.bass_utils import run_bass_kernel_spmd

F32 = mybir.dt.float32
BF16 = mybir.dt.bfloat16
ALU = mybir.AluOpType
AF = mybir.ActivationFunctionType
AX = mybir.AxisListType

import os
NBLK = int(os.environ.get("NBLK", "24"))
ADAQ = os.environ.get("ADAQ", "pool")
D = 2048
T = 2048
KC = 16
NT = 16
EPS = 1e-6


class Res:
    __slots__ = ("name", "w", "r")

    def __init__(self, name=""):
        self.name = name
        self.w = None
        self.r = []


class Sched:
    SEM_LIMIT = 30000

    def __init__(self, nc, es):
        self.nc = nc
        self.es = es
        self.engs = {"pe": nc.tensor, "act": nc.scalar, "dve": nc.vector,
                     "pool": nc.gpsimd, "sp": nc.sync}
        self.sem = {}
        self.cnt = {}
        self.nsem = 0
        self.pe_sems = []
        self.pend = {}
        for e in ("pe", "act", "dve", "pool"):
            self._new_sem(e)
        self.waited = {e: {} for e in self.engs}
        self.dma_slots = {}
        self.dma_next = {}
        for q, n in (("sp", 8), ("pool", 2), ("act", 4)):
            self.dma_slots[q] = [[self._mk(f"d{q}{i}"), 0] for i in range(n)]
            self.dma_next[q] = 0
        self.ninstr = 0

    def _mk(self, name):
        self.nsem += 1
        return self.es.enter_context(self.nc.semaphore(f"{name}_{self.nsem}"))

    def _new_sem(self, e):
        self.sem[e] = self._mk(f"s{e}")
        self.cnt[e] = 0
        if e == "pe":
            self.pe_sems.append(self.sem[e])

    def _wait(self, e, tok):
        sem, val = tok
        if e == "pe" and sem in self.pe_sems:
            return
        key = id(sem)
        if self.waited[e].get(key, 0) >= val:
            return
        self.engs[e].wait_ge(sem, val)
        self.waited[e][key] = val

    def _deps(self, e, reads, writes):
        toks = []
        for r in reads:
            if r.w is not None:
                toks.append(r.w)
        for w in writes:
            if w.w is not None:
                toks.append(w.w)
            toks.extend(w.r)
        for t in toks:
            self._wait(e, t)

    def _mark(self, tok, reads, writes):
        for w in writes:
            w.w = tok
            w.r = []
        for r in reads:
            if r in writes:
                continue
            r.r.append(tok)
            if len(r.r) > 24:
                r.r = r.r[-24:]

    def op(self, e, fn, reads=(), writes=(), inc=True):
        self._deps(e, reads, writes)
        if not self.pend.get(e, False) and self.cnt[e] >= self.SEM_LIMIT:
            self._new_sem(e)
        self.pend[e] = not inc
        ins = fn()
        self.ninstr += 1
        if inc:
            ins.then_inc(self.sem[e], 1)
            self.cnt[e] += 1
            tok = (self.sem[e], self.cnt[e])
            self._pending_ok = True
        else:
            tok = (self.sem[e], self.cnt[e] + 1)
        self._mark(tok, reads, writes)
        return tok

    def dma(self, q, out, in_, reads=(), writes=(), **kw):
        slots = self.dma_slots[q]
        i = self.dma_next[q]
        self.dma_next[q] = (i + 1) % len(slots)
        sem, val = slots[i]
        if val > 0:
            self._wait(q, (sem, val))
        self._deps(q, reads, writes)
        ins = self.engs[q].dma_start(out=out, in_=in_, **kw)
        ins.then_inc(sem, 16)
        slots[i][1] = val + 16
        tok = (sem, val + 16)
        self._mark(tok, reads, writes)
        self.ninstr += 1
        return tok

    def barrier(self):
        toks = []
        for e in ("pe", "act", "dve", "pool"):
            if self.cnt[e] > 0:
                assert not self.pend.get(e, False), f"open group on {e} at barrier"
                toks.append((self.sem[e], self.cnt[e]))
        for q in self.dma_slots:
            for sem, val in self.dma_slots[q]:
                if val > 0:
                    toks.append((sem, val))
        for e in self.engs:
            for t in toks:
                self._wait(e, t)

    def wait_all(self, e, ress):
        for r in ress:
            if r.w is not None:
                self._wait(e, r.w)


class Buf:
    def __init__(self, t, name):
        self.t = t
        self.r = Res(name)

    def __getitem__(self, idx):
        return self.t[idx]


class Ring:
    def __init__(self, bufs):
        self.bufs = bufs
        self.i = 0

    def next(self):
        b = self.bufs[self.i]
        self.i = (self.i + 1) % len(self.bufs)
        return b


class Ctx:
    def __init__(self, nc, es):
        self.nc = nc
        self.es = es
        self.S = Sched(nc, es)
        self.n = 0

    def sb(self, shape, dt, name, es=None):
        self.n += 1
        t = (es or self.es).enter_context(self.nc.sbuf_tensor(f"{name}_{self.n}", list(shape), dt))
        return Buf(t, name)

    def ps(self, shape, dt, name, es=None):
        self.n += 1
        t = (es or self.es).enter_context(self.nc.psum_tensor(f"{name}_{self.n}", list(shape), dt))
        return Buf(t, name)

    def sbring(self, n, shape, dt, name, es=None):
        return Ring([self.sb(shape, dt, f"{name}{i}", es) for i in range(n)])


NE1 = int(os.environ.get("NE1", "128"))
NQ = int(os.environ.get("NQ", "4"))
NFOX = int(os.environ.get("NFOX", "8"))
NML = int(os.environ.get("NML", "4"))
GK = 4
NEG = -1.0e30
CORES_PER_LAUNCH = 8


def build_program(stage=99, dbg=False):
    nc = bass.Bass("TRN2", target_bir_lowering=False)

    def din(name, shape, dt=F32):
        return nc.dram_tensor(name, list(shape), dt, kind="ExternalInput").ap()

    x_d = din("x", [T, D])
    c_d = din("c_r", [128, KC])
    wada_d = din("wada_r", [24, 128, KC, 512])
    bada_d = din("bada_r", [128, 96])
    g1_d = din("g1_r", [128, KC])
    g2_d = din("g2_r", [128, KC])
    wfm_d = din("wfm_r", [56, 128, KC, 128])
    wg_d = din("wg_r", [128, KC, 16])
    gb_d = din("gb_r", [16, 1])
    qkg_d = din("qkg_r", [128, 2])
    cw_d = din("convw_r", [128, 16, 4])
    cb_d = din("convb_r", [128, 16])
    hg_d = din("hg_r", [128, 8])
    wout_d = din("wout_r", [4, 128, KC, 512])
    wq_d = din("wq_r", [16, 128, KC, 128])
    k1t_d = din("k1t_r", [128, 128])
    k2t_d = din("k2t_r", [128, 128])
    edt_d = din("edt_r", [128, 128, KC, 128])
    eu_d = din("eu_r", [128, 128, D])
    out_d = nc.dram_tensor("out", [T, D], F32, kind="ExternalOutput").ap()
    mix_d = nc.dram_tensor("mix_scr", [KC, 128, T], BF16, kind="Internal").ap()
    if dbg:
        dbg_d = nc.dram_tensor("dbg", [128, 4096], F32, kind="ExternalOutput").ap()

    with ExitStack() as es:
        C = Ctx(nc, es)
        S = C.S
        V, A, P, G = nc.vector, nc.scalar, nc.tensor, nc.gpsimd

        psb = Ring([C.ps([128, 512], F32, f"ps{i}") for i in range(4)])
        acc_ps = [C.ps([128, 512], F32, f"pacc{i}") for i in range(4)]
        out_res = [Res(f"out{i}") for i in range(NT)]
        mix_res = [Res(f"mix{i}") for i in range(KC)]

        ident_f = C.sb([128, 128], F32, "ident_f")
        ident_b = C.sb([128, 128], BF16, "ident_b")
        ones_f = C.sb([128, 128], F32, "ones_f")
        ones_b = C.sb([128, 128], BF16, "ones_b")
        sel127 = C.sb([128, 128], F32, "sel127")
        tri_b = C.sb([128, 128], BF16, "tri_b")
        zcol = C.sb([128, 1], F32, "zcol")
        lncol = C.sb([128, 1], F32, "lncol")
        S.op("pool", lambda: G.memset(ones_f[:], 1.0), writes=[ones_f.r])
        S.op("pool", lambda: G.memset(ones_b[:], 1.0), writes=[ones_b.r])
        S.op("pool", lambda: G.memset(zcol[:], 0.0), writes=[zcol.r])
        S.op("pool", lambda: G.memset(lncol[:], float(np.log(1.0 / 16.0))), writes=[lncol.r])
        S.op("pool", lambda: G.affine_select(out=ident_f[:], in_=ones_f[:], pattern=[[-1, 128]],
                                             compare_op=ALU.is_equal, fill=0.0, base=0,
                                             channel_multiplier=1),
             reads=[ones_f.r], writes=[ident_f.r])
        S.op("pool", lambda: G.tensor_copy(out=ident_b[:], in_=ident_f[:]),
             reads=[ident_f.r], writes=[ident_b.r])
        S.op("pool", lambda: G.affine_select(out=sel127[:], in_=ones_f[:], pattern=[[0, 128]],
                                             compare_op=ALU.is_equal, fill=0.0, base=-127,
                                             channel_multiplier=1),
             reads=[ones_f.r], writes=[sel127.r])
        S.op("pool", lambda: G.affine_select(out=tri_b[:], in_=ones_b[:], pattern=[[1, 128]],
                                             compare_op=ALU.is_ge, fill=0.0, base=0,
                                             channel_multiplier=-1),
             reads=[ones_b.r], writes=[tri_b.r])

        def load_small(d_ap, shape, name, dt=F32):
            b = C.sb(shape, dt, name)
            S.dma("sp", b[:], d_ap, writes=[b.r])
            return b

        c_sb = load_small(c_d, [128, KC], "c_sb")
        bada_sb = load_small(bada_d, [128, 96], "bada")
        g1_sb = load_small(g1_d, [128, KC], "g1")
        g2_sb = load_small(g2_d, [128, KC], "g2")
        gb_sb = load_small(gb_d, [16, 1], "gb")
        qkg_sb = load_small(qkg_d, [128, 2], "qkg")
        cw_sb = load_small(cw_d, [128, 16, 4], "cw")
        cb_sb = load_small(cb_d, [128, 16], "cb")
        hg_sb = load_small(hg_d, [128, 8], "hg")
        k1t_f = load_small(k1t_d, [128, 128], "k1tf")
        k2t_f = load_small(k2t_d, [128, 128], "k2tf")
        wg_f = load_small(wg_d, [128, KC, 16], "wgf")
        k1t_b = C.sb([128, 128], BF16, "k1tb")
        k2t_b = C.sb([128, 128], BF16, "k2tb")
        wg_b = C.sb([128, KC, 16], BF16, "wgb")
        S.op("pool", lambda: G.tensor_copy(out=k1t_b[:], in_=k1t_f[:]), reads=[k1t_f.r], writes=[k1t_b.r])
        S.op("pool", lambda: G.tensor_copy(out=k2t_b[:], in_=k2t_f[:]), reads=[k2t_f.r], writes=[k2t_b.r])
        S.op("pool", lambda: G.tensor_copy(out=wg_b[:], in_=wg_f[:]), reads=[wg_f.r], writes=[wg_b.r])
        qsc = C.sb([128, 2], F32, "qsc")
        S.op("dve", lambda: V.tensor_scalar(out=qsc[:, 0:1], in0=qkg_sb[:, 0:1], scalar1=128.0 ** -0.5,
                                            scalar2=None, op0=ALU.mult), reads=[qkg_sb.r], writes=[qsc.r])
        S.op("dve", lambda: V.tensor_copy(out=qsc[:, 1:2], in_=qkg_sb[:, 1:2]), reads=[qkg_sb.r, qsc.r], writes=[qsc.r])

        sc_sb = C.sb([128, KC], F32, "sc_sb")
        mod = C.sb([128, 96], F32, "mod")
        S.op("act", lambda: A.activation(out=sc_sb[:], in_=c_sb[:], func=AF.Silu),
             reads=[c_sb.r], writes=[sc_sb.r])
        es_ada = ExitStack()
        wada_ring = C.sbring(2, [128, KC, 512], F32, "wada", es_ada)
        for jb in range(24):
            wb = wada_ring.next()
            S.dma("sp" if jb % 2 == 0 else "act", wb[:], wada_d[jb], writes=[wb.r])
            pm = psb.next()
            for jj in range(4):
                for kc in range(KC):
                    S.op("pe", lambda kc=kc, jj=jj, pm=pm, wb=wb: P.matmul(
                        pm[:, jj:jj + 1], lhsT=wb[:, kc, jj * 128:(jj + 1) * 128],
                        rhs=sc_sb[:, kc:kc + 1], start=(kc == 0), stop=(kc == KC - 1)),
                        reads=[wb.r, sc_sb.r], writes=[pm.r], inc=(kc == KC - 1 and jj == 3))
            S.op("dve", lambda jb=jb, pm=pm: V.tensor_tensor(
                out=mod[:, jb * 4:jb * 4 + 4], in0=pm[:, 0:4],
                in1=bada_sb[:, jb * 4:jb * 4 + 4], op=ALU.add),
                reads=[pm.r, bada_sb.r], writes=[mod.r])
        S.barrier()
        es_ada.close()
        A1 = C.sb([128, KC], F32, "A1")
        A2 = C.sb([128, KC], F32, "A2")
        S.op("dve", lambda: V.scalar_tensor_tensor(out=A1[:], in0=mod[:, 16:32], scalar=1.0, in1=g1_sb[:],
                                                   op0=ALU.add, op1=ALU.mult),
             reads=[mod.r, g1_sb.r], writes=[A1.r])
        S.op("dve", lambda: V.scalar_tensor_tensor(out=A2[:], in0=mod[:, 64:80], scalar=1.0, in1=g2_sb[:],
                                                   op0=ALU.add, op1=ALU.mult),
             reads=[mod.r, g2_sb.r], writes=[A2.r])

        dg = C.sb([128, 128], F32, "diag")

        def build_gate(gt, c0):
            for kc in range(KC):
                S.op("dve", lambda kc=kc: V.tensor_scalar(
                    out=dg[:], in0=ident_f[:], scalar1=mod[:, c0 + kc:c0 + kc + 1], scalar2=None,
                    op0=ALU.mult), reads=[ident_f.r, mod.r], writes=[dg.r])
                pm = psb.next()
                S.op("pe", lambda pm=pm: P.matmul(pm[:, 0:128], lhsT=ones_f[:], rhs=dg[:], start=True, stop=True),
                     reads=[ones_f.r, dg.r], writes=[pm.r])
                S.op("act", lambda pm=pm, kc=kc: A.copy(out=gt[:, kc * 128:(kc + 1) * 128], in_=pm[:, 0:128]),
                     reads=[pm.r], writes=[gt.r])

        def norm_to_T(es_l, src_rows, src_res, Asc, shift_c0, dstT, ntiles, nring):
            xr = C.sbring(nring, [128, D], F32, "xt", es_l)
            xn_r = C.sbring(nring, [128, D], BF16, "xn", es_l)
            ssr = C.sbring(2, [128, 2], F32, "ss", es_l)
            for i in range(ntiles):
                xt = xr.next()
                S.dma("sp", xt[:], src_rows(i), reads=[src_res(i)] if src_res else [], writes=[xt.r])
                ss = ssr.next()
                S.op("dve", lambda ss=ss: V.memset(ss[:], 0.0), writes=[ss.r])
                xn = xn_r.next()
                S.op("act", lambda xt=xt, ss=ss, xn=xn: A.activation(out=xn[:], in_=xt[:], func=AF.Square,
                                                                     accum_out=ss[:, 0:1]),
                     reads=[xt.r, ss.r], writes=[xn.r, ss.r])
                S.op("dve", lambda ss=ss: V.tensor_scalar(out=ss[:, 1:2], in0=ss[:, 0:1], scalar1=1.0 / D,
                                                          scalar2=EPS, op0=ALU.mult, op1=ALU.add),
                     reads=[ss.r], writes=[ss.r])
                S.op("act", lambda ss=ss: A.activation(out=ss[:, 1:2], in_=ss[:, 1:2], func=AF.Sqrt),
                     reads=[ss.r], writes=[ss.r])
                S.op("dve", lambda ss=ss: V.reciprocal(out=ss[:, 1:2], in_=ss[:, 1:2]),
                     reads=[ss.r], writes=[ss.r])
                S.op("act", lambda xt=xt, ss=ss, xn=xn: A.activation(out=xn[:], in_=xt[:], func=AF.Copy,
                                                                     scale=ss[:, 1:2]),
                     reads=[xt.r, ss.r], writes=[xn.r])
                for k4 in range(4):
                    pm = psb.next()
                    pmb = pm.t.bitcast(BF16)
                    for j in range(4):
                        kc = k4 * 4 + j
                        S.op("pe", lambda kc=kc, j=j, pmb=pmb, xn=xn: P.transpose(
                            out=pmb[:, j * 128:(j + 1) * 128], in_=xn[:, kc * 128:(kc + 1) * 128],
                            identity=ident_b[:]), reads=[xn.r, ident_b.r], writes=[pm.r], inc=(j == 3))
                    for j in range(4):
                        kc = k4 * 4 + j
                        S.op("dve", lambda kc=kc, j=j, pmb=pmb, i=i: V.tensor_scalar(
                            out=dstT[:, kc, i * 128:(i + 1) * 128], in0=pmb[:, j * 128:(j + 1) * 128],
                            scalar1=Asc[:, kc:kc + 1], scalar2=mod[:, shift_c0 + kc:shift_c0 + kc + 1],
                            op0=ALU.mult, op1=ALU.add),
                            reads=[pm.r, Asc.r, mod.r], writes=[dstT.r])

        es_m = ExitStack()
        wst = C.sbring(2, [128, KC, 128], F32, "wst", es_m)
        wbf = C.sbring(2, [128, KC, 128], BF16, "wbf", es_m)
        wq_flip = [0]

        def load_wchunk(d_ap):
            st = wst.next()
            q = "sp"
            wq_flip[0] += 1
            S.dma(q, st[:], d_ap, writes=[st.r])
            wb = wbf.next()
            S.op("pool", lambda: G.tensor_copy(out=wb[:], in_=st[:]), reads=[st.r], writes=[wb.r])
            return wb

        h1T = C.sb([128, KC, T], BF16, "h1T", es_m)
        Ltok = C.sb([128, NT, 16], F32, "Ltok", es_m)
        Gtok = C.sb([128, NT, 16], F32, "Gtok", es_m)
        LrefB = C.sb([128, 64], F32, "LrefB", es_m)
        EQ = C.sb([16, T], BF16, "EQ", es_m)
        selm = C.sb([16, 4 * 128], BF16, "selm", es_m)
        qTr = [C.sb([128, T], BF16, f"qT{i}", es_m) for i in range(2)]
        kTr = [C.sb([128, T], BF16, f"kT{i}", es_m) for i in range(2)]
        qpr = [C.sbring(2, [128, 512], BF16, f"qp{i}", es_m) for i in range(2)]
        vtok = C.sb([128, NT, 256], BF16, "vtok", es_m)
        sigo = [C.sb([128, T], BF16, f"sigo{i}", es_m) for i in range(2)]
        rawc = C.sb([128, T + 4], F32, "rawc", es_m)
        cv = C.sb([128, T], F32, "cv", es_m)
        rawf_r = C.sbring(2, [128, 512], F32, "rawf", es_m)
        sqb_r = C.sbring(2, [128, 512], BF16, "sqb", es_m)
        rs_r = C.sbring(2, [128, 512], F32, "rs", es_m)
        pt_r = C.sbring(3, [128, 512], BF16, "pt", es_m)
        kb_r = C.sbring(2, [128, NT], F32, "kb", es_m)
        ktmp = C.sb([128, NT], F32, "ktmp", es_m)
        hT_r = [C.sbring(1, [128, 512], F32, f"hT{i}", es_m) for i in range(2)]
        mixo_r = C.sbring(2, [128, T], BF16, "mixo", es_m)
        es_n1 = ExitStack()
        norm_to_T(es_n1, lambda i: x_d[i * 128:(i + 1) * 128, :], None, A1, 0, h1T, NT, 2)
        S.barrier()
        es_n1.close()

        es_g = ExitStack()
        graw = C.sb([16, T], F32, "graw", es_g)
        lsp = C.sb([16, T], F32, "lsp", es_g)
        Lc = C.sb([16, T], F32, "Lc", es_g)
        for c in range(4):
            pm = psb.next()
            for kc in range(KC):
                S.op("pe", lambda kc=kc, pm=pm, c=c: P.matmul(
                    pm[0:16, :], lhsT=wg_b[:, kc, :], rhs=h1T[:, kc, c * 512:(c + 1) * 512],
                    start=(kc == 0), stop=(kc == KC - 1)),
                    reads=[wg_b.r, h1T.r], writes=[pm.r], inc=(kc == KC - 1))
            S.op("act", lambda pm=pm, c=c: A.activation(out=graw[:, c * 512:(c + 1) * 512], in_=pm[0:16, :],
                                                        func=AF.Identity, bias=gb_sb[:, 0:1]),
                 reads=[pm.r, gb_sb.r], writes=[graw.r])
        S.op("act", lambda: A.activation(out=lsp[:], in_=graw[:], func=AF.Exp, scale=-1.0),
             reads=[graw.r], writes=[lsp.r])
        S.op("act", lambda: A.activation(out=lsp[:], in_=lsp[:], func=AF.Ln, bias=ones_f[0:16, 0:1]),
             reads=[lsp.r, ones_f.r], writes=[lsp.r])
        S.op("dve", lambda: V.tensor_tensor_scan(out=Lc[:], data0=ones_f[0:16, 0:1].broadcast_to([16, T]), data1=lsp[:],
                                                 initial=zcol[0:16, 0:1], op0=ALU.mult, op1=ALU.add),
             reads=[ones_f.r, lsp.r, zcol.r], writes=[Lc.r])
        for (src, dst) in ((Lc, Ltok), (graw, Gtok)):
            for i4 in range(4):
                pm = psb.next()
                for j in range(4):
                    i = i4 * 4 + j
                    S.op("pe", lambda i=i, j=j, pm=pm, src=src: P.transpose(
                        out=pm[:, j * 16:(j + 1) * 16], in_=src[0:16, i * 128:(i + 1) * 128],
                        identity=ident_f[0:16, 0:16]), reads=[src.r, ident_f.r], writes=[pm.r], inc=(j == 3))
                S.op("dve", lambda i4=i4, pm=pm, dst=dst: V.tensor_copy(
                    out=dst[:, i4 * 4:(i4 + 1) * 4, :],
                    in_=pm[:, 0:64].rearrange("p (a b) -> p a b", a=4)),
                    reads=[pm.r], writes=[dst.r])
        pm = psb.next()
        for c in range(4):
            S.op("pe", lambda c=c, pm=pm: P.matmul(pm[:, c * 16:(c + 1) * 16], lhsT=sel127[:],
                                                   rhs=Ltok[:, 4 * c + 3, :], start=True, stop=True),
                 reads=[sel127.r, Ltok.r], writes=[pm.r], inc=(c == 3))
        S.op("dve", lambda pm=pm: V.tensor_copy(out=LrefB[:], in_=pm[:, 0:64]), reads=[pm.r], writes=[LrefB.r])
        for c in range(4):
            S.op("act", lambda c=c: A.activation(out=EQ[0:12, c * 512:(c + 1) * 512], in_=Lc[0:12, c * 512:(c + 1) * 512],
                                                 func=AF.Exp, scale=-1.0,
                                                 bias=Lc[0:12, c * 512 + 511:c * 512 + 512]),
                 reads=[Lc.r], writes=[EQ.r])
        for m in range(4):
            S.op("dve", lambda m=m: V.tensor_scalar(out=selm[:, m * 128:(m + 1) * 128], in0=ones_f[0:16, :],
                                                    scalar1=ident_f[0:16, 8 + m:9 + m], scalar2=None,
                                                    op0=ALU.mult),
                 reads=[ones_f.r, ident_f.r], writes=[selm.r])

        S.barrier()
        es_g.close()
        S.op("pool", lambda: G.memset(rawc[:, 0:4], 0.0), writes=[rawc.r])

        def proj_fm(wb, c, pm):
            for kc in range(KC):
                S.op("pe", lambda kc=kc: P.matmul(pm[:], lhsT=wb[:, kc, :], rhs=h1T[:, kc, c * 512:(c + 1) * 512],
                                                  start=(kc == 0), stop=(kc == KC - 1)),
                     reads=[wb.r, h1T.r], writes=[pm.r], inc=(kc == KC - 1))

        def fox_qk(wb, dst, gcol):
            def post(c, sqb, rawf):
                pn = psb.next()
                S.op("pe", lambda: P.matmul(pn[:], lhsT=ones_b[:], rhs=sqb[:], start=True, stop=True),
                     reads=[ones_b.r, sqb.r], writes=[pn.r])
                rs = rs_r.next()
                S.op("dve", lambda: V.tensor_scalar(out=rs[:], in0=pn[:], scalar1=1.0 / 128, scalar2=EPS,
                                                    op0=ALU.mult, op1=ALU.add), reads=[pn.r], writes=[rs.r])
                S.op("act", lambda: A.activation(out=rs[:], in_=rs[:], func=AF.Sqrt), reads=[rs.r], writes=[rs.r])
                S.op("dve", lambda: V.reciprocal(out=rs[:], in_=rs[:]), reads=[rs.r], writes=[rs.r])
                S.op("dve", lambda: V.scalar_tensor_tensor(out=dst[:, c * 512:(c + 1) * 512], in0=rawf[:],
                                                           scalar=qsc[:, gcol:gcol + 1], in1=rs[:],
                                                           op0=ALU.mult, op1=ALU.mult),
                     reads=[rawf.r, qsc.r, rs.r], writes=[dst.r])

            prev = None
            for c in range(4):
                pm = psb.next()
                proj_fm(wb, c, pm)
                sqb = sqb_r.next()
                rawf = rawf_r.next()
                S.op("act", lambda: A.activation(out=sqb[:], in_=pm[:], func=AF.Square), reads=[pm.r], writes=[sqb.r])
                S.op("act", lambda: A.copy(out=rawf[:], in_=pm[:]), reads=[pm.r], writes=[rawf.r])
                if prev is not None:
                    post(*prev)
                prev = (c, sqb, rawf)
            post(*prev)

        def v_tok(wb, dvc):
            for i4 in range(4):
                pm = psb.next()
                for j in range(4):
                    i = i4 * 4 + j
                    for kc in range(KC):
                        S.op("pe", lambda kc=kc, i=i, j=j: P.matmul(
                            pm[:, j * 128:(j + 1) * 128], lhsT=h1T[:, kc, i * 128:(i + 1) * 128], rhs=wb[:, kc, :],
                            start=(kc == 0), stop=(kc == KC - 1)),
                            reads=[h1T.r, wb.r], writes=[pm.r], inc=(kc == KC - 1 and j == 3))
                S.op("act", lambda: A.copy(out=vtok[:, i4 * 4:(i4 + 1) * 4, dvc * 128:(dvc + 1) * 128],
                                           in_=pm[:].rearrange("p (a b) -> p a b", a=4)),
                     reads=[pm.r], writes=[vtok.r])

        def ml_qk(wb, dst, cch):
            for c in range(4):
                pm = psb.next()
                proj_fm(wb, c, pm)
                S.op("act", lambda: A.copy(out=rawc[:, 4 + c * 512:4 + (c + 1) * 512], in_=pm[:]),
                     reads=[pm.r], writes=[rawc.r])
            S.op("dve", lambda: V.tensor_scalar(out=cv[:], in0=rawc[:, 4:4 + T], scalar1=cw_sb[:, cch, 3:4],
                                                scalar2=cb_sb[:, cch:cch + 1], op0=ALU.mult, op1=ALU.add),
                 reads=[rawc.r, cw_sb.r, cb_sb.r], writes=[cv.r])
            for j in range(3):
                S.op("dve", lambda j=j: V.scalar_tensor_tensor(out=cv[:], in0=rawc[:, 1 + j:1 + j + T],
                                                               scalar=cw_sb[:, cch, j:j + 1], in1=cv[:],
                                                               op0=ALU.mult, op1=ALU.add),
                     reads=[rawc.r, cw_sb.r, cv.r], writes=[cv.r])
            S.op("act", lambda: A.activation(out=dst[:], in_=cv[:], func=AF.Silu), reads=[cv.r], writes=[dst.r])

        def ml_o(wb, dst):
            for c in range(4):
                pm = psb.next()
                proj_fm(wb, c, pm)
                S.op("act", lambda: A.activation(out=dst[:, c * 512:(c + 1) * 512], in_=pm[:], func=AF.Sigmoid),
                     reads=[pm.r], writes=[dst.r])

        def attend(nd, ndv, row, is_fox, mrow, out_chunks, hgc0):
            mixo = [mixo_r.next() for _ in range(ndv)]
            for c in range(4):
                kb = kb_r.next()
                if is_fox:
                    S.op("dve", lambda: V.tensor_scalar(out=kb[:], in0=Ltok[:, :, row],
                                                        scalar1=LrefB[:, c * 16 + row:c * 16 + row + 1],
                                                        scalar2=None, op0=ALU.subtract),
                         reads=[Ltok.r, LrefB.r], writes=[kb.r])
                    qs = [qTr[d][:, c * 512:(c + 1) * 512] for d in range(nd)]
                    qres = [qTr[d].r for d in range(nd)]
                else:
                    S.op("dve", lambda: V.scalar_tensor_tensor(out=ktmp[:, 0:4 * c + 4], in0=Ltok[:, 0:4 * c + 4, row],
                                                               scalar=LrefB[:, c * 16 + row:c * 16 + row + 1],
                                                               in1=Gtok[:, 0:4 * c + 4, 12 + mrow],
                                                               op0=ALU.subtract, op1=ALU.add),
                         reads=[Ltok.r, LrefB.r, Gtok.r], writes=[ktmp.r])
                    S.op("act", lambda: A.activation(out=kb[:, 0:4 * c + 4], in_=ktmp[:, 0:4 * c + 4], func=AF.Exp, bias=lncol[:, 0:1]),
                         reads=[ktmp.r, lncol.r], writes=[kb.r])
                    pe_ = psb.next()
                    S.op("pe", lambda: P.matmul(pe_[:], lhsT=selm[0:12, mrow * 128:(mrow + 1) * 128],
                                                rhs=EQ[0:12, c * 512:(c + 1) * 512], start=True, stop=True),
                         reads=[selm.r, EQ.r], writes=[pe_.r])
                    qs, qres = [], []
                    for d in range(nd):
                        qp = qpr[d].next()
                        S.op("dve", lambda d=d, qp=qp: V.tensor_tensor(out=qp[:], in0=qTr[d][:, c * 512:(c + 1) * 512],
                                                                       in1=pe_[:], op=ALU.mult),
                             reads=[qTr[d].r, pe_.r], writes=[qp.r])
                        qs.append(qp[:])
                        qres.append(qp.r)
                pO = [acc_ps[d] for d in range(ndv)]
                pD = acc_ps[2]
                nj = 4 * c + 4

                def emit_pv(j, lo, pt):
                    for dv in range(ndv):
                        S.op("pe", lambda dv=dv: P.matmul(pO[dv][:, lo:512], lhsT=vtok[:, j, dv * 128:(dv + 1) * 128],
                                                          rhs=pt[:, lo:512], start=(j == 0), stop=(j == nj - 1)),
                             reads=[vtok.r, pt.r], writes=[pO[dv].r], inc=False)
                    S.op("pe", lambda: P.matmul(pD[:, lo:512], lhsT=ones_b[:], rhs=pt[:, lo:512],
                                                start=(j == 0), stop=(j == nj - 1)),
                         reads=[ones_b.r, pt.r], writes=[pD.r])

                prevpv = None
                for j in range(nj):
                    lo = 128 * (j - 4 * c) if j >= 4 * c else 0
                    pS = psb.next()
                    for d in range(nd):
                        S.op("pe", lambda d=d: P.matmul(pS[:, lo:512], lhsT=kTr[d][:, j * 128:(j + 1) * 128],
                                                        rhs=qs[d][:, lo:512], start=(d == 0), stop=(d == nd - 1)),
                             reads=[kTr[d].r, qres[d]], writes=[pS.r], inc=(d == nd - 1))
                    pt = pt_r.next()
                    if is_fox:
                        S.op("act", lambda: A.activation(out=pt[:, lo:512], in_=pS[:, lo:512], func=AF.Exp,
                                                         bias=kb[:, j:j + 1]),
                             reads=[pS.r, kb.r], writes=[pt.r])
                    else:
                        S.op("dve", lambda: V.tensor_scalar(out=pt[:, lo:512], in0=pS[:, lo:512],
                                                            scalar1=kb[:, j:j + 1], scalar2=None, op0=ALU.mult),
                             reads=[pS.r, kb.r], writes=[pt.r])
                    if j >= 4 * c:
                        S.op("pool", lambda: G.tensor_tensor(out=pt[:, lo:lo + 128], in0=pt[:, lo:lo + 128],
                                                             in1=tri_b[:], op=ALU.mult),
                             reads=[pt.r, tri_b.r], writes=[pt.r])
                    if prevpv is not None:
                        emit_pv(*prevpv)
                    prevpv = (j, lo, pt)
                emit_pv(*prevpv)
                rs = rs_r.next()
                if is_fox:
                    S.op("dve", lambda: V.reciprocal(out=rs[:], in_=pD[:]), reads=[pD.r], writes=[rs.r])
                    S.op("dve", lambda: V.tensor_tensor(out=mixo[0][:, c * 512:(c + 1) * 512], in0=pO[0][:],
                                                        in1=rs[:], op=ALU.mult),
                         reads=[pO[0].r, rs.r], writes=[mixo[0].r])
                else:
                    S.op("dve", lambda: V.tensor_scalar(out=rs[:], in0=pD[:], scalar1=-1.0, scalar2=1.0,
                                                        op0=ALU.mult, op1=ALU.max), reads=[pD.r], writes=[rs.r])
                    S.op("dve", lambda: V.scalar_tensor_tensor(out=rs[:], in0=pD[:], scalar=1.0, in1=rs[:],
                                                               op0=ALU.max, op1=ALU.max),
                         reads=[pD.r, rs.r], writes=[rs.r])
                    S.op("dve", lambda: V.reciprocal(out=rs[:], in_=rs[:]), reads=[rs.r], writes=[rs.r])
                    hTs = []
                    pn = psb.next()
                    for dv in range(ndv):
                        hT = hT_r[dv].next()
                        S.op("dve", lambda dv=dv, hT=hT: V.tensor_tensor(out=hT[:], in0=pO[dv][:], in1=rs[:], op=ALU.mult),
                             reads=[pO[dv].r, rs.r], writes=[hT.r])
                        sqb = sqb_r.next()
                        S.op("act", lambda hT=hT, sqb=sqb: A.activation(out=sqb[:], in_=hT[:], func=AF.Square),
                             reads=[hT.r], writes=[sqb.r])
                        S.op("pe", lambda dv=dv, sqb=sqb: P.matmul(pn[:], lhsT=ones_b[:], rhs=sqb[:],
                                                                    start=(dv == 0), stop=(dv == ndv - 1)),
                             reads=[ones_b.r, sqb.r], writes=[pn.r], inc=(dv == ndv - 1))
                        hTs.append(hT)
                    rs2 = rs_r.next()
                    S.op("dve", lambda: V.tensor_scalar(out=rs2[:], in0=pn[:], scalar1=1.0 / 256, scalar2=EPS,
                                                        op0=ALU.mult, op1=ALU.add), reads=[pn.r], writes=[rs2.r])
                    S.op("act", lambda: A.activation(out=rs2[:], in_=rs2[:], func=AF.Sqrt), reads=[rs2.r], writes=[rs2.r])
                    S.op("dve", lambda: V.reciprocal(out=rs2[:], in_=rs2[:]), reads=[rs2.r], writes=[rs2.r])
                    for dv in range(ndv):
                        S.op("dve", lambda dv=dv: V.scalar_tensor_tensor(
                            out=hTs[dv][:], in0=hTs[dv][:], scalar=hg_sb[:, hgc0 + dv:hgc0 + dv + 1], in1=rs2[:],
                            op0=ALU.mult, op1=ALU.mult), reads=[hTs[dv].r, hg_sb.r, rs2.r], writes=[hTs[dv].r])
                        S.op("dve", lambda dv=dv: V.tensor_tensor(
                            out=mixo[dv][:, c * 512:(c + 1) * 512], in0=hTs[dv][:],
                            in1=sigo[dv][:, c * 512:(c + 1) * 512], op=ALU.mult),
                            reads=[hTs[dv].r, sigo[dv].r], writes=[mixo[dv].r])
            for dv in range(ndv):
                S.dma("sp", mix_d[out_chunks[dv]], mixo[dv][:], reads=[mixo[dv].r],
                      writes=[mix_res[out_chunks[dv]]])

        for h in range(NFOX):
            fox_qk(load_wchunk(wfm_d[3 * h]), qTr[0], 0)
            fox_qk(load_wchunk(wfm_d[3 * h + 1]), kTr[0], 1)
            v_tok(load_wchunk(wfm_d[3 * h + 2]), 0)
            attend(1, 1, h, True, 0, [h], 0)
        for m in range(NML):
            b0 = 24 + 8 * m
            ml_qk(load_wchunk(wfm_d[b0 + 0]), qTr[0], 2 * m)
            ml_qk(load_wchunk(wfm_d[b0 + 1]), qTr[1], 2 * m + 1)
            ml_qk(load_wchunk(wfm_d[b0 + 2]), kTr[0], 8 + 2 * m)
            ml_qk(load_wchunk(wfm_d[b0 + 3]), kTr[1], 8 + 2 * m + 1)
            v_tok(load_wchunk(wfm_d[b0 + 4]), 0)
            v_tok(load_wchunk(wfm_d[b0 + 5]), 1)
            ml_o(load_wchunk(wfm_d[b0 + 6]), sigo[0])
            ml_o(load_wchunk(wfm_d[b0 + 7]), sigo[1])
            attend(2, 2, 8 + m, False, m, [8 + 2 * m, 8 + 2 * m + 1], 2 * m)
        S.barrier()
        es_m.close()

        es_o = ExitStack()
        mixT = C.sb([128, KC, T], BF16, "mixT", es_o)
        written = list(range(NFOX)) + [8 + j for j in range(2 * NML)]
        if len(written) < KC:
            S.op("pool", lambda: G.memset(mixT[:], 0.0), writes=[mixT.r])
        for kc in written:
            S.dma("sp" if kc % 2 == 0 else "act", mixT[:, kc, :], mix_d[kc], reads=[mix_res[kc]], writes=[mixT.r])
        gate1b = C.sb([128, D], F32, "gate1b", es_o)
        build_gate(gate1b, 32)
        wo_st = C.sb([128, KC, 512], F32, "wo_st", es_o)
        wo_bf = C.sbring(2, [128, KC, 512], BF16, "wo_bf", es_o)
        xs_r = C.sbring(3, [128, 512], F32, "xs", es_o)
        t1_r = C.sbring(3, [128, 512], F32, "t1", es_o)
        for cb in range(4):
            S.dma("sp", wo_st[:], wout_d[cb], writes=[wo_st.r])
            wob = wo_bf.next()
            S.op("pool", lambda: G.tensor_copy(out=wob[:], in_=wo_st[:]), reads=[wo_st.r], writes=[wob.r])
            for i in range(NT):
                pm = psb.next()
                for kc in range(KC):
                    S.op("pe", lambda kc=kc: P.matmul(pm[:], lhsT=mixT[:, kc, i * 128:(i + 1) * 128], rhs=wob[:, kc, :],
                                                      start=(kc == 0), stop=(kc == KC - 1)),
                         reads=[mixT.r, wob.r], writes=[pm.r], inc=(kc == KC - 1))
                xs = xs_r.next()
                S.dma("act", xs[:], x_d[i * 128:(i + 1) * 128, cb * 512:(cb + 1) * 512], writes=[xs.r])
                t1 = t1_r.next()
                S.op("dve", lambda: V.tensor_tensor(out=t1[:], in0=pm[:], in1=gate1b[:, cb * 512:(cb + 1) * 512],
                                                    op=ALU.mult), reads=[pm.r, gate1b.r], writes=[t1.r])
                S.op("pool", lambda: G.tensor_tensor(out=t1[:], in0=t1[:], in1=xs[:], op=ALU.add),
                     reads=[t1.r, xs.r], writes=[t1.r])
                S.dma("sp", out_d[i * 128:(i + 1) * 128, cb * 512:(cb + 1) * 512], t1[:], reads=[t1.r],
                      writes=[out_res[i]])
        S.barrier()
        es_o.close()

        es_p = ExitStack()
        psb = Ring(psb.bufs + acc_ps)
        wst = C.sbring(2, [128, KC, 128], F32, "wstp", es_p)
        wbf = C.sbring(2, [128, KC, 128], BF16, "wbfp", es_p)
        gate2b = C.sb([128, D], F32, "gate2b", es_p)
        build_gate(gate2b, 80)
        h2T = C.sb([128, KC, 512], BF16, "h2T", es_p)
        qT = C.sb([128, 16, 512], BF16, "qTp", es_p)
        acc = C.sb([128, 4, D], F32, "acc", es_p)
        Cb = C.sb([128, 8, 512], BF16, "Cb", es_p)
        statsT = C.sb([32, 512], BF16, "statsT", es_p)
        selT = C.sb([32, 8 * 128], BF16, "selT", es_p)
        selg = C.sb([32, 8 * 128], BF16, "selg", es_p)
        ucol = C.sb([32, 8], F32, "ucol", es_p)
        for h in range(8):
            S.op("dve", lambda h=h: V.tensor_tensor(out=ucol[:, h:h + 1], in0=ident_f[0:32, h:h + 1],
                                                    in1=ident_f[0:32, 8 + h:9 + h], op=ALU.add),
                 reads=[ident_f.r, ucol.r], writes=[ucol.r])
            S.op("dve", lambda h=h: V.scalar_tensor_tensor(out=ucol[:, h:h + 1], in0=ucol[:, h:h + 1], scalar=-1.0,
                                                           in1=ident_f[0:32, 16 + h:17 + h],
                                                           op0=ALU.mult, op1=ALU.subtract),
                 reads=[ident_f.r, ucol.r], writes=[ucol.r])
            S.op("dve", lambda h=h: V.tensor_scalar(out=selT[:, h * 128:(h + 1) * 128], in0=ones_f[0:32, :],
                                                    scalar1=ucol[:, h:h + 1], scalar2=None, op0=ALU.mult),
                 reads=[ones_f.r, ucol.r], writes=[selT.r])
            S.op("dve", lambda h=h: V.tensor_scalar(out=selg[:, h * 128:(h + 1) * 128], in0=ones_f[0:32, :],
                                                    scalar1=ident_f[0:32, 24 + h:25 + h], scalar2=None, op0=ALU.mult),
                 reads=[ones_f.r, ident_f.r], writes=[selg.r])
        k1bc_r = C.sbring(2, [128, 128], BF16, "k1bc", es_p)
        eu_st = C.sbring(2, [128, D], F32, "eu_st", es_p)
        eu_bf = C.sbring(GK + 2, [128, D], BF16, "eu_bf", es_p)
        wT_r = C.sbring(2 * GK, [128, 512], BF16, "wT", es_p)
        gA_r = C.sbring(2, [128, 512], BF16, "gA", es_p)
        E_r = C.sbring(6, [128, 512], BF16, "E", es_p)
        Mm2_r = C.sbring(3, [128, 2, 512], BF16, "Mm2", es_p)
        Tt2_r = C.sbring(4, [128, 2, 512], BF16, "Tt2", es_p)
        Ga2_r = C.sbring(2, [128, 2, 512], BF16, "Ga2", es_p)
        sc_r = C.sbring(1, [128, 256], F32, "sc", es_p)
        sc2_r = C.sbring(1, [128, 256], F32, "sc2", es_p)
        v12_r = C.sbring(2, [128, 32], F32, "v12", es_p)
        cand_r = C.sbring(1, [128, 256], F32, "cand", es_p)
        cand2_r = C.sbring(1, [128, 256], F32, "cand2", es_p)
        c16_r = C.sbring(2, [128, 16], F32, "c16", es_p)
        e16_r = C.sbring(2, [128, 16], F32, "e16", es_p)
        sm_r = C.sbring(2, [128, 4], F32, "sm", es_p)
        statf = C.sb([128, 48], F32, "statf", es_p)
        statb = C.sb([128, 32], BF16, "statb", es_p)

        for Q in range(NQ):
            es_n2 = ExitStack()
            norm_to_T(es_n2, lambda i: out_d[(Q * 4 + i) * 128:(Q * 4 + i + 1) * 128, :],
                      lambda i: out_res[Q * 4 + i], A2, 48, h2T, 4, 1)
            S.barrier()
            es_n2.close()
            for cc in range(16):
                st = wst.next()
                S.dma("sp", st[:], wq_d[cc], writes=[st.r])
                wb = wbf.next()
                S.op("pool", lambda: G.tensor_copy(out=wb[:], in_=st[:]), reads=[st.r], writes=[wb.r])
                pm = psb.next()
                for kc in range(KC):
                    S.op("pe", lambda kc=kc: P.matmul(pm[:], lhsT=wb[:, kc, :], rhs=h2T[:, kc, :],
                                                      start=(kc == 0), stop=(kc == KC - 1)),
                         reads=[wb.r, h2T.r], writes=[pm.r], inc=(kc == KC - 1))
                S.op("act", lambda: A.copy(out=qT[:, cc, :], in_=pm[:]), reads=[pm.r], writes=[qT.r])
            for ti in range(4):
                for h in range(8):
                    pm = psb.next()
                    S.op("pe", lambda: P.matmul(pm[:, 0:128], lhsT=qT[:, 2 * h, ti * 128:(ti + 1) * 128],
                                                rhs=k1t_b[:], start=True, stop=True),
                         reads=[qT.r, k1t_b.r], writes=[pm.r], inc=False)
                    S.op("pe", lambda: P.matmul(pm[:, 128:256], lhsT=qT[:, 2 * h + 1, ti * 128:(ti + 1) * 128],
                                                rhs=k2t_b[:], start=True, stop=True),
                         reads=[qT.r, k2t_b.r], writes=[pm.r])
                    sc = sc_r.next()
                    sc2 = sc2_r.next()
                    v12 = v12_r.next()
                    S.op("act", lambda: A.copy(out=sc[:], in_=pm[:, 0:256]), reads=[pm.r], writes=[sc.r])
                    for half in range(2):
                        sl = slice(half * 128, (half + 1) * 128)
                        vo = half * 16
                        S.op("dve", lambda: V.max(out=v12[:, vo:vo + 8], in_=sc[:, sl]), reads=[sc.r, v12.r], writes=[v12.r])
                        S.op("dve", lambda: V.match_replace(out=sc2[:, sl], in_to_replace=v12[:, vo:vo + 8],
                                                            in_values=sc[:, sl], imm_value=NEG),
                             reads=[sc.r, v12.r, sc2.r], writes=[sc2.r])
                        S.op("dve", lambda: V.max(out=v12[:, vo + 8:vo + 16], in_=sc2[:, sl]),
                             reads=[sc2.r, v12.r], writes=[v12.r])
                    cand = cand_r.next()
                    cand2 = cand2_r.next()
                    c16 = c16_r.next()
                    e16 = e16_r.next()
                    sm = sm_r.next()
                    S.op("dve", lambda: V.tensor_tensor(
                        out=cand[:].rearrange("p (a b) -> p a b", a=16),
                        in0=v12[:, 0:16].unsqueeze(2).broadcast_to([128, 16, 16]),
                        in1=v12[:, 16:32].unsqueeze(1).broadcast_to([128, 16, 16]), op=ALU.add),
                        reads=[v12.r], writes=[cand.r])
                    S.op("dve", lambda: V.max(out=c16[:, 0:8], in_=cand[:]), reads=[cand.r, c16.r], writes=[c16.r])
                    S.op("dve", lambda: V.match_replace(out=cand2[:], in_to_replace=c16[:, 0:8], in_values=cand[:],
                                                        imm_value=NEG), reads=[cand.r, c16.r], writes=[cand2.r])
                    S.op("dve", lambda: V.max(out=c16[:, 8:16], in_=cand2[:]), reads=[cand2.r, c16.r], writes=[c16.r])
                    S.op("dve", lambda: V.tensor_scalar(out=sm[:, 0:1], in0=c16[:, 0:1], scalar1=-1.0, scalar2=None,
                                                        op0=ALU.mult), reads=[c16.r, sm.r], writes=[sm.r])
                    S.op("dve", lambda: V.memset(sm[:, 1:2], 0.0), reads=[sm.r], writes=[sm.r])
                    S.op("act", lambda: A.activation(out=e16[:], in_=c16[:], func=AF.Exp, bias=sm[:, 0:1],
                                                     accum_out=sm[:, 1:2]),
                         reads=[c16.r, sm.r], writes=[e16.r, sm.r])
                    S.op("dve", lambda: V.reciprocal(out=sm[:, 2:3], in_=sm[:, 1:2]), reads=[sm.r], writes=[sm.r])
                    S.op("dve", lambda: V.tensor_scalar(out=statf[:, h:h + 1], in0=c16[:, 15:16], scalar1=-3.0e-5,
                                                        scalar2=None, op0=ALU.add),
                         reads=[c16.r, statf.r], writes=[statf.r])
                    S.op("dve", lambda: V.tensor_tensor(out=statf[:, 32 + h:33 + h], in0=e16[:, 15:16], in1=sm[:, 2:3],
                                                        op=ALU.mult), reads=[e16.r, sm.r, statf.r], writes=[statf.r])
                S.op("dve", lambda: V.tensor_copy(out=statb[:, 0:8], in_=statf[:, 0:8]), reads=[statf.r, statb.r], writes=[statb.r])
                S.op("dve", lambda: V.tensor_tensor(out=statf[:, 8:16], in0=statf[:, 0:8], in1=statb[:, 0:8],
                                                    op=ALU.subtract), reads=[statf.r, statb.r], writes=[statf.r])
                S.op("dve", lambda: V.tensor_copy(out=statb[:, 8:16], in_=statf[:, 8:16]), reads=[statf.r, statb.r], writes=[statb.r])
                S.op("dve", lambda: V.tensor_tensor(out=statf[:, 16:24], in0=statf[:, 8:16], in1=statb[:, 8:16],
                                                    op=ALU.subtract), reads=[statf.r, statb.r], writes=[statf.r])
                S.op("dve", lambda: V.tensor_copy(out=statb[:, 16:24], in_=statf[:, 16:24]), reads=[statf.r, statb.r], writes=[statb.r])
                S.op("dve", lambda: V.tensor_copy(out=statb[:, 24:32], in_=statf[:, 32:40]), reads=[statf.r, statb.r], writes=[statb.r])
                pm = psb.next()
                pmb = pm.t.bitcast(BF16)
                S.op("pe", lambda: P.transpose(out=pmb[0:32, 0:128], in_=statb[:, 0:32], identity=ident_b[:]),
                     reads=[statb.r, ident_b.r], writes=[pm.r])
                S.op("act", lambda: A.copy(out=statsT[:, ti * 128:(ti + 1) * 128], in_=pmb[0:32, 0:128]),
                     reads=[pm.r], writes=[statsT.r])
            for h in range(8):
                pm = psb.next()
                S.op("pe", lambda: P.matmul(pm[:], lhsT=selg[:, h * 128:(h + 1) * 128], rhs=statsT[:],
                                            start=True, stop=True), reads=[selg.r, statsT.r], writes=[pm.r])
                S.op("act", lambda: A.copy(out=Cb[:, h, :], in_=pm[:]), reads=[pm.r], writes=[Cb.r])

            grp = []
            ngrp = 0
            pending = []

            def emit_units(n):
                for _ in range(min(n, len(pending))):
                    g_, ti, cbk, first = pending.pop(0)
                    pm = psb.next()
                    for gi, (wT_, eub_) in enumerate(g_):
                        S.op("pe", lambda gi=gi, wT_=wT_, eub_=eub_: P.matmul(
                            pm[:], lhsT=wT_[:, ti * 128:(ti + 1) * 128], rhs=eub_[:, cbk * 512:(cbk + 1) * 512],
                            start=(gi == 0), stop=(gi == len(g_) - 1)),
                            reads=[wT_.r, eub_.r], writes=[pm.r], inc=(gi == len(g_) - 1))
                    if first:
                        S.op("act", lambda: A.copy(out=acc[:, ti, cbk * 512:(cbk + 1) * 512], in_=pm[:]),
                             reads=[pm.r], writes=[acc.r])
                    else:
                        S.op("dve", lambda: V.tensor_tensor(out=acc[:, ti, cbk * 512:(cbk + 1) * 512],
                                                            in0=pm[:], in1=acc[:, ti, cbk * 512:(cbk + 1) * 512],
                                                            op=ALU.add), reads=[pm.r, acc.r], writes=[acc.r])

            prepped = {}

            def prep(e1):
                st = wst.next()
                S.dma("sp", st[:], edt_d[e1], writes=[st.r])
                edb = wbf.next()
                S.op("act", lambda: A.copy(out=edb[:], in_=st[:]), reads=[st.r], writes=[edb.r])
                es_ = eu_st.next()
                S.dma("sp", es_[:], eu_d[e1], writes=[es_.r])
                eub = eu_bf.next()
                S.op("act", lambda: A.copy(out=eub[:], in_=es_[:]), reads=[es_.r], writes=[eub.r])
                k1bc = k1bc_r.next()
                S.op("pool", lambda: G.tensor_copy(out=k1bc[:], in_=k1t_b[:, e1:e1 + 1].broadcast_to([128, 128])),
                     reads=[k1t_b.r], writes=[k1bc.r])
                prepped[e1] = (edb, eub, k1bc)

            prep(0)
            for e1 in range(NE1):
                if e1 + 1 < NE1:
                    prep(e1 + 1)
                edb, eub, k1bc = prepped.pop(e1)
                pA = psb.next()
                for kc in range(KC):
                    S.op("pe", lambda kc=kc: P.matmul(pA[:], lhsT=edb[:, kc, :], rhs=h2T[:, kc, :],
                                                      start=(kc == 0), stop=(kc == KC - 1)),
                         reads=[edb.r, h2T.r], writes=[pA.r], inc=(kc == KC - 1))
                gA = gA_r.next()
                S.op("act", lambda: A.activation(out=gA[:], in_=pA[:], func=AF.Gelu), reads=[pA.r], writes=[gA.r])
                Ga2 = Ga2_r.next()
                for hp in range(4):
                    Mm2 = Mm2_r.next()
                    for hh in range(2):
                        h = 2 * hp + hh
                        pX = psb.next()
                        S.op("pe", lambda: P.matmul(pX[:], lhsT=k2t_b[:], rhs=qT[:, 2 * h + 1, :], start=True, stop=False),
                             reads=[k2t_b.r, qT.r], writes=[pX.r], inc=False)
                        S.op("pe", lambda: P.matmul(pX[:], lhsT=k1bc[:], rhs=qT[:, 2 * h, :], start=False, stop=False),
                             reads=[k1bc.r, qT.r], writes=[pX.r], inc=False)
                        S.op("pe", lambda: P.matmul(pX[:], lhsT=selT[:, h * 128:(h + 1) * 128], rhs=statsT[:],
                                                    start=False, stop=True),
                             reads=[selT.r, statsT.r], writes=[pX.r])
                        E = E_r.next()
                        S.op("act", lambda: A.activation(out=E[:], in_=pX[:], func=AF.Exp), reads=[pX.r], writes=[E.r])
                        S.op("dve", lambda: V.scalar_tensor_tensor(out=Mm2[:, hh, :], in0=pX[:], scalar=0.0, in1=E[:],
                                                                   op0=ALU.is_ge, op1=ALU.mult),
                             reads=[pX.r, E.r, Mm2.r], writes=[Mm2.r])
                    if hp == 0:
                        S.op("dve", lambda: V.tensor_tensor(out=Ga2[:], in0=Mm2[:], in1=Cb[:, 0:2, :], op=ALU.mult),
                             reads=[Mm2.r, Cb.r], writes=[Ga2.r])
                    else:
                        Tt2 = Tt2_r.next()
                        S.op("dve", lambda: V.tensor_tensor(out=Tt2[:], in0=Mm2[:], in1=Cb[:, 2 * hp:2 * hp + 2, :],
                                                            op=ALU.mult), reads=[Mm2.r, Cb.r], writes=[Tt2.r])
                        S.op("pool", lambda: G.tensor_tensor(out=Ga2[:], in0=Ga2[:], in1=Tt2[:], op=ALU.add),
                             reads=[Ga2.r, Tt2.r], writes=[Ga2.r])
                S.op("pool", lambda: G.tensor_tensor(out=Ga2[:, 0, :], in0=Ga2[:, 0, :], in1=Ga2[:, 1, :], op=ALU.add),
                     reads=[Ga2.r], writes=[Ga2.r])
                wT = wT_r.next()
                S.op("dve", lambda: V.tensor_tensor(out=wT[:], in0=Ga2[:, 0, :], in1=gA[:], op=ALU.mult),
                     reads=[Ga2.r, gA.r], writes=[wT.r])
                grp.append((wT, eub))
                emit_units(16)
                if len(grp) == GK or e1 == NE1 - 1:
                    for ti in range(4):
                        for cbk in range(4):
                            pending.append((list(grp), ti, cbk, ngrp == 0))
                    grp = []
                    ngrp += 1
            emit_units(len(pending))
            es_f = ExitStack()
            x1_r = C.sbring(1, [128, D], F32, "x1t", es_f)
            for ti in range(4):
                i = Q * 4 + ti
                x1 = x1_r.next()
                S.dma("sp", x1[:], out_d[i * 128:(i + 1) * 128, :], reads=[out_res[i]], writes=[x1.r])
                S.op("dve", lambda: V.tensor_tensor(out=acc[:, ti, :], in0=acc[:, ti, :], in1=gate2b[:], op=ALU.mult),
                     reads=[acc.r, gate2b.r], writes=[acc.r])
                S.op("pool", lambda: G.tensor_tensor(out=acc[:, ti, :], in0=acc[:, ti, :], in1=x1[:], op=ALU.add),
                     reads=[acc.r, x1.r], writes=[acc.r])
                S.dma("sp", out_d[i * 128:(i + 1) * 128, :], acc[:, ti, :], reads=[acc.r, out_res[i]], writes=[out_res[i]])
            S.barrier()
            es_f.close()
        S.barrier()
        es_p.close()

        for q in ("sp", "pool", "act"):
            for sem, val in S.dma_slots[q]:
                if val > 0:
                    S._wait("sp", (sem, val))
        print("instructions:", S.ninstr)
    return nc


def _host_layouts(inp):
    f = lambda a: np.ascontiguousarray(a, dtype=np.float32)
    L = {}
    w_ada = inp["w_ada"][0]
    L["wada_r"] = f(w_ada.reshape(KC, 128, 24, 512).transpose(2, 1, 0, 3))
    L["bada_r"] = f(inp["b_ada"][0].reshape(96, 128).T)
    L["g1_r"] = f(inp["norm1_gain"][0].reshape(KC, 128).T)
    L["g2_r"] = f(inp["norm2_gain"][0].reshape(KC, 128).T)
    w_in = inp["w_in"][0]
    o_fq, o_fk, o_fv, o_ff, o_mq, o_mk, o_mv, o_mo, o_mi, o_mf = 0, 1024, 2048, 3072, 3080, 4104, 5128, 6152, 7176, 7180
    cols = []
    for h in range(8):
        cols += [o_fq + 128 * h, o_fk + 128 * h, o_fv + 128 * h]
    for m in range(4):
        cols += [o_mq + 256 * m, o_mq + 256 * m + 128, o_mk + 256 * m, o_mk + 256 * m + 128,
                 o_mv + 256 * m, o_mv + 256 * m + 128, o_mo + 256 * m, o_mo + 256 * m + 128]
    wfm = np.empty((56, 128, KC, 128), np.float32)
    for i, c0 in enumerate(cols):
        wfm[i] = w_in[:, c0:c0 + 128].reshape(KC, 128, 128).transpose(1, 0, 2)
    L["wfm_r"] = wfm
    gcols = list(range(o_ff, o_ff + 8)) + list(range(o_mf, o_mf + 4)) + list(range(o_mi, o_mi + 4))
    L["wg_r"] = f(w_in[:, gcols].reshape(KC, 128, 16).transpose(1, 0, 2))
    L["gb_r"] = f(np.concatenate([inp["fox_f_bias"][0], inp["mlstm_f_bias"][0], inp["mlstm_i_bias"][0]]).reshape(16, 1))
    L["qkg_r"] = f(np.stack([inp["fox_q_gain"][0], inp["fox_k_gain"][0]], axis=1))
    L["convw_r"] = f(inp["mlstm_conv_w"][0].reshape(4, 16, 128).transpose(2, 1, 0))
    L["convb_r"] = f(inp["mlstm_conv_b"][0].reshape(16, 128).T)
    L["hg_r"] = f(inp["mlstm_head_gain"][0].reshape(8, 128).T)
    L["wout_r"] = f(inp["w_out"][0].reshape(KC, 128, 4, 512).transpose(2, 1, 0, 3))
    L["wq_r"] = f(inp["peer_w_query"][0].reshape(KC, 128, 16, 128).transpose(2, 1, 0, 3))
    L["k1t_r"] = f(inp["peer_sub_keys_1"][0].T)
    L["k2t_r"] = f(inp["peer_sub_keys_2"][0].T)
    ed = inp["peer_expert_down"][0]
    L["edt_r"] = f(ed.reshape(128, 128, KC, 128).transpose(0, 3, 2, 1))
    L["eu_r"] = f(inp["peer_expert_up"][0].reshape(128, 128, D))
    return L


def _core_inputs(inputs, b):
    return {
        "x": np.ascontiguousarray(inputs["x"][b], dtype=np.float32),
        "c_r": np.ascontiguousarray(inputs["c"][b].reshape(KC, 128).T, dtype=np.float32),
    }


def kernel(**inputs):
    inputs = {k: np.asarray(v) for k, v in inputs.items()}
    L = _host_layouts(inputs)
    nc = build_program()
    outs = []
    for g0 in range(0, 8, CORES_PER_LAUNCH):
        in_maps = []
        for b in range(g0, g0 + CORES_PER_LAUNCH):
            m = dict(L)
            m.update(_core_inputs(inputs, b))
            in_maps.append(m)
        res = run_bass_kernel_spmd(nc, in_maps, core_ids=list(range(CORES_PER_LAUNCH)))
        outs += [np.asarray(r["out"], dtype=np.float32) for r in res.results]
    return np.stack(outs, axis=0)
```
